# Optimizing a Trainium2 kernel written in Bass

```python
import math
import jax, jax.numpy as jnp
from jax import lax
import numpy as np

D_MODEL = 1024
BATCH = 2
SEQ = 16384
DEPTH = 4

N_MIXERS = 3
ALPHA = (2.0 * DEPTH) ** 0.25
BETA = (8.0 * DEPTH) ** -0.25
LN_EPS = 1e-5
RMS_EPS = 1e-6

SSD_D_INNER = 2 * D_MODEL
SSD_HEAD_DIM = 64
SSD_N_HEADS = SSD_D_INNER // SSD_HEAD_DIM
SSD_N_GROUPS = 8
SSD_HPG = SSD_N_HEADS // SSD_N_GROUPS
SSD_D_STATE = 128
SSD_CONV = 4
SSD_CHUNK = 128
SSD_CONV_DIM = SSD_D_INNER + 2 * SSD_N_GROUPS * SSD_D_STATE
SSD_IN_DIM = SSD_D_INNER + SSD_CONV_DIM + SSD_N_HEADS

MLA_N_HEADS = 16
MLA_NOPE = 64
MLA_ROPE = 32
MLA_V = 64
MLA_Q_RANK = 512
MLA_KV_RANK = 256
MLA_IN_DIM = MLA_Q_RANK + MLA_KV_RANK + MLA_ROPE
MLA_BLOCK = 128
ROPE_THETA = 10000.0

SG_D = 2 * D_MODEL
SG_GROUPS = 8
SG_GROUP_DIM = SG_D // SG_GROUPS
SG_CHUNK = 128

D_FF = 2816
N_EXPERTS = 8
TOP_K = 2

N_SSD = (DEPTH + 2) // 3
N_MLA = (DEPTH + 1) // 3
N_SG = DEPTH // 3
N_DENSE = (DEPTH + 1) // 2
N_MOE = DEPTH // 2

kernel_name = "hybrid_ssd_mla_gmlp_moe_deepnorm"


def layer_norm(x, g, b):
    xf = x.astype(jnp.float32)
    mu = jnp.mean(xf, -1, keepdims=True)
    var = jnp.mean(jnp.square(xf - mu), -1, keepdims=True)
    return ((xf - mu) * lax.rsqrt(var + LN_EPS)).astype(x.dtype) * g + b


def rms_norm(x, w):
    xf = x.astype(jnp.float32)
    return (xf * lax.rsqrt(jnp.mean(xf * xf, -1, keepdims=True) + RMS_EPS)).astype(x.dtype) * w


def rope(x, positions):
    half = x.shape[-1] // 2
    freqs = ROPE_THETA ** (-jnp.arange(half, dtype=jnp.float32) / half)
    ang = positions[..., None].astype(jnp.float32) * freqs
    cos = jnp.cos(ang)[:, :, None, :].astype(x.dtype)
    sin = jnp.sin(ang)[:, :, None, :].astype(x.dtype)
    x1, x2 = x[..., :half], x[..., half:]
    return jnp.concatenate([x1 * cos - x2 * sin, x2 * cos + x1 * sin], axis=-1)


def ssd_mixer(h, w_in, conv_w, conv_b, dt_bias, a_log, d_skip, norm_w, w_out):
    b, s, _ = h.shape
    G, R, P, N, L = SSD_N_GROUPS, SSD_HPG, SSD_HEAD_DIM, SSD_D_STATE, SSD_CHUNK
    z, xbc, dt = jnp.split(h @ w_in, [SSD_D_INNER, SSD_D_INNER + SSD_CONV_DIM], axis=-1)
    xbc = lax.conv_general_dilated(xbc, conv_w[:, None, :], window_strides=(1,),
                                   padding=[(SSD_CONV - 1, 0)],
                                   dimension_numbers=('NWC', 'WIO', 'NWC'),
                                   feature_group_count=SSD_CONV_DIM) + conv_b
    xbc = jax.nn.silu(xbc)
    xs, Bm, Cm = jnp.split(xbc, [SSD_D_INNER, SSD_D_INNER + G * N], axis=-1)
    dt = jax.nn.softplus(dt + dt_bias).astype(jnp.float32)
    A = -jnp.exp(a_log.astype(jnp.float32)).reshape(G, R)
    nc = s // L
    x = xs.reshape(b, nc, L, G, R, P)
    Bc = Bm.reshape(b, nc, L, G, N)
    Cc = Cm.reshape(b, nc, L, G, N)
    dt = dt.reshape(b, nc, L, G, R)
    a_cum = jnp.cumsum(dt * A, axis=2)
    xdt = x * dt[..., None]
    acum_t = jnp.transpose(a_cum, (0, 1, 3, 4, 2))
    seg = acum_t[..., :, None] - acum_t[..., None, :]
    causal = jnp.tril(jnp.ones((L, L), dtype=bool))
    decay = jnp.exp(jnp.where(causal, seg, -jnp.inf))
    cb = jnp.einsum('bclgn,bcsgn->bcgls', Cc, Bc)
    y_diag = jnp.einsum('bcgls,bcgrls,bcsgrp->bclgrp', cb, decay, xdt)
    decay_to_end = jnp.exp(a_cum[:, :, -1:] - a_cum)
    states = jnp.einsum('bclgn,bclgr,bclgrp->bcgrpn', Bc, decay_to_end, xdt)
    chunk_decay = jnp.exp(a_cum[:, :, -1])

    def step(carry, inp):
        st, dec = inp
        return carry * dec[..., None, None] + st, carry

    init = jnp.zeros((b, G, R, P, N), states.dtype)
    _, prev = lax.scan(step, init, (jnp.moveaxis(states, 1, 0), jnp.moveaxis(chunk_decay, 1, 0)))
    prev = jnp.moveaxis(prev, 0, 1)
    y_off = jnp.einsum('bclgn,bcgrpn,bclgr->bclgrp', Cc, prev, jnp.exp(a_cum))
    y = y_diag + y_off + x * d_skip.reshape(G, R)[..., None]
    y = y.reshape(b, s, SSD_D_INNER)
    yg = (y * jax.nn.silu(z)).reshape(b, s, G, SSD_D_INNER // G)
    yg = rms_norm(yg, norm_w.reshape(G, SSD_D_INNER // G)).reshape(b, s, SSD_D_INNER)
    return (yg @ w_out).astype(h.dtype)


def mla_mixer(h, positions, w_in, q_norm, kv_norm, w_uq, w_ukv, w_out):
    b, s, _ = h.shape
    H = MLA_N_HEADS
    cq, ckv, k_rope = jnp.split(h @ w_in, [MLA_Q_RANK, MLA_Q_RANK + MLA_KV_RANK], axis=-1)
    q = (rms_norm(cq, q_norm) @ w_uq).reshape(b, s, H, MLA_NOPE + MLA_ROPE)
    q_nope = q[..., :MLA_NOPE]
    q_rope = rope(q[..., MLA_NOPE:], positions)
    kv = (rms_norm(ckv, kv_norm) @ w_ukv).reshape(b, s, H, MLA_NOPE + MLA_V)
    k_nope, v = kv[..., :MLA_NOPE], kv[..., MLA_NOPE:]
    k_rope = rope(k_rope[:, :, None, :], positions)[:, :, 0]
    scale = (MLA_NOPE + MLA_ROPE) ** -0.5
    nb = s // MLA_BLOCK
    qn_blocks = jnp.moveaxis(q_nope.reshape(b, nb, MLA_BLOCK, H, MLA_NOPE), 1, 0)
    qr_blocks = jnp.moveaxis(q_rope.reshape(b, nb, MLA_BLOCK, H, MLA_ROPE), 1, 0)
    key_idx = jnp.arange(s)

    def attend(args):
        qn, qr, i = args
        scores = (jnp.einsum('bqhd,bkhd->bhqk', qn, k_nope)
                  + jnp.einsum('bqhd,bkd->bhqk', qr, k_rope)).astype(jnp.float32) * scale
        q_idx = i * MLA_BLOCK + jnp.arange(MLA_BLOCK)
        scores = jnp.where(key_idx[None, :] <= q_idx[:, None], scores, -jnp.inf)
        p = jax.nn.softmax(scores, axis=-1).astype(v.dtype)
        return jnp.einsum('bhqk,bkhd->bqhd', p, v)

    out = lax.map(attend, (qn_blocks, qr_blocks, jnp.arange(nb)))
    out = jnp.moveaxis(out, 0, 1).reshape(b, s, H * MLA_V)
    return (out @ w_out).astype(h.dtype)


def sg_mixer(h, w_in, b_in, ln_g, ln_b, w_s, b_s, w_out):
    b, s, _ = h.shape
    u, v = jnp.split(jax.nn.gelu(h @ w_in + b_in), 2, axis=-1)
    v = layer_norm(v, ln_g, ln_b)
    nc = s // SG_CHUNK
    v = v.reshape(b, nc, SG_CHUNK, SG_GROUPS, SG_GROUP_DIM)
    causal = jnp.tril(jnp.ones((SG_CHUNK, SG_CHUNK), dtype=w_s.dtype))
    mixed = jnp.einsum('gts,bcsgd->bctgd', w_s * causal, v) + b_s.T[:, :, None]
    return ((u * mixed.reshape(b, s, SG_D)) @ w_out).astype(h.dtype)


def swiglu(h, w_gate, w_up, w_down):
    return (jax.nn.silu(h @ w_gate) * (h @ w_up)) @ w_down


def moe(h, w_router, w_gate, w_up, w_down):
    logits = (h @ w_router).astype(jnp.float32)
    top_vals, top_idx = lax.top_k(logits, TOP_K)
    weights = jax.nn.softmax(top_vals, axis=-1)
    combine = jnp.sum(jax.nn.one_hot(top_idx, N_EXPERTS, dtype=jnp.float32) * weights[..., None], axis=-2)
    combine = combine.astype(h.dtype)
    out = combine[..., 0:1] * swiglu(h, w_gate[0], w_up[0], w_down[0])
    for e in range(1, N_EXPERTS):
        out = out + combine[..., e:e + 1] * swiglu(h, w_gate[e], w_up[e], w_down[e])
    return out


def setup_inputs(seed: int = 0) -> dict:
    key = jax.random.key(seed)
    ks = iter(jax.random.split(key, 48))

    def nrm(shape, scale):
        return jax.random.normal(next(ks), shape, jnp.float32) * scale

    D = D_MODEL
    x = nrm((BATCH, SEQ, D), 1.0)
    c = nrm((BATCH, D), 1.0)
    offset = jax.random.randint(next(ks), (BATCH, 1), 0, 4096, dtype=jnp.int32)
    positions = offset + jnp.arange(SEQ, dtype=jnp.int32)[None, :]
    ada_w = nrm((DEPTH, D, 6 * D), 0.1 * D ** -0.5)
    ada_b = nrm((DEPTH, 6 * D), 0.01)
    ln_g = 1.0 + nrm((DEPTH, 2, D), 0.02)
    ln_b = nrm((DEPTH, 2, D), 0.02)
    ssd_w_in = nrm((N_SSD, D, SSD_IN_DIM), D ** -0.5)
    ssd_conv_w = nrm((N_SSD, SSD_CONV, SSD_CONV_DIM), SSD_CONV ** -0.5)
    ssd_conv_b = nrm((N_SSD, SSD_CONV_DIM), 0.02)
    dt0 = jnp.exp(jax.random.uniform(next(ks), (N_SSD, SSD_N_HEADS), jnp.float32)
                  * (math.log(0.1) - math.log(1e-3)) + math.log(1e-3))
    ssd_dt_bias = dt0 + jnp.log(-jnp.expm1(-dt0))
    ssd_a_log = jnp.log(jax.random.uniform(next(ks), (N_SSD, SSD_N_HEADS), jnp.float32, 1.0, 16.0))
    ssd_d_skip = 1.0 + nrm((N_SSD, SSD_N_HEADS), 0.02)
    ssd_norm_w = 1.0 + nrm((N_SSD, SSD_D_INNER), 0.02)
    ssd_w_out = nrm((N_SSD, SSD_D_INNER, D), BETA * SSD_D_INNER ** -0.5)
    mla_w_in = nrm((N_MLA, D, MLA_IN_DIM), D ** -0.5)
    mla_q_norm = 1.0 + nrm((N_MLA, MLA_Q_RANK), 0.02)
    mla_kv_norm = 1.0 + nrm((N_MLA, MLA_KV_RANK), 0.02)
    mla_w_uq = nrm((N_MLA, MLA_Q_RANK, MLA_N_HEADS * (MLA_NOPE + MLA_ROPE)), MLA_Q_RANK ** -0.5)
    mla_w_ukv = nrm((N_MLA, MLA_KV_RANK, MLA_N_HEADS * (MLA_NOPE + MLA_V)), MLA_KV_RANK ** -0.5)
    mla_w_out = nrm((N_MLA, MLA_N_HEADS * MLA_V, D), BETA * (MLA_N_HEADS * MLA_V) ** -0.5)
    sg_w_in = nrm((N_SG, D, 2 * SG_D), D ** -0.5)
    sg_b_in = nrm((N_SG, 2 * SG_D), 0.02)
    sg_ln_g = 1.0 + nrm((N_SG, SG_D), 0.02)
    sg_ln_b = nrm((N_SG, SG_D), 0.02)
    sg_w_s = nrm((N_SG, SG_GROUPS, SG_CHUNK, SG_CHUNK), SG_CHUNK ** -0.5)
    sg_b_s = 1.0 + nrm((N_SG, SG_GROUPS, SG_CHUNK), 0.02)
    sg_w_out = nrm((N_SG, SG_D, D), BETA * SG_D ** -0.5)
    ffn_w_gate = nrm((N_DENSE, D, D_FF), D ** -0.5)
    ffn_w_up = nrm((N_DENSE, D, D_FF), D ** -0.5)
    ffn_w_down = nrm((N_DENSE, D_FF, D), BETA * D_FF ** -0.5)
    moe_w_router = nrm((N_MOE, D, N_EXPERTS), D ** -0.5)
    moe_w_gate = nrm((N_MOE, N_EXPERTS, D, D_FF), D ** -0.5)
    moe_w_up = nrm((N_MOE, N_EXPERTS, D, D_FF), D ** -0.5)
    moe_w_down = nrm((N_MOE, N_EXPERTS, D_FF, D), BETA * D_FF ** -0.5)
    return {
        'x': x, 'c': c, 'positions': positions,
        'ada_w': ada_w, 'ada_b': ada_b, 'ln_g': ln_g, 'ln_b': ln_b,
        'ssd_w_in': ssd_w_in, 'ssd_conv_w': ssd_conv_w, 'ssd_conv_b': ssd_conv_b,
        'ssd_dt_bias': ssd_dt_bias, 'ssd_a_log': ssd_a_log, 'ssd_d_skip': ssd_d_skip,
        'ssd_norm_w': ssd_norm_w, 'ssd_w_out': ssd_w_out,
        'mla_w_in': mla_w_in, 'mla_q_norm': mla_q_norm, 'mla_kv_norm': mla_kv_norm,
        'mla_w_uq': mla_w_uq, 'mla_w_ukv': mla_w_ukv, 'mla_w_out': mla_w_out,
        'sg_w_in': sg_w_in, 'sg_b_in': sg_b_in, 'sg_ln_g': sg_ln_g, 'sg_ln_b': sg_ln_b,
        'sg_w_s': sg_w_s, 'sg_b_s': sg_b_s, 'sg_w_out': sg_w_out,
        'ffn_w_gate': ffn_w_gate, 'ffn_w_up': ffn_w_up, 'ffn_w_down': ffn_w_down,
        'moe_w_router': moe_w_router, 'moe_w_gate': moe_w_gate, 'moe_w_up': moe_w_up,
        'moe_w_down': moe_w_down,
    }


def reference(x, c, positions, ada_w, ada_b, ln_g, ln_b,
              ssd_w_in, ssd_conv_w, ssd_conv_b, ssd_dt_bias, ssd_a_log, ssd_d_skip,
              ssd_norm_w, ssd_w_out,
              mla_w_in, mla_q_norm, mla_kv_norm, mla_w_uq, mla_w_ukv, mla_w_out,
              sg_w_in, sg_b_in, sg_ln_g, sg_ln_b, sg_w_s, sg_b_s, sg_w_out,
              ffn_w_gate, ffn_w_up, ffn_w_down,
              moe_w_router, moe_w_gate, moe_w_up, moe_w_down):
    cond = jax.nn.silu(c)
    for i in range(DEPTH):
        mod = cond @ ada_w[i] + ada_b[i]
        sh_m, sc_m, g_m, sh_f, sc_f, g_f = [m[:, None, :] for m in jnp.split(mod, 6, axis=-1)]
        hm = x * (1.0 + sc_m) + sh_m
        kind, j = i % N_MIXERS, i // N_MIXERS
        if kind == 0:
            y = ssd_mixer(hm, ssd_w_in[j], ssd_conv_w[j], ssd_conv_b[j], ssd_dt_bias[j],
                          ssd_a_log[j], ssd_d_skip[j], ssd_norm_w[j], ssd_w_out[j])
        elif kind == 1:
            y = mla_mixer(hm, positions, mla_w_in[j], mla_q_norm[j], mla_kv_norm[j],
                          mla_w_uq[j], mla_w_ukv[j], mla_w_out[j])
        else:
            y = sg_mixer(hm, sg_w_in[j], sg_b_in[j], sg_ln_g[j], sg_ln_b[j],
                         sg_w_s[j], sg_b_s[j], sg_w_out[j])
        x = layer_norm(ALPHA * x + (1.0 + g_m) * y, ln_g[i, 0], ln_b[i, 0])
        hf = x * (1.0 + sc_f) + sh_f
        k = i // 2
        if i % 2 == 0:
            y = swiglu(hf, ffn_w_gate[k], ffn_w_up[k], ffn_w_down[k])
        else:
            y = moe(hf, moe_w_router[k], moe_w_gate[k], moe_w_up[k], moe_w_down[k])
        x = layer_norm(ALPHA * x + (1.0 + g_f) * y, ln_g[i, 1], ln_b[i, 1])
    return x
```

```python
import math
from contextlib import ExitStack

import numpy as np
import concourse.bass as bass
import concourse.mybir as mybir
from concourse.bass_utils import run_bass_kernel_spmd

F32 = mybir.dt.float32
BF16 = mybir.dt.bfloat16
I32 = mybir.dt.int32
AF = mybir.ActivationFunctionType
ALU = mybir.AluOpType

D = 1024
DFF = 2816
NE = 8
ALPHA = 8.0 ** 0.25
LN_EPS = 1e-5
RMS_EPS = 1e-6


class Dep:
    __slots__ = ("w", "r")

    def __init__(self):
        self.w = None
        self.r = {}


class DmaSem:
    def __init__(self, sem):
        self.sem = sem
        self.val = 0


class Eng:
    def __init__(self, name, engine, sem):
        self.name = name
        self.e = engine
        self.sem = sem
        self.count = 0
        self.seen = {}


class Sync:
    def __init__(self, nc, stack):
        self.nc = nc
        self.stack = stack
        self.engs = {}
        for name in ("tensor", "vector", "scalar", "gpsimd", "sync"):
            sem = stack.enter_context(nc.semaphore("s_" + name))
            self.engs[name] = Eng(name, getattr(nc, name), sem)
        self.dma_sems = []
        self.n_inst = 0

    def new_dma_sem(self):
        if getattr(self, "free", None):
            d = self.free.pop()
        else:
            sem = self.stack.enter_context(self.nc.semaphore(None))
            d = DmaSem(sem)
            self.dma_sems.append(d)
        if not hasattr(self, "handed"):
            self.handed = []
            self.free = []
        self.handed.append(d)
        return d

    def mark(self):
        if not hasattr(self, "handed"):
            self.handed = []
            self.free = []
        return len(self.handed)

    def release(self, mk):
        self.free.extend(self.handed[mk:])
        del self.handed[mk:]

    def _waits(self, eng, reads, writes):
        need = {}
        for d in reads:
            if d.w is not None and need.get(d.w[0], 0) < d.w[1]:
                need[d.w[0]] = d.w[1]
        for d in writes:
            if d.w is not None and need.get(d.w[0], 0) < d.w[1]:
                need[d.w[0]] = d.w[1]
            for k, v in d.r.items():
                if need.get(k, 0) < v:
                    need[k] = v
        for k, v in need.items():
            if eng.seen.get(k, 0) < v:
                eng.e.wait_ge(k, v)
                eng.seen[k] = v

    def _mark(self, key, val, reads, writes):
        for d in reads:
            d.r[key] = val
        for d in writes:
            d.w = (key, val)
            d.r = {}

    def op(self, en, fn, reads=(), writes=()):
        eng = self.engs[en]
        self._waits(eng, reads, writes)
        ins = fn(eng.e)
        eng.count += 1
        ins.then_inc(eng.sem, 1)
        self.n_inst += 1
        self._mark(eng.sem, eng.count, reads, writes)

    def group(self, en, fns, reads=(), writes=()):
        eng = self.engs[en]
        self._waits(eng, reads, writes)
        ins = None
        for fn in fns:
            ins = fn(eng.e)
            self.n_inst += 1
        eng.count += 1
        ins.then_inc(eng.sem, 1)
        self._mark(eng.sem, eng.count, reads, writes)

    def dma(self, en, dsem, out, in_, reads=(), writes=(), **kw):
        eng = self.engs[en]
        self._waits(eng, reads, writes)
        ins = eng.e.dma_start(out=out, in_=in_, **kw)
        dsem.val += 16
        ins.then_inc(dsem.sem, 16)
        self.n_inst += 1
        self._mark(dsem.sem, dsem.val, reads, writes)

    def barrier(self):
        for eng in self.engs.values():
            for e2 in self.engs.values():
                if e2.count > 0 and eng.seen.get(e2.sem, 0) < e2.count:
                    eng.e.wait_ge(e2.sem, e2.count)
                    eng.seen[e2.sem] = e2.count
            for d in self.dma_sems:
                if d.val > 0 and eng.seen.get(d.sem, 0) < d.val:
                    eng.e.wait_ge(d.sem, d.val)
                    eng.seen[d.sem] = d.val

    def finish(self):
        self.barrier()


class Buf:
    def __init__(self, t, n=1):
        self.t = t
        self.ds = [Dep() for _ in range(n)]

    @property
    def d(self):
        return self.ds[0]


class KB:
    def __init__(self):
        self.nc = bass.Bass("TRN2", target_bir_lowering=False)
        self.st = ExitStack()
        self.S = Sync(self.nc, self.st)
        self.scope = self.st
        self._n = 0
        self.psum = None

    def name(self, p):
        self._n += 1
        return f"{p}{self._n}"

    def din(self, name, shape, dt=F32):
        return self.nc.dram_tensor(name, list(shape), dt, kind="ExternalInput").ap()

    def dout(self, name, shape, dt=F32):
        return self.nc.dram_tensor(name, list(shape), dt, kind="ExternalOutput").ap()

    def dtmp(self, name, shape, dt=F32):
        return self.nc.dram_tensor(name, list(shape), dt, kind="Internal").ap()

    def sb(self, shape, dt, n=1, tag="t"):
        t = self.scope.enter_context(self.nc.sbuf_tensor(self.name(tag), list(shape), dt))
        return Buf(t, n)

    def alloc_psum(self):
        self.psum = [Buf(self.st.enter_context(self.nc.psum_tensor(f"ps{i}", [128, 1024], F32)), 2)
                     for i in range(4)]

    def consts(self):
        S, nc = self.S, self.nc
        ii = self.sb([128, 128], I32, tag="ii")
        self.ident = self.sb([128, 128], F32, tag="ident")
        S.op("gpsimd", lambda e: e.iota(ii.t[:], pattern=[[1, 128]], base=0, channel_multiplier=-1),
             writes=[ii.d])
        S.op("vector", lambda e: e.tensor_single_scalar(out=self.ident.t[:], in_=ii.t[:], scalar=0,
                                                        op=ALU.is_equal), reads=[ii.d], writes=[self.ident.d])
        self.eps_ln = self.sb([128, 1], F32, tag="eps")
        S.op("vector", lambda e: e.memset(self.eps_ln.t[:], LN_EPS), writes=[self.eps_ln.d])

    def setup_cond(self, c_ap):
        S, nc = self.S, self.nc
        ccol = self.sb([128, 8], F32, tag="ccol")
        self.cbc = self.sb([128, 8, 128], F32, tag="cbc")
        self.modw = self.sb([128, 8, 512], F32, tag="modw")
        self.modb = self.sb([128, 1024], F32, tag="modb")
        self.sem_c = S.new_dma_sem()
        self.sem_mw = S.new_dma_sem()
        self.sem_mb = S.new_dma_sem()
        with nc.allow_non_contiguous_dma(reason="tiny column load"):
            S.dma("sync", self.sem_c, ccol.t[:], c_ap.rearrange("(c p) -> p c", p=128), writes=[ccol.d])
        S.op("scalar", lambda e: e.activation(out=ccol.t[:], in_=ccol.t[:], func=AF.Silu),
             reads=[ccol.d], writes=[ccol.d])
        S.op("vector", lambda e: e.tensor_copy(out=self.cbc.t[:],
                                               in_=ccol.t[:].unsqueeze(2).to_broadcast([128, 8, 128])),
             reads=[ccol.d], writes=[self.cbc.d])

    def mod_vec(self, out_buf, ada_w_l, ada_b_l, idx, plus_one):
        S = self.S
        ps = self.psum[0]
        S.dma("sync", self.sem_mb, self.modb.t[:],
              ada_b_l[idx * 1024:(idx + 1) * 1024].unsqueeze(0).to_broadcast([128, 1024]),
              writes=[self.modb.d])
        for hb in range(2):
            c0 = idx * 1024 + hb * 512
            S.dma("sync", self.sem_mw, self.modw.t[:],
                  ada_w_l[:, c0:c0 + 512].rearrange("(c p) f -> p c f", p=128), writes=[self.modw.d])
            fns = [(lambda e, k=k: e.matmul(ps.t[:, hb * 512:(hb + 1) * 512], lhsT=self.cbc.t[:, k, :],
                                             rhs=self.modw.t[:, k, :], start=(k == 0), stop=(k == 7)))
                   for k in range(8)]
            S.group("tensor", fns, reads=[self.cbc.d, self.modw.d], writes=[ps.ds[hb]])
        if plus_one:
            S.op("vector", lambda e: e.scalar_tensor_tensor(out=out_buf.t[:], in0=ps.t[:], scalar=1.0,
                                                            in1=self.modb.t[:], op0=ALU.add, op1=ALU.add),
                 reads=[ps.ds[0], ps.ds[1], self.modb.d], writes=[out_buf.d])
        else:
            S.op("vector", lambda e: e.tensor_tensor(out=out_buf.t[:], in0=ps.t[:], in1=self.modb.t[:],
                                                     op=ALU.add),
                 reads=[ps.ds[0], ps.ds[1], self.modb.d], writes=[out_buf.d])

    def bcast_vec(self, out_buf, vec_ap, sem):
        n = vec_ap.shape[0]
        self.S.dma("sync", sem, out_buf.t[:], vec_ap.unsqueeze(0).to_broadcast([128, n]), writes=[out_buf.d])

    def ln_alloc(self):
        self.ln_st = self.sb([128, 2, 6], F32, tag="lnst")
        self.ln_mv = self.sb([128, 2], F32, tag="lnmv")
        self.ln_rs = self.sb([128, 1], F32, tag="lnrs")

    def layer_norm(self, r, out, g_bc, b_bc):
        S = self.S
        st, mv, rs = self.ln_st, self.ln_mv, self.ln_rs
        for hb in range(2):
            S.op("vector", lambda e, hb=hb: e.bn_stats(out=st.t[:, hb, :], in_=r.t[:, hb * 512:(hb + 1) * 512]),
                 reads=[r.d], writes=[st.d])
        S.op("vector", lambda e: e.bn_aggr(out=mv.t[:], in_=st.t[:].rearrange("p a b -> p (a b)")),
             reads=[st.d], writes=[mv.d])
        S.op("scalar", lambda e: e.activation(out=rs.t[:], in_=mv.t[:, 1:2], func=AF.Sqrt,
                                              bias=self.eps_ln.t[:], scale=1.0),
             reads=[mv.d, self.eps_ln.d], writes=[rs.d])
        S.op("vector", lambda e: e.reciprocal(out=rs.t[:], in_=rs.t[:]), reads=[rs.d], writes=[rs.d])
        S.op("vector", lambda e: e.tensor_scalar(out=r.t[:], in0=r.t[:], scalar1=mv.t[:, 0:1], scalar2=rs.t[:],
                                                 op0=ALU.subtract, op1=ALU.mult),
             reads=[r.d, mv.d, rs.d], writes=[r.d])
        S.op("vector", lambda e: e.tensor_tensor(out=r.t[:], in0=r.t[:], in1=g_bc.t[:], op=ALU.mult),
             reads=[r.d, g_bc.d], writes=[r.d])
        S.op("vector", lambda e: e.tensor_tensor(out=out.t[:], in0=r.t[:], in1=b_bc.t[:], op=ALU.add),
             reads=[r.d, b_bc.d], writes=[out.d])


class DramStream:
    def __init__(self, ap, nt):
        self.ap = ap
        self.ds = [Dep() for _ in range(nt // 128)]


def sub_ffn(K, xs, xd, NT, ada_w_l, ada_b_l, lng_ap, lnb_ap, wg_ap, wu_ap, wd_ap, wr_ap=None):
    S, nc = K.S, K.nc
    moe = wr_ap is not None
    import os as _os
    E = int(_os.environ.get('MOE_E', NE)) if moe else 1
    T = min(NT, 2048)
    NSUP, NSUB, NB = NT // T, T // 128, T // 512
    JG = 2
    NG = DFF // (128 * JG)
    old_scope = K.scope
    _mk = K.S.mark()
    with ExitStack() as sc:
        K.scope = sc
        xT = K.sb([128, 8, T], BF16, n=NB, tag="xT")
        acc = K.sb([128, NSUB, 1024], F32, n=NSUB, tag="acc")
        wgb = [K.sb([128, 8, 128 * JG], BF16, tag="wg") for _ in range(2)]
        wub = [K.sb([128, 8, 128 * JG], BF16, tag="wu") for _ in range(2)]
        wdb = [K.sb([128, JG, 1024], BF16, tag="wd") for _ in range(2)]
        wsem = [S.new_dma_sem() for _ in range(2)]
        hT = [K.sb([128, JG, 512], BF16, n=JG, tag="hT") for _ in range(2)]
        sg = [K.sb([128, 512], F32, tag="sg") for _ in range(2)]
        xin = [K.sb([128, 1024], F32, tag="xin") for _ in range(2)]
        xsem = [S.new_dma_sem() for _ in range(2)]
        hf = [K.sb([128, 1024], F32, tag="hf") for _ in range(2)]
        xo = [K.sb([128, 1024], F32, tag="xo") for _ in range(2)]
        osem = [S.new_dma_sem() for _ in range(2)]
        sc1p = K.sb([128, 1024], F32, tag="sc1p")
        shf = K.sb([128, 1024], F32, tag="shf")
        g1p = K.sb([128, 1024], F32, tag="g1p")
        lng = K.sb([128, 1024], F32, tag="lng")
        lnb = K.sb([128, 1024], F32, tag="lnb")
        csem = S.new_dma_sem()
        K.ln_alloc()
        K.mod_vec(shf, ada_w_l, ada_b_l, 3, False)
        K.mod_vec(sc1p, ada_w_l, ada_b_l, 4, True)
        K.mod_vec(g1p, ada_w_l, ada_b_l, 5, True)
        K.bcast_vec(lng, lng_ap, csem)
        K.bcast_vec(lnb, lnb_ap, S.new_dma_sem())
        if moe:
            hfT32 = K.sb([128, 8, 128], F32, tag="hfT32")
            wr = K.sb([128, 8, NE], F32, tag="wr")
            if _os.environ.get("DBG4") != "1":
                with nc.allow_non_contiguous_dma(reason="router weights, tiny"):
                    S.dma("sync", S.new_dma_sem(), wr.t[:], wr_ap.rearrange("(c p) e -> p c e", p=128), writes=[wr.d])
            comb = K.sb([128, NSUB, NE], F32, tag="comb")
            lgall = K.sb([128, NSUB, NE], F32, tag="lgall")
            l2 = K.sb([128, NSUB, NE], F32, tag="l2")
            mk1 = K.sb([128, NSUB, NE], F32, tag="mk1")
            mk2 = K.sb([128, NSUB, NE], F32, tag="mk2")
            m1 = K.sb([128, NSUB], F32, tag="m1")
            m2 = K.sb([128, NSUB], F32, tag="m2")
            w1 = K.sb([128, NSUB], F32, tag="w1")
            w2 = K.sb([128, NSUB], F32, tag="w2")
        psG = [K.psum[0], K.psum[1]]
        psO = [K.psum[2], K.psum[3]]

        def wsrc(ap, e):
            return ap[e % ap.shape[0]] if moe else ap

        def load_w(e, jg, slot):
            f0 = jg * 128 * JG
            S.dma("gpsimd", wsem[slot], wgb[slot].t[:],
                  wsrc(wg_ap, e)[:, f0:f0 + 128 * JG].rearrange("(c p) f -> p c f", p=128), writes=[wgb[slot].d])
            S.dma("gpsimd", wsem[slot], wub[slot].t[:],
                  wsrc(wu_ap, e)[:, f0:f0 + 128 * JG].rearrange("(c p) f -> p c f", p=128), writes=[wub[slot].d])
            S.dma("gpsimd", wsem[slot], wdb[slot].t[:],
                  wsrc(wd_ap, e)[f0:f0 + 128 * JG, :].rearrange("(j p) d -> p j d", p=128), writes=[wdb[slot].d])
            wgb[slot].d.w = wub[slot].d.w = wdb[slot].d.w

        wlist = [(e, jg) for e in range(E) for jg in range(NG)]
        for sup in range(NSUP):
            t0 = sup * T
            load_w(*wlist[0], 0)
            for ts in range(NSUB):
                b = ts % 2
                gi = (t0 // 128) + ts
                S.dma("sync", xsem[b], xin[b].t[:], xs.ap[gi * 128:(gi + 1) * 128, :],
                      reads=[xs.ds[gi]], writes=[xin[b].d])
                S.op("vector", lambda e, b=b: e.tensor_tensor(out=hf[b].t[:], in0=xin[b].t[:], in1=sc1p.t[:],
                                                              op=ALU.mult),
                     reads=[xin[b].d, sc1p.d], writes=[hf[b].d])
                S.op("vector", lambda e, b=b: e.tensor_tensor(out=hf[b].t[:], in0=hf[b].t[:], in1=shf.t[:],
                                                              op=ALU.add),
                     reads=[hf[b].d, shf.d], writes=[hf[b].d])
                po = psO[b]
                for hb in range(2):
                    fns = [(lambda e, k=k, b=b, po=po: e.transpose(out=po.t[:, k * 128:(k + 1) * 128],
                                                                    in_=hf[b].t[:, k * 128:(k + 1) * 128],
                                                                    identity=K.ident.t[:]))
                           for k in range(hb * 4, hb * 4 + 4)]
                    S.group("tensor", fns, reads=[hf[b].d, K.ident.d], writes=[po.ds[hb]])
                if not moe:
                    S.op("scalar", lambda e, po=po, ts=ts: e.activation(
                        out=xT.t[:, :, ts * 128:(ts + 1) * 128], in_=po.t[:].rearrange("p (c t) -> p c t", c=8),
                        func=AF.Copy), reads=[po.ds[0], po.ds[1]], writes=[xT.ds[ts // 4]])
                else:
                    S.op("scalar", lambda e, po=po: e.activation(
                        out=hfT32.t[:], in_=po.t[:].rearrange("p (c t) -> p c t", c=8), func=AF.Copy),
                         reads=[po.ds[0], po.ds[1]], writes=[hfT32.d])
                    S.op("gpsimd", lambda e, ts=ts: e.tensor_copy(out=xT.t[:, :, ts * 128:(ts + 1) * 128],
                                                                  in_=hfT32.t[:]),
                         reads=[hfT32.d], writes=[xT.ds[ts // 4]])
                if moe:
                    pl = psG[0]
                    fns = [(lambda e, k=k, pl=pl: e.matmul(pl.t[:, 0:NE], lhsT=hfT32.t[:, k, :], rhs=wr.t[:, k, :],
                                                           start=(k == 0), stop=(k == 7))) for k in range(8)]
                    import os as _os
                    if _os.environ.get("MOEDBG") == "1":
                        S.op("vector", lambda e, ts=ts: e.memset(lgall.t[:, ts, :], 0.0), writes=[lgall.d])
                    else:
                        S.group("tensor", fns, reads=[hfT32.d, wr.d], writes=[pl.ds[0]])
                        S.op("vector", lambda e, pl=pl, ts=ts: e.tensor_copy(out=lgall.t[:, ts, :], in_=pl.t[:, 0:NE]),
                             reads=[pl.ds[0]], writes=[lgall.d])
            if moe and _os.environ.get('DBG3') == '1':
                S.op('vector', lambda e: e.memset(comb.t[:], 0.125), writes=[comb.d])
            elif moe:
                X = mybir.AxisListType.X
                bc = lambda b_: b_.t[:].unsqueeze(2).to_broadcast([128, NSUB, NE])
                S.op("vector", lambda e: e.tensor_reduce(out=m1.t[:], in_=lgall.t[:], axis=X, op=ALU.max),
                     reads=[lgall.d], writes=[m1.d])
                S.op("vector", lambda e: e.tensor_tensor(out=mk1.t[:], in0=lgall.t[:], in1=bc(m1), op=ALU.is_equal),
                     reads=[lgall.d, m1.d], writes=[mk1.d])
                S.op("vector", lambda e: e.scalar_tensor_tensor(out=l2.t[:], in0=mk1.t[:], scalar=-1e30,
                                                                in1=lgall.t[:], op0=ALU.mult, op1=ALU.add),
                     reads=[mk1.d, lgall.d], writes=[l2.d])
                S.op("vector", lambda e: e.tensor_reduce(out=m2.t[:], in_=l2.t[:], axis=X, op=ALU.max),
                     reads=[l2.d], writes=[m2.d])
                S.op("vector", lambda e: e.tensor_tensor(out=mk2.t[:], in0=l2.t[:], in1=bc(m2), op=ALU.is_equal),
                     reads=[l2.d, m2.d], writes=[mk2.d])
                S.op("vector", lambda e: e.tensor_tensor(out=w2.t[:], in0=m2.t[:], in1=m1.t[:], op=ALU.subtract),
                     reads=[m1.d, m2.d], writes=[w2.d])
                S.op("scalar", lambda e: e.activation(out=w2.t[:], in_=w2.t[:], func=AF.Exp),
                     reads=[w2.d], writes=[w2.d])
                S.op("vector", lambda e: e.tensor_scalar(out=w1.t[:], in0=w2.t[:], scalar1=1.0, scalar2=None,
                                                         op0=ALU.add), reads=[w2.d], writes=[w1.d])
                S.op("vector", lambda e: e.reciprocal(out=w1.t[:], in_=w1.t[:]), reads=[w1.d], writes=[w1.d])
                S.op("vector", lambda e: e.tensor_tensor(out=w2.t[:], in0=w2.t[:], in1=w1.t[:], op=ALU.mult),
                     reads=[w1.d, w2.d], writes=[w2.d])
                S.op("vector", lambda e: e.tensor_tensor(out=mk1.t[:], in0=mk1.t[:], in1=bc(w1), op=ALU.mult),
                     reads=[mk1.d, w1.d], writes=[mk1.d])
                S.op("vector", lambda e: e.tensor_tensor(out=mk2.t[:], in0=mk2.t[:], in1=bc(w2), op=ALU.mult),
                     reads=[mk2.d, w2.d], writes=[mk2.d])
                S.op("vector", lambda e: e.tensor_tensor(out=comb.t[:], in0=mk1.t[:], in1=mk2.t[:], op=ALU.add),
                     reads=[mk1.d, mk2.d], writes=[comb.d])
            for ts in range(NSUB):
                S.op("gpsimd", lambda e, ts=ts: e.memset(acc.t[:, ts, :], 0.0), writes=[acc.ds[ts]])
            its = [(wi, tb) for wi in range(len(wlist)) for tb in range(NB)]

            def emit_gu(n):
                wi, tb = its[n]
                slot = wi % 2
                hb_ = hT[n % 2]
                for j in range(JG):
                    pg = psG[j % 2]
                    for which, wbuf in ((0, wgb[slot]), (1, wub[slot])):
                        fns = [(lambda e, k=k, j=j, which=which, wbuf=wbuf, pg=pg, tb=tb: e.matmul(
                            pg.t[:, which * 512:(which + 1) * 512], lhsT=wbuf.t[:, k, j * 128:(j + 1) * 128],
                            rhs=xT.t[:, k, tb * 512:(tb + 1) * 512], start=(k == 0), stop=(k == 7)))
                            for k in range(8)]
                        S.group("tensor", fns, reads=[wbuf.d, xT.ds[tb]], writes=[pg.ds[which]])
                    sgb = sg[j % 2]
                    S.op("scalar", lambda e, pg=pg, sgb=sgb: e.activation(out=sgb.t[:], in_=pg.t[:, 0:512],
                                                                         func=AF.Silu),
                         reads=[pg.ds[0]], writes=[sgb.d])
                    S.op("vector", lambda e, pg=pg, sgb=sgb, hb_=hb_, j=j: e.tensor_tensor(
                        out=hb_.t[:, j, :], in0=sgb.t[:], in1=pg.t[:, 512:1024], op=ALU.mult),
                         reads=[sgb.d, pg.ds[1]], writes=[hb_.ds[j]])

            def emit_d(n):
                wi, tb = its[n]
                slot = wi % 2
                e_ = wlist[wi][0]
                hb_ = hT[n % 2]
                for q in range(4):
                    ts = tb * 4 + q
                    po = psO[q % 2]
                    for db in range(2):
                        fns = [(lambda e, j=j, q=q, db=db, po=po, hb_=hb_, slot=slot: e.matmul(
                            po.t[:, db * 512:(db + 1) * 512], lhsT=hb_.t[:, j, q * 128:(q + 1) * 128],
                            rhs=wdb[slot].t[:, j, db * 512:(db + 1) * 512], start=(j == 0),
                            stop=(j == JG - 1))) for j in range(JG)]
                        S.group("tensor", fns, reads=[hb_.ds[0], hb_.ds[1], wdb[slot].d], writes=[po.ds[db]])
                    cs = comb.t[:, ts, e_:e_ + 1] if (moe and _os.environ.get('DBG2') != '1') else 1.0
                    rd = [po.ds[0], po.ds[1], acc.ds[ts]] + ([comb.d] if moe else [])
                    S.op("vector", lambda e, po=po, ts=ts, cs=cs: e.scalar_tensor_tensor(
                        out=acc.t[:, ts, :], in0=po.t[:], scalar=cs, in1=acc.t[:, ts, :], op0=ALU.mult,
                        op1=ALU.add), reads=rd, writes=[acc.ds[ts]])

            for n in range(len(its)):
                wi, tb = its[n]
                emit_gu(n)
                if n > 0:
                    emit_d(n - 1)
                if tb == 0 and wi + 1 < len(wlist):
                    load_w(*wlist[wi + 1], (wi + 1) % 2)
            emit_d(len(its) - 1)
            for ts in range(NSUB):
                b = ts % 2
                gi = (t0 // 128) + ts
                S.dma("sync", xsem[b], xin[b].t[:], xs.ap[gi * 128:(gi + 1) * 128, :],
                      reads=[xs.ds[gi]], writes=[xin[b].d])
                S.op("vector", lambda e, ts=ts, b=b: e.tensor_tensor(out=hf[b].t[:], in0=acc.t[:, ts, :],
                                                                    in1=g1p.t[:], op=ALU.mult),
                     reads=[acc.ds[ts], g1p.d], writes=[hf[b].d])
                S.op("vector", lambda e, b=b: e.scalar_tensor_tensor(out=hf[b].t[:], in0=xin[b].t[:], scalar=ALPHA,
                                                                     in1=hf[b].t[:], op0=ALU.mult, op1=ALU.add),
                     reads=[xin[b].d, hf[b].d], writes=[hf[b].d])
                K.layer_norm(hf[b], xo[b], lng, lnb)
                S.dma("gpsimd", osem[b], xd.ap[gi * 128:(gi + 1) * 128, :], xo[b].t[:],
                      reads=[xo[b].d], writes=[xd.ds[gi]])
        S.barrier()
    K.scope = old_scope
    K.S.release(_mk)


def residual_ln(K, y_ap, y_deps, xin, g1p, lng, lnb, tmp, xo):
    S = K.S
    S.op("vector", lambda e: e.tensor_tensor(out=tmp.t[:], in0=y_ap, in1=g1p.t[:], op=ALU.mult),
         reads=list(y_deps) + [g1p.d], writes=[tmp.d])
    S.op("vector", lambda e: e.scalar_tensor_tensor(out=tmp.t[:], in0=xin.t[:], scalar=ALPHA, in1=tmp.t[:],
                                                    op0=ALU.mult, op1=ALU.add),
         reads=[xin.d, tmp.d], writes=[tmp.d])
    K.layer_norm(tmp, xo, lng, lnb)


def sub_proj(K, xs, xd, NT, mT_ap, DM, wout_ap, ada_w_l, ada_b_l, lng_ap, lnb_ap):
    S, nc = K.S, K.nc
    NC_ = DM // 128
    old_scope = K.scope
    _mk = K.S.mark()
    with ExitStack() as sc:
        K.scope = sc
        wout = K.sb([128, NC_, 1024], BF16, tag="wout")
        S.dma("gpsimd", S.new_dma_sem(), wout.t[:], wout_ap.rearrange("(c p) d -> p c d", p=128), writes=[wout.d])
        g1p = K.sb([128, 1024], F32, tag="g1p")
        lng = K.sb([128, 1024], F32, tag="lng")
        lnb = K.sb([128, 1024], F32, tag="lnb")
        K.ln_alloc()
        K.mod_vec(g1p, ada_w_l, ada_b_l, 2, True)
        K.bcast_vec(lng, lng_ap, S.new_dma_sem())
        K.bcast_vec(lnb, lnb_ap, S.new_dma_sem())
        mt = [K.sb([128, NC_, 128], BF16, tag="mt") for _ in range(2)]
        msem = [S.new_dma_sem() for _ in range(2)]
        xin = [K.sb([128, 1024], F32, tag="xin") for _ in range(2)]
        xsem = [S.new_dma_sem() for _ in range(2)]
        tmp = [K.sb([128, 1024], F32, tag="tmp") for _ in range(2)]
        xo = [K.sb([128, 1024], F32, tag="xo") for _ in range(2)]
        osem = [S.new_dma_sem() for _ in range(2)]
        for ts in range(NT // 128):
            b = ts % 2
            S.dma("sync", msem[b], mt[b].t[:], mT_ap[:, ts * 128:(ts + 1) * 128].rearrange("(c p) t -> p c t", p=128),
                  writes=[mt[b].d])
            S.dma("sync", xsem[b], xin[b].t[:], xs.ap[ts * 128:(ts + 1) * 128, :], reads=[xs.ds[ts]],
                  writes=[xin[b].d])
            po = K.psum[2 + b]
            for db in range(2):
                fns = [(lambda e, c=c, db=db, po=po, b=b: e.matmul(
                    po.t[:, db * 512:(db + 1) * 512], lhsT=mt[b].t[:, c, :], rhs=wout.t[:, c, db * 512:(db + 1) * 512],
                    start=(c == 0), stop=(c == NC_ - 1))) for c in range(NC_)]
                S.group("tensor", fns, reads=[mt[b].d, wout.d], writes=[po.ds[db]])
            residual_ln(K, po.t[:], po.ds, xin[b], g1p, lng, lnb, tmp[b], xo[b])
            S.dma("gpsimd", osem[b], xd.ap[ts * 128:(ts + 1) * 128, :], xo[b].t[:], reads=[xo[b].d],
                  writes=[xd.ds[ts]])
        S.barrier()
    K.scope = old_scope
    K.S.release(_mk)


def sub_sg(K, xs, xd, NT, ada_w_l, ada_b_l, lng_ap, lnb_ap, w_in_ap, b_in_ap, sglng_ap, sglnb_ap, w_s_ap, b_s_ap,
           w_out_ap):
    S, nc = K.S, K.nc
    old_scope = K.scope
    _mk = K.S.mark()
    with ExitStack() as sc:
        K.scope = sc
        win = K.sb([128, 8, 4096], BF16, tag="win")
        S.dma("gpsimd", S.new_dma_sem(), win.t[:, :, 0:2048],
              w_in_ap[:, 0:2048].rearrange("(c p) f -> p c f", p=128), writes=[win.d])
        S.dma("gpsimd", S.new_dma_sem(), win.t[:, :, 2048:4096],
              w_in_ap[:, 2048:4096].rearrange("(c p) f -> p c f", p=128), writes=[win.d])
        wout = K.sb([128, 16, 1024], BF16, tag="wout")
        S.dma("gpsimd", S.new_dma_sem(), wout.t[:], w_out_ap.rearrange("(c p) d -> p c d", p=128), writes=[wout.d])
        binb = K.sb([1, 4096], BF16, tag="binb")
        S.dma("gpsimd", S.new_dma_sem(), binb.t[:], b_in_ap.unsqueeze(0), writes=[binb.d])
        bsb = K.sb([1, 8, 128], BF16, tag="bsb")
        S.dma("gpsimd", S.new_dma_sem(), bsb.t[:], b_s_ap.unsqueeze(0), writes=[bsb.d])
        ones = K.sb([1, 512], BF16, tag="ones")
        S.op("vector", lambda e: e.memset(ones.t[:], 1.0), writes=[ones.d])
        sglng = K.sb([128, 2048], F32, tag="sglng")
        sglnb = K.sb([128, 2048], F32, tag="sglnb")
        K.bcast_vec(sglng, sglng_ap, S.new_dma_sem())
        K.bcast_vec(sglnb, sglnb_ap, S.new_dma_sem())
        sc1p = K.sb([128, 1024], F32, tag="sc1p")
        shm = K.sb([128, 1024], F32, tag="shm")
        g1p = K.sb([128, 1024], F32, tag="g1p")
        lng = K.sb([128, 1024], F32, tag="lng")
        lnb = K.sb([128, 1024], F32, tag="lnb")
        K.ln_alloc()
        K.mod_vec(shm, ada_w_l, ada_b_l, 0, False)
        K.mod_vec(sc1p, ada_w_l, ada_b_l, 1, True)
        K.mod_vec(g1p, ada_w_l, ada_b_l, 2, True)
        K.bcast_vec(lng, lng_ap, S.new_dma_sem())
        K.bcast_vec(lnb, lnb_ap, S.new_dma_sem())
        wmT = K.sb([128, 8, 128], BF16, tag="wmT")
        sc_setup = ExitStack()
        K.scope = sc_setup
        wsf = K.sb([128, 8, 128], F32, tag="wsf")
        S.dma("sync", S.new_dma_sem(), wsf.t[:], w_s_ap.rearrange("g t s -> t g s"), writes=[wsf.d])
        ii = K.sb([128, 128], I32, tag="ii2")
        msk = K.sb([128, 128], F32, tag="msk")
        S.op("gpsimd", lambda e: e.iota(ii.t[:], pattern=[[1, 128]], base=0, channel_multiplier=-1), writes=[ii.d])
        S.op("vector", lambda e: e.tensor_single_scalar(out=msk.t[:], in_=ii.t[:], scalar=0, op=ALU.is_le),
             reads=[ii.d], writes=[msk.d])
        S.op("vector", lambda e: e.tensor_tensor(out=wsf.t[:], in0=wsf.t[:],
                                                 in1=msk.t[:].unsqueeze(1).to_broadcast([128, 8, 128]), op=ALU.mult),
             reads=[wsf.d, msk.d], writes=[wsf.d])
        pt = K.psum[0]
        for hb in range(2):
            fns = [(lambda e, g=g: e.transpose(out=pt.t[:, g * 128:(g + 1) * 128], in_=wsf.t[:, g, :],
                                               identity=K.ident.t[:])) for g in range(hb * 4, hb * 4 + 4)]
            S.group("tensor", fns, reads=[wsf.d, K.ident.d], writes=[pt.ds[hb]])
        S.op("scalar", lambda e: e.activation(out=wmT.t[:], in_=pt.t[:].rearrange("p (g t) -> p g t", g=8),
                                              func=AF.Copy), reads=[pt.ds[0], pt.ds[1]], writes=[wmT.d])
        S.barrier()
        sc_setup.close()
        K.scope = sc
        xin = K.sb([128, 1024], F32, tag="xin")
        xsem = S.new_dma_sem()
        hf = K.sb([128, 1024], F32, tag="hf")
        hmT = K.sb([128, 8, 128], BF16, tag="hmT")
        u = K.sb([128, 2048], F32, tag="u")
        v = K.sb([128, 2048], F32, n=1, tag="v")
        vn = K.sb([128, 2048], BF16, tag="vn")
        gT = Buf(vn.t, 1)
        gT.ds = vn.ds
        gTv = vn.t[:].rearrange("p (c t) -> p c t", c=16)
        xo = hf
        osem = S.new_dma_sem()
        st4 = K.sb([128, 4, 6], F32, tag="st4")
        mv = K.sb([128, 2], F32, tag="mv2")
        rs = K.sb([128, 1], F32, tag="rs2")
        for ts in range(NT // 128):
            S.dma("sync", xsem, xin.t[:], xs.ap[ts * 128:(ts + 1) * 128, :], reads=[xs.ds[ts]], writes=[xin.d])
            S.op("vector", lambda e: e.tensor_tensor(out=hf.t[:], in0=xin.t[:], in1=sc1p.t[:], op=ALU.mult),
                 reads=[xin.d, sc1p.d], writes=[hf.d])
            S.op("vector", lambda e: e.tensor_tensor(out=hf.t[:], in0=hf.t[:], in1=shm.t[:], op=ALU.add),
                 reads=[hf.d, shm.d], writes=[hf.d])
            po = K.psum[0]
            for hb in range(2):
                fns = [(lambda e, k=k: e.transpose(out=po.t[:, k * 128:(k + 1) * 128],
                                                   in_=hf.t[:, k * 128:(k + 1) * 128], identity=K.ident.t[:]))
                       for k in range(hb * 4, hb * 4 + 4)]
                S.group("tensor", fns, reads=[hf.d, K.ident.d], writes=[po.ds[hb]])
            S.op("scalar", lambda e: e.activation(out=hmT.t[:], in_=po.t[:].rearrange("p (c t) -> p c t", c=8),
                                                  func=AF.Copy), reads=[po.ds[0], po.ds[1]], writes=[hmT.d])
            for cb in range(8):
                pb = K.psum[(cb // 2) % 2 + 0]
                half = cb % 2
                fns = [(lambda e, k=k, cb=cb, pb=pb, half=half: e.matmul(
                    pb.t[:, half * 512:(half + 1) * 512], lhsT=hmT.t[:, k, :], rhs=win.t[:, k, cb * 512:(cb + 1) * 512],
                    start=(k == 0), stop=False)) for k in range(8)]
                fns.append(lambda e, cb=cb, pb=pb, half=half: e.matmul(
                    pb.t[:, half * 512:(half + 1) * 512], lhsT=ones.t[0:1, 0:128], rhs=binb.t[0:1, cb * 512:(cb + 1) * 512],
                    start=False, stop=True))
                S.group("tensor", fns, reads=[hmT.d, win.d, ones.d, binb.d], writes=[pb.ds[half]])
                dst = u if cb < 4 else v
                c0 = (cb % 4) * 512
                S.op("scalar", lambda e, pb=pb, half=half, dst=dst, c0=c0: e.activation(
                    out=dst.t[:, c0:c0 + 512], in_=pb.t[:, half * 512:(half + 1) * 512], func=AF.Gelu_apprx_tanh),
                     reads=[pb.ds[half]], writes=[dst.d])
            for q in range(4):
                S.op("vector", lambda e, q=q: e.bn_stats(out=st4.t[:, q, :], in_=v.t[:, q * 512:(q + 1) * 512]),
                     reads=[v.d], writes=[st4.d])
            S.op("vector", lambda e: e.bn_aggr(out=mv.t[:], in_=st4.t[:].rearrange("p a b -> p (a b)")),
                 reads=[st4.d], writes=[mv.d])
            S.op("scalar", lambda e: e.activation(out=rs.t[:], in_=mv.t[:, 1:2], func=AF.Sqrt, bias=K.eps_ln.t[:],
                                                  scale=1.0), reads=[mv.d, K.eps_ln.d], writes=[rs.d])
            S.op("vector", lambda e: e.reciprocal(out=rs.t[:], in_=rs.t[:]), reads=[rs.d], writes=[rs.d])
            S.op("vector", lambda e: e.tensor_scalar(out=v.t[:], in0=v.t[:], scalar1=mv.t[:, 0:1], scalar2=rs.t[:],
                                                     op0=ALU.subtract, op1=ALU.mult),
                 reads=[v.d, mv.d, rs.d], writes=[v.d])
            S.op("vector", lambda e: e.tensor_tensor(out=v.t[:], in0=v.t[:], in1=sglng.t[:], op=ALU.mult),
                 reads=[v.d, sglng.d], writes=[v.d])
            S.op("vector", lambda e: e.tensor_tensor(out=vn.t[:], in0=v.t[:], in1=sglnb.t[:], op=ALU.add),
                 reads=[v.d, sglnb.d], writes=[vn.d])
            for g in range(8):
                pm = K.psum[2 + g // 4]
                half = (g % 4) // 2
                c0 = (g % 4) * 256
                fns = [lambda e, g=g, pm=pm, c0=c0: e.matmul(pm.t[:, c0:c0 + 256], lhsT=wmT.t[:, g, :],
                                                              rhs=vn.t[:, g * 256:(g + 1) * 256], start=True, stop=False),
                       lambda e, g=g, pm=pm, c0=c0: e.matmul(pm.t[:, c0:c0 + 256], lhsT=bsb.t[0:1, g, :],
                                                              rhs=ones.t[0:1, 0:256], start=False, stop=True)]
                S.group("tensor", fns, reads=[wmT.d, vn.d, bsb.d, ones.d], writes=[pm.ds[half]])
            for h2 in range(2):
                pm = K.psum[2 + h2]
                S.op("vector", lambda e, pm=pm, h2=h2: e.tensor_tensor(
                    out=v.t[:, h2 * 1024:(h2 + 1) * 1024], in0=pm.t[:], in1=u.t[:, h2 * 1024:(h2 + 1) * 1024],
                    op=ALU.mult), reads=[pm.ds[0], pm.ds[1], u.d], writes=[v.d])
            for h2 in range(2):
                pt2 = K.psum[h2]
                for hb in range(2):
                    fns = [(lambda e, c=c, pt2=pt2, h2=h2: e.transpose(
                        out=pt2.t[:, (c % 8) * 128:(c % 8 + 1) * 128], in_=v.t[:, c * 128:(c + 1) * 128],
                        identity=K.ident.t[:])) for c in range(h2 * 8 + hb * 4, h2 * 8 + hb * 4 + 4)]
                    S.group("tensor", fns, reads=[v.d, K.ident.d], writes=[pt2.ds[hb]])
                S.op("scalar", lambda e, pt2=pt2, h2=h2: e.activation(
                    out=gTv[:, h2 * 8:(h2 + 1) * 8, :], in_=pt2.t[:].rearrange("p (c t) -> p c t", c=8),
                    func=AF.Copy), reads=[pt2.ds[0], pt2.ds[1]], writes=[gT.d])
            py = K.psum[2]
            for db in range(2):
                fns = [(lambda e, c=c, db=db: e.matmul(py.t[:, db * 512:(db + 1) * 512], lhsT=gTv[:, c, :],
                                                        rhs=wout.t[:, c, db * 512:(db + 1) * 512], start=(c == 0),
                                                        stop=(c == 15))) for c in range(16)]
                S.group("tensor", fns, reads=[gT.d, wout.d], writes=[py.ds[db]])
            residual_ln(K, py.t[:], py.ds, xin, g1p, lng, lnb, hf, xo)
            S.dma("gpsimd", osem, xd.ap[ts * 128:(ts + 1) * 128, :], xo.t[:], reads=[xo.d], writes=[xd.ds[ts]])
        S.barrier()
    K.scope = old_scope
    K.S.release(_mk)


def build_ssd(NTok):
    K = KB()
    x = K.din("x", [NTok, D]); c = K.din("c", [D]); aw = K.din("aw", [D, 6 * D]); ab = K.din("ab", [6 * D])
    w = K.din("w", [D, 1544]); cw = K.din("cw", [4, 1024]); cbv = K.din("cb", [1024])
    dtb = K.din("dtb", [8]); alog = K.din("alog", [8]); dsk = K.din("dsk", [8]); nw = K.din("nw", [512])
    out = K.dout("mT", [512, NTok], BF16)
    K.alloc_psum(); K.consts(); K.setup_cond(c)
    sub_ssd(K, x, out, NTok, aw, ab, w, cw, cbv, dtb, alog, dsk, nw)
    K.S.finish()
    return K


def sub_ssd(K, x, out, NTok, aw, ab, w, cw, cbv, dtb, alog, dsk, nw):
    S, nc = K.S, K.nc
    X = mybir.AxisListType.X
    P0, P1, P2, P3 = K.psum
    old_scope = K.scope
    _mk = K.S.mark()
    sc_ssd = ExitStack()
    K.scope = sc_ssd
    wz = K.sb([128, 8, 512], BF16, tag="wz")
    wx = K.sb([128, 8, 1024], BF16, tag="wx")
    wdt = K.sb([128, 8, 8], BF16, tag="wdt")
    S.dma("gpsimd", S.new_dma_sem(), wz.t[:], w[:, 0:512].rearrange("(c p) f -> p c f", p=128), writes=[wz.d])
    S.dma("gpsimd", S.new_dma_sem(), wx.t[:], w[:, 512:1536].rearrange("(c p) f -> p c f", p=128), writes=[wx.d])
    with nc.allow_non_contiguous_dma(reason="tiny"):
        S.dma("gpsimd", S.new_dma_sem(), wdt.t[:], w[:, 1536:1544].rearrange("(c p) f -> p c f", p=128),
              writes=[wdt.d])
    cwT = K.sb([128, 4, 8], F32, tag="cwT")
    cbT = K.sb([128, 8], F32, tag="cbT")
    with nc.allow_non_contiguous_dma(reason="tiny"):
        S.dma("sync", S.new_dma_sem(), cwT.t[:], cw.rearrange("k (c p) -> p k c", p=128), writes=[cwT.d])
        S.dma("sync", S.new_dma_sem(), cbT.t[:], cbv.rearrange("(c p) -> p c", p=128), writes=[cbT.d])
    dtb_bc = K.sb([128, 8], F32, tag="dtb"); A_bc = K.sb([128, 8], F32, tag="A"); dsk_bc = K.sb([128, 8], F32, tag="dsk")
    nw_bc = K.sb([128, 512], F32, tag="nw")
    K.bcast_vec(dtb_bc, dtb, S.new_dma_sem()); K.bcast_vec(A_bc, alog, S.new_dma_sem())
    K.bcast_vec(dsk_bc, dsk, S.new_dma_sem()); K.bcast_vec(nw_bc, nw, S.new_dma_sem())
    S.op("scalar", lambda e: e.activation(out=A_bc.t[:], in_=A_bc.t[:], func=AF.Exp), reads=[A_bc.d], writes=[A_bc.d])
    S.op("vector", lambda e: e.tensor_scalar(out=A_bc.t[:], in0=A_bc.t[:], scalar1=-1.0, scalar2=None, op0=ALU.mult),
         reads=[A_bc.d], writes=[A_bc.d])
    sc1p = K.sb([128, 1024], F32, tag="sc1p"); shm = K.sb([128, 1024], F32, tag="shm")
    K.mod_vec(shm, aw, ab, 0, False)
    K.mod_vec(sc1p, aw, ab, 1, True)
    ii = K.sb([128, 128], I32, tag="ii3")
    triU = K.sb([128, 128], F32, tag="triU")
    ones = K.sb([128, 128], F32, tag="ones")
    eps_r = K.sb([128, 1], F32, tag="epsr")
    S.op("gpsimd", lambda e: e.iota(ii.t[:], pattern=[[1, 128]], base=0, channel_multiplier=-1), writes=[ii.d])
    S.op("vector", lambda e: e.tensor_single_scalar(out=triU.t[:], in_=ii.t[:], scalar=0, op=ALU.is_ge),
         reads=[ii.d], writes=[triU.d])
    S.op("vector", lambda e: e.memset(ones.t[:], 1.0), writes=[ones.d])
    S.op("vector", lambda e: e.memset(eps_r.t[:], RMS_EPS), writes=[eps_r.d])
    S32 = K.sb([128, 8, 64], F32, tag="S32"); Sbf = K.sb([128, 8, 64], BF16, tag="Sbf")
    S.op("vector", lambda e: e.memset(S32.t[:], 0.0), writes=[S32.d])
    S.op("vector", lambda e: e.memset(Sbf.t[:], 0.0), writes=[Sbf.d])
    xr = K.sb([128, 8, 131], F32, tag="xr")
    S.op("vector", lambda e: e.memset(xr.t[:], 0.0), writes=[xr.d])
    xin = K.sb([128, 1024], F32, tag="xin"); xsem = S.new_dma_sem()
    hf = K.sb([128, 1024], F32, tag="hf")
    hmT = K.sb([128, 8, 128], BF16, tag="hmT")
    cacc = K.sb([128, 8, 128], F32, tag="cacc"); ctmp = K.sb([128, 8, 128], F32, tag="ctmp")
    xa = K.sb([128, 8, 128], F32, tag="xa")
    bcT = K.sb([128, 4, 128], BF16, tag="bcT")
    xtok = K.sb([128, 768], F32, tag="xtok")
    btok = K.sb([128, 256], BF16, tag="btok")
    dtv = K.sb([128, 8], F32, tag="dtv"); dtA = K.sb([128, 8], F32, tag="dtA")
    dtAb = K.sb([128, 8, 128], F32, tag="dtAb")
    acs = K.sb([128, 24], F32, tag="acs")
    dte = K.sb([128, 8], F32, tag="dte"); cd = K.sb([128, 8], F32, tag="cd")
    Lx = K.sb([128, 8, 128], F32, tag="Lx"); Eb = K.sb([128, 8, 128], F32, tag="Eb")
    cbm = K.sb([128, 2, 128], F32, tag="cbm")
    MT = K.sb([128, 8, 128], BF16, tag="MT"); CsT = K.sb([128, 8, 128], BF16, tag="CsT")
    xdt = K.sb([128, 8, 64], BF16, tag="xdt"); xdte = K.sb([128, 8, 64], BF16, tag="xdte")
    y = K.sb([128, 512], F32, tag="y"); sz = K.sb([128, 512], F32, tag="sz"); sq = K.sb([128, 512], F32, tag="sq")
    ss = K.sb([128, 2], F32, tag="ss")
    ygT = K.sb([128, 4, 128], BF16, tag="ygT"); osem = S.new_dma_sem()
    bc3 = lambda ap, n: ap.unsqueeze(2).to_broadcast([128, 8, n])

    for ck in range(NTok // 128):
        S.dma("sync", xsem, xin.t[:], (x(ck) if callable(x) else x[ck * 128:(ck + 1) * 128, :]), writes=[xin.d])
        S.op("vector", lambda e: e.tensor_tensor(out=hf.t[:], in0=xin.t[:], in1=sc1p.t[:], op=ALU.mult),
             reads=[xin.d, sc1p.d], writes=[hf.d])
        S.op("vector", lambda e: e.tensor_tensor(out=hf.t[:], in0=hf.t[:], in1=shm.t[:], op=ALU.add),
             reads=[hf.d, shm.d], writes=[hf.d])
        for hb in range(2):
            fns = [(lambda e, k=k: e.transpose(out=P0.t[:, k * 128:(k + 1) * 128], in_=hf.t[:, k * 128:(k + 1) * 128],
                                               identity=K.ident.t[:])) for k in range(hb * 4, hb * 4 + 4)]
            S.group("tensor", fns, reads=[hf.d, K.ident.d], writes=[P0.ds[hb]])
        S.op("scalar", lambda e: e.activation(out=hmT.t[:], in_=P0.t[:].rearrange("p (c t) -> p c t", c=8),
                                              func=AF.Copy), reads=[P0.ds[0], P0.ds[1]], writes=[hmT.d])
        fns = [(lambda e, k=k: e.matmul(P1.t[:, 0:512], lhsT=hmT.t[:, k, :], rhs=wz.t[:, k, :], start=(k == 0),
                                        stop=(k == 7))) for k in range(8)]
        S.group("tensor", fns, reads=[hmT.d, wz.d], writes=[P1.ds[0]])
        fns = [(lambda e, k=k: e.matmul(P1.t[:, 512:520], lhsT=hmT.t[:, k, :], rhs=wdt.t[:, k, :], start=(k == 0),
                                        stop=(k == 7))) for k in range(8)]
        S.group("tensor", fns, reads=[hmT.d, wdt.d], writes=[P1.ds[1]])
        for hb in range(2):
            fns = []
            for ch in range(hb * 4, hb * 4 + 4):
                fns += [(lambda e, k=k, ch=ch: e.matmul(P2.t[:, ch * 128:(ch + 1) * 128],
                                                        lhsT=wx.t[:, k, ch * 128:(ch + 1) * 128], rhs=hmT.t[:, k, :],
                                                        start=(k == 0), stop=(k == 7))) for k in range(8)]
            S.group("tensor", fns, reads=[hmT.d, wx.d], writes=[P2.ds[hb]])
        S.op("scalar", lambda e: e.activation(out=xr.t[:, :, 3:131], in_=P2.t[:].rearrange("p (c t) -> p c t", c=8),
                                              func=AF.Copy), reads=[P2.ds[0], P2.ds[1]], writes=[xr.d])
        S.op("vector", lambda e: e.tensor_tensor(out=dtv.t[:], in0=P1.t[:, 512:520], in1=dtb_bc.t[:], op=ALU.add),
             reads=[P1.ds[1], dtb_bc.d], writes=[dtv.d])
        S.op("scalar", lambda e: e.activation(out=dtv.t[:], in_=dtv.t[:], func=AF.Exp), reads=[dtv.d], writes=[dtv.d])
        S.op("scalar", lambda e: e.activation(out=dtv.t[:], in_=dtv.t[:], func=AF.Ln, bias=1.0, scale=1.0),
             reads=[dtv.d], writes=[dtv.d])
        S.op("vector", lambda e: e.tensor_tensor(out=dtA.t[:], in0=dtv.t[:], in1=A_bc.t[:], op=ALU.mult),
             reads=[dtv.d, A_bc.d], writes=[dtA.d])
        S.op("vector", lambda e: e.tensor_copy(out=dtAb.t[:], in_=bc3(dtA.t[:], 128)), reads=[dtA.d], writes=[dtAb.d])
        S.op("tensor", lambda e: e.matmul(P1.t[:, 520:528], lhsT=triU.t[:], rhs=dtA.t[:], start=True, stop=True),
             reads=[triU.d, dtA.d], writes=[P1.ds[1]])
        S.op("tensor", lambda e: e.matmul(P1.t[:, 528:536], lhsT=ones.t[:], rhs=dtA.t[:], start=True, stop=True),
             reads=[ones.d, dtA.d], writes=[P1.ds[1]])
        S.op("scalar", lambda e: e.activation(out=acs.t[:, 0:16], in_=P1.t[:, 520:536], func=AF.Copy),
             reads=[P1.ds[1]], writes=[acs.d])
        for hb in range(2):
            fns = [(lambda e, h=h: e.matmul(P0.t[:, h * 128:(h + 1) * 128], lhsT=dtAb.t[:, h, :], rhs=triU.t[:],
                                            start=True, stop=True)) for h in range(hb * 4, hb * 4 + 4)]
            S.group("tensor", fns, reads=[dtAb.d, triU.d], writes=[P0.ds[hb]])
        P0v = P0.t[:].rearrange("p (h l) -> p h l", h=8)
        S.op("vector", lambda e: e.tensor_tensor(out=Lx.t[:], in0=P0v, in1=bc3(acs.t[:, 0:8], 128), op=ALU.subtract),
             reads=[P0.ds[0], P0.ds[1], acs.d], writes=[Lx.d])
        S.op("vector", lambda e: e.tensor_scalar(out=Lx.t[:], in0=Lx.t[:], scalar1=0.0, scalar2=None, op0=ALU.min),
             reads=[Lx.d], writes=[Lx.d])
        S.op("scalar", lambda e: e.activation(out=Lx.t[:], in_=Lx.t[:], func=AF.Exp), reads=[Lx.d], writes=[Lx.d])
        S.op("scalar", lambda e: e.activation(out=Eb.t[:], in_=P0v, func=AF.Exp), reads=[P0.ds[0], P0.ds[1]],
             writes=[Eb.d])
        S.op("vector", lambda e: e.tensor_tensor(out=dte.t[:], in0=acs.t[:, 8:16], in1=acs.t[:, 0:8], op=ALU.subtract),
             reads=[acs.d], writes=[dte.d])
        S.op("scalar", lambda e: e.activation(out=dte.t[:], in_=dte.t[:], func=AF.Exp), reads=[dte.d], writes=[dte.d])
        S.op("scalar", lambda e: e.activation(out=cd.t[:], in_=acs.t[:, 8:16], func=AF.Exp), reads=[acs.d], writes=[cd.d])
        for k in range(4):
            src = xr.t[:, :, k:k + 128]
            wk = bc3(cwT.t[:, k, :], 128)
            if k == 0:
                S.op("vector", lambda e, src=src, wk=wk: e.tensor_tensor(out=cacc.t[:], in0=src, in1=wk, op=ALU.mult),
                     reads=[xr.d, cwT.d], writes=[cacc.d])
            else:
                S.op("vector", lambda e, src=src, wk=wk: e.tensor_tensor(out=ctmp.t[:], in0=src, in1=wk, op=ALU.mult),
                     reads=[xr.d, cwT.d], writes=[ctmp.d])
                S.op("vector", lambda e: e.tensor_tensor(out=cacc.t[:], in0=cacc.t[:], in1=ctmp.t[:], op=ALU.add),
                     reads=[cacc.d, ctmp.d], writes=[cacc.d])
        S.op("vector", lambda e: e.tensor_copy(out=xr.t[:, :, 0:3], in_=xr.t[:, :, 128:131]), reads=[xr.d], writes=[xr.d])
        for ch in range(8):
            S.op("scalar", lambda e, ch=ch: e.activation(out=xa.t[:, ch, :], in_=cacc.t[:, ch, :], func=AF.Silu,
                                                         bias=cbT.t[:, ch:ch + 1], scale=1.0),
                 reads=[cacc.d, cbT.d], writes=[xa.d])
        S.op("vector", lambda e: e.tensor_copy(out=bcT.t[:], in_=xa.t[:, 4:8, :]), reads=[xa.d], writes=[bcT.d])
        for hb in range(2):
            rng_ = range(0, 4) if hb == 0 else range(4, 6)
            fns = [(lambda e, j=j: e.transpose(out=P2.t[:, j * 128:(j + 1) * 128], in_=xa.t[:, j, :],
                                               identity=K.ident.t[:])) for j in rng_]
            S.group("tensor", fns, reads=[xa.d, K.ident.d], writes=[P2.ds[hb]])
        S.op("scalar", lambda e: e.activation(out=xtok.t[:], in_=P2.t[:, 0:768], func=AF.Copy),
             reads=[P2.ds[0], P2.ds[1]], writes=[xtok.d])
        S.op("vector", lambda e: e.tensor_copy(out=btok.t[:], in_=xtok.t[:, 512:768]), reads=[xtok.d], writes=[btok.d])
        xt3 = xtok.t[:, 0:512].rearrange("p (h d) -> p h d", h=8)
        S.op("vector", lambda e: e.tensor_tensor(out=xdt.t[:], in0=xt3, in1=bc3(dtv.t[:], 64), op=ALU.mult),
             reads=[xtok.d, dtv.d], writes=[xdt.d])
        S.op("vector", lambda e: e.tensor_tensor(out=dte.t[:], in0=dte.t[:], in1=dtv.t[:], op=ALU.mult),
             reads=[dte.d, dtv.d], writes=[dte.d])
        S.op("vector", lambda e: e.tensor_tensor(out=xdte.t[:], in0=xt3, in1=bc3(dte.t[:], 64), op=ALU.mult),
             reads=[xtok.d, dte.d], writes=[xdte.d])
        fns = [(lambda e, g=g: e.matmul(P3.t[:, g * 128:(g + 1) * 128], lhsT=bcT.t[:, g, :], rhs=bcT.t[:, 2 + g, :],
                                        start=True, stop=True)) for g in range(2)]
        S.group("tensor", fns, reads=[bcT.d], writes=[P3.ds[0]])
        S.op("vector", lambda e: e.tensor_tensor(out=cbm.t[:], in0=P3.t[:, 0:256].rearrange("p (g l) -> p g l", g=2),
                                                 in1=triU.t[:].unsqueeze(1).to_broadcast([128, 2, 128]), op=ALU.mult),
             reads=[P3.ds[0], triU.d], writes=[cbm.d])
        for g in range(2):
            S.op("vector", lambda e, g=g: e.tensor_tensor(
                out=MT.t[:, 4 * g:4 * g + 4, :], in0=Lx.t[:, 4 * g:4 * g + 4, :],
                in1=cbm.t[:, g, :].unsqueeze(1).to_broadcast([128, 4, 128]), op=ALU.mult),
                 reads=[Lx.d, cbm.d], writes=[MT.d])
            S.op("vector", lambda e, g=g: e.tensor_tensor(
                out=CsT.t[:, 4 * g:4 * g + 4, :], in0=Eb.t[:, 4 * g:4 * g + 4, :],
                in1=xa.t[:, 6 + g, :].unsqueeze(1).to_broadcast([128, 4, 128]), op=ALU.mult),
                 reads=[Eb.d, xa.d], writes=[CsT.d])
        fns = []
        for h in range(8):
            fns.append(lambda e, h=h: e.matmul(P2.t[:, h * 64:(h + 1) * 64], lhsT=MT.t[:, h, :], rhs=xdt.t[:, h, :],
                                               start=True, stop=False))
            fns.append(lambda e, h=h: e.matmul(P2.t[:, h * 64:(h + 1) * 64], lhsT=CsT.t[:, h, :], rhs=Sbf.t[:, h, :],
                                               start=False, stop=True))
        S.group("tensor", fns, reads=[MT.d, xdt.d, CsT.d, Sbf.d], writes=[P2.ds[0]])
        fns = [(lambda e, h=h: e.matmul(P2.t[:, 512 + h * 64:512 + (h + 1) * 64], lhsT=btok.t[:, (h // 4) * 128:(h // 4 + 1) * 128],
                                        rhs=xdte.t[:, h, :], start=True, stop=True)) for h in range(8)]
        S.group("tensor", fns, reads=[btok.d, xdte.d], writes=[P2.ds[1]])
        S.op("vector", lambda e: e.tensor_tensor(out=S32.t[:], in0=S32.t[:], in1=bc3(cd.t[:], 64), op=ALU.mult),
             reads=[S32.d, cd.d], writes=[S32.d])
        S.op("vector", lambda e: e.tensor_tensor(out=S32.t[:], in0=S32.t[:],
                                                 in1=P2.t[:, 512:1024].rearrange("p (h d) -> p h d", h=8), op=ALU.add),
             reads=[S32.d, P2.ds[1]], writes=[S32.d])
        S.op("vector", lambda e: e.tensor_copy(out=Sbf.t[:], in_=S32.t[:]), reads=[S32.d], writes=[Sbf.d])
        S.op("vector", lambda e: e.tensor_tensor(out=y.t[:].rearrange("p (h d) -> p h d", h=8), in0=xt3,
                                                 in1=bc3(dsk_bc.t[:], 64), op=ALU.mult),
             reads=[xtok.d, dsk_bc.d], writes=[y.d])
        S.op("vector", lambda e: e.tensor_tensor(out=y.t[:], in0=y.t[:], in1=P2.t[:, 0:512], op=ALU.add),
             reads=[y.d, P2.ds[0]], writes=[y.d])
        S.op("scalar", lambda e: e.activation(out=sz.t[:], in_=P1.t[:, 0:512], func=AF.Silu), reads=[P1.ds[0]],
             writes=[sz.d])
        S.op("vector", lambda e: e.tensor_tensor(out=y.t[:], in0=y.t[:], in1=sz.t[:], op=ALU.mult),
             reads=[y.d, sz.d], writes=[y.d])
        S.op("vector", lambda e: e.tensor_tensor(out=sq.t[:], in0=y.t[:], in1=y.t[:], op=ALU.mult),
             reads=[y.d], writes=[sq.d])
        S.op("vector", lambda e: e.tensor_reduce(out=ss.t[:], in_=sq.t[:].rearrange("p (g d) -> p g d", g=2), axis=X,
                                                 op=ALU.add), reads=[sq.d], writes=[ss.d])
        S.op("scalar", lambda e: e.activation(out=ss.t[:], in_=ss.t[:], func=AF.Sqrt, bias=eps_r.t[:], scale=1.0 / 256.0),
             reads=[ss.d, eps_r.d], writes=[ss.d])
        S.op("vector", lambda e: e.reciprocal(out=ss.t[:], in_=ss.t[:]), reads=[ss.d], writes=[ss.d])
        S.op("vector", lambda e: e.tensor_tensor(out=y.t[:].rearrange("p (g d) -> p g d", g=2),
                                                 in0=y.t[:].rearrange("p (g d) -> p g d", g=2),
                                                 in1=ss.t[:].unsqueeze(2).to_broadcast([128, 2, 256]), op=ALU.mult),
             reads=[y.d, ss.d], writes=[y.d])
        S.op("vector", lambda e: e.tensor_tensor(out=y.t[:], in0=y.t[:], in1=nw_bc.t[:], op=ALU.mult),
             reads=[y.d, nw_bc.d], writes=[y.d])
        fns = [(lambda e, j=j: e.transpose(out=P3.t[:, 512 + j * 128:512 + (j + 1) * 128], in_=y.t[:, j * 128:(j + 1) * 128],
                                           identity=K.ident.t[:])) for j in range(4)]
        S.group("tensor", fns, reads=[y.d, K.ident.d], writes=[P3.ds[1]])
        S.op("scalar", lambda e: e.activation(out=ygT.t[:], in_=P3.t[:, 512:1024].rearrange("p (c t) -> p c t", c=4),
                                              func=AF.Copy), reads=[P3.ds[1]], writes=[ygT.d])
        S.dma("gpsimd", osem, out[:, ck * 128:(ck + 1) * 128].rearrange("(c p) t -> p c t", p=128), ygT.t[:],
              reads=[ygT.d])
    S.barrier()
    sc_ssd.close()
    K.scope = old_scope
    K.S.release(_mk)


def build_mla(NTok):
    K = KB()
    x = K.din("x", [NTok, D]); c = K.din("c", [D]); aw = K.din("aw", [D, 6 * D]); ab = K.din("ab", [6 * D])
    pos = K.din("pos", [NTok], I32)
    w_in = K.din("w_in", [D, 800]); qn = K.din("qn", [512]); kvn = K.din("kvn", [256])
    wuq_d = K.din("wuq", [512, 384]); wukv_d = K.din("wukv", [256, 512])
    out = K.dout("mT", [256, NTok], BF16)
    K.alloc_psum(); K.consts(); K.setup_cond(c)
    sub_mla(K, x, out, NTok, aw, ab, pos, w_in, qn, kvn, wuq_d, wukv_d)
    K.S.finish()
    return K


def sub_mla(K, x, out, NTok, aw, ab, pos, w_in, qn, kvn, wuq_d, wukv_d):
    S, nc = K.S, K.nc
    X = mybir.AxisListType.X
    NTL = NTok // 128
    QT_d = K.dtmp("QT_d", [4, 96, NTok], BF16); KT_d = K.dtmp("KT_d", [4, 96, NTok], BF16)
    V_d = K.dtmp("V_d", [NTL, 128, 4, 65], BF16)
    P0, P1, P2, P3 = K.psum
    old_scope = K.scope
    _mk = K.S.mark()
    SCALE = 96.0 ** -0.5
    TWO_PI = 2.0 * math.pi
    with ExitStack() as sc:
        K.scope = sc
        win = K.sb([128, 8, 800], BF16, tag="win")
        wuq = K.sb([128, 4, 384], BF16, tag="wuq"); wukv = K.sb([128, 2, 512], BF16, tag="wukv")
        S.dma("gpsimd", S.new_dma_sem(), win.t[:], w_in.rearrange("(c p) f -> p c f", p=128), writes=[win.d])
        S.dma("gpsimd", S.new_dma_sem(), wuq.t[:], wuq_d.rearrange("(c p) f -> p c f", p=128), writes=[wuq.d])
        S.dma("gpsimd", S.new_dma_sem(), wukv.t[:], wukv_d.rearrange("(c p) f -> p c f", p=128), writes=[wukv.d])
        nbc = K.sb([128, 768], F32, tag="nbc")
        S.dma("sync", S.new_dma_sem(), nbc.t[:, 0:512], qn.unsqueeze(0).to_broadcast([128, 512]), writes=[nbc.d])
        S.dma("sync", S.new_dma_sem(), nbc.t[:, 512:768], kvn.unsqueeze(0).to_broadcast([128, 256]), writes=[nbc.d])
        sc1p = K.sb([128, 1024], F32, tag="sc1p"); shm = K.sb([128, 1024], F32, tag="shm")
        K.mod_vec(shm, aw, ab, 0, False)
        K.mod_vec(sc1p, aw, ab, 1, True)
        eps_r = K.sb([128, 1], F32, tag="epsr")
        S.op("vector", lambda e: e.memset(eps_r.t[:], RMS_EPS), writes=[eps_r.d])
        ji = K.sb([128, 16], I32, tag="ji"); freq = K.sb([128, 16], F32, tag="freq")
        S.op("gpsimd", lambda e: e.iota(ji.t[:], pattern=[[1, 16]], base=0, channel_multiplier=0), writes=[ji.d])
        S.op("vector", lambda e: e.tensor_copy(out=freq.t[:], in_=ji.t[:]), reads=[ji.d], writes=[freq.d])
        S.op("scalar", lambda e: e.activation(out=freq.t[:], in_=freq.t[:], func=AF.Exp,
                                              scale=-math.log(10000.0) / 16.0), reads=[freq.d], writes=[freq.d])
        xin = K.sb([128, 1024], F32, tag="xin"); xsem = S.new_dma_sem()
        hf = K.sb([128, 1024], F32, tag="hf"); hmT = K.sb([128, 8, 128], BF16, tag="hmT")
        lat = K.sb([128, 800], F32, tag="lat"); sq = K.sb([128, 768], F32, tag="sq")
        ss = K.sb([128, 2], F32, tag="ss"); nrm = K.sb([128, 768], F32, tag="nrm"); nT = K.sb([128, 6, 128], BF16, tag="nT")
        posi = K.sb([128, 1], I32, tag="posi"); psem = S.new_dma_sem(); posf = K.sb([128, 1], F32, tag="posf")
        tt = K.sb([128, 32], F32, tag="tt"); ti = K.sb([128, 32], I32, tag="ti"); tf = K.sb([128, 32], F32, tag="tf")
        mm = K.sb([128, 32], F32, tag="mm"); scs = K.sb([128, 32], F32, tag="scs")
        qf = K.sb([128, 4, 96], F32, tag="qf"); kvf = K.sb([128, 4, 128], F32, tag="kvf")
        Qh = K.sb([128, 4, 96], F32, tag="Qh"); Kh = K.sb([128, 4, 96], F32, tag="Kh")
        ra = K.sb([128, 4, 16], F32, tag="ra"); rb = K.sb([128, 4, 16], F32, tag="rb")
        kr = K.sb([128, 32], F32, tag="kr")
        Va = K.sb([128, 4, 65], BF16, tag="Va")
        S.op("vector", lambda e: e.memset(Va.t[:], 1.0), writes=[Va.d])
        QTs = K.sb([128, 4, 128], BF16, tag="QTs"); KTs = K.sb([128, 4, 128], BF16, tag="KTs")
        qsem = S.new_dma_sem(); ksem = S.new_dma_sem(); vsem = S.new_dma_sem()
        b4 = lambda ap: ap.unsqueeze(1).to_broadcast([128, 4, 16])

        def rope(src1, src2, dst1, dst2, cosb, sinb, shape_bc):
            S.op("vector", lambda e: e.tensor_tensor(out=ra.t[:] if shape_bc else ra.t[:, 0, :], in0=src1, in1=cosb, op=ALU.mult),
                 reads=[qf.d, lat.d, scs.d], writes=[ra.d])
            S.op("vector", lambda e: e.tensor_tensor(out=rb.t[:] if shape_bc else rb.t[:, 0, :], in0=src2, in1=sinb, op=ALU.mult),
                 reads=[qf.d, lat.d, scs.d], writes=[rb.d])
            S.op("vector", lambda e: e.tensor_tensor(out=dst1, in0=ra.t[:] if shape_bc else ra.t[:, 0, :],
                                                     in1=rb.t[:] if shape_bc else rb.t[:, 0, :], op=ALU.subtract),
                 reads=[ra.d, rb.d], writes=[Qh.d, kr.d])
            S.op("vector", lambda e: e.tensor_tensor(out=ra.t[:] if shape_bc else ra.t[:, 0, :], in0=src2, in1=cosb, op=ALU.mult),
                 reads=[qf.d, lat.d, scs.d], writes=[ra.d])
            S.op("vector", lambda e: e.tensor_tensor(out=rb.t[:] if shape_bc else rb.t[:, 0, :], in0=src1, in1=sinb, op=ALU.mult),
                 reads=[qf.d, lat.d, scs.d], writes=[rb.d])
            S.op("vector", lambda e: e.tensor_tensor(out=dst2, in0=ra.t[:] if shape_bc else ra.t[:, 0, :],
                                                     in1=rb.t[:] if shape_bc else rb.t[:, 0, :], op=ALU.add),
                 reads=[ra.d, rb.d], writes=[Qh.d, kr.d])

        for t in range(NTL):
            S.dma("sync", xsem, xin.t[:], (x(t) if callable(x) else x[t * 128:(t + 1) * 128, :]), writes=[xin.d])
            with nc.allow_non_contiguous_dma(reason="positions column"):
                S.dma("sync", psem, posi.t[:], pos[t * 128:(t + 1) * 128].unsqueeze(1), writes=[posi.d])
            S.op("vector", lambda e: e.tensor_tensor(out=hf.t[:], in0=xin.t[:], in1=sc1p.t[:], op=ALU.mult),
                 reads=[xin.d, sc1p.d], writes=[hf.d])
            S.op("vector", lambda e: e.tensor_tensor(out=hf.t[:], in0=hf.t[:], in1=shm.t[:], op=ALU.add),
                 reads=[hf.d, shm.d], writes=[hf.d])
            for hb in range(2):
                fns = [(lambda e, k=k: e.transpose(out=P0.t[:, k * 128:(k + 1) * 128], in_=hf.t[:, k * 128:(k + 1) * 128],
                                                   identity=K.ident.t[:])) for k in range(hb * 4, hb * 4 + 4)]
                S.group("tensor", fns, reads=[hf.d, K.ident.d], writes=[P0.ds[hb]])
            S.op("scalar", lambda e: e.activation(out=hmT.t[:], in_=P0.t[:].rearrange("p (c t) -> p c t", c=8),
                                                  func=AF.Copy), reads=[P0.ds[0], P0.ds[1]], writes=[hmT.d])
            fns = [(lambda e, k=k: e.matmul(P1.t[:, 0:512], lhsT=hmT.t[:, k, :], rhs=win.t[:, k, 0:512], start=(k == 0),
                                            stop=(k == 7))) for k in range(8)]
            S.group("tensor", fns, reads=[hmT.d, win.d], writes=[P1.ds[0]])
            fns = [(lambda e, k=k: e.matmul(P1.t[:, 512:800], lhsT=hmT.t[:, k, :], rhs=win.t[:, k, 512:800], start=(k == 0),
                                            stop=(k == 7))) for k in range(8)]
            S.group("tensor", fns, reads=[hmT.d, win.d], writes=[P1.ds[1]])
            S.op("scalar", lambda e: e.activation(out=lat.t[:], in_=P1.t[:, 0:800], func=AF.Copy),
                 reads=[P1.ds[0], P1.ds[1]], writes=[lat.d])
            S.op("vector", lambda e: e.tensor_tensor(out=sq.t[:], in0=lat.t[:, 0:768], in1=lat.t[:, 0:768], op=ALU.mult),
                 reads=[lat.d], writes=[sq.d])
            S.op("vector", lambda e: e.tensor_reduce(out=ss.t[:, 0:1], in_=sq.t[:, 0:512], axis=X, op=ALU.add),
                 reads=[sq.d], writes=[ss.d])
            S.op("vector", lambda e: e.tensor_reduce(out=ss.t[:, 1:2], in_=sq.t[:, 512:768], axis=X, op=ALU.add),
                 reads=[sq.d], writes=[ss.d])
            S.op("scalar", lambda e: e.activation(out=ss.t[:, 0:1], in_=ss.t[:, 0:1], func=AF.Sqrt, bias=eps_r.t[:],
                                                  scale=1.0 / 512.0), reads=[ss.d, eps_r.d], writes=[ss.d])
            S.op("scalar", lambda e: e.activation(out=ss.t[:, 1:2], in_=ss.t[:, 1:2], func=AF.Sqrt, bias=eps_r.t[:],
                                                  scale=1.0 / 256.0), reads=[ss.d, eps_r.d], writes=[ss.d])
            S.op("vector", lambda e: e.reciprocal(out=ss.t[:], in_=ss.t[:]), reads=[ss.d], writes=[ss.d])
            S.op("vector", lambda e: e.scalar_tensor_tensor(out=nrm.t[:, 0:512], in0=lat.t[:, 0:512], scalar=ss.t[:, 0:1],
                                                            in1=nbc.t[:, 0:512], op0=ALU.mult, op1=ALU.mult),
                 reads=[lat.d, ss.d, nbc.d], writes=[nrm.d])
            S.op("vector", lambda e: e.scalar_tensor_tensor(out=nrm.t[:, 512:768], in0=lat.t[:, 512:768],
                                                            scalar=ss.t[:, 1:2], in1=nbc.t[:, 512:768], op0=ALU.mult,
                                                            op1=ALU.mult), reads=[lat.d, ss.d, nbc.d], writes=[nrm.d])
            for hb in range(2):
                rng_ = range(0, 4) if hb == 0 else range(4, 6)
                fns = [(lambda e, j=j: e.transpose(out=P0.t[:, j * 128:(j + 1) * 128], in_=nrm.t[:, j * 128:(j + 1) * 128],
                                                   identity=K.ident.t[:])) for j in rng_]
                S.group("tensor", fns, reads=[nrm.d, K.ident.d], writes=[P0.ds[hb]])
            S.op("scalar", lambda e: e.activation(out=nT.t[:], in_=P0.t[:, 0:768].rearrange("p (c t) -> p c t", c=6),
                                                  func=AF.Copy), reads=[P0.ds[0], P0.ds[1]], writes=[nT.d])
            fns = [(lambda e, c_=c_: e.matmul(P2.t[:, 0:384], lhsT=nT.t[:, c_, :], rhs=wuq.t[:, c_, :], start=(c_ == 0),
                                              stop=(c_ == 3))) for c_ in range(4)]
            S.group("tensor", fns, reads=[nT.d, wuq.d], writes=[P2.ds[0]])
            fns = [(lambda e, c_=c_: e.matmul(P2.t[:, 512:1024], lhsT=nT.t[:, 4 + c_, :], rhs=wukv.t[:, c_, :],
                                              start=(c_ == 0), stop=(c_ == 1))) for c_ in range(2)]
            S.group("tensor", fns, reads=[nT.d, wukv.d], writes=[P2.ds[1]])
            S.op("scalar", lambda e: e.activation(out=qf.t[:], in_=P2.t[:, 0:384].rearrange("p (h d) -> p h d", h=4),
                                                  func=AF.Copy), reads=[P2.ds[0]], writes=[qf.d])
            S.op("scalar", lambda e: e.activation(out=kvf.t[:], in_=P2.t[:, 512:1024].rearrange("p (h d) -> p h d", h=4),
                                                  func=AF.Copy), reads=[P2.ds[1]], writes=[kvf.d])
            S.op("vector", lambda e: e.tensor_copy(out=posf.t[:], in_=posi.t[:]), reads=[posi.d], writes=[posf.d])
            S.op("vector", lambda e: e.tensor_scalar(out=tt.t[:, 0:16], in0=freq.t[:], scalar1=posf.t[:],
                                                     scalar2=1.0 / TWO_PI, op0=ALU.mult, op1=ALU.mult),
                 reads=[freq.d, posf.d], writes=[tt.d])
            S.op("vector", lambda e: e.tensor_scalar(out=tt.t[:, 16:32], in0=tt.t[:, 0:16], scalar1=0.25, scalar2=None,
                                                     op0=ALU.add), reads=[tt.d], writes=[tt.d])
            S.op("vector", lambda e: e.tensor_copy(out=ti.t[:], in_=tt.t[:]), reads=[tt.d], writes=[ti.d])
            S.op("vector", lambda e: e.tensor_copy(out=tf.t[:], in_=ti.t[:]), reads=[ti.d], writes=[tf.d])
            S.op("vector", lambda e: e.tensor_tensor(out=tt.t[:], in0=tt.t[:], in1=tf.t[:], op=ALU.subtract),
                 reads=[tt.d, tf.d], writes=[tt.d])
            S.op("vector", lambda e: e.tensor_single_scalar(out=mm.t[:], in_=tt.t[:], scalar=0.5, op=ALU.is_gt),
                 reads=[tt.d], writes=[mm.d])
            S.op("vector", lambda e: e.tensor_tensor(out=tt.t[:], in0=tt.t[:], in1=mm.t[:], op=ALU.subtract),
                 reads=[tt.d, mm.d], writes=[tt.d])
            S.op("vector", lambda e: e.tensor_single_scalar(out=mm.t[:], in_=tt.t[:], scalar=-0.5, op=ALU.is_lt),
                 reads=[tt.d], writes=[mm.d])
            S.op("vector", lambda e: e.tensor_tensor(out=tt.t[:], in0=tt.t[:], in1=mm.t[:], op=ALU.add),
                 reads=[tt.d, mm.d], writes=[tt.d])
            S.op("scalar", lambda e: e.activation(out=scs.t[:], in_=tt.t[:], func=AF.Sin, scale=TWO_PI),
                 reads=[tt.d], writes=[scs.d])
            sinb, cosb = scs.t[:, 0:16], scs.t[:, 16:32]
            rope(qf.t[:, :, 64:80], qf.t[:, :, 80:96], Qh.t[:, :, 64:80], Qh.t[:, :, 80:96], b4(cosb), b4(sinb), True)
            rope(lat.t[:, 768:784], lat.t[:, 784:800], kr.t[:, 0:16], kr.t[:, 16:32], cosb, sinb, False)
            S.op("vector", lambda e: e.tensor_copy(out=Qh.t[:, :, 0:64], in_=qf.t[:, :, 0:64]), reads=[qf.d], writes=[Qh.d])
            S.op("vector", lambda e: e.tensor_copy(out=Kh.t[:, :, 0:64], in_=kvf.t[:, :, 0:64]), reads=[kvf.d], writes=[Kh.d])
            S.op("vector", lambda e: e.tensor_copy(out=Kh.t[:, :, 64:96], in_=kr.t[:].unsqueeze(1).to_broadcast([128, 4, 32])),
                 reads=[kr.d], writes=[Kh.d])
            S.op("vector", lambda e: e.tensor_copy(out=Va.t[:, :, 0:64], in_=kvf.t[:, :, 64:128]), reads=[kvf.d], writes=[Va.d])
            fns = [(lambda e, h=h: e.transpose(out=P3.t[0:96, h * 128:(h + 1) * 128], in_=Qh.t[:, h, :],
                                               identity=K.ident.t[:])) for h in range(4)]
            S.group("tensor", fns, reads=[Qh.d, K.ident.d], writes=[P3.ds[0]])
            fns = [(lambda e, h=h: e.transpose(out=P3.t[0:96, 512 + h * 128:512 + (h + 1) * 128], in_=Kh.t[:, h, :],
                                               identity=K.ident.t[:])) for h in range(4)]
            S.group("tensor", fns, reads=[Kh.d, K.ident.d], writes=[P3.ds[1]])
            S.op("scalar", lambda e: e.activation(out=QTs.t[0:96], in_=P3.t[0:96, 0:512].rearrange("p (h t) -> p h t", h=4),
                                                  func=AF.Copy), reads=[P3.ds[0]], writes=[QTs.d])
            S.op("scalar", lambda e: e.activation(out=KTs.t[0:96], in_=P3.t[0:96, 512:1024].rearrange("p (h t) -> p h t", h=4),
                                                  func=AF.Copy), reads=[P3.ds[1]], writes=[KTs.d])
            S.dma("gpsimd", qsem, QT_d[:, :, t * 128:(t + 1) * 128].rearrange("h d t -> d h t"), QTs.t[0:96], reads=[QTs.d])
            S.dma("gpsimd", ksem, KT_d[:, :, t * 128:(t + 1) * 128].rearrange("h d t -> d h t"), KTs.t[0:96], reads=[KTs.d])
            S.dma("gpsimd", vsem, V_d[t], Va.t[:], reads=[Va.d])
        S.barrier()
    with ExitStack() as sc:
        K.scope = sc
        KTh = K.sb([128, NTok], BF16, tag="KTh"); Vh = K.sb([128, NTL, 65], BF16, tag="Vh")
        khs = S.new_dma_sem(); vhs = S.new_dma_sem()
        QTt = [K.sb([128, 512], BF16, tag="QTt") for _ in range(2)]; qts = [S.new_dma_sem() for _ in range(2)]
        Pb = [K.sb([128, 512], BF16, tag="Pb") for _ in range(2)]
        mk = K.sb([128, 4, 512], BF16, tag="mk")
        mi = K.sb([128, 512], I32, tag="mi")
        for j in range(4):
            S.op("gpsimd", lambda e, j=j: e.iota(mi.t[:], pattern=[[1, 512]], base=-128 * j, channel_multiplier=-1),
                 writes=[mi.d])
            S.op("vector", lambda e, j=j: e.tensor_single_scalar(out=mk.t[:, j, :], in_=mi.t[:], scalar=0, op=ALU.is_ge),
                 reads=[mi.d], writes=[mk.d])
        sel = K.sb([128, 64], F32, tag="sel")
        S.op("vector", lambda e: e.memset(sel.t[:], 0.0), writes=[sel.d])
        S.op("vector", lambda e: e.memset(sel.t[64:65, :], 1.0), writes=[sel.d])
        Osb = K.sb([128, 512], F32, tag="Osb"); rec = K.sb([64, 512], F32, tag="rec")
        ob = [K.sb([64, 512], BF16, tag="ob") for _ in range(2)]; obs = [S.new_dma_sem() for _ in range(2)]
        it = 0
        for h in range(4):
            S.dma("sync", khs, KTh.t[0:96, :], KT_d[h], writes=[KTh.d])
            with nc.allow_non_contiguous_dma(reason="V rows of 130B"):
                S.dma("sync", vhs, Vh.t[:], V_d[:, :, h, :].rearrange("c p d -> p c d"), writes=[Vh.d])
            for qt in range(NTok // 512):
                qb = (h * (NTok // 512) + qt) % 2
                S.dma("sync", qts[qb], QTt[qb].t[0:96, :], QT_d[h][:, qt * 512:(qt + 1) * 512], writes=[QTt[qb].d])
                nkb = 4 * qt + 4
                for kb in range(nkb):
                    pb = Pb[it % 2]
                    S.op("tensor", lambda e, kb=kb, it=it, qb=qb: e.matmul(
                        P0.t[:, (it % 2) * 512:(it % 2 + 1) * 512], lhsT=KTh.t[0:96, kb * 128:(kb + 1) * 128],
                        rhs=QTt[qb].t[0:96, :], start=True, stop=True),
                         reads=[KTh.d, QTt[qb].d], writes=[P0.ds[it % 2]])
                    S.op("scalar", lambda e, it=it, pb=pb: e.activation(
                        out=pb.t[:], in_=P0.t[:, (it % 2) * 512:(it % 2 + 1) * 512], func=AF.Exp, scale=SCALE),
                         reads=[P0.ds[it % 2]], writes=[pb.d])
                    if kb >= 4 * qt:
                        S.op("vector", lambda e, pb=pb, j=kb - 4 * qt: e.tensor_tensor(
                            out=pb.t[:], in0=pb.t[:], in1=mk.t[:, j, :], op=ALU.mult), reads=[pb.d, mk.d], writes=[pb.d])
                    S.op("tensor", lambda e, kb=kb, pb=pb, nkb=nkb: e.matmul(
                        P1.t[0:65, 0:512], lhsT=Vh.t[:, kb, :], rhs=pb.t[:], start=(kb == 0), stop=(kb == nkb - 1)),
                         reads=[Vh.d, pb.d], writes=[P1.ds[0]])
                    it += 1
                S.op("scalar", lambda e: e.activation(out=Osb.t[0:65, :], in_=P1.t[0:65, 0:512], func=AF.Copy),
                     reads=[P1.ds[0]], writes=[Osb.d])
                S.op("tensor", lambda e: e.matmul(P2.t[0:64, 0:512], lhsT=sel.t[0:65, :], rhs=Osb.t[0:65, :], start=True,
                                                  stop=True), reads=[sel.d, Osb.d], writes=[P2.ds[0]])
                S.op("scalar", lambda e: e.activation(out=rec.t[:], in_=P2.t[0:64, 0:512], func=AF.Copy),
                     reads=[P2.ds[0]], writes=[rec.d])
                S.op("vector", lambda e: e.reciprocal(out=rec.t[:], in_=rec.t[:]), reads=[rec.d], writes=[rec.d])
                S.op("vector", lambda e, qb=qb: e.tensor_tensor(out=ob[qb].t[:], in0=Osb.t[0:64, :], in1=rec.t[:],
                                                               op=ALU.mult), reads=[Osb.d, rec.d], writes=[ob[qb].d])
                S.dma("gpsimd", obs[qb], out[h * 64:(h + 1) * 64, qt * 512:(qt + 1) * 512], ob[qb].t[:], reads=[ob[qb].d])
        S.barrier()
    K.scope = old_scope
    K.S.release(_mk)


def build_tok(NT, steps):
    K = KB()
    x = K.din("x", [NT, D]); c = K.din("c", [D])
    y = K.dout("y", [NT, D])
    K.alloc_psum(); K.consts(); K.setup_cond(c)
    aw, ab, lnp = {}, {}, {}

    def layer_in(l):
        if l not in aw:
            aw[l] = K.din(f"aw{l}", [D, 6 * D]); ab[l] = K.din(f"ab{l}", [6 * D])
        return aw[l], ab[l]

    def ln_in(l, j):
        if (l, j) not in lnp:
            lnp[(l, j)] = (K.din(f"lng{l}_{j}", [D]), K.din(f"lnb{l}_{j}", [D]))
        return lnp[(l, j)]

    cur = DramStream(x, NT)
    for si, (kind, l, dm) in enumerate(steps):
        last = si == len(steps) - 1
        nxt = DramStream(y if last else K.dtmp(f"xs{si}", [NT, D]), NT)
        a_w, a_b = layer_in(l)
        if kind == "proj":
            g, b = ln_in(l, 0)
            mT = K.din(f"s{si}_mT", [dm, NT], BF16); wo = K.din(f"s{si}_wo", [dm, D])
            sub_proj(K, cur, nxt, NT, mT, dm, wo, a_w, a_b, g, b)
        elif kind == "ffn":
            g, b = ln_in(l, 1)
            wg = K.din(f"s{si}_wg", [D, DFF]); wu = K.din(f"s{si}_wu", [D, DFF]); wd = K.din(f"s{si}_wd", [DFF, D])
            sub_ffn(K, cur, nxt, NT, a_w, a_b, g, b, wg, wu, wd, None)
        elif kind == "moe":
            g, b = ln_in(l, 1)
            wg = K.din(f"s{si}_wg", [NE, D, DFF]); wu = K.din(f"s{si}_wu", [NE, D, DFF]); wd = K.din(f"s{si}_wd", [NE, DFF, D])
            wr = K.din(f"s{si}_wr", [D, NE])
            sub_ffn(K, cur, nxt, NT, a_w, a_b, g, b, wg, wu, wd, wr)
        elif kind == "sg":
            g, b = ln_in(l, 0)
            w_in = K.din(f"s{si}_win", [D, 4096]); b_in = K.din(f"s{si}_bin", [4096])
            sg_g = K.din(f"s{si}_sg", [2048]); sg_b = K.din(f"s{si}_sb", [2048])
            w_s = K.din(f"s{si}_ws", [8, 128, 128]); b_s = K.din(f"s{si}_bs", [8, 128]); wo = K.din(f"s{si}_wo", [2048, D])
            sub_sg(K, cur, nxt, NT, a_w, a_b, g, b, w_in, b_in, sg_g, sg_b, w_s, b_s, wo)
        cur = nxt
    K.S.finish()
    return K


def _ssd_sel(I, j, q):
    w = I['ssd_w_in'][j]
    cols = np.concatenate([np.arange(512 * q, 512 * q + 512), 2048 + np.arange(512 * q, 512 * q + 512),
                           4096 + np.arange(256 * q, 256 * q + 256), 4096 + 1024 + np.arange(256 * q, 256 * q + 256),
                           6144 + np.arange(8 * q, 8 * q + 8)])
    ccols = np.concatenate([np.arange(512 * q, 512 * q + 512), 2048 + np.arange(256 * q, 256 * q + 256),
                            2048 + 1024 + np.arange(256 * q, 256 * q + 256)])
    return dict(w=np.ascontiguousarray(w[:, cols]), cw=np.ascontiguousarray(I['ssd_conv_w'][j][:, ccols]),
                cb=np.ascontiguousarray(I['ssd_conv_b'][j][ccols]),
                dtb=np.ascontiguousarray(I['ssd_dt_bias'][j][8 * q:8 * q + 8]),
                alog=np.ascontiguousarray(I['ssd_a_log'][j][8 * q:8 * q + 8]),
                dsk=np.ascontiguousarray(I['ssd_d_skip'][j][8 * q:8 * q + 8]),
                nw=np.ascontiguousarray(I['ssd_norm_w'][j][512 * q:512 * q + 512]))


def _mla_sel(I, hq):
    uq = I['mla_w_uq'][0].reshape(512, 16, 96)[:, 4 * hq:4 * hq + 4].reshape(512, 384)
    ukv = I['mla_w_ukv'][0].reshape(256, 16, 128)[:, 4 * hq:4 * hq + 4].reshape(256, 512)
    return dict(w_in=I['mla_w_in'][0], qn=I['mla_q_norm'][0], kvn=I['mla_kv_norm'][0],
                wuq=np.ascontiguousarray(uq), wukv=np.ascontiguousarray(ukv))


def _run(K, in_maps):
    res = run_bass_kernel_spmd(K.nc, in_maps, core_ids=list(range(8)))
    return res.results


def kernel_unfused(**I):
    I = {k: np.asarray(v) for k, v in I.items()}
    B, SEQ = I['x'].shape[0], I['x'].shape[1]
    NT = SEQ // 4
    cores = [(k // 4, k % 4) for k in range(8)]

    def run_mixer_ssd(xcur, layer, j):
        K = build_ssd(SEQ)
        maps = [dict(x=xcur[b], c=I['c'][b], aw=I['ada_w'][layer], ab=I['ada_b'][layer], **_ssd_sel(I, j, q))
                for b, q in cores]
        r = _run(K, maps)
        return [np.concatenate([r[b * 4 + q]["mT"] for q in range(4)], 0) for b in range(B)]

    def run_mixer_mla(xcur, layer):
        K = build_mla(SEQ)
        maps = [dict(x=xcur[b], c=I['c'][b], aw=I['ada_w'][layer], ab=I['ada_b'][layer],
                     pos=np.ascontiguousarray(I['positions'][b].astype(np.int32)), **_mla_sel(I, q)) for b, q in cores]
        r = _run(K, maps)
        return [np.concatenate([r[b * 4 + q]["mT"] for q in range(4)], 0) for b in range(B)]

    def run_tok(xcur, steps, extra):
        K = build_tok(NT, steps)
        maps = []
        for b, r_ in cores:
            m = dict(x=np.ascontiguousarray(xcur[b][r_ * NT:(r_ + 1) * NT]), c=I['c'][b])
            for (kind, l, dm) in steps:
                m[f"aw{l}"] = I['ada_w'][l]; m[f"ab{l}"] = I['ada_b'][l]
                jj = 0 if kind in ("proj", "sg") else 1
                m[f"lng{l}_{jj}"] = I['ln_g'][l, jj]; m[f"lnb{l}_{jj}"] = I['ln_b'][l, jj]
            for k_, v in extra.items():
                m[k_] = v(b, r_) if callable(v) else v
            maps.append(m)
        r = _run(K, maps)
        return [np.concatenate([r[b * 4 + q]["y"] for q in range(4)], 0) for b in range(B)]

    def ffn_w(si, k):
        return {f"s{si}_wg": I['ffn_w_gate'][k], f"s{si}_wu": I['ffn_w_up'][k], f"s{si}_wd": I['ffn_w_down'][k]}

    def moe_w(si, k):
        return {f"s{si}_wg": I['moe_w_gate'][k], f"s{si}_wu": I['moe_w_up'][k], f"s{si}_wd": I['moe_w_down'][k],
                f"s{si}_wr": I['moe_w_router'][k]}

    def mt_slice(mT):
        return lambda b, r_: np.ascontiguousarray(mT[b][:, r_ * NT:(r_ + 1) * NT])

    xcur = [I['x'][b] for b in range(B)]
    mT = run_mixer_ssd(xcur, 0, 0)
    xcur = run_tok(xcur, [("proj", 0, 2048), ("ffn", 0, 0)],
                   {"s0_mT": mt_slice(mT), "s0_wo": I['ssd_w_out'][0], **ffn_w(1, 0)})
    mT = run_mixer_mla(xcur, 1)
    sgw = {"s2_win": I['sg_w_in'][0], "s2_bin": I['sg_b_in'][0], "s2_sg": I['sg_ln_g'][0], "s2_sb": I['sg_ln_b'][0],
           "s2_ws": I['sg_w_s'][0], "s2_bs": I['sg_b_s'][0], "s2_wo": I['sg_w_out'][0]}
    xcur = run_tok(xcur, [("proj", 1, 1024), ("moe", 1, 0), ("sg", 2, 0), ("ffn", 2, 0)],
                   {"s0_mT": mt_slice(mT), "s0_wo": I['mla_w_out'][0], **moe_w(1, 0), **sgw, **ffn_w(3, 1)})
    mT = run_mixer_ssd(xcur, 3, 1)
    xcur = run_tok(xcur, [("proj", 3, 2048), ("moe", 3, 0)],
                   {"s0_mT": mt_slice(mT), "s0_wo": I['ssd_w_out'][1], **moe_w(1, 1)})
    return np.stack(xcur, 0).astype(np.float32)


RG = [[0, 1, 2, 3], [4, 5, 6, 7]]


def collective(K, kind, in_ap, out_ap):
    S = K.S
    if not hasattr(K, "cc"):
        K.cc = S.new_dma_sem()
    S.barrier()
    op = ALU.add if kind == "ReduceScatter" else ALU.bypass
    ins = K.nc.gpsimd.collective_compute(kind, op, replica_groups=RG, ins=[in_ap], outs=[out_ap])
    K.cc.val += 1
    ins.then_inc(K.cc.sem)
    S.barrier()


def sub_pproj(K, mT_ap, DM, wo_ap, ypart_ap, NTok):
    S, nc = K.S, K.nc
    NC_ = DM // 128
    old_scope = K.scope
    _mk = K.S.mark()
    with ExitStack() as sc:
        K.scope = sc
        wout = K.sb([128, NC_, 1024], BF16, tag="wout")
        S.dma("gpsimd", S.new_dma_sem(), wout.t[:], wo_ap.rearrange("(c p) d -> p c d", p=128), writes=[wout.d])
        mt = [K.sb([128, NC_, 128], BF16, tag="mt") for _ in range(2)]
        msem = [S.new_dma_sem() for _ in range(2)]
        yo = [K.sb([128, 1024], F32, tag="yo") for _ in range(2)]
        osem = [S.new_dma_sem() for _ in range(2)]
        for ts in range(NTok // 128):
            b = ts % 2
            S.dma("sync", msem[b], mt[b].t[:], mT_ap[:, ts * 128:(ts + 1) * 128].rearrange("(c p) t -> p c t", p=128),
                  writes=[mt[b].d])
            po = K.psum[2 + b]
            for db in range(2):
                fns = [(lambda e, c=c, db=db, po=po, b=b: e.matmul(
                    po.t[:, db * 512:(db + 1) * 512], lhsT=mt[b].t[:, c, :], rhs=wout.t[:, c, db * 512:(db + 1) * 512],
                    start=(c == 0), stop=(c == NC_ - 1))) for c in range(NC_)]
                S.group("tensor", fns, reads=[mt[b].d, wout.d], writes=[po.ds[db]])
            S.op("scalar", lambda e, po=po, b=b: e.activation(out=yo[b].t[:], in_=po.t[:], func=AF.Copy),
                 reads=[po.ds[0], po.ds[1]], writes=[yo[b].d])
            S.dma("gpsimd", osem[b], ypart_ap(ts), yo[b].t[:], reads=[yo[b].d])
        S.barrier()
    K.scope = old_scope
    K.S.release(_mk)


def sub_resln(K, xs, xd, NT, y_ap, ada_w_l, ada_b_l, lng_ap, lnb_ap):
    S, nc = K.S, K.nc
    old_scope = K.scope
    _mk = K.S.mark()
    with ExitStack() as sc:
        K.scope = sc
        g1p = K.sb([128, 1024], F32, tag="g1p")
        lng = K.sb([128, 1024], F32, tag="lng")
        lnb = K.sb([128, 1024], F32, tag="lnb")
        K.ln_alloc()
        K.mod_vec(g1p, ada_w_l, ada_b_l, 2, True)
        K.bcast_vec(lng, lng_ap, S.new_dma_sem())
        K.bcast_vec(lnb, lnb_ap, S.new_dma_sem())
        yin = [K.sb([128, 1024], F32, tag="yin") for _ in range(2)]
        ysem = [S.new_dma_sem() for _ in range(2)]
        xin = [K.sb([128, 1024], F32, tag="xin") for _ in range(2)]
        xsem = [S.new_dma_sem() for _ in range(2)]
        tmp = [K.sb([128, 1024], F32, tag="tmp") for _ in range(2)]
        xo = [K.sb([128, 1024], F32, tag="xo") for _ in range(2)]
        osem = [S.new_dma_sem() for _ in range(2)]
        for ts in range(NT // 128):
            b = ts % 2
            S.dma("sync", ysem[b], yin[b].t[:], y_ap[ts * 128:(ts + 1) * 128, :], writes=[yin[b].d])
            S.dma("sync", xsem[b], xin[b].t[:], xs.ap[ts * 128:(ts + 1) * 128, :], reads=[xs.ds[ts]],
                  writes=[xin[b].d])
            residual_ln(K, yin[b].t[:], [yin[b].d], xin[b], g1p, lng, lnb, tmp[b], xo[b])
            S.dma("gpsimd", osem[b], xd.ap[ts * 128:(ts + 1) * 128, :], xo[b].t[:], reads=[xo[b].d],
                  writes=[xd.ds[ts]])
        S.barrier()
    K.scope = old_scope
    K.S.release(_mk)


def build_fused(SEQ):
    NT = SEQ // 4
    K = KB()
    S, nc = K.S, K.nc
    x_in = K.din("x", [NT, D]); c = K.din("c", [D]); pos = K.din("pos", [SEQ], I32)
    aw = [K.din(f"aw{l}", [D, 6 * D]) for l in range(4)]
    ab = [K.din(f"ab{l}", [6 * D]) for l in range(4)]
    lng = [[K.din(f"lng{l}_{j}", [D]) for j in range(2)] for l in range(4)]
    lnb = [[K.din(f"lnb{l}_{j}", [D]) for j in range(2)] for l in range(4)]
    ssd = []
    for j in range(2):
        ssd.append(dict(w=K.din(f"ssd{j}_w", [D, 1544]), cw=K.din(f"ssd{j}_cw", [4, 1024]),
                        cbv=K.din(f"ssd{j}_cb", [1024]), dtb=K.din(f"ssd{j}_dtb", [8]),
                        alog=K.din(f"ssd{j}_alog", [8]), dsk=K.din(f"ssd{j}_dsk", [8]),
                        nw=K.din(f"ssd{j}_nw", [512]), wo=K.din(f"ssd{j}_wo", [512, D])))
    mla = dict(w_in=K.din("mla_w_in", [D, 800]), qn=K.din("mla_qn", [512]), kvn=K.din("mla_kvn", [256]),
               wuq_d=K.din("mla_wuq", [512, 384]), wukv_d=K.din("mla_wukv", [256, 512]))
    mla_wo = K.din("mla_wo", [256, D])
    sg = dict(w_in=K.din("sg_win", [D, 4096]), b_in=K.din("sg_bin", [4096]), sg_g=K.din("sg_g", [2048]),
              sg_b=K.din("sg_b", [2048]), w_s=K.din("sg_ws", [8, 128, 128]), b_s=K.din("sg_bs", [8, 128]),
              wo=K.din("sg_wo", [2048, D]))
    ffn = [dict(wg=K.din(f"ffn{k}_wg", [D, DFF]), wu=K.din(f"ffn{k}_wu", [D, DFF]), wd=K.din(f"ffn{k}_wd", [DFF, D]))
           for k in range(2)]
    moe = [dict(wg=K.din(f"moe{k}_wg", [NE, D, DFF]), wu=K.din(f"moe{k}_wu", [NE, D, DFF]),
                wd=K.din(f"moe{k}_wd", [NE, DFF, D]), wr=K.din(f"moe{k}_wr", [D, NE])) for k in range(2)]
    y = K.dout("y", [NT, D])
    K.alloc_psum(); K.consts(); K.setup_cond(c)
    CH = 256
    NCH = NT // CH
    xfull_c = K.dtmp("xfull", [NCH, 4 * CH, D])
    ypart_c = K.dtmp("ypart", [NCH, 4 * CH, D]); yred = K.dtmp("yred", [NT, D])

    def rows(buf):
        def f(tile):
            t = tile * 128
            r, rem = t // NT, t % NT
            ch, i = rem // CH, rem % CH
            return buf[ch, r * CH + i:r * CH + i + 128, :]
        return f

    xfull = rows(xfull_c)
    ypart = rows(ypart_c)

    def gather_x(src):
        for ch in range(NCH):
            collective(K, "AllGather", src[ch * CH:(ch + 1) * CH, :], xfull_c[ch])

    def scatter_y():
        for ch in range(NCH):
            collective(K, "ReduceScatter", ypart_c[ch], yred[ch * CH:(ch + 1) * CH, :])
    mTs = K.dtmp("mTs", [512, SEQ], BF16); mTm = K.dtmp("mTm", [256, SEQ], BF16)
    xl = [K.dtmp(f"xl{i}", [NT, D]) for i in range(8)]

    def stream(ap):
        return DramStream(ap, NT)

    cps = S.new_dma_sem()
    for ch in range(NCH):
        S.dma("sync", cps, xl[0][ch * CH:(ch + 1) * CH, :], x_in[ch * CH:(ch + 1) * CH, :])
    gather_x(xl[0])
    s = ssd[0]
    sub_ssd(K, xfull, mTs, SEQ, aw[0], ab[0], s["w"], s["cw"], s["cbv"], s["dtb"], s["alog"], s["dsk"], s["nw"])
    sub_pproj(K, mTs, 512, s["wo"], ypart, SEQ)
    scatter_y()
    sub_resln(K, stream(xl[0]), stream(xl[1]), NT, yred, aw[0], ab[0], lng[0][0], lnb[0][0])
    f = ffn[0]
    sub_ffn(K, stream(xl[1]), stream(xl[2]), NT, aw[0], ab[0], lng[0][1], lnb[0][1], f["wg"], f["wu"], f["wd"], None)
    gather_x(xl[2])
    sub_mla(K, xfull, mTm, SEQ, aw[1], ab[1], pos, **mla)
    sub_pproj(K, mTm, 256, mla_wo, ypart, SEQ)
    scatter_y()
    sub_resln(K, stream(xl[2]), stream(xl[3]), NT, yred, aw[1], ab[1], lng[1][0], lnb[1][0])
    m = moe[0]
    sub_ffn(K, stream(xl[3]), stream(xl[4]), NT, aw[1], ab[1], lng[1][1], lnb[1][1], m["wg"], m["wu"], m["wd"], m["wr"])
    sub_sg(K, stream(xl[4]), stream(xl[5]), NT, aw[2], ab[2], lng[2][0], lnb[2][0], sg["w_in"], sg["b_in"], sg["sg_g"],
           sg["sg_b"], sg["w_s"], sg["b_s"], sg["wo"])
    f = ffn[1]
    sub_ffn(K, stream(xl[5]), stream(xl[6]), NT, aw[2], ab[2], lng[2][1], lnb[2][1], f["wg"], f["wu"], f["wd"], None)
    gather_x(xl[6])
    s = ssd[1]
    sub_ssd(K, xfull, mTs, SEQ, aw[3], ab[3], s["w"], s["cw"], s["cbv"], s["dtb"], s["alog"], s["dsk"], s["nw"])
    sub_pproj(K, mTs, 512, s["wo"], ypart, SEQ)
    scatter_y()
    sub_resln(K, stream(xl[6]), stream(xl[7]), NT, yred, aw[3], ab[3], lng[3][0], lnb[3][0])
    m = moe[1]
    sub_ffn(K, stream(xl[7]), stream(y), NT, aw[3], ab[3], lng[3][1], lnb[3][1], m["wg"], m["wu"], m["wd"], m["wr"])
    S.finish()
    return K


def fused_inputs(I, SEQ, b, q):
    NT = SEQ // 4
    m = dict(x=np.ascontiguousarray(I['x'][b][q * NT:(q + 1) * NT]), c=np.ascontiguousarray(I['c'][b]),
             pos=np.ascontiguousarray(I['positions'][b].astype(np.int32)))
    for l in range(4):
        m[f"aw{l}"] = I['ada_w'][l]; m[f"ab{l}"] = I['ada_b'][l]
        for j in range(2):
            m[f"lng{l}_{j}"] = I['ln_g'][l, j]; m[f"lnb{l}_{j}"] = I['ln_b'][l, j]
    for j in range(2):
        s = _ssd_sel(I, j, q)
        for k_, v in s.items():
            m[f"ssd{j}_{k_}"] = v
        m[f"ssd{j}_wo"] = np.ascontiguousarray(I['ssd_w_out'][j][512 * q:512 * q + 512])
    s = _mla_sel(I, q)
    m["mla_w_in"] = s["w_in"]; m["mla_qn"] = s["qn"]; m["mla_kvn"] = s["kvn"]; m["mla_wuq"] = s["wuq"]; m["mla_wukv"] = s["wukv"]
    m["mla_wo"] = np.ascontiguousarray(I['mla_w_out'][0][256 * q:256 * q + 256])
    m["sg_win"] = I['sg_w_in'][0]; m["sg_bin"] = I['sg_b_in'][0]; m["sg_g"] = I['sg_ln_g'][0]; m["sg_b"] = I['sg_ln_b'][0]
    m["sg_ws"] = I['sg_w_s'][0]; m["sg_bs"] = I['sg_b_s'][0]; m["sg_wo"] = I['sg_w_out'][0]
    for k in range(2):
        m[f"ffn{k}_wg"] = I['ffn_w_gate'][k]; m[f"ffn{k}_wu"] = I['ffn_w_up'][k]; m[f"ffn{k}_wd"] = I['ffn_w_down'][k]
        m[f"moe{k}_wg"] = I['moe_w_gate'][k]; m[f"moe{k}_wu"] = I['moe_w_up'][k]; m[f"moe{k}_wd"] = I['moe_w_down'][k]
        m[f"moe{k}_wr"] = I['moe_w_router'][k]
    return m


def kernel(**I):
    I = {k: np.asarray(v) for k, v in I.items()}
    B, SEQ = I['x'].shape[0], I['x'].shape[1]
    NT = SEQ // 4
    K = build_fused(SEQ)
    maps = [fused_inputs(I, SEQ, k // 4, k % 4) for k in range(8)]
    r = _run(K, maps)
    return np.stack([np.concatenate([r[b * 4 + q]["y"] for q in range(4)], 0) for b in range(B)], 0).astype(np.float32)
```

```python
import math
from contextlib import ExitStack

import numpy as np
import concourse.bass as bass
import concourse.mybir as mybir
from concourse.bass_utils import run_bass_kernel_spmd

F32 = mybir.dt.float32
BF16 = mybir.dt.bfloat16
I32 = mybir.dt.int32
AF = mybir.ActivationFunctionType
ALU = mybir.AluOpType

D = 1024
DFF = 2816
NE = 8
ALPHA = 8.0 ** 0.25
LN_EPS = 1e-5
RMS_EPS = 1e-6


class Dep:
    __slots__ = ("w", "r")

    def __init__(self):
        self.w = None
        self.r = {}


class DmaSem:
    def __init__(self, sem):
        self.sem = sem
        self.val = 0


class Eng:
    def __init__(self, name, engine, sem):
        self.name = name
        self.e = engine
        self.sem = sem
        self.count = 0
        self.seen = {}


class Sync:
    def __init__(self, nc, stack):
        self.nc = nc
        self.stack = stack
        self.engs = {}
        for name in ("tensor", "vector", "scalar", "gpsimd", "sync"):
            sem = stack.enter_context(nc.semaphore("s_" + name))
            self.engs[name] = Eng(name, getattr(nc, name), sem)
        self.dma_sems = []
        self.n_inst = 0

    def new_dma_sem(self):
        if getattr(self, "free", None):
            d = self.free.pop()
        else:
            sem = self.stack.enter_context(self.nc.semaphore(None))
            d = DmaSem(sem)
            self.dma_sems.append(d)
        if not hasattr(self, "handed"):
            self.handed = []
            self.free = []
        self.handed.append(d)
        return d

    def mark(self):
        if not hasattr(self, "handed"):
            self.handed = []
            self.free = []
        return len(self.handed)

    def release(self, mk):
        self.free.extend(self.handed[mk:])
        del self.handed[mk:]

    def _waits(self, eng, reads, writes):
        need = {}
        for d in reads:
            if d.w is not None and need.get(d.w[0], 0) < d.w[1]:
                need[d.w[0]] = d.w[1]
        for d in writes:
            if d.w is not None and need.get(d.w[0], 0) < d.w[1]:
                need[d.w[0]] = d.w[1]
            for k, v in d.r.items():
                if need.get(k, 0) < v:
                    need[k] = v
        for k, v in need.items():
            if eng.seen.get(k, 0) < v:
                eng.e.wait_ge(k, v)
                eng.seen[k] = v

    def _mark(self, key, val, reads, writes):
        for d in reads:
            d.r[key] = val
        for d in writes:
            d.w = (key, val)
            d.r = {}

    def op(self, en, fn, reads=(), writes=()):
        eng = self.engs[en]
        self._waits(eng, reads, writes)
        ins = fn(eng.e)
        eng.count += 1
        ins.then_inc(eng.sem, 1)
        self.n_inst += 1
        self._mark(eng.sem, eng.count, reads, writes)

    def group(self, en, fns, reads=(), writes=()):
        eng = self.engs[en]
        self._waits(eng, reads, writes)
        ins = None
        for fn in fns:
            ins = fn(eng.e)
            self.n_inst += 1
        eng.count += 1
        ins.then_inc(eng.sem, 1)
        self._mark(eng.sem, eng.count, reads, writes)

    def dma(self, en, dsem, out, in_, reads=(), writes=(), **kw):
        eng = self.engs[en]
        self._waits(eng, reads, writes)
        ins = eng.e.dma_start(out=out, in_=in_, **kw)
        dsem.val += 16
        ins.then_inc(dsem.sem, 16)
        self.n_inst += 1
        self._mark(dsem.sem, dsem.val, reads, writes)

    def barrier(self):
        for eng in self.engs.values():
            for e2 in self.engs.values():
                if e2.count > 0 and eng.seen.get(e2.sem, 0) < e2.count:
                    eng.e.wait_ge(e2.sem, e2.count)
                    eng.seen[e2.sem] = e2.count
            for d in self.dma_sems:
                if d.val > 0 and eng.seen.get(d.sem, 0) < d.val:
                    eng.e.wait_ge(d.sem, d.val)
                    eng.seen[d.sem] = d.val

    def finish(self):
        self.barrier()


class Buf:
    def __init__(self, t, n=1):
        self.t = t
        self.ds = [Dep() for _ in range(n)]

    @property
    def d(self):
        return self.ds[0]


class KB:
    def __init__(self):
        self.nc = bass.Bass("TRN2", target_bir_lowering=False)
        self.st = ExitStack()
        self.S = Sync(self.nc, self.st)
        self.scope = self.st
        self._n = 0
        self.psum = None

    def name(self, p):
        self._n += 1
        return f"{p}{self._n}"

    def din(self, name, shape, dt=F32):
        return self.nc.dram_tensor(name, list(shape), dt, kind="ExternalInput").ap()

    def dout(self, name, shape, dt=F32):
        return self.nc.dram_tensor(name, list(shape), dt, kind="ExternalOutput").ap()

    def dtmp(self, name, shape, dt=F32):
        return self.nc.dram_tensor(name, list(shape), dt, kind="Internal").ap()

    def sb(self, shape, dt, n=1, tag="t"):
        t = self.scope.enter_context(self.nc.sbuf_tensor(self.name(tag), list(shape), dt))
        return Buf(t, n)

    def alloc_psum(self):
        self.psum = [Buf(self.st.enter_context(self.nc.psum_tensor(f"ps{i}", [128, 1024], F32)), 2)
                     for i in range(4)]

    def consts(self):
        S, nc = self.S, self.nc
        ii = self.sb([128, 128], I32, tag="ii")
        self.ident = self.sb([128, 128], F32, tag="ident")
        S.op("gpsimd", lambda e: e.iota(ii.t[:], pattern=[[1, 128]], base=0, channel_multiplier=-1),
             writes=[ii.d])
        S.op("vector", lambda e: e.tensor_single_scalar(out=self.ident.t[:], in_=ii.t[:], scalar=0,
                                                        op=ALU.is_equal), reads=[ii.d], writes=[self.ident.d])
        self.eps_ln = self.sb([128, 1], F32, tag="eps")
        S.op("vector", lambda e: e.memset(self.eps_ln.t[:], LN_EPS), writes=[self.eps_ln.d])

    def setup_cond(self, c_ap):
        S, nc = self.S, self.nc
        ccol = self.sb([128, 8], F32, tag="ccol")
        self.cbc = self.sb([128, 8, 128], F32, tag="cbc")
        self.modw = self.sb([128, 8, 512], F32, tag="modw")
        self.modb = self.sb([128, 1024], F32, tag="modb")
        self.sem_c = S.new_dma_sem()
        self.sem_mw = S.new_dma_sem()
        self.sem_mb = S.new_dma_sem()
        with nc.allow_non_contiguous_dma(reason="tiny column load"):
            S.dma("sync", self.sem_c, ccol.t[:], c_ap.rearrange("(c p) -> p c", p=128), writes=[ccol.d])
        S.op("scalar", lambda e: e.activation(out=ccol.t[:], in_=ccol.t[:], func=AF.Silu),
             reads=[ccol.d], writes=[ccol.d])
        S.op("vector", lambda e: e.tensor_copy(out=self.cbc.t[:],
                                               in_=ccol.t[:].unsqueeze(2).to_broadcast([128, 8, 128])),
             reads=[ccol.d], writes=[self.cbc.d])

    def mod_vec(self, out_buf, ada_w_l, ada_b_l, idx, plus_one):
        S = self.S
        ps = self.psum[0]
        S.dma("sync", self.sem_mb, self.modb.t[:],
              ada_b_l[idx * 1024:(idx + 1) * 1024].unsqueeze(0).to_broadcast([128, 1024]),
              writes=[self.modb.d])
        for hb in range(2):
            c0 = idx * 1024 + hb * 512
            S.dma("sync", self.sem_mw, self.modw.t[:],
                  ada_w_l[:, c0:c0 + 512].rearrange("(c p) f -> p c f", p=128), writes=[self.modw.d])
            fns = [(lambda e, k=k: e.matmul(ps.t[:, hb * 512:(hb + 1) * 512], lhsT=self.cbc.t[:, k, :],
                                             rhs=self.modw.t[:, k, :], start=(k == 0), stop=(k == 7)))
                   for k in range(8)]
            S.group("tensor", fns, reads=[self.cbc.d, self.modw.d], writes=[ps.ds[hb]])
        if plus_one:
            S.op("vector", lambda e: e.scalar_tensor_tensor(out=out_buf.t[:], in0=ps.t[:], scalar=1.0,
                                                            in1=self.modb.t[:], op0=ALU.add, op1=ALU.add),
                 reads=[ps.ds[0], ps.ds[1], self.modb.d], writes=[out_buf.d])
        else:
            S.op("vector", lambda e: e.tensor_tensor(out=out_buf.t[:], in0=ps.t[:], in1=self.modb.t[:],
                                                     op=ALU.add),
                 reads=[ps.ds[0], ps.ds[1], self.modb.d], writes=[out_buf.d])

    def bcast_vec(self, out_buf, vec_ap, sem):
        n = vec_ap.shape[0]
        self.S.dma("sync", sem, out_buf.t[:], vec_ap.unsqueeze(0).to_broadcast([128, n]), writes=[out_buf.d])

    def ln_alloc(self):
        self.ln_st = self.sb([128, 2, 6], F32, tag="lnst")
        self.ln_mv = self.sb([128, 2], F32, tag="lnmv")
        self.ln_rs = self.sb([128, 1], F32, tag="lnrs")

    def layer_norm(self, r, out, g_bc, b_bc):
        S = self.S
        st, mv, rs = self.ln_st, self.ln_mv, self.ln_rs
        for hb in range(2):
            S.op("vector", lambda e, hb=hb: e.bn_stats(out=st.t[:, hb, :], in_=r.t[:, hb * 512:(hb + 1) * 512]),
                 reads=[r.d], writes=[st.d])
        S.op("vector", lambda e: e.bn_aggr(out=mv.t[:], in_=st.t[:].rearrange("p a b -> p (a b)")),
             reads=[st.d], writes=[mv.d])
        S.op("scalar", lambda e: e.activation(out=rs.t[:], in_=mv.t[:, 1:2], func=AF.Sqrt,
                                              bias=self.eps_ln.t[:], scale=1.0),
             reads=[mv.d, self.eps_ln.d], writes=[rs.d])
        S.op("vector", lambda e: e.reciprocal(out=rs.t[:], in_=rs.t[:]), reads=[rs.d], writes=[rs.d])
        S.op("vector", lambda e: e.tensor_scalar(out=r.t[:], in0=r.t[:], scalar1=mv.t[:, 0:1], scalar2=rs.t[:],
                                                 op0=ALU.subtract, op1=ALU.mult),
             reads=[r.d, mv.d, rs.d], writes=[r.d])
        S.op("vector", lambda e: e.tensor_tensor(out=r.t[:], in0=r.t[:], in1=g_bc.t[:], op=ALU.mult),
             reads=[r.d, g_bc.d], writes=[r.d])
        S.op("vector", lambda e: e.tensor_tensor(out=out.t[:], in0=r.t[:], in1=b_bc.t[:], op=ALU.add),
             reads=[r.d, b_bc.d], writes=[out.d])


class DramStream:
    def __init__(self, ap, nt):
        self.ap = ap
        self.ds = [Dep() for _ in range(nt // 128)]


def sub_ffn(K, xs, xd, NT, ada_w_l, ada_b_l, lng_ap, lnb_ap, wg_ap, wu_ap, wd_ap, wr_ap=None):
    S, nc = K.S, K.nc
    moe = wr_ap is not None
    import os as _os
    E = int(_os.environ.get('MOE_E', NE)) if moe else 1
    T = min(NT, 2048)
    NSUP, NSUB, NB = NT // T, T // 128, T // 512
    JG = 2
    NG = DFF // (128 * JG)
    old_scope = K.scope
    _mk = K.S.mark()
    with ExitStack() as sc:
        K.scope = sc
        xT = K.sb([128, 8, T], BF16, n=NB, tag="xT")
        acc = K.sb([128, NSUB, 1024], F32, n=NSUB, tag="acc")
        wgb = [K.sb([128, 8, 128 * JG], BF16, tag="wg") for _ in range(2)]
        wub = [K.sb([128, 8, 128 * JG], BF16, tag="wu") for _ in range(2)]
        wdb = [K.sb([128, JG, 1024], BF16, tag="wd") for _ in range(2)]
        wsem = [S.new_dma_sem() for _ in range(2)]
        hT = [K.sb([128, JG, 512], BF16, n=JG, tag="hT") for _ in range(2)]
        sg = [K.sb([128, 512], F32, tag="sg") for _ in range(2)]
        xin = [K.sb([128, 1024], F32, tag="xin") for _ in range(2)]
        xsem = [S.new_dma_sem() for _ in range(2)]
        hf = [K.sb([128, 1024], F32, tag="hf") for _ in range(2)]
        xo = [K.sb([128, 1024], F32, tag="xo") for _ in range(2)]
        osem = [S.new_dma_sem() for _ in range(2)]
        sc1p = K.sb([128, 1024], F32, tag="sc1p")
        shf = K.sb([128, 1024], F32, tag="shf")
        g1p = K.sb([128, 1024], F32, tag="g1p")
        lng = K.sb([128, 1024], F32, tag="lng")
        lnb = K.sb([128, 1024], F32, tag="lnb")
        csem = S.new_dma_sem()
        K.ln_alloc()
        K.mod_vec(shf, ada_w_l, ada_b_l, 3, False)
        K.mod_vec(sc1p, ada_w_l, ada_b_l, 4, True)
        K.mod_vec(g1p, ada_w_l, ada_b_l, 5, True)
        K.bcast_vec(lng, lng_ap, csem)
        K.bcast_vec(lnb, lnb_ap, S.new_dma_sem())
        if moe:
            hfT32 = K.sb([128, 8, 128], F32, tag="hfT32")
            wr = K.sb([128, 8, NE], F32, tag="wr")
            if _os.environ.get("DBG4") != "1":
                with nc.allow_non_contiguous_dma(reason="router weights, tiny"):
                    S.dma("sync", S.new_dma_sem(), wr.t[:], wr_ap.rearrange("(c p) e -> p c e", p=128), writes=[wr.d])
            comb = K.sb([128, NSUB, NE], F32, tag="comb")
            lgall = K.sb([128, NSUB, NE], F32, tag="lgall")
            l2 = K.sb([128, NSUB, NE], F32, tag="l2")
            mk1 = K.sb([128, NSUB, NE], F32, tag="mk1")
            mk2 = K.sb([128, NSUB, NE], F32, tag="mk2")
            m1 = K.sb([128, NSUB], F32, tag="m1")
            m2 = K.sb([128, NSUB], F32, tag="m2")
            w1 = K.sb([128, NSUB], F32, tag="w1")
            w2 = K.sb([128, NSUB], F32, tag="w2")
        psG = [K.psum[0], K.psum[1]]
        psO = [K.psum[2], K.psum[3]]

        def wsrc(ap, e):
            return ap[e % ap.shape[0]] if moe else ap

        def load_w(e, jg, slot):
            f0 = jg * 128 * JG
            S.dma("gpsimd", wsem[slot], wgb[slot].t[:],
                  wsrc(wg_ap, e)[:, f0:f0 + 128 * JG].rearrange("(c p) f -> p c f", p=128), writes=[wgb[slot].d])
            S.dma("gpsimd", wsem[slot], wub[slot].t[:],
                  wsrc(wu_ap, e)[:, f0:f0 + 128 * JG].rearrange("(c p) f -> p c f", p=128), writes=[wub[slot].d])
            S.dma("gpsimd", wsem[slot], wdb[slot].t[:],
                  wsrc(wd_ap, e)[f0:f0 + 128 * JG, :].rearrange("(j p) d -> p j d", p=128), writes=[wdb[slot].d])
            wgb[slot].d.w = wub[slot].d.w = wdb[slot].d.w

        wlist = [(e, jg) for e in range(E) for jg in range(NG)]
        for sup in range(NSUP):
            t0 = sup * T
            load_w(*wlist[0], 0)
            for ts in range(NSUB):
                b = ts % 2
                gi = (t0 // 128) + ts
                S.dma("sync", xsem[b], xin[b].t[:], xs.ap[gi * 128:(gi + 1) * 128, :],
                      reads=[xs.ds[gi]], writes=[xin[b].d])
                S.op("vector", lambda e, b=b: e.tensor_tensor(out=hf[b].t[:], in0=xin[b].t[:], in1=sc1p.t[:],
                                                              op=ALU.mult),
                     reads=[xin[b].d, sc1p.d], writes=[hf[b].d])
                S.op("vector", lambda e, b=b: e.tensor_tensor(out=hf[b].t[:], in0=hf[b].t[:], in1=shf.t[:],
                                                              op=ALU.add),
                     reads=[hf[b].d, shf.d], writes=[hf[b].d])
                po = psO[b]
                for hb in range(2):
                    fns = [(lambda e, k=k, b=b, po=po: e.transpose(out=po.t[:, k * 128:(k + 1) * 128],
                                                                    in_=hf[b].t[:, k * 128:(k + 1) * 128],
                                                                    identity=K.ident.t[:]))
                           for k in range(hb * 4, hb * 4 + 4)]
                    S.group("tensor", fns, reads=[hf[b].d, K.ident.d], writes=[po.ds[hb]])
                if not moe:
                    S.op("scalar", lambda e, po=po, ts=ts: e.activation(
                        out=xT.t[:, :, ts * 128:(ts + 1) * 128], in_=po.t[:].rearrange("p (c t) -> p c t", c=8),
                        func=AF.Copy), reads=[po.ds[0], po.ds[1]], writes=[xT.ds[ts // 4]])
                else:
                    S.op("scalar", lambda e, po=po: e.activation(
                        out=hfT32.t[:], in_=po.t[:].rearrange("p (c t) -> p c t", c=8), func=AF.Copy),
                         reads=[po.ds[0], po.ds[1]], writes=[hfT32.d])
                    S.op("gpsimd", lambda e, ts=ts: e.tensor_copy(out=xT.t[:, :, ts * 128:(ts + 1) * 128],
                                                                  in_=hfT32.t[:]),
                         reads=[hfT32.d], writes=[xT.ds[ts // 4]])
                if moe:
                    pl = psG[0]
                    fns = [(lambda e, k=k, pl=pl: e.matmul(pl.t[:, 0:NE], lhsT=hfT32.t[:, k, :], rhs=wr.t[:, k, :],
                                                           start=(k == 0), stop=(k == 7))) for k in range(8)]
                    import os as _os
                    if _os.environ.get("MOEDBG") == "1":
                        S.op("vector", lambda e, ts=ts: e.memset(lgall.t[:, ts, :], 0.0), writes=[lgall.d])
                    else:
                        S.group("tensor", fns, reads=[hfT32.d, wr.d], writes=[pl.ds[0]])
                        S.op("vector", lambda e, pl=pl, ts=ts: e.tensor_copy(out=lgall.t[:, ts, :], in_=pl.t[:, 0:NE]),
                             reads=[pl.ds[0]], writes=[lgall.d])
            if moe and _os.environ.get('DBG3') == '1':
                S.op('vector', lambda e: e.memset(comb.t[:], 0.125), writes=[comb.d])
            elif moe:
                X = mybir.AxisListType.X
                bc = lambda b_: b_.t[:].unsqueeze(2).to_broadcast([128, NSUB, NE])
                S.op("vector", lambda e: e.tensor_reduce(out=m1.t[:], in_=lgall.t[:], axis=X, op=ALU.max),
                     reads=[lgall.d], writes=[m1.d])
                S.op("vector", lambda e: e.tensor_tensor(out=mk1.t[:], in0=lgall.t[:], in1=bc(m1), op=ALU.is_equal),
                     reads=[lgall.d, m1.d], writes=[mk1.d])
                S.op("vector", lambda e: e.scalar_tensor_tensor(out=l2.t[:], in0=mk1.t[:], scalar=-1e30,
                                                                in1=lgall.t[:], op0=ALU.mult, op1=ALU.add),
                     reads=[mk1.d, lgall.d], writes=[l2.d])
                S.op("vector", lambda e: e.tensor_reduce(out=m2.t[:], in_=l2.t[:], axis=X, op=ALU.max),
                     reads=[l2.d], writes=[m2.d])
                S.op("vector", lambda e: e.tensor_tensor(out=mk2.t[:], in0=l2.t[:], in1=bc(m2), op=ALU.is_equal),
                     reads=[l2.d, m2.d], writes=[mk2.d])
                S.op("vector", lambda e: e.tensor_tensor(out=w2.t[:], in0=m2.t[:], in1=m1.t[:], op=ALU.subtract),
                     reads=[m1.d, m2.d], writes=[w2.d])
                S.op("scalar", lambda e: e.activation(out=w2.t[:], in_=w2.t[:], func=AF.Exp),
                     reads=[w2.d], writes=[w2.d])
                S.op("vector", lambda e: e.tensor_scalar(out=w1.t[:], in0=w2.t[:], scalar1=1.0, scalar2=None,
                                                         op0=ALU.add), reads=[w2.d], writes=[w1.d])
                S.op("vector", lambda e: e.reciprocal(out=w1.t[:], in_=w1.t[:]), reads=[w1.d], writes=[w1.d])
                S.op("vector", lambda e: e.tensor_tensor(out=w2.t[:], in0=w2.t[:], in1=w1.t[:], op=ALU.mult),
                     reads=[w1.d, w2.d], writes=[w2.d])
                S.op("vector", lambda e: e.tensor_tensor(out=mk1.t[:], in0=mk1.t[:], in1=bc(w1), op=ALU.mult),
                     reads=[mk1.d, w1.d], writes=[mk1.d])
                S.op("vector", lambda e: e.tensor_tensor(out=mk2.t[:], in0=mk2.t[:], in1=bc(w2), op=ALU.mult),
                     reads=[mk2.d, w2.d], writes=[mk2.d])
                S.op("vector", lambda e: e.tensor_tensor(out=comb.t[:], in0=mk1.t[:], in1=mk2.t[:], op=ALU.add),
                     reads=[mk1.d, mk2.d], writes=[comb.d])
            for ts in range(NSUB):
                S.op("gpsimd", lambda e, ts=ts: e.memset(acc.t[:, ts, :], 0.0), writes=[acc.ds[ts]])
            its = [(wi, tb) for wi in range(len(wlist)) for tb in range(NB)]

            def emit_gu(n):
                wi, tb = its[n]
                slot = wi % 2
                hb_ = hT[n % 2]
                for j in range(JG):
                    pg = psG[j % 2]
                    for which, wbuf in ((0, wgb[slot]), (1, wub[slot])):
                        fns = [(lambda e, k=k, j=j, which=which, wbuf=wbuf, pg=pg, tb=tb: e.matmul(
                            pg.t[:, which * 512:(which + 1) * 512], lhsT=wbuf.t[:, k, j * 128:(j + 1) * 128],
                            rhs=xT.t[:, k, tb * 512:(tb + 1) * 512], start=(k == 0), stop=(k == 7)))
                            for k in range(8)]
                        S.group("tensor", fns, reads=[wbuf.d, xT.ds[tb]], writes=[pg.ds[which]])
                    sgb = sg[j % 2]
                    S.op("scalar", lambda e, pg=pg, sgb=sgb: e.activation(out=sgb.t[:], in_=pg.t[:, 0:512],
                                                                         func=AF.Silu),
                         reads=[pg.ds[0]], writes=[sgb.d])
                    S.op("vector", lambda e, pg=pg, sgb=sgb, hb_=hb_, j=j: e.tensor_tensor(
                        out=hb_.t[:, j, :], in0=sgb.t[:], in1=pg.t[:, 512:1024], op=ALU.mult),
                         reads=[sgb.d, pg.ds[1]], writes=[hb_.ds[j]])

            def emit_d(n):
                wi, tb = its[n]
                slot = wi % 2
                e_ = wlist[wi][0]
                hb_ = hT[n % 2]
                for q in range(4):
                    ts = tb * 4 + q
                    po = psO[q % 2]
                    for db in range(2):
                        fns = [(lambda e, j=j, q=q, db=db, po=po, hb_=hb_, slot=slot: e.matmul(
                            po.t[:, db * 512:(db + 1) * 512], lhsT=hb_.t[:, j, q * 128:(q + 1) * 128],
                            rhs=wdb[slot].t[:, j, db * 512:(db + 1) * 512], start=(j == 0),
                            stop=(j == JG - 1))) for j in range(JG)]
                        S.group("tensor", fns, reads=[hb_.ds[0], hb_.ds[1], wdb[slot].d], writes=[po.ds[db]])
                    cs = comb.t[:, ts, e_:e_ + 1] if (moe and _os.environ.get('DBG2') != '1') else 1.0
                    rd = [po.ds[0], po.ds[1], acc.ds[ts]] + ([comb.d] if moe else [])
                    S.op("vector", lambda e, po=po, ts=ts, cs=cs: e.scalar_tensor_tensor(
                        out=acc.t[:, ts, :], in0=po.t[:], scalar=cs, in1=acc.t[:, ts, :], op0=ALU.mult,
                        op1=ALU.add), reads=rd, writes=[acc.ds[ts]])

            for n in range(len(its)):
                wi, tb = its[n]
                emit_gu(n)
                if n > 0:
                    emit_d(n - 1)
                if tb == 0 and wi + 1 < len(wlist):
                    load_w(*wlist[wi + 1], (wi + 1) % 2)
            emit_d(len(its) - 1)
            for ts in range(NSUB):
                b = ts % 2
                gi = (t0 // 128) + ts
                S.dma("sync", xsem[b], xin[b].t[:], xs.ap[gi * 128:(gi + 1) * 128, :],
                      reads=[xs.ds[gi]], writes=[xin[b].d])
                S.op("vector", lambda e, ts=ts, b=b: e.tensor_tensor(out=hf[b].t[:], in0=acc.t[:, ts, :],
                                                                    in1=g1p.t[:], op=ALU.mult),
                     reads=[acc.ds[ts], g1p.d], writes=[hf[b].d])
                S.op("vector", lambda e, b=b: e.scalar_tensor_tensor(out=hf[b].t[:], in0=xin[b].t[:], scalar=ALPHA,
                                                                     in1=hf[b].t[:], op0=ALU.mult, op1=ALU.add),
                     reads=[xin[b].d, hf[b].d], writes=[hf[b].d])
                K.layer_norm(hf[b], xo[b], lng, lnb)
                S.dma("gpsimd", osem[b], xd.ap[gi * 128:(gi + 1) * 128, :], xo[b].t[:],
                      reads=[xo[b].d], writes=[xd.ds[gi]])
        S.barrier()
    K.scope = old_scope
    K.S.release(_mk)


def residual_ln(K, y_ap, y_deps, xin, g1p, lng, lnb, tmp, xo):
    S = K.S
    S.op("vector", lambda e: e.tensor_tensor(out=tmp.t[:], in0=y_ap, in1=g1p.t[:], op=ALU.mult),
         reads=list(y_deps) + [g1p.d], writes=[tmp.d])
    S.op("vector", lambda e: e.scalar_tensor_tensor(out=tmp.t[:], in0=xin.t[:], scalar=ALPHA, in1=tmp.t[:],
                                                    op0=ALU.mult, op1=ALU.add),
         reads=[xin.d, tmp.d], writes=[tmp.d])
    K.layer_norm(tmp, xo, lng, lnb)


def sub_proj(K, xs, xd, NT, mT_ap, DM, wout_ap, ada_w_l, ada_b_l, lng_ap, lnb_ap):
    S, nc = K.S, K.nc
    NC_ = DM // 128
    old_scope = K.scope
    _mk = K.S.mark()
    with ExitStack() as sc:
        K.scope = sc
        wout = K.sb([128, NC_, 1024], BF16, tag="wout")
        S.dma("gpsimd", S.new_dma_sem(), wout.t[:], wout_ap.rearrange("(c p) d -> p c d", p=128), writes=[wout.d])
        g1p = K.sb([128, 1024], F32, tag="g1p")
        lng = K.sb([128, 1024], F32, tag="lng")
        lnb = K.sb([128, 1024], F32, tag="lnb")
        K.ln_alloc()
        K.mod_vec(g1p, ada_w_l, ada_b_l, 2, True)
        K.bcast_vec(lng, lng_ap, S.new_dma_sem())
        K.bcast_vec(lnb, lnb_ap, S.new_dma_sem())
        mt = [K.sb([128, NC_, 128], BF16, tag="mt") for _ in range(2)]
        msem = [S.new_dma_sem() for _ in range(2)]
        xin = [K.sb([128, 1024], F32, tag="xin") for _ in range(2)]
        xsem = [S.new_dma_sem() for _ in range(2)]
        tmp = [K.sb([128, 1024], F32, tag="tmp") for _ in range(2)]
        xo = [K.sb([128, 1024], F32, tag="xo") for _ in range(2)]
        osem = [S.new_dma_sem() for _ in range(2)]
        for ts in range(NT // 128):
            b = ts % 2
            S.dma("sync", msem[b], mt[b].t[:], mT_ap[:, ts * 128:(ts + 1) * 128].rearrange("(c p) t -> p c t", p=128),
                  writes=[mt[b].d])
            S.dma("sync", xsem[b], xin[b].t[:], xs.ap[ts * 128:(ts + 1) * 128, :], reads=[xs.ds[ts]],
                  writes=[xin[b].d])
            po = K.psum[2 + b]
            for db in range(2):
                fns = [(lambda e, c=c, db=db, po=po, b=b: e.matmul(
                    po.t[:, db * 512:(db + 1) * 512], lhsT=mt[b].t[:, c, :], rhs=wout.t[:, c, db * 512:(db + 1) * 512],
                    start=(c == 0), stop=(c == NC_ - 1))) for c in range(NC_)]
                S.group("tensor", fns, reads=[mt[b].d, wout.d], writes=[po.ds[db]])
            residual_ln(K, po.t[:], po.ds, xin[b], g1p, lng, lnb, tmp[b], xo[b])
            S.dma("gpsimd", osem[b], xd.ap[ts * 128:(ts + 1) * 128, :], xo[b].t[:], reads=[xo[b].d],
                  writes=[xd.ds[ts]])
        S.barrier()
    K.scope = old_scope
    K.S.release(_mk)


def sub_sg(K, xs, xd, NT, ada_w_l, ada_b_l, lng_ap, lnb_ap, w_in_ap, b_in_ap, sglng_ap, sglnb_ap, w_s_ap, b_s_ap,
           w_out_ap):
    S, nc = K.S, K.nc
    old_scope = K.scope
    _mk = K.S.mark()
    with ExitStack() as sc:
        K.scope = sc
        win = K.sb([128, 8, 4096], BF16, tag="win")
        S.dma("gpsimd", S.new_dma_sem(), win.t[:, :, 0:2048],
              w_in_ap[:, 0:2048].rearrange("(c p) f -> p c f", p=128), writes=[win.d])
        S.dma("gpsimd", S.new_dma_sem(), win.t[:, :, 2048:4096],
              w_in_ap[:, 2048:4096].rearrange("(c p) f -> p c f", p=128), writes=[win.d])
        wout = K.sb([128, 16, 1024], BF16, tag="wout")
        S.dma("gpsimd", S.new_dma_sem(), wout.t[:], w_out_ap.rearrange("(c p) d -> p c d", p=128), writes=[wout.d])
        binb = K.sb([1, 4096], BF16, tag="binb")
        S.dma("gpsimd", S.new_dma_sem(), binb.t[:], b_in_ap.unsqueeze(0), writes=[binb.d])
        bsb = K.sb([1, 8, 128], BF16, tag="bsb")
        S.dma("gpsimd", S.new_dma_sem(), bsb.t[:], b_s_ap.unsqueeze(0), writes=[bsb.d])
        ones = K.sb([1, 512], BF16, tag="ones")
        S.op("vector", lambda e: e.memset(ones.t[:], 1.0), writes=[ones.d])
        sglng = K.sb([128, 2048], F32, tag="sglng")
        sglnb = K.sb([128, 2048], F32, tag="sglnb")
        K.bcast_vec(sglng, sglng_ap, S.new_dma_sem())
        K.bcast_vec(sglnb, sglnb_ap, S.new_dma_sem())
        sc1p = K.sb([128, 1024], F32, tag="sc1p")
        shm = K.sb([128, 1024], F32, tag="shm")
        g1p = K.sb([128, 1024], F32, tag="g1p")
        lng = K.sb([128, 1024], F32, tag="lng")
        lnb = K.sb([128, 1024], F32, tag="lnb")
        K.ln_alloc()
        K.mod_vec(shm, ada_w_l, ada_b_l, 0, False)
        K.mod_vec(sc1p, ada_w_l, ada_b_l, 1, True)
        K.mod_vec(g1p, ada_w_l, ada_b_l, 2, True)
        K.bcast_vec(lng, lng_ap, S.new_dma_sem())
        K.bcast_vec(lnb, lnb_ap, S.new_dma_sem())
        wmT = K.sb([128, 8, 128], BF16, tag="wmT")
        sc_setup = ExitStack()
        K.scope = sc_setup
        wsf = K.sb([128, 8, 128], F32, tag="wsf")
        S.dma("sync", S.new_dma_sem(), wsf.t[:], w_s_ap.rearrange("g t s -> t g s"), writes=[wsf.d])
        ii = K.sb([128, 128], I32, tag="ii2")
        msk = K.sb([128, 128], F32, tag="msk")
        S.op("gpsimd", lambda e: e.iota(ii.t[:], pattern=[[1, 128]], base=0, channel_multiplier=-1), writes=[ii.d])
        S.op("vector", lambda e: e.tensor_single_scalar(out=msk.t[:], in_=ii.t[:], scalar=0, op=ALU.is_le),
             reads=[ii.d], writes=[msk.d])
        S.op("vector", lambda e: e.tensor_tensor(out=wsf.t[:], in0=wsf.t[:],
                                                 in1=msk.t[:].unsqueeze(1).to_broadcast([128, 8, 128]), op=ALU.mult),
             reads=[wsf.d, msk.d], writes=[wsf.d])
        pt = K.psum[0]
        for hb in range(2):
            fns = [(lambda e, g=g: e.transpose(out=pt.t[:, g * 128:(g + 1) * 128], in_=wsf.t[:, g, :],
                                               identity=K.ident.t[:])) for g in range(hb * 4, hb * 4 + 4)]
            S.group("tensor", fns, reads=[wsf.d, K.ident.d], writes=[pt.ds[hb]])
        S.op("scalar", lambda e: e.activation(out=wmT.t[:], in_=pt.t[:].rearrange("p (g t) -> p g t", g=8),
                                              func=AF.Copy), reads=[pt.ds[0], pt.ds[1]], writes=[wmT.d])
        S.barrier()
        sc_setup.close()
        K.scope = sc
        xin = K.sb([128, 1024], F32, tag="xin")
        xsem = S.new_dma_sem()
        hf = K.sb([128, 1024], F32, tag="hf")
        hmT = K.sb([128, 8, 128], BF16, tag="hmT")
        u = K.sb([128, 2048], F32, tag="u")
        v = K.sb([128, 2048], F32, n=1, tag="v")
        vn = K.sb([128, 2048], BF16, tag="vn")
        gT = Buf(vn.t, 1)
        gT.ds = vn.ds
        gTv = vn.t[:].rearrange("p (c t) -> p c t", c=16)
        xo = hf
        osem = S.new_dma_sem()
        st4 = K.sb([128, 4, 6], F32, tag="st4")
        mv = K.sb([128, 2], F32, tag="mv2")
        rs = K.sb([128, 1], F32, tag="rs2")
        for ts in range(NT // 128):
            S.dma("sync", xsem, xin.t[:], xs.ap[ts * 128:(ts + 1) * 128, :], reads=[xs.ds[ts]], writes=[xin.d])
            S.op("vector", lambda e: e.tensor_tensor(out=hf.t[:], in0=xin.t[:], in1=sc1p.t[:], op=ALU.mult),
                 reads=[xin.d, sc1p.d], writes=[hf.d])
            S.op("vector", lambda e: e.tensor_tensor(out=hf.t[:], in0=hf.t[:], in1=shm.t[:], op=ALU.add),
                 reads=[hf.d, shm.d], writes=[hf.d])
            po = K.psum[0]
            for hb in range(2):
                fns = [(lambda e, k=k: e.transpose(out=po.t[:, k * 128:(k + 1) * 128],
                                                   in_=hf.t[:, k * 128:(k + 1) * 128], identity=K.ident.t[:]))
                       for k in range(hb * 4, hb * 4 + 4)]
                S.group("tensor", fns, reads=[hf.d, K.ident.d], writes=[po.ds[hb]])
            S.op("scalar", lambda e: e.activation(out=hmT.t[:], in_=po.t[:].rearrange("p (c t) -> p c t", c=8),
                                                  func=AF.Copy), reads=[po.ds[0], po.ds[1]], writes=[hmT.d])
            for cb in range(8):
                pb = K.psum[(cb // 2) % 2 + 0]
                half = cb % 2
                fns = [(lambda e, k=k, cb=cb, pb=pb, half=half: e.matmul(
                    pb.t[:, half * 512:(half + 1) * 512], lhsT=hmT.t[:, k, :], rhs=win.t[:, k, cb * 512:(cb + 1) * 512],
                    start=(k == 0), stop=False)) for k in range(8)]
                fns.append(lambda e, cb=cb, pb=pb, half=half: e.matmul(
                    pb.t[:, half * 512:(half + 1) * 512], lhsT=ones.t[0:1, 0:128], rhs=binb.t[0:1, cb * 512:(cb + 1) * 512],
                    start=False, stop=True))
                S.group("tensor", fns, reads=[hmT.d, win.d, ones.d, binb.d], writes=[pb.ds[half]])
                dst = u if cb < 4 else v
                c0 = (cb % 4) * 512
                S.op("scalar", lambda e, pb=pb, half=half, dst=dst, c0=c0: e.activation(
                    out=dst.t[:, c0:c0 + 512], in_=pb.t[:, half * 512:(half + 1) * 512], func=AF.Gelu_apprx_tanh),
                     reads=[pb.ds[half]], writes=[dst.d])
            for q in range(4):
                S.op("vector", lambda e, q=q: e.bn_stats(out=st4.t[:, q, :], in_=v.t[:, q * 512:(q + 1) * 512]),
                     reads=[v.d], writes=[st4.d])
            S.op("vector", lambda e: e.bn_aggr(out=mv.t[:], in_=st4.t[:].rearrange("p a b -> p (a b)")),
                 reads=[st4.d], writes=[mv.d])
            S.op("scalar", lambda e: e.activation(out=rs.t[:], in_=mv.t[:, 1:2], func=AF.Sqrt, bias=K.eps_ln.t[:],
                                                  scale=1.0), reads=[mv.d, K.eps_ln.d], writes=[rs.d])
            S.op("vector", lambda e: e.reciprocal(out=rs.t[:], in_=rs.t[:]), reads=[rs.d], writes=[rs.d])
            S.op("vector", lambda e: e.tensor_scalar(out=v.t[:], in0=v.t[:], scalar1=mv.t[:, 0:1], scalar2=rs.t[:],
                                                     op0=ALU.subtract, op1=ALU.mult),
                 reads=[v.d, mv.d, rs.d], writes=[v.d])
            S.op("vector", lambda e: e.tensor_tensor(out=v.t[:], in0=v.t[:], in1=sglng.t[:], op=ALU.mult),
                 reads=[v.d, sglng.d], writes=[v.d])
            S.op("vector", lambda e: e.tensor_tensor(out=vn.t[:], in0=v.t[:], in1=sglnb.t[:], op=ALU.add),
                 reads=[v.d, sglnb.d], writes=[vn.d])
            for g in range(8):
                pm = K.psum[2 + g // 4]
                half = (g % 4) // 2
                c0 = (g % 4) * 256
                fns = [lambda e, g=g, pm=pm, c0=c0: e.matmul(pm.t[:, c0:c0 + 256], lhsT=wmT.t[:, g, :],
                                                              rhs=vn.t[:, g * 256:(g + 1) * 256], start=True, stop=False),
                       lambda e, g=g, pm=pm, c0=c0: e.matmul(pm.t[:, c0:c0 + 256], lhsT=bsb.t[0:1, g, :],
                                                              rhs=ones.t[0:1, 0:256], start=False, stop=True)]
                S.group("tensor", fns, reads=[wmT.d, vn.d, bsb.d, ones.d], writes=[pm.ds[half]])
            for h2 in range(2):
                pm = K.psum[2 + h2]
                S.op("vector", lambda e, pm=pm, h2=h2: e.tensor_tensor(
                    out=v.t[:, h2 * 1024:(h2 + 1) * 1024], in0=pm.t[:], in1=u.t[:, h2 * 1024:(h2 + 1) * 1024],
                    op=ALU.mult), reads=[pm.ds[0], pm.ds[1], u.d], writes=[v.d])
            for h2 in range(2):
                pt2 = K.psum[h2]
                for hb in range(2):
                    fns = [(lambda e, c=c, pt2=pt2, h2=h2: e.transpose(
                        out=pt2.t[:, (c % 8) * 128:(c % 8 + 1) * 128], in_=v.t[:, c * 128:(c + 1) * 128],
                        identity=K.ident.t[:])) for c in range(h2 * 8 + hb * 4, h2 * 8 + hb * 4 + 4)]
                    S.group("tensor", fns, reads=[v.d, K.ident.d], writes=[pt2.ds[hb]])
                S.op("scalar", lambda e, pt2=pt2, h2=h2: e.activation(
                    out=gTv[:, h2 * 8:(h2 + 1) * 8, :], in_=pt2.t[:].rearrange("p (c t) -> p c t", c=8),
                    func=AF.Copy), reads=[pt2.ds[0], pt2.ds[1]], writes=[gT.d])
            py = K.psum[2]
            for db in range(2):
                fns = [(lambda e, c=c, db=db: e.matmul(py.t[:, db * 512:(db + 1) * 512], lhsT=gTv[:, c, :],
                                                        rhs=wout.t[:, c, db * 512:(db + 1) * 512], start=(c == 0),
                                                        stop=(c == 15))) for c in range(16)]
                S.group("tensor", fns, reads=[gT.d, wout.d], writes=[py.ds[db]])
            residual_ln(K, py.t[:], py.ds, xin, g1p, lng, lnb, hf, xo)
            S.dma("gpsimd", osem, xd.ap[ts * 128:(ts + 1) * 128, :], xo.t[:], reads=[xo.d], writes=[xd.ds[ts]])
        S.barrier()
    K.scope = old_scope
    K.S.release(_mk)


def build_ssd(NTok):
    K = KB()
    x = K.din("x", [NTok, D]); c = K.din("c", [D]); aw = K.din("aw", [D, 6 * D]); ab = K.din("ab", [6 * D])
    w = K.din("w", [D, 1544]); cw = K.din("cw", [4, 1024]); cbv = K.din("cb", [1024])
    dtb = K.din("dtb", [8]); alog = K.din("alog", [8]); dsk = K.din("dsk", [8]); nw = K.din("nw", [512])
    out = K.dout("mT", [512, NTok], BF16)
    K.alloc_psum(); K.consts(); K.setup_cond(c)
    sub_ssd(K, x, out, NTok, aw, ab, w, cw, cbv, dtb, alog, dsk, nw)
    K.S.finish()
    return K


def sub_ssd(K, x, out, NTok, aw, ab, w, cw, cbv, dtb, alog, dsk, nw):
    S, nc = K.S, K.nc
    X = mybir.AxisListType.X
    P0, P1, P2, P3 = K.psum
    old_scope = K.scope
    _mk = K.S.mark()
    sc_ssd = ExitStack()
    K.scope = sc_ssd
    wz = K.sb([128, 8, 512], BF16, tag="wz")
    wx = K.sb([128, 8, 1024], BF16, tag="wx")
    wdt = K.sb([128, 8, 8], BF16, tag="wdt")
    S.dma("gpsimd", S.new_dma_sem(), wz.t[:], w[:, 0:512].rearrange("(c p) f -> p c f", p=128), writes=[wz.d])
    S.dma("gpsimd", S.new_dma_sem(), wx.t[:], w[:, 512:1536].rearrange("(c p) f -> p c f", p=128), writes=[wx.d])
    with nc.allow_non_contiguous_dma(reason="tiny"):
        S.dma("gpsimd", S.new_dma_sem(), wdt.t[:], w[:, 1536:1544].rearrange("(c p) f -> p c f", p=128),
              writes=[wdt.d])
    cwT = K.sb([128, 4, 8], F32, tag="cwT")
    cbT = K.sb([128, 8], F32, tag="cbT")
    with nc.allow_non_contiguous_dma(reason="tiny"):
        S.dma("sync", S.new_dma_sem(), cwT.t[:], cw.rearrange("k (c p) -> p k c", p=128), writes=[cwT.d])
        S.dma("sync", S.new_dma_sem(), cbT.t[:], cbv.rearrange("(c p) -> p c", p=128), writes=[cbT.d])
    dtb_bc = K.sb([128, 8], F32, tag="dtb"); A_bc = K.sb([128, 8], F32, tag="A"); dsk_bc = K.sb([128, 8], F32, tag="dsk")
    nw_bc = K.sb([128, 512], F32, tag="nw")
    K.bcast_vec(dtb_bc, dtb, S.new_dma_sem()); K.bcast_vec(A_bc, alog, S.new_dma_sem())
    K.bcast_vec(dsk_bc, dsk, S.new_dma_sem()); K.bcast_vec(nw_bc, nw, S.new_dma_sem())
    S.op("scalar", lambda e: e.activation(out=A_bc.t[:], in_=A_bc.t[:], func=AF.Exp), reads=[A_bc.d], writes=[A_bc.d])
    S.op("vector", lambda e: e.tensor_scalar(out=A_bc.t[:], in0=A_bc.t[:], scalar1=-1.0, scalar2=None, op0=ALU.mult),
         reads=[A_bc.d], writes=[A_bc.d])
    sc1p = K.sb([128, 1024], F32, tag="sc1p"); shm = K.sb([128, 1024], F32, tag="shm")
    K.mod_vec(shm, aw, ab, 0, False)
    K.mod_vec(sc1p, aw, ab, 1, True)
    ii = K.sb([128, 128], I32, tag="ii3")
    triU = K.sb([128, 128], F32, tag="triU")
    ones = K.sb([128, 128], F32, tag="ones")
    eps_r = K.sb([128, 1], F32, tag="epsr")
    S.op("gpsimd", lambda e: e.iota(ii.t[:], pattern=[[1, 128]], base=0, channel_multiplier=-1), writes=[ii.d])
    S.op("vector", lambda e: e.tensor_single_scalar(out=triU.t[:], in_=ii.t[:], scalar=0, op=ALU.is_ge),
         reads=[ii.d], writes=[triU.d])
    S.op("vector", lambda e: e.memset(ones.t[:], 1.0), writes=[ones.d])
    S.op("vector", lambda e: e.memset(eps_r.t[:], RMS_EPS), writes=[eps_r.d])
    S32 = K.sb([128, 8, 64], F32, tag="S32"); Sbf = K.sb([128, 8, 64], BF16, tag="Sbf")
    S.op("vector", lambda e: e.memset(S32.t[:], 0.0), writes=[S32.d])
    S.op("vector", lambda e: e.memset(Sbf.t[:], 0.0), writes=[Sbf.d])
    xr = K.sb([128, 8, 131], F32, tag="xr")
    S.op("vector", lambda e: e.memset(xr.t[:], 0.0), writes=[xr.d])
    xin = K.sb([128, 1024], F32, tag="xin"); xsem = S.new_dma_sem()
    hf = K.sb([128, 1024], F32, tag="hf")
    hmT = K.sb([128, 8, 128], BF16, tag="hmT")
    cacc = K.sb([128, 8, 128], F32, tag="cacc"); ctmp = K.sb([128, 8, 128], F32, tag="ctmp")
    xa = K.sb([128, 8, 128], F32, tag="xa")
    bcT = K.sb([128, 4, 128], BF16, tag="bcT")
    xtok = K.sb([128, 768], F32, tag="xtok")
    btok = K.sb([128, 256], BF16, tag="btok")
    dtv = K.sb([128, 8], F32, tag="dtv"); dtA = K.sb([128, 8], F32, tag="dtA")
    dtAb = K.sb([128, 8, 128], F32, tag="dtAb")
    acs = K.sb([128, 24], F32, tag="acs")
    dte = K.sb([128, 8], F32, tag="dte"); cd = K.sb([128, 8], F32, tag="cd")
    Lx = K.sb([128, 8, 128], F32, tag="Lx"); Eb = K.sb([128, 8, 128], F32, tag="Eb")
    cbm = K.sb([128, 2, 128], F32, tag="cbm")
    MT = K.sb([128, 8, 128], BF16, tag="MT"); CsT = K.sb([128, 8, 128], BF16, tag="CsT")
    xdt = K.sb([128, 8, 64], BF16, tag="xdt"); xdte = K.sb([128, 8, 64], BF16, tag="xdte")
    y = K.sb([128, 512], F32, tag="y"); sz = K.sb([128, 512], F32, tag="sz"); sq = K.sb([128, 512], F32, tag="sq")
    ss = K.sb([128, 2], F32, tag="ss")
    ygT = K.sb([128, 4, 128], BF16, tag="ygT"); osem = S.new_dma_sem()
    bc3 = lambda ap, n: ap.unsqueeze(2).to_broadcast([128, 8, n])

    for ck in range(NTok // 128):
        S.dma("sync", xsem, xin.t[:], (x(ck) if callable(x) else x[ck * 128:(ck + 1) * 128, :]), writes=[xin.d])
        S.op("vector", lambda e: e.tensor_tensor(out=hf.t[:], in0=xin.t[:], in1=sc1p.t[:], op=ALU.mult),
             reads=[xin.d, sc1p.d], writes=[hf.d])
        S.op("vector", lambda e: e.tensor_tensor(out=hf.t[:], in0=hf.t[:], in1=shm.t[:], op=ALU.add),
             reads=[hf.d, shm.d], writes=[hf.d])
        for hb in range(2):
            fns = [(lambda e, k=k: e.transpose(out=P0.t[:, k * 128:(k + 1) * 128], in_=hf.t[:, k * 128:(k + 1) * 128],
                                               identity=K.ident.t[:])) for k in range(hb * 4, hb * 4 + 4)]
            S.group("tensor", fns, reads=[hf.d, K.ident.d], writes=[P0.ds[hb]])
        S.op("scalar", lambda e: e.activation(out=hmT.t[:], in_=P0.t[:].rearrange("p (c t) -> p c t", c=8),
                                              func=AF.Copy), reads=[P0.ds[0], P0.ds[1]], writes=[hmT.d])
        fns = [(lambda e, k=k: e.matmul(P1.t[:, 0:512], lhsT=hmT.t[:, k, :], rhs=wz.t[:, k, :], start=(k == 0),
                                        stop=(k == 7))) for k in range(8)]
        S.group("tensor", fns, reads=[hmT.d, wz.d], writes=[P1.ds[0]])
        fns = [(lambda e, k=k: e.matmul(P1.t[:, 512:520], lhsT=hmT.t[:, k, :], rhs=wdt.t[:, k, :], start=(k == 0),
                                        stop=(k == 7))) for k in range(8)]
        S.group("tensor", fns, reads=[hmT.d, wdt.d], writes=[P1.ds[1]])
        for hb in range(2):
            fns = []
            for ch in range(hb * 4, hb * 4 + 4):
                fns += [(lambda e, k=k, ch=ch: e.matmul(P2.t[:, ch * 128:(ch + 1) * 128],
                                                        lhsT=wx.t[:, k, ch * 128:(ch + 1) * 128], rhs=hmT.t[:, k, :],
                                                        start=(k == 0), stop=(k == 7))) for k in range(8)]
            S.group("tensor", fns, reads=[hmT.d, wx.d], writes=[P2.ds[hb]])
        S.op("scalar", lambda e: e.activation(out=xr.t[:, :, 3:131], in_=P2.t[:].rearrange("p (c t) -> p c t", c=8),
                                              func=AF.Copy), reads=[P2.ds[0], P2.ds[1]], writes=[xr.d])
        S.op("vector", lambda e: e.tensor_tensor(out=dtv.t[:], in0=P1.t[:, 512:520], in1=dtb_bc.t[:], op=ALU.add),
             reads=[P1.ds[1], dtb_bc.d], writes=[dtv.d])
        S.op("scalar", lambda e: e.activation(out=dtv.t[:], in_=dtv.t[:], func=AF.Exp), reads=[dtv.d], writes=[dtv.d])
        S.op("scalar", lambda e: e.activation(out=dtv.t[:], in_=dtv.t[:], func=AF.Ln, bias=1.0, scale=1.0),
             reads=[dtv.d], writes=[dtv.d])
        S.op("vector", lambda e: e.tensor_tensor(out=dtA.t[:], in0=dtv.t[:], in1=A_bc.t[:], op=ALU.mult),
             reads=[dtv.d, A_bc.d], writes=[dtA.d])
        S.op("vector", lambda e: e.tensor_copy(out=dtAb.t[:], in_=bc3(dtA.t[:], 128)), reads=[dtA.d], writes=[dtAb.d])
        S.op("tensor", lambda e: e.matmul(P1.t[:, 520:528], lhsT=triU.t[:], rhs=dtA.t[:], start=True, stop=True),
             reads=[triU.d, dtA.d], writes=[P1.ds[1]])
        S.op("tensor", lambda e: e.matmul(P1.t[:, 528:536], lhsT=ones.t[:], rhs=dtA.t[:], start=True, stop=True),
             reads=[ones.d, dtA.d], writes=[P1.ds[1]])
        S.op("scalar", lambda e: e.activation(out=acs.t[:, 0:16], in_=P1.t[:, 520:536], func=AF.Copy),
             reads=[P1.ds[1]], writes=[acs.d])
        for hb in range(2):
            fns = [(lambda e, h=h: e.matmul(P0.t[:, h * 128:(h + 1) * 128], lhsT=dtAb.t[:, h, :], rhs=triU.t[:],
                                            start=True, stop=True)) for h in range(hb * 4, hb * 4 + 4)]
            S.group("tensor", fns, reads=[dtAb.d, triU.d], writes=[P0.ds[hb]])
        P0v = P0.t[:].rearrange("p (h l) -> p h l", h=8)
        S.op("vector", lambda e: e.tensor_tensor(out=Lx.t[:], in0=P0v, in1=bc3(acs.t[:, 0:8], 128), op=ALU.subtract),
             reads=[P0.ds[0], P0.ds[1], acs.d], writes=[Lx.d])
        S.op("vector", lambda e: e.tensor_scalar(out=Lx.t[:], in0=Lx.t[:], scalar1=0.0, scalar2=None, op0=ALU.min),
             reads=[Lx.d], writes=[Lx.d])
        S.op("scalar", lambda e: e.activation(out=Lx.t[:], in_=Lx.t[:], func=AF.Exp), reads=[Lx.d], writes=[Lx.d])
        S.op("scalar", lambda e: e.activation(out=Eb.t[:], in_=P0v, func=AF.Exp), reads=[P0.ds[0], P0.ds[1]],
             writes=[Eb.d])
        S.op("vector", lambda e: e.tensor_tensor(out=dte.t[:], in0=acs.t[:, 8:16], in1=acs.t[:, 0:8], op=ALU.subtract),
             reads=[acs.d], writes=[dte.d])
        S.op("scalar", lambda e: e.activation(out=dte.t[:], in_=dte.t[:], func=AF.Exp), reads=[dte.d], writes=[dte.d])
        S.op("scalar", lambda e: e.activation(out=cd.t[:], in_=acs.t[:, 8:16], func=AF.Exp), reads=[acs.d], writes=[cd.d])
        for k in range(4):
            src = xr.t[:, :, k:k + 128]
            wk = bc3(cwT.t[:, k, :], 128)
            if k == 0:
                S.op("vector", lambda e, src=src, wk=wk: e.tensor_tensor(out=cacc.t[:], in0=src, in1=wk, op=ALU.mult),
                     reads=[xr.d, cwT.d], writes=[cacc.d])
            else:
                S.op("vector", lambda e, src=src, wk=wk: e.tensor_tensor(out=ctmp.t[:], in0=src, in1=wk, op=ALU.mult),
                     reads=[xr.d, cwT.d], writes=[ctmp.d])
                S.op("vector", lambda e: e.tensor_tensor(out=cacc.t[:], in0=cacc.t[:], in1=ctmp.t[:], op=ALU.add),
                     reads=[cacc.d, ctmp.d], writes=[cacc.d])
        S.op("vector", lambda e: e.tensor_copy(out=xr.t[:, :, 0:3], in_=xr.t[:, :, 128:131]), reads=[xr.d], writes=[xr.d])
        for ch in range(8):
            S.op("scalar", lambda e, ch=ch: e.activation(out=xa.t[:, ch, :], in_=cacc.t[:, ch, :], func=AF.Silu,
                                                         bias=cbT.t[:, ch:ch + 1], scale=1.0),
                 reads=[cacc.d, cbT.d], writes=[xa.d])
        S.op("vector", lambda e: e.tensor_copy(out=bcT.t[:], in_=xa.t[:, 4:8, :]), reads=[xa.d], writes=[bcT.d])
        for hb in range(2):
            rng_ = range(0, 4) if hb == 0 else range(4, 6)
            fns = [(lambda e, j=j: e.transpose(out=P2.t[:, j * 128:(j + 1) * 128], in_=xa.t[:, j, :],
                                               identity=K.ident.t[:])) for j in rng_]
            S.group("tensor", fns, reads=[xa.d, K.ident.d], writes=[P2.ds[hb]])
        S.op("scalar", lambda e: e.activation(out=xtok.t[:], in_=P2.t[:, 0:768], func=AF.Copy),
             reads=[P2.ds[0], P2.ds[1]], writes=[xtok.d])
        S.op("vector", lambda e: e.tensor_copy(out=btok.t[:], in_=xtok.t[:, 512:768]), reads=[xtok.d], writes=[btok.d])
        xt3 = xtok.t[:, 0:512].rearrange("p (h d) -> p h d", h=8)
        S.op("vector", lambda e: e.tensor_tensor(out=xdt.t[:], in0=xt3, in1=bc3(dtv.t[:], 64), op=ALU.mult),
             reads=[xtok.d, dtv.d], writes=[xdt.d])
        S.op("vector", lambda e: e.tensor_tensor(out=dte.t[:], in0=dte.t[:], in1=dtv.t[:], op=ALU.mult),
             reads=[dte.d, dtv.d], writes=[dte.d])
        S.op("vector", lambda e: e.tensor_tensor(out=xdte.t[:], in0=xt3, in1=bc3(dte.t[:], 64), op=ALU.mult),
             reads=[xtok.d, dte.d], writes=[xdte.d])
        fns = [(lambda e, g=g: e.matmul(P3.t[:, g * 128:(g + 1) * 128], lhsT=bcT.t[:, g, :], rhs=bcT.t[:, 2 + g, :],
                                        start=True, stop=True)) for g in range(2)]
        S.group("tensor", fns, reads=[bcT.d], writes=[P3.ds[0]])
        S.op("vector", lambda e: e.tensor_tensor(out=cbm.t[:], in0=P3.t[:, 0:256].rearrange("p (g l) -> p g l", g=2),
                                                 in1=triU.t[:].unsqueeze(1).to_broadcast([128, 2, 128]), op=ALU.mult),
             reads=[P3.ds[0], triU.d], writes=[cbm.d])
        for g in range(2):
            S.op("vector", lambda e, g=g: e.tensor_tensor(
                out=MT.t[:, 4 * g:4 * g + 4, :], in0=Lx.t[:, 4 * g:4 * g + 4, :],
                in1=cbm.t[:, g, :].unsqueeze(1).to_broadcast([128, 4, 128]), op=ALU.mult),
                 reads=[Lx.d, cbm.d], writes=[MT.d])
            S.op("vector", lambda e, g=g: e.tensor_tensor(
                out=CsT.t[:, 4 * g:4 * g + 4, :], in0=Eb.t[:, 4 * g:4 * g + 4, :],
                in1=xa.t[:, 6 + g, :].unsqueeze(1).to_broadcast([128, 4, 128]), op=ALU.mult),
                 reads=[Eb.d, xa.d], writes=[CsT.d])
        fns = []
        for h in range(8):
            fns.append(lambda e, h=h: e.matmul(P2.t[:, h * 64:(h + 1) * 64], lhsT=MT.t[:, h, :], rhs=xdt.t[:, h, :],
                                               start=True, stop=False))
            fns.append(lambda e, h=h: e.matmul(P2.t[:, h * 64:(h + 1) * 64], lhsT=CsT.t[:, h, :], rhs=Sbf.t[:, h, :],
                                               start=False, stop=True))
        S.group("tensor", fns, reads=[MT.d, xdt.d, CsT.d, Sbf.d], writes=[P2.ds[0]])
        fns = [(lambda e, h=h: e.matmul(P2.t[:, 512 + h * 64:512 + (h + 1) * 64], lhsT=btok.t[:, (h // 4) * 128:(h // 4 + 1) * 128],
                                        rhs=xdte.t[:, h, :], start=True, stop=True)) for h in range(8)]
        S.group("tensor", fns, reads=[btok.d, xdte.d], writes=[P2.ds[1]])
        S.op("vector", lambda e: e.tensor_tensor(out=S32.t[:], in0=S32.t[:], in1=bc3(cd.t[:], 64), op=ALU.mult),
             reads=[S32.d, cd.d], writes=[S32.d])
        S.op("vector", lambda e: e.tensor_tensor(out=S32.t[:], in0=S32.t[:],
                                                 in1=P2.t[:, 512:1024].rearrange("p (h d) -> p h d", h=8), op=ALU.add),
             reads=[S32.d, P2.ds[1]], writes=[S32.d])
        S.op("vector", lambda e: e.tensor_copy(out=Sbf.t[:], in_=S32.t[:]), reads=[S32.d], writes=[Sbf.d])
        S.op("vector", lambda e: e.tensor_tensor(out=y.t[:].rearrange("p (h d) -> p h d", h=8), in0=xt3,
                                                 in1=bc3(dsk_bc.t[:], 64), op=ALU.mult),
             reads=[xtok.d, dsk_bc.d], writes=[y.d])
        S.op("vector", lambda e: e.tensor_tensor(out=y.t[:], in0=y.t[:], in1=P2.t[:, 0:512], op=ALU.add),
             reads=[y.d, P2.ds[0]], writes=[y.d])
        S.op("scalar", lambda e: e.activation(out=sz.t[:], in_=P1.t[:, 0:512], func=AF.Silu), reads=[P1.ds[0]],
             writes=[sz.d])
        S.op("vector", lambda e: e.tensor_tensor(out=y.t[:], in0=y.t[:], in1=sz.t[:], op=ALU.mult),
             reads=[y.d, sz.d], writes=[y.d])
        S.op("vector", lambda e: e.tensor_tensor(out=sq.t[:], in0=y.t[:], in1=y.t[:], op=ALU.mult),
             reads=[y.d], writes=[sq.d])
        S.op("vector", lambda e: e.tensor_reduce(out=ss.t[:], in_=sq.t[:].rearrange("p (g d) -> p g d", g=2), axis=X,
                                                 op=ALU.add), reads=[sq.d], writes=[ss.d])
        S.op("scalar", lambda e: e.activation(out=ss.t[:], in_=ss.t[:], func=AF.Sqrt, bias=eps_r.t[:], scale=1.0 / 256.0),
             reads=[ss.d, eps_r.d], writes=[ss.d])
        S.op("vector", lambda e: e.reciprocal(out=ss.t[:], in_=ss.t[:]), reads=[ss.d], writes=[ss.d])
        S.op("vector", lambda e: e.tensor_tensor(out=y.t[:].rearrange("p (g d) -> p g d", g=2),
                                                 in0=y.t[:].rearrange("p (g d) -> p g d", g=2),
                                                 in1=ss.t[:].unsqueeze(2).to_broadcast([128, 2, 256]), op=ALU.mult),
             reads=[y.d, ss.d], writes=[y.d])
        S.op("vector", lambda e: e.tensor_tensor(out=y.t[:], in0=y.t[:], in1=nw_bc.t[:], op=ALU.mult),
             reads=[y.d, nw_bc.d], writes=[y.d])
        fns = [(lambda e, j=j: e.transpose(out=P3.t[:, 512 + j * 128:512 + (j + 1) * 128], in_=y.t[:, j * 128:(j + 1) * 128],
                                           identity=K.ident.t[:])) for j in range(4)]
        S.group("tensor", fns, reads=[y.d, K.ident.d], writes=[P3.ds[1]])
        S.op("scalar", lambda e: e.activation(out=ygT.t[:], in_=P3.t[:, 512:1024].rearrange("p (c t) -> p c t", c=4),
                                              func=AF.Copy), reads=[P3.ds[1]], writes=[ygT.d])
        S.dma("gpsimd", osem, out[:, ck * 128:(ck + 1) * 128].rearrange("(c p) t -> p c t", p=128), ygT.t[:],
              reads=[ygT.d])
    S.barrier()
    sc_ssd.close()
    K.scope = old_scope
    K.S.release(_mk)


def build_mla(NTok):
    K = KB()
    x = K.din("x", [NTok, D]); c = K.din("c", [D]); aw = K.din("aw", [D, 6 * D]); ab = K.din("ab", [6 * D])
    pos = K.din("pos", [NTok], I32)
    w_in = K.din("w_in", [D, 800]); qn = K.din("qn", [512]); kvn = K.din("kvn", [256])
    wuq_d = K.din("wuq", [512, 384]); wukv_d = K.din("wukv", [256, 512])
    out = K.dout("mT", [256, NTok], BF16)
    K.alloc_psum(); K.consts(); K.setup_cond(c)
    sub_mla(K, x, out, NTok, aw, ab, pos, w_in, qn, kvn, wuq_d, wukv_d)
    K.S.finish()
    return K


def sub_mla(K, x, out, NTok, aw, ab, pos, w_in, qn, kvn, wuq_d, wukv_d):
    S, nc = K.S, K.nc
    X = mybir.AxisListType.X
    NTL = NTok // 128
    QT_d = K.dtmp("QT_d", [4, 96, NTok], BF16); KT_d = K.dtmp("KT_d", [4, 96, NTok], BF16)
    V_d = K.dtmp("V_d", [NTL, 128, 4, 65], BF16)
    P0, P1, P2, P3 = K.psum
    old_scope = K.scope
    _mk = K.S.mark()
    SCALE = 96.0 ** -0.5
    TWO_PI = 2.0 * math.pi
    with ExitStack() as sc:
        K.scope = sc
        win = K.sb([128, 8, 800], BF16, tag="win")
        wuq = K.sb([128, 4, 384], BF16, tag="wuq"); wukv = K.sb([128, 2, 512], BF16, tag="wukv")
        S.dma("gpsimd", S.new_dma_sem(), win.t[:], w_in.rearrange("(c p) f -> p c f", p=128), writes=[win.d])
        S.dma("gpsimd", S.new_dma_sem(), wuq.t[:], wuq_d.rearrange("(c p) f -> p c f", p=128), writes=[wuq.d])
        S.dma("gpsimd", S.new_dma_sem(), wukv.t[:], wukv_d.rearrange("(c p) f -> p c f", p=128), writes=[wukv.d])
        nbc = K.sb([128, 768], F32, tag="nbc")
        S.dma("sync", S.new_dma_sem(), nbc.t[:, 0:512], qn.unsqueeze(0).to_broadcast([128, 512]), writes=[nbc.d])
        S.dma("sync", S.new_dma_sem(), nbc.t[:, 512:768], kvn.unsqueeze(0).to_broadcast([128, 256]), writes=[nbc.d])
        sc1p = K.sb([128, 1024], F32, tag="sc1p"); shm = K.sb([128, 1024], F32, tag="shm")
        K.mod_vec(shm, aw, ab, 0, False)
        K.mod_vec(sc1p, aw, ab, 1, True)
        eps_r = K.sb([128, 1], F32, tag="epsr")
        S.op("vector", lambda e: e.memset(eps_r.t[:], RMS_EPS), writes=[eps_r.d])
        ji = K.sb([128, 16], I32, tag="ji"); freq = K.sb([128, 16], F32, tag="freq")
        S.op("gpsimd", lambda e: e.iota(ji.t[:], pattern=[[1, 16]], base=0, channel_multiplier=0), writes=[ji.d])
        S.op("vector", lambda e: e.tensor_copy(out=freq.t[:], in_=ji.t[:]), reads=[ji.d], writes=[freq.d])
        S.op("scalar", lambda e: e.activation(out=freq.t[:], in_=freq.t[:], func=AF.Exp,
                                              scale=-math.log(10000.0) / 16.0), reads=[freq.d], writes=[freq.d])
        xin = K.sb([128, 1024], F32, tag="xin"); xsem = S.new_dma_sem()
        hf = K.sb([128, 1024], F32, tag="hf"); hmT = K.sb([128, 8, 128], BF16, tag="hmT")
        lat = K.sb([128, 800], F32, tag="lat"); sq = K.sb([128, 768], F32, tag="sq")
        ss = K.sb([128, 2], F32, tag="ss"); nrm = K.sb([128, 768], F32, tag="nrm"); nT = K.sb([128, 6, 128], BF16, tag="nT")
        posi = K.sb([128, 1], I32, tag="posi"); psem = S.new_dma_sem(); posf = K.sb([128, 1], F32, tag="posf")
        tt = K.sb([128, 32], F32, tag="tt"); ti = K.sb([128, 32], I32, tag="ti"); tf = K.sb([128, 32], F32, tag="tf")
        mm = K.sb([128, 32], F32, tag="mm"); scs = K.sb([128, 32], F32, tag="scs")
        qf = K.sb([128, 4, 96], F32, tag="qf"); kvf = K.sb([128, 4, 128], F32, tag="kvf")
        Qh = K.sb([128, 4, 96], F32, tag="Qh"); Kh = K.sb([128, 4, 96], F32, tag="Kh")
        ra = K.sb([128, 4, 16], F32, tag="ra"); rb = K.sb([128, 4, 16], F32, tag="rb")
        kr = K.sb([128, 32], F32, tag="kr")
        Va = K.sb([128, 4, 65], BF16, tag="Va")
        S.op("vector", lambda e: e.memset(Va.t[:], 1.0), writes=[Va.d])
        QTs = K.sb([128, 4, 128], BF16, tag="QTs"); KTs = K.sb([128, 4, 128], BF16, tag="KTs")
        qsem = S.new_dma_sem(); ksem = S.new_dma_sem(); vsem = S.new_dma_sem()
        b4 = lambda ap: ap.unsqueeze(1).to_broadcast([128, 4, 16])

        def rope(src1, src2, dst1, dst2, cosb, sinb, shape_bc):
            S.op("vector", lambda e: e.tensor_tensor(out=ra.t[:] if shape_bc else ra.t[:, 0, :], in0=src1, in1=cosb, op=ALU.mult),
                 reads=[qf.d, lat.d, scs.d], writes=[ra.d])
            S.op("vector", lambda e: e.tensor_tensor(out=rb.t[:] if shape_bc else rb.t[:, 0, :], in0=src2, in1=sinb, op=ALU.mult),
                 reads=[qf.d, lat.d, scs.d], writes=[rb.d])
            S.op("vector", lambda e: e.tensor_tensor(out=dst1, in0=ra.t[:] if shape_bc else ra.t[:, 0, :],
                                                     in1=rb.t[:] if shape_bc else rb.t[:, 0, :], op=ALU.subtract),
                 reads=[ra.d, rb.d], writes=[Qh.d, kr.d])
            S.op("vector", lambda e: e.tensor_tensor(out=ra.t[:] if shape_bc else ra.t[:, 0, :], in0=src2, in1=cosb, op=ALU.mult),
                 reads=[qf.d, lat.d, scs.d], writes=[ra.d])
            S.op("vector", lambda e: e.tensor_tensor(out=rb.t[:] if shape_bc else rb.t[:, 0, :], in0=src1, in1=sinb, op=ALU.mult),
                 reads=[qf.d, lat.d, scs.d], writes=[rb.d])
            S.op("vector", lambda e: e.tensor_tensor(out=dst2, in0=ra.t[:] if shape_bc else ra.t[:, 0, :],
                                                     in1=rb.t[:] if shape_bc else rb.t[:, 0, :], op=ALU.add),
                 reads=[ra.d, rb.d], writes=[Qh.d, kr.d])

        for t in range(NTL):
            S.dma("sync", xsem, xin.t[:], (x(t) if callable(x) else x[t * 128:(t + 1) * 128, :]), writes=[xin.d])
            with nc.allow_non_contiguous_dma(reason="positions column"):
                S.dma("sync", psem, posi.t[:], pos[t * 128:(t + 1) * 128].unsqueeze(1), writes=[posi.d])
            S.op("vector", lambda e: e.tensor_tensor(out=hf.t[:], in0=xin.t[:], in1=sc1p.t[:], op=ALU.mult),
                 reads=[xin.d, sc1p.d], writes=[hf.d])
            S.op("vector", lambda e: e.tensor_tensor(out=hf.t[:], in0=hf.t[:], in1=shm.t[:], op=ALU.add),
                 reads=[hf.d, shm.d], writes=[hf.d])
            for hb in range(2):
                fns = [(lambda e, k=k: e.transpose(out=P0.t[:, k * 128:(k + 1) * 128], in_=hf.t[:, k * 128:(k + 1) * 128],
                                                   identity=K.ident.t[:])) for k in range(hb * 4, hb * 4 + 4)]
                S.group("tensor", fns, reads=[hf.d, K.ident.d], writes=[P0.ds[hb]])
            S.op("scalar", lambda e: e.activation(out=hmT.t[:], in_=P0.t[:].rearrange("p (c t) -> p c t", c=8),
                                                  func=AF.Copy), reads=[P0.ds[0], P0.ds[1]], writes=[hmT.d])
            fns = [(lambda e, k=k: e.matmul(P1.t[:, 0:512], lhsT=hmT.t[:, k, :], rhs=win.t[:, k, 0:512], start=(k == 0),
                                            stop=(k == 7))) for k in range(8)]
            S.group("tensor", fns, reads=[hmT.d, win.d], writes=[P1.ds[0]])
            fns = [(lambda e, k=k: e.matmul(P1.t[:, 512:800], lhsT=hmT.t[:, k, :], rhs=win.t[:, k, 512:800], start=(k == 0),
                                            stop=(k == 7))) for k in range(8)]
            S.group("tensor", fns, reads=[hmT.d, win.d], writes=[P1.ds[1]])
            S.op("scalar", lambda e: e.activation(out=lat.t[:], in_=P1.t[:, 0:800], func=AF.Copy),
                 reads=[P1.ds[0], P1.ds[1]], writes=[lat.d])
            S.op("vector", lambda e: e.tensor_tensor(out=sq.t[:], in0=lat.t[:, 0:768], in1=lat.t[:, 0:768], op=ALU.mult),
                 reads=[lat.d], writes=[sq.d])
            S.op("vector", lambda e: e.tensor_reduce(out=ss.t[:, 0:1], in_=sq.t[:, 0:512], axis=X, op=ALU.add),
                 reads=[sq.d], writes=[ss.d])
            S.op("vector", lambda e: e.tensor_reduce(out=ss.t[:, 1:2], in_=sq.t[:, 512:768], axis=X, op=ALU.add),
                 reads=[sq.d], writes=[ss.d])
            S.op("scalar", lambda e: e.activation(out=ss.t[:, 0:1], in_=ss.t[:, 0:1], func=AF.Sqrt, bias=eps_r.t[:],
                                                  scale=1.0 / 512.0), reads=[ss.d, eps_r.d], writes=[ss.d])
            S.op("scalar", lambda e: e.activation(out=ss.t[:, 1:2], in_=ss.t[:, 1:2], func=AF.Sqrt, bias=eps_r.t[:],
                                                  scale=1.0 / 256.0), reads=[ss.d, eps_r.d], writes=[ss.d])
            S.op("vector", lambda e: e.reciprocal(out=ss.t[:], in_=ss.t[:]), reads=[ss.d], writes=[ss.d])
            S.op("vector", lambda e: e.scalar_tensor_tensor(out=nrm.t[:, 0:512], in0=lat.t[:, 0:512], scalar=ss.t[:, 0:1],
                                                            in1=nbc.t[:, 0:512], op0=ALU.mult, op1=ALU.mult),
                 reads=[lat.d, ss.d, nbc.d], writes=[nrm.d])
            S.op("vector", lambda e: e.scalar_tensor_tensor(out=nrm.t[:, 512:768], in0=lat.t[:, 512:768],
                                                            scalar=ss.t[:, 1:2], in1=nbc.t[:, 512:768], op0=ALU.mult,
                                                            op1=ALU.mult), reads=[lat.d, ss.d, nbc.d], writes=[nrm.d])
            for hb in range(2):
                rng_ = range(0, 4) if hb == 0 else range(4, 6)
                fns = [(lambda e, j=j: e.transpose(out=P0.t[:, j * 128:(j + 1) * 128], in_=nrm.t[:, j * 128:(j + 1) * 128],
                                                   identity=K.ident.t[:])) for j in rng_]
                S.group("tensor", fns, reads=[nrm.d, K.ident.d], writes=[P0.ds[hb]])
            S.op("scalar", lambda e: e.activation(out=nT.t[:], in_=P0.t[:, 0:768].rearrange("p (c t) -> p c t", c=6),
                                                  func=AF.Copy), reads=[P0.ds[0], P0.ds[1]], writes=[nT.d])
            fns = [(lambda e, c_=c_: e.matmul(P2.t[:, 0:384], lhsT=nT.t[:, c_, :], rhs=wuq.t[:, c_, :], start=(c_ == 0),
                                              stop=(c_ == 3))) for c_ in range(4)]
            S.group("tensor", fns, reads=[nT.d, wuq.d], writes=[P2.ds[0]])
            fns = [(lambda e, c_=c_: e.matmul(P2.t[:, 512:1024], lhsT=nT.t[:, 4 + c_, :], rhs=wukv.t[:, c_, :],
                                              start=(c_ == 0), stop=(c_ == 1))) for c_ in range(2)]
            S.group("tensor", fns, reads=[nT.d, wukv.d], writes=[P2.ds[1]])
            S.op("scalar", lambda e: e.activation(out=qf.t[:], in_=P2.t[:, 0:384].rearrange("p (h d) -> p h d", h=4),
                                                  func=AF.Copy), reads=[P2.ds[0]], writes=[qf.d])
            S.op("scalar", lambda e: e.activation(out=kvf.t[:], in_=P2.t[:, 512:1024].rearrange("p (h d) -> p h d", h=4),
                                                  func=AF.Copy), reads=[P2.ds[1]], writes=[kvf.d])
            S.op("vector", lambda e: e.tensor_copy(out=posf.t[:], in_=posi.t[:]), reads=[posi.d], writes=[posf.d])
            S.op("vector", lambda e: e.tensor_scalar(out=tt.t[:, 0:16], in0=freq.t[:], scalar1=posf.t[:],
                                                     scalar2=1.0 / TWO_PI, op0=ALU.mult, op1=ALU.mult),
                 reads=[freq.d, posf.d], writes=[tt.d])
            S.op("vector", lambda e: e.tensor_scalar(out=tt.t[:, 16:32], in0=tt.t[:, 0:16], scalar1=0.25, scalar2=None,
                                                     op0=ALU.add), reads=[tt.d], writes=[tt.d])
            S.op("vector", lambda e: e.tensor_copy(out=ti.t[:], in_=tt.t[:]), reads=[tt.d], writes=[ti.d])
            S.op("vector", lambda e: e.tensor_copy(out=tf.t[:], in_=ti.t[:]), reads=[ti.d], writes=[tf.d])
            S.op("vector", lambda e: e.tensor_tensor(out=tt.t[:], in0=tt.t[:], in1=tf.t[:], op=ALU.subtract),
                 reads=[tt.d, tf.d], writes=[tt.d])
            S.op("vector", lambda e: e.tensor_single_scalar(out=mm.t[:], in_=tt.t[:], scalar=0.5, op=ALU.is_gt),
                 reads=[tt.d], writes=[mm.d])
            S.op("vector", lambda e: e.tensor_tensor(out=tt.t[:], in0=tt.t[:], in1=mm.t[:], op=ALU.subtract),
                 reads=[tt.d, mm.d], writes=[tt.d])
            S.op("vector", lambda e: e.tensor_single_scalar(out=mm.t[:], in_=tt.t[:], scalar=-0.5, op=ALU.is_lt),
                 reads=[tt.d], writes=[mm.d])
            S.op("vector", lambda e: e.tensor_tensor(out=tt.t[:], in0=tt.t[:], in1=mm.t[:], op=ALU.add),
                 reads=[tt.d, mm.d], writes=[tt.d])
            S.op("scalar", lambda e: e.activation(out=scs.t[:], in_=tt.t[:], func=AF.Sin, scale=TWO_PI),
                 reads=[tt.d], writes=[scs.d])
            sinb, cosb = scs.t[:, 0:16], scs.t[:, 16:32]
            rope(qf.t[:, :, 64:80], qf.t[:, :, 80:96], Qh.t[:, :, 64:80], Qh.t[:, :, 80:96], b4(cosb), b4(sinb), True)
            rope(lat.t[:, 768:784], lat.t[:, 784:800], kr.t[:, 0:16], kr.t[:, 16:32], cosb, sinb, False)
            S.op("vector", lambda e: e.tensor_copy(out=Qh.t[:, :, 0:64], in_=qf.t[:, :, 0:64]), reads=[qf.d], writes=[Qh.d])
            S.op("vector", lambda e: e.tensor_copy(out=Kh.t[:, :, 0:64], in_=kvf.t[:, :, 0:64]), reads=[kvf.d], writes=[Kh.d])
            S.op("vector", lambda e: e.tensor_copy(out=Kh.t[:, :, 64:96], in_=kr.t[:].unsqueeze(1).to_broadcast([128, 4, 32])),
                 reads=[kr.d], writes=[Kh.d])
            S.op("vector", lambda e: e.tensor_copy(out=Va.t[:, :, 0:64], in_=kvf.t[:, :, 64:128]), reads=[kvf.d], writes=[Va.d])
            fns = [(lambda e, h=h: e.transpose(out=P3.t[0:96, h * 128:(h + 1) * 128], in_=Qh.t[:, h, :],
                                               identity=K.ident.t[:])) for h in range(4)]
            S.group("tensor", fns, reads=[Qh.d, K.ident.d], writes=[P3.ds[0]])
            fns = [(lambda e, h=h: e.transpose(out=P3.t[0:96, 512 + h * 128:512 + (h + 1) * 128], in_=Kh.t[:, h, :],
                                               identity=K.ident.t[:])) for h in range(4)]
            S.group("tensor", fns, reads=[Kh.d, K.ident.d], writes=[P3.ds[1]])
            S.op("scalar", lambda e: e.activation(out=QTs.t[0:96], in_=P3.t[0:96, 0:512].rearrange("p (h t) -> p h t", h=4),
                                                  func=AF.Copy), reads=[P3.ds[0]], writes=[QTs.d])
            S.op("scalar", lambda e: e.activation(out=KTs.t[0:96], in_=P3.t[0:96, 512:1024].rearrange("p (h t) -> p h t", h=4),
                                                  func=AF.Copy), reads=[P3.ds[1]], writes=[KTs.d])
            S.dma("gpsimd", qsem, QT_d[:, :, t * 128:(t + 1) * 128].rearrange("h d t -> d h t"), QTs.t[0:96], reads=[QTs.d])
            S.dma("gpsimd", ksem, KT_d[:, :, t * 128:(t + 1) * 128].rearrange("h d t -> d h t"), KTs.t[0:96], reads=[KTs.d])
            S.dma("gpsimd", vsem, V_d[t], Va.t[:], reads=[Va.d])
        S.barrier()
    with ExitStack() as sc:
        K.scope = sc
        KTh = K.sb([128, NTok], BF16, tag="KTh"); Vh = K.sb([128, NTL, 65], BF16, tag="Vh")
        khs = S.new_dma_sem(); vhs = S.new_dma_sem()
        QTt = [K.sb([128, 512], BF16, tag="QTt") for _ in range(2)]; qts = [S.new_dma_sem() for _ in range(2)]
        Pb = [K.sb([128, 512], BF16, tag="Pb") for _ in range(2)]
        mk = K.sb([128, 4, 512], BF16, tag="mk")
        mi = K.sb([128, 512], I32, tag="mi")
        for j in range(4):
            S.op("gpsimd", lambda e, j=j: e.iota(mi.t[:], pattern=[[1, 512]], base=-128 * j, channel_multiplier=-1),
                 writes=[mi.d])
            S.op("vector", lambda e, j=j: e.tensor_single_scalar(out=mk.t[:, j, :], in_=mi.t[:], scalar=0, op=ALU.is_ge),
                 reads=[mi.d], writes=[mk.d])
        sel = K.sb([128, 64], F32, tag="sel")
        S.op("vector", lambda e: e.memset(sel.t[:], 0.0), writes=[sel.d])
        S.op("vector", lambda e: e.memset(sel.t[64:65, :], 1.0), writes=[sel.d])
        Osb = K.sb([128, 512], F32, tag="Osb"); rec = K.sb([64, 512], F32, tag="rec")
        ob = [K.sb([64, 512], BF16, tag="ob") for _ in range(2)]; obs = [S.new_dma_sem() for _ in range(2)]
        it = 0
        for h in range(4):
            S.dma("sync", khs, KTh.t[0:96, :], KT_d[h], writes=[KTh.d])
            with nc.allow_non_contiguous_dma(reason="V rows of 130B"):
                S.dma("sync", vhs, Vh.t[:], V_d[:, :, h, :].rearrange("c p d -> p c d"), writes=[Vh.d])
            for qt in range(NTok // 512):
                qb = (h * (NTok // 512) + qt) % 2
                S.dma("sync", qts[qb], QTt[qb].t[0:96, :], QT_d[h][:, qt * 512:(qt + 1) * 512], writes=[QTt[qb].d])
                nkb = 4 * qt + 4

                def emit_s(kb, it_):
                    pb = Pb[it_ % 2]
                    S.op("tensor", lambda e, kb=kb, it_=it_, qb=qb: e.matmul(
                        P0.t[:, (it_ % 2) * 512:(it_ % 2 + 1) * 512], lhsT=KTh.t[0:96, kb * 128:(kb + 1) * 128],
                        rhs=QTt[qb].t[0:96, :], start=True, stop=True),
                         reads=[KTh.d, QTt[qb].d], writes=[P0.ds[it_ % 2]])
                    S.op("scalar", lambda e, it_=it_, pb=pb: e.activation(
                        out=pb.t[:], in_=P0.t[:, (it_ % 2) * 512:(it_ % 2 + 1) * 512], func=AF.Exp, scale=SCALE),
                         reads=[P0.ds[it_ % 2]], writes=[pb.d])
                    if kb >= 4 * qt:
                        S.op("vector", lambda e, pb=pb, j=kb - 4 * qt: e.tensor_tensor(
                            out=pb.t[:], in0=pb.t[:], in1=mk.t[:, j, :], op=ALU.mult), reads=[pb.d, mk.d], writes=[pb.d])

                def emit_pv(kb, it_):
                    pb = Pb[it_ % 2]
                    S.op("tensor", lambda e, kb=kb, pb=pb: e.matmul(
                        P1.t[0:65, 0:512], lhsT=Vh.t[:, kb, :], rhs=pb.t[:], start=(kb == 0), stop=(kb == nkb - 1)),
                         reads=[Vh.d, pb.d], writes=[P1.ds[0]])

                emit_s(0, it)
                for kb in range(nkb):
                    if kb + 1 < nkb:
                        emit_s(kb + 1, it + 1)
                    emit_pv(kb, it)
                    it += 1
                S.op("scalar", lambda e: e.activation(out=Osb.t[0:65, :], in_=P1.t[0:65, 0:512], func=AF.Copy),
                     reads=[P1.ds[0]], writes=[Osb.d])
                S.op("tensor", lambda e: e.matmul(P2.t[0:64, 0:512], lhsT=sel.t[0:65, :], rhs=Osb.t[0:65, :], start=True,
                                                  stop=True), reads=[sel.d, Osb.d], writes=[P2.ds[0]])
                S.op("scalar", lambda e: e.activation(out=rec.t[:], in_=P2.t[0:64, 0:512], func=AF.Copy),
                     reads=[P2.ds[0]], writes=[rec.d])
                S.op("vector", lambda e: e.reciprocal(out=rec.t[:], in_=rec.t[:]), reads=[rec.d], writes=[rec.d])
                S.op("vector", lambda e, qb=qb: e.tensor_tensor(out=ob[qb].t[:], in0=Osb.t[0:64, :], in1=rec.t[:],
                                                               op=ALU.mult), reads=[Osb.d, rec.d], writes=[ob[qb].d])
                S.dma("gpsimd", obs[qb], out[h * 64:(h + 1) * 64, qt * 512:(qt + 1) * 512], ob[qb].t[:], reads=[ob[qb].d])
        S.barrier()
    K.scope = old_scope
    K.S.release(_mk)


def build_tok(NT, steps):
    K = KB()
    x = K.din("x", [NT, D]); c = K.din("c", [D])
    y = K.dout("y", [NT, D])
    K.alloc_psum(); K.consts(); K.setup_cond(c)
    aw, ab, lnp = {}, {}, {}

    def layer_in(l):
        if l not in aw:
            aw[l] = K.din(f"aw{l}", [D, 6 * D]); ab[l] = K.din(f"ab{l}", [6 * D])
        return aw[l], ab[l]

    def ln_in(l, j):
        if (l, j) not in lnp:
            lnp[(l, j)] = (K.din(f"lng{l}_{j}", [D]), K.din(f"lnb{l}_{j}", [D]))
        return lnp[(l, j)]

    cur = DramStream(x, NT)
    for si, (kind, l, dm) in enumerate(steps):
        last = si == len(steps) - 1
        nxt = DramStream(y if last else K.dtmp(f"xs{si}", [NT, D]), NT)
        a_w, a_b = layer_in(l)
        if kind == "proj":
            g, b = ln_in(l, 0)
            mT = K.din(f"s{si}_mT", [dm, NT], BF16); wo = K.din(f"s{si}_wo", [dm, D])
            sub_proj(K, cur, nxt, NT, mT, dm, wo, a_w, a_b, g, b)
        elif kind == "ffn":
            g, b = ln_in(l, 1)
            wg = K.din(f"s{si}_wg", [D, DFF]); wu = K.din(f"s{si}_wu", [D, DFF]); wd = K.din(f"s{si}_wd", [DFF, D])
            sub_ffn(K, cur, nxt, NT, a_w, a_b, g, b, wg, wu, wd, None)
        elif kind == "moe":
            g, b = ln_in(l, 1)
            wg = K.din(f"s{si}_wg", [NE, D, DFF]); wu = K.din(f"s{si}_wu", [NE, D, DFF]); wd = K.din(f"s{si}_wd", [NE, DFF, D])
            wr = K.din(f"s{si}_wr", [D, NE])
            sub_ffn(K, cur, nxt, NT, a_w, a_b, g, b, wg, wu, wd, wr)
        elif kind == "sg":
            g, b = ln_in(l, 0)
            w_in = K.din(f"s{si}_win", [D, 4096]); b_in = K.din(f"s{si}_bin", [4096])
            sg_g = K.din(f"s{si}_sg", [2048]); sg_b = K.din(f"s{si}_sb", [2048])
            w_s = K.din(f"s{si}_ws", [8, 128, 128]); b_s = K.din(f"s{si}_bs", [8, 128]); wo = K.din(f"s{si}_wo", [2048, D])
            sub_sg(K, cur, nxt, NT, a_w, a_b, g, b, w_in, b_in, sg_g, sg_b, w_s, b_s, wo)
        cur = nxt
    K.S.finish()
    return K


def _ssd_sel(I, j, q):
    w = I['ssd_w_in'][j]
    cols = np.concatenate([np.arange(512 * q, 512 * q + 512), 2048 + np.arange(512 * q, 512 * q + 512),
                           4096 + np.arange(256 * q, 256 * q + 256), 4096 + 1024 + np.arange(256 * q, 256 * q + 256),
                           6144 + np.arange(8 * q, 8 * q + 8)])
    ccols = np.concatenate([np.arange(512 * q, 512 * q + 512), 2048 + np.arange(256 * q, 256 * q + 256),
                            2048 + 1024 + np.arange(256 * q, 256 * q + 256)])
    return dict(w=np.ascontiguousarray(w[:, cols]), cw=np.ascontiguousarray(I['ssd_conv_w'][j][:, ccols]),
                cb=np.ascontiguousarray(I['ssd_conv_b'][j][ccols]),
                dtb=np.ascontiguousarray(I['ssd_dt_bias'][j][8 * q:8 * q + 8]),
                alog=np.ascontiguousarray(I['ssd_a_log'][j][8 * q:8 * q + 8]),
                dsk=np.ascontiguousarray(I['ssd_d_skip'][j][8 * q:8 * q + 8]),
                nw=np.ascontiguousarray(I['ssd_norm_w'][j][512 * q:512 * q + 512]))


def _mla_sel(I, hq):
    uq = I['mla_w_uq'][0].reshape(512, 16, 96)[:, 4 * hq:4 * hq + 4].reshape(512, 384)
    ukv = I['mla_w_ukv'][0].reshape(256, 16, 128)[:, 4 * hq:4 * hq + 4].reshape(256, 512)
    return dict(w_in=I['mla_w_in'][0], qn=I['mla_q_norm'][0], kvn=I['mla_kv_norm'][0],
                wuq=np.ascontiguousarray(uq), wukv=np.ascontiguousarray(ukv))


def _run(K, in_maps):
    res = run_bass_kernel_spmd(K.nc, in_maps, core_ids=list(range(8)))
    return res.results


def kernel_unfused(**I):
    I = {k: np.asarray(v) for k, v in I.items()}
    B, SEQ = I['x'].shape[0], I['x'].shape[1]
    NT = SEQ // 4
    cores = [(k // 4, k % 4) for k in range(8)]

    def run_mixer_ssd(xcur, layer, j):
        K = build_ssd(SEQ)
        maps = [dict(x=xcur[b], c=I['c'][b], aw=I['ada_w'][layer], ab=I['ada_b'][layer], **_ssd_sel(I, j, q))
                for b, q in cores]
        r = _run(K, maps)
        return [np.concatenate([r[b * 4 + q]["mT"] for q in range(4)], 0) for b in range(B)]

    def run_mixer_mla(xcur, layer):
        K = build_mla(SEQ)
        maps = [dict(x=xcur[b], c=I['c'][b], aw=I['ada_w'][layer], ab=I['ada_b'][layer],
                     pos=np.ascontiguousarray(I['positions'][b].astype(np.int32)), **_mla_sel(I, q)) for b, q in cores]
        r = _run(K, maps)
        return [np.concatenate([r[b * 4 + q]["mT"] for q in range(4)], 0) for b in range(B)]

    def run_tok(xcur, steps, extra):
        K = build_tok(NT, steps)
        maps = []
        for b, r_ in cores:
            m = dict(x=np.ascontiguousarray(xcur[b][r_ * NT:(r_ + 1) * NT]), c=I['c'][b])
            for (kind, l, dm) in steps:
                m[f"aw{l}"] = I['ada_w'][l]; m[f"ab{l}"] = I['ada_b'][l]
                jj = 0 if kind in ("proj", "sg") else 1
                m[f"lng{l}_{jj}"] = I['ln_g'][l, jj]; m[f"lnb{l}_{jj}"] = I['ln_b'][l, jj]
            for k_, v in extra.items():
                m[k_] = v(b, r_) if callable(v) else v
            maps.append(m)
        r = _run(K, maps)
        return [np.concatenate([r[b * 4 + q]["y"] for q in range(4)], 0) for b in range(B)]

    def ffn_w(si, k):
        return {f"s{si}_wg": I['ffn_w_gate'][k], f"s{si}_wu": I['ffn_w_up'][k], f"s{si}_wd": I['ffn_w_down'][k]}

    def moe_w(si, k):
        return {f"s{si}_wg": I['moe_w_gate'][k], f"s{si}_wu": I['moe_w_up'][k], f"s{si}_wd": I['moe_w_down'][k],
                f"s{si}_wr": I['moe_w_router'][k]}

    def mt_slice(mT):
        return lambda b, r_: np.ascontiguousarray(mT[b][:, r_ * NT:(r_ + 1) * NT])

    xcur = [I['x'][b] for b in range(B)]
    mT = run_mixer_ssd(xcur, 0, 0)
    xcur = run_tok(xcur, [("proj", 0, 2048), ("ffn", 0, 0)],
                   {"s0_mT": mt_slice(mT), "s0_wo": I['ssd_w_out'][0], **ffn_w(1, 0)})
    mT = run_mixer_mla(xcur, 1)
    sgw = {"s2_win": I['sg_w_in'][0], "s2_bin": I['sg_b_in'][0], "s2_sg": I['sg_ln_g'][0], "s2_sb": I['sg_ln_b'][0],
           "s2_ws": I['sg_w_s'][0], "s2_bs": I['sg_b_s'][0], "s2_wo": I['sg_w_out'][0]}
    xcur = run_tok(xcur, [("proj", 1, 1024), ("moe", 1, 0), ("sg", 2, 0), ("ffn", 2, 0)],
                   {"s0_mT": mt_slice(mT), "s0_wo": I['mla_w_out'][0], **moe_w(1, 0), **sgw, **ffn_w(3, 1)})
    mT = run_mixer_ssd(xcur, 3, 1)
    xcur = run_tok(xcur, [("proj", 3, 2048), ("moe", 3, 0)],
                   {"s0_mT": mt_slice(mT), "s0_wo": I['ssd_w_out'][1], **moe_w(1, 1)})
    return np.stack(xcur, 0).astype(np.float32)


RG = [[0, 1, 2, 3], [4, 5, 6, 7]]


def collective(K, kind, in_ap, out_ap):
    S = K.S
    if not hasattr(K, "cc"):
        K.cc = S.new_dma_sem()
    S.barrier()
    op = ALU.add if kind == "ReduceScatter" else ALU.bypass
    ins = K.nc.gpsimd.collective_compute(kind, op, replica_groups=RG, ins=[in_ap], outs=[out_ap])
    K.cc.val += 1
    ins.then_inc(K.cc.sem)
    S.barrier()


def sub_pproj(K, mT_ap, DM, wo_ap, ypart_ap, NTok):
    S, nc = K.S, K.nc
    NC_ = DM // 128
    old_scope = K.scope
    _mk = K.S.mark()
    with ExitStack() as sc:
        K.scope = sc
        wout = K.sb([128, NC_, 1024], BF16, tag="wout")
        S.dma("gpsimd", S.new_dma_sem(), wout.t[:], wo_ap.rearrange("(c p) d -> p c d", p=128), writes=[wout.d])
        mt = [K.sb([128, NC_, 128], BF16, tag="mt") for _ in range(2)]
        msem = [S.new_dma_sem() for _ in range(2)]
        yo = [K.sb([128, 1024], F32, tag="yo") for _ in range(2)]
        osem = [S.new_dma_sem() for _ in range(2)]
        for ts in range(NTok // 128):
            b = ts % 2
            S.dma("sync", msem[b], mt[b].t[:], mT_ap[:, ts * 128:(ts + 1) * 128].rearrange("(c p) t -> p c t", p=128),
                  writes=[mt[b].d])
            po = K.psum[2 + b]
            for db in range(2):
                fns = [(lambda e, c=c, db=db, po=po, b=b: e.matmul(
                    po.t[:, db * 512:(db + 1) * 512], lhsT=mt[b].t[:, c, :], rhs=wout.t[:, c, db * 512:(db + 1) * 512],
                    start=(c == 0), stop=(c == NC_ - 1))) for c in range(NC_)]
                S.group("tensor", fns, reads=[mt[b].d, wout.d], writes=[po.ds[db]])
            S.op("scalar", lambda e, po=po, b=b: e.activation(out=yo[b].t[:], in_=po.t[:], func=AF.Copy),
                 reads=[po.ds[0], po.ds[1]], writes=[yo[b].d])
            S.dma("gpsimd", osem[b], ypart_ap(ts), yo[b].t[:], reads=[yo[b].d])
        S.barrier()
    K.scope = old_scope
    K.S.release(_mk)


def sub_resln(K, xs, xd, NT, y_ap, ada_w_l, ada_b_l, lng_ap, lnb_ap):
    S, nc = K.S, K.nc
    old_scope = K.scope
    _mk = K.S.mark()
    with ExitStack() as sc:
        K.scope = sc
        g1p = K.sb([128, 1024], F32, tag="g1p")
        lng = K.sb([128, 1024], F32, tag="lng")
        lnb = K.sb([128, 1024], F32, tag="lnb")
        K.ln_alloc()
        K.mod_vec(g1p, ada_w_l, ada_b_l, 2, True)
        K.bcast_vec(lng, lng_ap, S.new_dma_sem())
        K.bcast_vec(lnb, lnb_ap, S.new_dma_sem())
        yin = [K.sb([128, 1024], F32, tag="yin") for _ in range(2)]
        ysem = [S.new_dma_sem() for _ in range(2)]
        xin = [K.sb([128, 1024], F32, tag="xin") for _ in range(2)]
        xsem = [S.new_dma_sem() for _ in range(2)]
        tmp = [K.sb([128, 1024], F32, tag="tmp") for _ in range(2)]
        xo = [K.sb([128, 1024], F32, tag="xo") for _ in range(2)]
        osem = [S.new_dma_sem() for _ in range(2)]
        for ts in range(NT // 128):
            b = ts % 2
            S.dma("sync", ysem[b], yin[b].t[:], y_ap[ts * 128:(ts + 1) * 128, :], writes=[yin[b].d])
            S.dma("sync", xsem[b], xin[b].t[:], xs.ap[ts * 128:(ts + 1) * 128, :], reads=[xs.ds[ts]],
                  writes=[xin[b].d])
            residual_ln(K, yin[b].t[:], [yin[b].d], xin[b], g1p, lng, lnb, tmp[b], xo[b])
            S.dma("gpsimd", osem[b], xd.ap[ts * 128:(ts + 1) * 128, :], xo[b].t[:], reads=[xo[b].d],
                  writes=[xd.ds[ts]])
        S.barrier()
    K.scope = old_scope
    K.S.release(_mk)


def build_fused(SEQ):
    NT = SEQ // 4
    K = KB()
    S, nc = K.S, K.nc
    x_in = K.din("x", [NT, D]); c = K.din("c", [D]); pos = K.din("pos", [SEQ], I32)
    aw = [K.din(f"aw{l}", [D, 6 * D]) for l in range(4)]
    ab = [K.din(f"ab{l}", [6 * D]) for l in range(4)]
    lng = [[K.din(f"lng{l}_{j}", [D]) for j in range(2)] for l in range(4)]
    lnb = [[K.din(f"lnb{l}_{j}", [D]) for j in range(2)] for l in range(4)]
    ssd = []
    for j in range(2):
        ssd.append(dict(w=K.din(f"ssd{j}_w", [D, 1544]), cw=K.din(f"ssd{j}_cw", [4, 1024]),
                        cbv=K.din(f"ssd{j}_cb", [1024]), dtb=K.din(f"ssd{j}_dtb", [8]),
                        alog=K.din(f"ssd{j}_alog", [8]), dsk=K.din(f"ssd{j}_dsk", [8]),
                        nw=K.din(f"ssd{j}_nw", [512]), wo=K.din(f"ssd{j}_wo", [512, D])))
    mla = dict(w_in=K.din("mla_w_in", [D, 800]), qn=K.din("mla_qn", [512]), kvn=K.din("mla_kvn", [256]),
               wuq_d=K.din("mla_wuq", [512, 384]), wukv_d=K.din("mla_wukv", [256, 512]))
    mla_wo = K.din("mla_wo", [256, D])
    sg = dict(w_in=K.din("sg_win", [D, 4096]), b_in=K.din("sg_bin", [4096]), sg_g=K.din("sg_g", [2048]),
              sg_b=K.din("sg_b", [2048]), w_s=K.din("sg_ws", [8, 128, 128]), b_s=K.din("sg_bs", [8, 128]),
              wo=K.din("sg_wo", [2048, D]))
    ffn = [dict(wg=K.din(f"ffn{k}_wg", [D, DFF]), wu=K.din(f"ffn{k}_wu", [D, DFF]), wd=K.din(f"ffn{k}_wd", [DFF, D]))
           for k in range(2)]
    moe = [dict(wg=K.din(f"moe{k}_wg", [NE, D, DFF]), wu=K.din(f"moe{k}_wu", [NE, D, DFF]),
                wd=K.din(f"moe{k}_wd", [NE, DFF, D]), wr=K.din(f"moe{k}_wr", [D, NE])) for k in range(2)]
    y = K.dout("y", [NT, D])
    K.alloc_psum(); K.consts(); K.setup_cond(c)
    CH = 256
    NCH = NT // CH
    xfull_c = K.dtmp("xfull", [NCH, 4 * CH, D])
    ypart_c = K.dtmp("ypart", [NCH, 4 * CH, D]); yred = K.dtmp("yred", [NT, D])

    def rows(buf):
        def f(tile):
            t = tile * 128
            r, rem = t // NT, t % NT
            ch, i = rem // CH, rem % CH
            return buf[ch, r * CH + i:r * CH + i + 128, :]
        return f

    xfull = rows(xfull_c)
    ypart = rows(ypart_c)

    def gather_x(src):
        for ch in range(NCH):
            collective(K, "AllGather", src[ch * CH:(ch + 1) * CH, :], xfull_c[ch])

    def scatter_y():
        for ch in range(NCH):
            collective(K, "ReduceScatter", ypart_c[ch], yred[ch * CH:(ch + 1) * CH, :])
    mTs = K.dtmp("mTs", [512, SEQ], BF16); mTm = K.dtmp("mTm", [256, SEQ], BF16)
    xl = [K.dtmp(f"xl{i}", [NT, D]) for i in range(8)]

    def stream(ap):
        return DramStream(ap, NT)

    cps = S.new_dma_sem()
    for ch in range(NCH):
        S.dma("sync", cps, xl[0][ch * CH:(ch + 1) * CH, :], x_in[ch * CH:(ch + 1) * CH, :])
    gather_x(xl[0])
    s = ssd[0]
    sub_ssd(K, xfull, mTs, SEQ, aw[0], ab[0], s["w"], s["cw"], s["cbv"], s["dtb"], s["alog"], s["dsk"], s["nw"])
    sub_pproj(K, mTs, 512, s["wo"], ypart, SEQ)
    scatter_y()
    sub_resln(K, stream(xl[0]), stream(xl[1]), NT, yred, aw[0], ab[0], lng[0][0], lnb[0][0])
    f = ffn[0]
    sub_ffn(K, stream(xl[1]), stream(xl[2]), NT, aw[0], ab[0], lng[0][1], lnb[0][1], f["wg"], f["wu"], f["wd"], None)
    gather_x(xl[2])
    sub_mla(K, xfull, mTm, SEQ, aw[1], ab[1], pos, **mla)
    sub_pproj(K, mTm, 256, mla_wo, ypart, SEQ)
    scatter_y()
    sub_resln(K, stream(xl[2]), stream(xl[3]), NT, yred, aw[1], ab[1], lng[1][0], lnb[1][0])
    m = moe[0]
    sub_ffn(K, stream(xl[3]), stream(xl[4]), NT, aw[1], ab[1], lng[1][1], lnb[1][1], m["wg"], m["wu"], m["wd"], m["wr"])
    sub_sg(K, stream(xl[4]), stream(xl[5]), NT, aw[2], ab[2], lng[2][0], lnb[2][0], sg["w_in"], sg["b_in"], sg["sg_g"],
           sg["sg_b"], sg["w_s"], sg["b_s"], sg["wo"])
    f = ffn[1]
    sub_ffn(K, stream(xl[5]), stream(xl[6]), NT, aw[2], ab[2], lng[2][1], lnb[2][1], f["wg"], f["wu"], f["wd"], None)
    gather_x(xl[6])
    s = ssd[1]
    sub_ssd(K, xfull, mTs, SEQ, aw[3], ab[3], s["w"], s["cw"], s["cbv"], s["dtb"], s["alog"], s["dsk"], s["nw"])
    sub_pproj(K, mTs, 512, s["wo"], ypart, SEQ)
    scatter_y()
    sub_resln(K, stream(xl[6]), stream(xl[7]), NT, yred, aw[3], ab[3], lng[3][0], lnb[3][0])
    m = moe[1]
    sub_ffn(K, stream(xl[7]), stream(y), NT, aw[3], ab[3], lng[3][1], lnb[3][1], m["wg"], m["wu"], m["wd"], m["wr"])
    S.finish()
    return K


def fused_inputs(I, SEQ, b, q):
    NT = SEQ // 4
    m = dict(x=np.ascontiguousarray(I['x'][b][q * NT:(q + 1) * NT]), c=np.ascontiguousarray(I['c'][b]),
             pos=np.ascontiguousarray(I['positions'][b].astype(np.int32)))
    for l in range(4):
        m[f"aw{l}"] = I['ada_w'][l]; m[f"ab{l}"] = I['ada_b'][l]
        for j in range(2):
            m[f"lng{l}_{j}"] = I['ln_g'][l, j]; m[f"lnb{l}_{j}"] = I['ln_b'][l, j]
    for j in range(2):
        s = _ssd_sel(I, j, q)
        for k_, v in s.items():
            m[f"ssd{j}_{k_}"] = v
        m[f"ssd{j}_wo"] = np.ascontiguousarray(I['ssd_w_out'][j][512 * q:512 * q + 512])
    s = _mla_sel(I, q)
    m["mla_w_in"] = s["w_in"]; m["mla_qn"] = s["qn"]; m["mla_kvn"] = s["kvn"]; m["mla_wuq"] = s["wuq"]; m["mla_wukv"] = s["wukv"]
    m["mla_wo"] = np.ascontiguousarray(I['mla_w_out'][0][256 * q:256 * q + 256])
    m["sg_win"] = I['sg_w_in'][0]; m["sg_bin"] = I['sg_b_in'][0]; m["sg_g"] = I['sg_ln_g'][0]; m["sg_b"] = I['sg_ln_b'][0]
    m["sg_ws"] = I['sg_w_s'][0]; m["sg_bs"] = I['sg_b_s'][0]; m["sg_wo"] = I['sg_w_out'][0]
    for k in range(2):
        m[f"ffn{k}_wg"] = I['ffn_w_gate'][k]; m[f"ffn{k}_wu"] = I['ffn_w_up'][k]; m[f"ffn{k}_wd"] = I['ffn_w_down'][k]
        m[f"moe{k}_wg"] = I['moe_w_gate'][k]; m[f"moe{k}_wu"] = I['moe_w_up'][k]; m[f"moe{k}_wd"] = I['moe_w_down'][k]
        m[f"moe{k}_wr"] = I['moe_w_router'][k]
    return m


def kernel(**I):
    I = {k: np.asarray(v) for k, v in I.items()}
    B, SEQ = I['x'].shape[0], I['x'].shape[1]
    NT = SEQ // 4
    K = build_fused(SEQ)
    maps = [fused_inputs(I, SEQ, k // 4, k % 4) for k in range(8)]
    r = _run(K, maps)
    return np.stack([np.concatenate([r[b * 4 + q]["y"] for q in range(4)], 0) for b in range(B)], 0).astype(np.float32)
```

```python
import math
from contextlib import ExitStack

import numpy as np
import concourse.bass as bass
import concourse.mybir as mybir
from concourse.bass_utils import run_bass_kernel_spmd

F32 = mybir.dt.float32
BF16 = mybir.dt.bfloat16
I32 = mybir.dt.int32
AF = mybir.ActivationFunctionType
ALU = mybir.AluOpType

D = 1024
DFF = 2816
NE = 8
ALPHA = 8.0 ** 0.25
LN_EPS = 1e-5
RMS_EPS = 1e-6


class Dep:
    __slots__ = ("w", "r")

    def __init__(self):
        self.w = None
        self.r = {}


class DmaSem:
    def __init__(self, sem):
        self.sem = sem
        self.val = 0


class Eng:
    def __init__(self, name, engine, sem):
        self.name = name
        self.e = engine
        self.sem = sem
        self.count = 0
        self.seen = {}


class Sync:
    def __init__(self, nc, stack):
        self.nc = nc
        self.stack = stack
        self.engs = {}
        for name in ("tensor", "vector", "scalar", "gpsimd", "sync"):
            sem = stack.enter_context(nc.semaphore("s_" + name))
            self.engs[name] = Eng(name, getattr(nc, name), sem)
        self.dma_sems = []
        self.n_inst = 0

    def new_dma_sem(self):
        if getattr(self, "free", None):
            d = self.free.pop()
        else:
            sem = self.stack.enter_context(self.nc.semaphore(None))
            d = DmaSem(sem)
            self.dma_sems.append(d)
        if not hasattr(self, "handed"):
            self.handed = []
            self.free = []
        self.handed.append(d)
        return d

    def mark(self):
        if not hasattr(self, "handed"):
            self.handed = []
            self.free = []
        return len(self.handed)

    def release(self, mk):
        self.free.extend(self.handed[mk:])
        del self.handed[mk:]

    def _waits(self, eng, reads, writes):
        need = {}
        for d in reads:
            if d.w is not None and need.get(d.w[0], 0) < d.w[1]:
                need[d.w[0]] = d.w[1]
        for d in writes:
            if d.w is not None and need.get(d.w[0], 0) < d.w[1]:
                need[d.w[0]] = d.w[1]
            for k, v in d.r.items():
                if need.get(k, 0) < v:
                    need[k] = v
        for k, v in need.items():
            if eng.seen.get(k, 0) < v:
                eng.e.wait_ge(k, v)
                eng.seen[k] = v

    def _mark(self, key, val, reads, writes):
        for d in reads:
            d.r[key] = val
        for d in writes:
            d.w = (key, val)
            d.r = {}

    def op(self, en, fn, reads=(), writes=()):
        eng = self.engs[en]
        self._waits(eng, reads, writes)
        ins = fn(eng.e)
        eng.count += 1
        ins.then_inc(eng.sem, 1)
        self.n_inst += 1
        self._mark(eng.sem, eng.count, reads, writes)

    def group(self, en, fns, reads=(), writes=()):
        eng = self.engs[en]
        self._waits(eng, reads, writes)
        ins = None
        for fn in fns:
            ins = fn(eng.e)
            self.n_inst += 1
        eng.count += 1
        ins.then_inc(eng.sem, 1)
        self._mark(eng.sem, eng.count, reads, writes)

    def dma(self, en, dsem, out, in_, reads=(), writes=(), **kw):
        eng = self.engs[en]
        self._waits(eng, reads, writes)
        ins = eng.e.dma_start(out=out, in_=in_, **kw)
        dsem.val += 16
        ins.then_inc(dsem.sem, 16)
        self.n_inst += 1
        self._mark(dsem.sem, dsem.val, reads, writes)

    def barrier(self):
        for eng in self.engs.values():
            for e2 in self.engs.values():
                if e2.count > 0 and eng.seen.get(e2.sem, 0) < e2.count:
                    eng.e.wait_ge(e2.sem, e2.count)
                    eng.seen[e2.sem] = e2.count
            for d in self.dma_sems:
                if d.val > 0 and eng.seen.get(d.sem, 0) < d.val:
                    eng.e.wait_ge(d.sem, d.val)
                    eng.seen[d.sem] = d.val

    def finish(self):
        self.barrier()


class Buf:
    def __init__(self, t, n=1):
        self.t = t
        self.ds = [Dep() for _ in range(n)]

    @property
    def d(self):
        return self.ds[0]


class KB:
    def __init__(self):
        self.nc = bass.Bass("TRN2", target_bir_lowering=False)
        self.st = ExitStack()
        self.S = Sync(self.nc, self.st)
        self.scope = self.st
        self._n = 0
        self.psum = None

    def name(self, p):
        self._n += 1
        return f"{p}{self._n}"

    def din(self, name, shape, dt=F32):
        return self.nc.dram_tensor(name, list(shape), dt, kind="ExternalInput").ap()

    def dout(self, name, shape, dt=F32):
        return self.nc.dram_tensor(name, list(shape), dt, kind="ExternalOutput").ap()

    def dtmp(self, name, shape, dt=F32):
        return self.nc.dram_tensor(name, list(shape), dt, kind="Internal").ap()

    def sb(self, shape, dt, n=1, tag="t"):
        t = self.scope.enter_context(self.nc.sbuf_tensor(self.name(tag), list(shape), dt))
        return Buf(t, n)

    def alloc_psum(self):
        self.psum = [Buf(self.st.enter_context(self.nc.psum_tensor(f"ps{i}", [128, 1024], F32)), 2)
                     for i in range(4)]

    def consts(self):
        S, nc = self.S, self.nc
        ii = self.sb([128, 128], I32, tag="ii")
        self.ident = self.sb([128, 128], F32, tag="ident")
        S.op("gpsimd", lambda e: e.iota(ii.t[:], pattern=[[1, 128]], base=0, channel_multiplier=-1),
             writes=[ii.d])
        S.op("vector", lambda e: e.tensor_single_scalar(out=self.ident.t[:], in_=ii.t[:], scalar=0,
                                                        op=ALU.is_equal), reads=[ii.d], writes=[self.ident.d])
        self.eps_ln = self.sb([128, 1], F32, tag="eps")
        S.op("vector", lambda e: e.memset(self.eps_ln.t[:], LN_EPS), writes=[self.eps_ln.d])

    def setup_cond(self, c_ap):
        S, nc = self.S, self.nc
        ccol = self.sb([128, 8], F32, tag="ccol")
        self.cbc = self.sb([128, 8, 128], F32, tag="cbc")
        self.modw = self.sb([128, 8, 512], F32, tag="modw")
        self.modb = self.sb([128, 1024], F32, tag="modb")
        self.sem_c = S.new_dma_sem()
        self.sem_mw = S.new_dma_sem()
        self.sem_mb = S.new_dma_sem()
        with nc.allow_non_contiguous_dma(reason="tiny column load"):
            S.dma("sync", self.sem_c, ccol.t[:], c_ap.rearrange("(c p) -> p c", p=128), writes=[ccol.d])
        S.op("scalar", lambda e: e.activation(out=ccol.t[:], in_=ccol.t[:], func=AF.Silu),
             reads=[ccol.d], writes=[ccol.d])
        S.op("vector", lambda e: e.tensor_copy(out=self.cbc.t[:],
                                               in_=ccol.t[:].unsqueeze(2).to_broadcast([128, 8, 128])),
             reads=[ccol.d], writes=[self.cbc.d])

    def mod_vec(self, out_buf, ada_w_l, ada_b_l, idx, plus_one):
        S = self.S
        ps = self.psum[0]
        S.dma("sync", self.sem_mb, self.modb.t[:],
              ada_b_l[idx * 1024:(idx + 1) * 1024].unsqueeze(0).to_broadcast([128, 1024]),
              writes=[self.modb.d])
        for hb in range(2):
            c0 = idx * 1024 + hb * 512
            S.dma("sync", self.sem_mw, self.modw.t[:],
                  ada_w_l[:, c0:c0 + 512].rearrange("(c p) f -> p c f", p=128), writes=[self.modw.d])
            fns = [(lambda e, k=k: e.matmul(ps.t[:, hb * 512:(hb + 1) * 512], lhsT=self.cbc.t[:, k, :],
                                             rhs=self.modw.t[:, k, :], start=(k == 0), stop=(k == 7)))
                   for k in range(8)]
            S.group("tensor", fns, reads=[self.cbc.d, self.modw.d], writes=[ps.ds[hb]])
        if plus_one:
            S.op("vector", lambda e: e.scalar_tensor_tensor(out=out_buf.t[:], in0=ps.t[:], scalar=1.0,
                                                            in1=self.modb.t[:], op0=ALU.add, op1=ALU.add),
                 reads=[ps.ds[0], ps.ds[1], self.modb.d], writes=[out_buf.d])
        else:
            S.op("vector", lambda e: e.tensor_tensor(out=out_buf.t[:], in0=ps.t[:], in1=self.modb.t[:],
                                                     op=ALU.add),
                 reads=[ps.ds[0], ps.ds[1], self.modb.d], writes=[out_buf.d])

    def bcast_vec(self, out_buf, vec_ap, sem):
        n = vec_ap.shape[0]
        self.S.dma("sync", sem, out_buf.t[:], vec_ap.unsqueeze(0).to_broadcast([128, n]), writes=[out_buf.d])

    def ln_alloc(self):
        self.ln_st = self.sb([128, 2, 6], F32, tag="lnst")
        self.ln_mv = self.sb([128, 2], F32, tag="lnmv")
        self.ln_rs = self.sb([128, 1], F32, tag="lnrs")

    def layer_norm(self, r, out, g_bc, b_bc):
        S = self.S
        st, mv, rs = self.ln_st, self.ln_mv, self.ln_rs
        for hb in range(2):
            S.op("vector", lambda e, hb=hb: e.bn_stats(out=st.t[:, hb, :], in_=r.t[:, hb * 512:(hb + 1) * 512]),
                 reads=[r.d], writes=[st.d])
        S.op("vector", lambda e: e.bn_aggr(out=mv.t[:], in_=st.t[:].rearrange("p a b -> p (a b)")),
             reads=[st.d], writes=[mv.d])
        S.op("scalar", lambda e: e.activation(out=rs.t[:], in_=mv.t[:, 1:2], func=AF.Sqrt,
                                              bias=self.eps_ln.t[:], scale=1.0),
             reads=[mv.d, self.eps_ln.d], writes=[rs.d])
        S.op("vector", lambda e: e.reciprocal(out=rs.t[:], in_=rs.t[:]), reads=[rs.d], writes=[rs.d])
        S.op("vector", lambda e: e.tensor_scalar(out=r.t[:], in0=r.t[:], scalar1=mv.t[:, 0:1], scalar2=rs.t[:],
                                                 op0=ALU.subtract, op1=ALU.mult),
             reads=[r.d, mv.d, rs.d], writes=[r.d])
        S.op("vector", lambda e: e.tensor_tensor(out=r.t[:], in0=r.t[:], in1=g_bc.t[:], op=ALU.mult),
             reads=[r.d, g_bc.d], writes=[r.d])
        S.op("vector", lambda e: e.tensor_tensor(out=out.t[:], in0=r.t[:], in1=b_bc.t[:], op=ALU.add),
             reads=[r.d, b_bc.d], writes=[out.d])


class DramStream:
    def __init__(self, ap, nt):
        self.ap = ap
        self.ds = [Dep() for _ in range(nt // 128)]


def sub_ffn(K, xs, xd, NT, ada_w_l, ada_b_l, lng_ap, lnb_ap, wg_ap, wu_ap, wd_ap, wr_ap=None):
    S, nc = K.S, K.nc
    moe = wr_ap is not None
    import os as _os
    E = int(_os.environ.get('MOE_E', NE)) if moe else 1
    T = min(NT, 2048)
    NSUP, NSUB, NB = NT // T, T // 128, T // 512
    JG = 2
    NG = DFF // (128 * JG)
    old_scope = K.scope
    _mk = K.S.mark()
    with ExitStack() as sc:
        K.scope = sc
        xT = K.sb([128, 8, T], BF16, n=NB, tag="xT")
        acc = K.sb([128, NSUB, 1024], F32, n=NSUB, tag="acc")
        wgb = [K.sb([128, 8, 128 * JG], BF16, tag="wg") for _ in range(2)]
        wub = [K.sb([128, 8, 128 * JG], BF16, tag="wu") for _ in range(2)]
        wdb = [K.sb([128, JG, 1024], BF16, tag="wd") for _ in range(2)]
        wsem = [S.new_dma_sem() for _ in range(2)]
        hT = [K.sb([128, JG, 512], BF16, n=JG, tag="hT") for _ in range(2)]
        sg = [K.sb([128, 512], F32, tag="sg") for _ in range(2)]
        xin = [K.sb([128, 1024], F32, tag="xin") for _ in range(2)]
        xsem = [S.new_dma_sem() for _ in range(2)]
        hf = [K.sb([128, 1024], F32, tag="hf") for _ in range(2)]
        xo = [K.sb([128, 1024], F32, tag="xo") for _ in range(2)]
        osem = [S.new_dma_sem() for _ in range(2)]
        sc1p = K.sb([128, 1024], F32, tag="sc1p")
        shf = K.sb([128, 1024], F32, tag="shf")
        g1p = K.sb([128, 1024], F32, tag="g1p")
        lng = K.sb([128, 1024], F32, tag="lng")
        lnb = K.sb([128, 1024], F32, tag="lnb")
        csem = S.new_dma_sem()
        K.ln_alloc()
        K.mod_vec(shf, ada_w_l, ada_b_l, 3, False)
        K.mod_vec(sc1p, ada_w_l, ada_b_l, 4, True)
        K.mod_vec(g1p, ada_w_l, ada_b_l, 5, True)
        K.bcast_vec(lng, lng_ap, csem)
        K.bcast_vec(lnb, lnb_ap, S.new_dma_sem())
        if moe:
            hfT32 = K.sb([128, 8, 128], F32, tag="hfT32")
            wr = K.sb([128, 8, NE], F32, tag="wr")
            if _os.environ.get("DBG4") != "1":
                with nc.allow_non_contiguous_dma(reason="router weights, tiny"):
                    S.dma("sync", S.new_dma_sem(), wr.t[:], wr_ap.rearrange("(c p) e -> p c e", p=128), writes=[wr.d])
            comb = K.sb([128, NSUB, NE], F32, tag="comb")
            lgall = K.sb([128, NSUB, NE], F32, tag="lgall")
            l2 = K.sb([128, NSUB, NE], F32, tag="l2")
            mk1 = K.sb([128, NSUB, NE], F32, tag="mk1")
            mk2 = K.sb([128, NSUB, NE], F32, tag="mk2")
            m1 = K.sb([128, NSUB], F32, tag="m1")
            m2 = K.sb([128, NSUB], F32, tag="m2")
            w1 = K.sb([128, NSUB], F32, tag="w1")
            w2 = K.sb([128, NSUB], F32, tag="w2")
        psG = [K.psum[0], K.psum[1]]
        psO = [K.psum[2], K.psum[3]]

        def wsrc(ap, e):
            return ap[e % ap.shape[0]] if moe else ap

        def load_w(e, jg, slot):
            f0 = jg * 128 * JG
            S.dma("gpsimd", wsem[slot], wgb[slot].t[:],
                  wsrc(wg_ap, e)[:, f0:f0 + 128 * JG].rearrange("(c p) f -> p c f", p=128), writes=[wgb[slot].d])
            S.dma("gpsimd", wsem[slot], wub[slot].t[:],
                  wsrc(wu_ap, e)[:, f0:f0 + 128 * JG].rearrange("(c p) f -> p c f", p=128), writes=[wub[slot].d])
            S.dma("gpsimd", wsem[slot], wdb[slot].t[:],
                  wsrc(wd_ap, e)[f0:f0 + 128 * JG, :].rearrange("(j p) d -> p j d", p=128), writes=[wdb[slot].d])
            wgb[slot].d.w = wub[slot].d.w = wdb[slot].d.w

        wlist = [(e, jg) for e in range(E) for jg in range(NG)]
        for sup in range(NSUP):
            t0 = sup * T
            load_w(*wlist[0], 0)
            for ts in range(NSUB):
                b = ts % 2
                gi = (t0 // 128) + ts
                S.dma("sync", xsem[b], xin[b].t[:], xs.ap[gi * 128:(gi + 1) * 128, :],
                      reads=[xs.ds[gi]], writes=[xin[b].d])
                S.op("vector", lambda e, b=b: e.tensor_tensor(out=hf[b].t[:], in0=xin[b].t[:], in1=sc1p.t[:],
                                                              op=ALU.mult),
                     reads=[xin[b].d, sc1p.d], writes=[hf[b].d])
                S.op("vector", lambda e, b=b: e.tensor_tensor(out=hf[b].t[:], in0=hf[b].t[:], in1=shf.t[:],
                                                              op=ALU.add),
                     reads=[hf[b].d, shf.d], writes=[hf[b].d])
                po = psO[b]
                for hb in range(2):
                    fns = [(lambda e, k=k, b=b, po=po: e.transpose(out=po.t[:, k * 128:(k + 1) * 128],
                                                                    in_=hf[b].t[:, k * 128:(k + 1) * 128],
                                                                    identity=K.ident.t[:]))
                           for k in range(hb * 4, hb * 4 + 4)]
                    S.group("tensor", fns, reads=[hf[b].d, K.ident.d], writes=[po.ds[hb]])
                if not moe:
                    S.op("scalar", lambda e, po=po, ts=ts: e.activation(
                        out=xT.t[:, :, ts * 128:(ts + 1) * 128], in_=po.t[:].rearrange("p (c t) -> p c t", c=8),
                        func=AF.Copy), reads=[po.ds[0], po.ds[1]], writes=[xT.ds[ts // 4]])
                else:
                    S.op("scalar", lambda e, po=po: e.activation(
                        out=hfT32.t[:], in_=po.t[:].rearrange("p (c t) -> p c t", c=8), func=AF.Copy),
                         reads=[po.ds[0], po.ds[1]], writes=[hfT32.d])
                    S.op("gpsimd", lambda e, ts=ts: e.tensor_copy(out=xT.t[:, :, ts * 128:(ts + 1) * 128],
                                                                  in_=hfT32.t[:]),
                         reads=[hfT32.d], writes=[xT.ds[ts // 4]])
                if moe:
                    pl = psG[0]
                    fns = [(lambda e, k=k, pl=pl: e.matmul(pl.t[:, 0:NE], lhsT=hfT32.t[:, k, :], rhs=wr.t[:, k, :],
                                                           start=(k == 0), stop=(k == 7))) for k in range(8)]
                    import os as _os
                    if _os.environ.get("MOEDBG") == "1":
                        S.op("vector", lambda e, ts=ts: e.memset(lgall.t[:, ts, :], 0.0), writes=[lgall.d])
                    else:
                        S.group("tensor", fns, reads=[hfT32.d, wr.d], writes=[pl.ds[0]])
                        S.op("vector", lambda e, pl=pl, ts=ts: e.tensor_copy(out=lgall.t[:, ts, :], in_=pl.t[:, 0:NE]),
                             reads=[pl.ds[0]], writes=[lgall.d])
            if moe and _os.environ.get('DBG3') == '1':
                S.op('vector', lambda e: e.memset(comb.t[:], 0.125), writes=[comb.d])
            elif moe:
                X = mybir.AxisListType.X
                bc = lambda b_: b_.t[:].unsqueeze(2).to_broadcast([128, NSUB, NE])
                S.op("vector", lambda e: e.tensor_reduce(out=m1.t[:], in_=lgall.t[:], axis=X, op=ALU.max),
                     reads=[lgall.d], writes=[m1.d])
                S.op("vector", lambda e: e.tensor_tensor(out=mk1.t[:], in0=lgall.t[:], in1=bc(m1), op=ALU.is_equal),
                     reads=[lgall.d, m1.d], writes=[mk1.d])
                S.op("vector", lambda e: e.scalar_tensor_tensor(out=l2.t[:], in0=mk1.t[:], scalar=-1e30,
                                                                in1=lgall.t[:], op0=ALU.mult, op1=ALU.add),
                     reads=[mk1.d, lgall.d], writes=[l2.d])
                S.op("vector", lambda e: e.tensor_reduce(out=m2.t[:], in_=l2.t[:], axis=X, op=ALU.max),
                     reads=[l2.d], writes=[m2.d])
                S.op("vector", lambda e: e.tensor_tensor(out=mk2.t[:], in0=l2.t[:], in1=bc(m2), op=ALU.is_equal),
                     reads=[l2.d, m2.d], writes=[mk2.d])
                S.op("vector", lambda e: e.tensor_tensor(out=w2.t[:], in0=m2.t[:], in1=m1.t[:], op=ALU.subtract),
                     reads=[m1.d, m2.d], writes=[w2.d])
                S.op("scalar", lambda e: e.activation(out=w2.t[:], in_=w2.t[:], func=AF.Exp),
                     reads=[w2.d], writes=[w2.d])
                S.op("vector", lambda e: e.tensor_scalar(out=w1.t[:], in0=w2.t[:], scalar1=1.0, scalar2=None,
                                                         op0=ALU.add), reads=[w2.d], writes=[w1.d])
                S.op("vector", lambda e: e.reciprocal(out=w1.t[:], in_=w1.t[:]), reads=[w1.d], writes=[w1.d])
                S.op("vector", lambda e: e.tensor_tensor(out=w2.t[:], in0=w2.t[:], in1=w1.t[:], op=ALU.mult),
                     reads=[w1.d, w2.d], writes=[w2.d])
                S.op("vector", lambda e: e.tensor_tensor(out=mk1.t[:], in0=mk1.t[:], in1=bc(w1), op=ALU.mult),
                     reads=[mk1.d, w1.d], writes=[mk1.d])
                S.op("vector", lambda e: e.tensor_tensor(out=mk2.t[:], in0=mk2.t[:], in1=bc(w2), op=ALU.mult),
                     reads=[mk2.d, w2.d], writes=[mk2.d])
                S.op("vector", lambda e: e.tensor_tensor(out=comb.t[:], in0=mk1.t[:], in1=mk2.t[:], op=ALU.add),
                     reads=[mk1.d, mk2.d], writes=[comb.d])
            for ts in range(NSUB):
                S.op("gpsimd", lambda e, ts=ts: e.memset(acc.t[:, ts, :], 0.0), writes=[acc.ds[ts]])
            its = [(wi, tb) for wi in range(len(wlist)) for tb in range(NB)]

            def emit_gu(n, js):
                wi, tb = its[n]
                slot = wi % 2
                hb_ = hT[n % 2]
                for j in js:
                    pg = psG[j % 2]
                    for which, wbuf in ((0, wgb[slot]), (1, wub[slot])):
                        fns = [(lambda e, k=k, j=j, which=which, wbuf=wbuf, pg=pg, tb=tb: e.matmul(
                            pg.t[:, which * 512:(which + 1) * 512], lhsT=wbuf.t[:, k, j * 128:(j + 1) * 128],
                            rhs=xT.t[:, k, tb * 512:(tb + 1) * 512], start=(k == 0), stop=(k == 7)))
                            for k in range(8)]
                        S.group("tensor", fns, reads=[wbuf.d, xT.ds[tb]], writes=[pg.ds[which]])
                    sgb = sg[j % 2]
                    S.op("scalar", lambda e, pg=pg, sgb=sgb: e.activation(out=sgb.t[:], in_=pg.t[:, 0:512],
                                                                         func=AF.Silu),
                         reads=[pg.ds[0]], writes=[sgb.d])
                    S.op("vector", lambda e, pg=pg, sgb=sgb, hb_=hb_, j=j: e.tensor_tensor(
                        out=hb_.t[:, j, :], in0=sgb.t[:], in1=pg.t[:, 512:1024], op=ALU.mult),
                         reads=[sgb.d, pg.ds[1]], writes=[hb_.ds[j]])

            def emit_d(n, qs):
                wi, tb = its[n]
                slot = wi % 2
                e_ = wlist[wi][0]
                hb_ = hT[n % 2]
                for q in qs:
                    ts = tb * 4 + q
                    po = psO[q % 2]
                    for db in range(2):
                        fns = [(lambda e, j=j, q=q, db=db, po=po, hb_=hb_, slot=slot: e.matmul(
                            po.t[:, db * 512:(db + 1) * 512], lhsT=hb_.t[:, j, q * 128:(q + 1) * 128],
                            rhs=wdb[slot].t[:, j, db * 512:(db + 1) * 512], start=(j == 0),
                            stop=(j == JG - 1))) for j in range(JG)]
                        S.group("tensor", fns, reads=[hb_.ds[0], hb_.ds[1], wdb[slot].d], writes=[po.ds[db]])
                    cs = comb.t[:, ts, e_:e_ + 1] if (moe and _os.environ.get('DBG2') != '1') else 1.0
                    rd = [po.ds[0], po.ds[1], acc.ds[ts]] + ([comb.d] if moe else [])
                    S.op("vector", lambda e, po=po, ts=ts, cs=cs: e.scalar_tensor_tensor(
                        out=acc.t[:, ts, :], in0=po.t[:], scalar=cs, in1=acc.t[:, ts, :], op0=ALU.mult,
                        op1=ALU.add), reads=rd, writes=[acc.ds[ts]])

            for n in range(len(its)):
                wi, tb = its[n]
                emit_gu(n, (0,))
                if n > 0:
                    emit_d(n - 1, (0, 1))
                emit_gu(n, (1,))
                if n > 0:
                    emit_d(n - 1, (2, 3))
                if tb == 0 and wi + 1 < len(wlist):
                    load_w(*wlist[wi + 1], (wi + 1) % 2)
            emit_d(len(its) - 1, (0, 1, 2, 3))
            for ts in range(NSUB):
                b = ts % 2
                gi = (t0 // 128) + ts
                S.dma("sync", xsem[b], xin[b].t[:], xs.ap[gi * 128:(gi + 1) * 128, :],
                      reads=[xs.ds[gi]], writes=[xin[b].d])
                S.op("vector", lambda e, ts=ts, b=b: e.tensor_tensor(out=hf[b].t[:], in0=acc.t[:, ts, :],
                                                                    in1=g1p.t[:], op=ALU.mult),
                     reads=[acc.ds[ts], g1p.d], writes=[hf[b].d])
                S.op("vector", lambda e, b=b: e.scalar_tensor_tensor(out=hf[b].t[:], in0=xin[b].t[:], scalar=ALPHA,
                                                                     in1=hf[b].t[:], op0=ALU.mult, op1=ALU.add),
                     reads=[xin[b].d, hf[b].d], writes=[hf[b].d])
                K.layer_norm(hf[b], xo[b], lng, lnb)
                S.dma("gpsimd", osem[b], xd.ap[gi * 128:(gi + 1) * 128, :], xo[b].t[:],
                      reads=[xo[b].d], writes=[xd.ds[gi]])
        S.barrier()
    K.scope = old_scope
    K.S.release(_mk)


def residual_ln(K, y_ap, y_deps, xin, g1p, lng, lnb, tmp, xo):
    S = K.S
    S.op("vector", lambda e: e.tensor_tensor(out=tmp.t[:], in0=y_ap, in1=g1p.t[:], op=ALU.mult),
         reads=list(y_deps) + [g1p.d], writes=[tmp.d])
    S.op("vector", lambda e: e.scalar_tensor_tensor(out=tmp.t[:], in0=xin.t[:], scalar=ALPHA, in1=tmp.t[:],
                                                    op0=ALU.mult, op1=ALU.add),
         reads=[xin.d, tmp.d], writes=[tmp.d])
    K.layer_norm(tmp, xo, lng, lnb)


def sub_proj(K, xs, xd, NT, mT_ap, DM, wout_ap, ada_w_l, ada_b_l, lng_ap, lnb_ap):
    S, nc = K.S, K.nc
    NC_ = DM // 128
    old_scope = K.scope
    _mk = K.S.mark()
    with ExitStack() as sc:
        K.scope = sc
        wout = K.sb([128, NC_, 1024], BF16, tag="wout")
        S.dma("gpsimd", S.new_dma_sem(), wout.t[:], wout_ap.rearrange("(c p) d -> p c d", p=128), writes=[wout.d])
        g1p = K.sb([128, 1024], F32, tag="g1p")
        lng = K.sb([128, 1024], F32, tag="lng")
        lnb = K.sb([128, 1024], F32, tag="lnb")
        K.ln_alloc()
        K.mod_vec(g1p, ada_w_l, ada_b_l, 2, True)
        K.bcast_vec(lng, lng_ap, S.new_dma_sem())
        K.bcast_vec(lnb, lnb_ap, S.new_dma_sem())
        mt = [K.sb([128, NC_, 128], BF16, tag="mt") for _ in range(2)]
        msem = [S.new_dma_sem() for _ in range(2)]
        xin = [K.sb([128, 1024], F32, tag="xin") for _ in range(2)]
        xsem = [S.new_dma_sem() for _ in range(2)]
        tmp = [K.sb([128, 1024], F32, tag="tmp") for _ in range(2)]
        xo = [K.sb([128, 1024], F32, tag="xo") for _ in range(2)]
        osem = [S.new_dma_sem() for _ in range(2)]
        for ts in range(NT // 128):
            b = ts % 2
            S.dma("sync", msem[b], mt[b].t[:], mT_ap[:, ts * 128:(ts + 1) * 128].rearrange("(c p) t -> p c t", p=128),
                  writes=[mt[b].d])
            S.dma("sync", xsem[b], xin[b].t[:], xs.ap[ts * 128:(ts + 1) * 128, :], reads=[xs.ds[ts]],
                  writes=[xin[b].d])
            po = K.psum[2 + b]
            for db in range(2):
                fns = [(lambda e, c=c, db=db, po=po, b=b: e.matmul(
                    po.t[:, db * 512:(db + 1) * 512], lhsT=mt[b].t[:, c, :], rhs=wout.t[:, c, db * 512:(db + 1) * 512],
                    start=(c == 0), stop=(c == NC_ - 1))) for c in range(NC_)]
                S.group("tensor", fns, reads=[mt[b].d, wout.d], writes=[po.ds[db]])
            residual_ln(K, po.t[:], po.ds, xin[b], g1p, lng, lnb, tmp[b], xo[b])
            S.dma("gpsimd", osem[b], xd.ap[ts * 128:(ts + 1) * 128, :], xo[b].t[:], reads=[xo[b].d],
                  writes=[xd.ds[ts]])
        S.barrier()
    K.scope = old_scope
    K.S.release(_mk)


def sub_sg(K, xs, xd, NT, ada_w_l, ada_b_l, lng_ap, lnb_ap, w_in_ap, b_in_ap, sglng_ap, sglnb_ap, w_s_ap, b_s_ap,
           w_out_ap):
    S, nc = K.S, K.nc
    old_scope = K.scope
    _mk = K.S.mark()
    with ExitStack() as sc:
        K.scope = sc
        win = K.sb([128, 8, 4096], BF16, tag="win")
        S.dma("gpsimd", S.new_dma_sem(), win.t[:, :, 0:2048],
              w_in_ap[:, 0:2048].rearrange("(c p) f -> p c f", p=128), writes=[win.d])
        S.dma("gpsimd", S.new_dma_sem(), win.t[:, :, 2048:4096],
              w_in_ap[:, 2048:4096].rearrange("(c p) f -> p c f", p=128), writes=[win.d])
        wout = K.sb([128, 16, 1024], BF16, tag="wout")
        S.dma("gpsimd", S.new_dma_sem(), wout.t[:], w_out_ap.rearrange("(c p) d -> p c d", p=128), writes=[wout.d])
        binb = K.sb([1, 4096], BF16, tag="binb")
        S.dma("gpsimd", S.new_dma_sem(), binb.t[:], b_in_ap.unsqueeze(0), writes=[binb.d])
        bsb = K.sb([1, 8, 128], BF16, tag="bsb")
        S.dma("gpsimd", S.new_dma_sem(), bsb.t[:], b_s_ap.unsqueeze(0), writes=[bsb.d])
        ones = K.sb([1, 512], BF16, tag="ones")
        S.op("vector", lambda e: e.memset(ones.t[:], 1.0), writes=[ones.d])
        sglng = K.sb([128, 2048], F32, tag="sglng")
        sglnb = K.sb([128, 2048], F32, tag="sglnb")
        K.bcast_vec(sglng, sglng_ap, S.new_dma_sem())
        K.bcast_vec(sglnb, sglnb_ap, S.new_dma_sem())
        sc1p = K.sb([128, 1024], F32, tag="sc1p")
        shm = K.sb([128, 1024], F32, tag="shm")
        g1p = K.sb([128, 1024], F32, tag="g1p")
        lng = K.sb([128, 1024], F32, tag="lng")
        lnb = K.sb([128, 1024], F32, tag="lnb")
        K.ln_alloc()
        K.mod_vec(shm, ada_w_l, ada_b_l, 0, False)
        K.mod_vec(sc1p, ada_w_l, ada_b_l, 1, True)
        K.mod_vec(g1p, ada_w_l, ada_b_l, 2, True)
        K.bcast_vec(lng, lng_ap, S.new_dma_sem())
        K.bcast_vec(lnb, lnb_ap, S.new_dma_sem())
        wmT = K.sb([128, 8, 128], BF16, tag="wmT")
        sc_setup = ExitStack()
        K.scope = sc_setup
        wsf = K.sb([128, 8, 128], F32, tag="wsf")
        S.dma("sync", S.new_dma_sem(), wsf.t[:], w_s_ap.rearrange("g t s -> t g s"), writes=[wsf.d])
        ii = K.sb([128, 128], I32, tag="ii2")
        msk = K.sb([128, 128], F32, tag="msk")
        S.op("gpsimd", lambda e: e.iota(ii.t[:], pattern=[[1, 128]], base=0, channel_multiplier=-1), writes=[ii.d])
        S.op("vector", lambda e: e.tensor_single_scalar(out=msk.t[:], in_=ii.t[:], scalar=0, op=ALU.is_le),
             reads=[ii.d], writes=[msk.d])
        S.op("vector", lambda e: e.tensor_tensor(out=wsf.t[:], in0=wsf.t[:],
                                                 in1=msk.t[:].unsqueeze(1).to_broadcast([128, 8, 128]), op=ALU.mult),
             reads=[wsf.d, msk.d], writes=[wsf.d])
        pt = K.psum[0]
        for hb in range(2):
            fns = [(lambda e, g=g: e.transpose(out=pt.t[:, g * 128:(g + 1) * 128], in_=wsf.t[:, g, :],
                                               identity=K.ident.t[:])) for g in range(hb * 4, hb * 4 + 4)]
            S.group("tensor", fns, reads=[wsf.d, K.ident.d], writes=[pt.ds[hb]])
        S.op("scalar", lambda e: e.activation(out=wmT.t[:], in_=pt.t[:].rearrange("p (g t) -> p g t", g=8),
                                              func=AF.Copy), reads=[pt.ds[0], pt.ds[1]], writes=[wmT.d])
        S.barrier()
        sc_setup.close()
        K.scope = sc
        xin = K.sb([128, 1024], F32, tag="xin")
        xsem = S.new_dma_sem()
        hf = K.sb([128, 1024], F32, tag="hf")
        hmT = K.sb([128, 8, 128], BF16, tag="hmT")
        u = K.sb([128, 2048], F32, tag="u")
        v = K.sb([128, 2048], F32, n=1, tag="v")
        vn = K.sb([128, 2048], BF16, tag="vn")
        gT = Buf(vn.t, 1)
        gT.ds = vn.ds
        gTv = vn.t[:].rearrange("p (c t) -> p c t", c=16)
        xo = hf
        osem = S.new_dma_sem()
        st4 = K.sb([128, 4, 6], F32, tag="st4")
        mv = K.sb([128, 2], F32, tag="mv2")
        rs = K.sb([128, 1], F32, tag="rs2")
        for ts in range(NT // 128):
            S.dma("sync", xsem, xin.t[:], xs.ap[ts * 128:(ts + 1) * 128, :], reads=[xs.ds[ts]], writes=[xin.d])
            S.op("vector", lambda e: e.tensor_tensor(out=hf.t[:], in0=xin.t[:], in1=sc1p.t[:], op=ALU.mult),
                 reads=[xin.d, sc1p.d], writes=[hf.d])
            S.op("vector", lambda e: e.tensor_tensor(out=hf.t[:], in0=hf.t[:], in1=shm.t[:], op=ALU.add),
                 reads=[hf.d, shm.d], writes=[hf.d])
            po = K.psum[0]
            for hb in range(2):
                fns = [(lambda e, k=k: e.transpose(out=po.t[:, k * 128:(k + 1) * 128],
                                                   in_=hf.t[:, k * 128:(k + 1) * 128], identity=K.ident.t[:]))
                       for k in range(hb * 4, hb * 4 + 4)]
                S.group("tensor", fns, reads=[hf.d, K.ident.d], writes=[po.ds[hb]])
            S.op("scalar", lambda e: e.activation(out=hmT.t[:], in_=po.t[:].rearrange("p (c t) -> p c t", c=8),
                                                  func=AF.Copy), reads=[po.ds[0], po.ds[1]], writes=[hmT.d])
            for cb in range(8):
                pb = K.psum[(cb // 2) % 2 + 0]
                half = cb % 2
                fns = [(lambda e, k=k, cb=cb, pb=pb, half=half: e.matmul(
                    pb.t[:, half * 512:(half + 1) * 512], lhsT=hmT.t[:, k, :], rhs=win.t[:, k, cb * 512:(cb + 1) * 512],
                    start=(k == 0), stop=False)) for k in range(8)]
                fns.append(lambda e, cb=cb, pb=pb, half=half: e.matmul(
                    pb.t[:, half * 512:(half + 1) * 512], lhsT=ones.t[0:1, 0:128], rhs=binb.t[0:1, cb * 512:(cb + 1) * 512],
                    start=False, stop=True))
                S.group("tensor", fns, reads=[hmT.d, win.d, ones.d, binb.d], writes=[pb.ds[half]])
                dst = u if cb < 4 else v
                c0 = (cb % 4) * 512
                S.op("scalar", lambda e, pb=pb, half=half, dst=dst, c0=c0: e.activation(
                    out=dst.t[:, c0:c0 + 512], in_=pb.t[:, half * 512:(half + 1) * 512], func=AF.Gelu_apprx_tanh),
                     reads=[pb.ds[half]], writes=[dst.d])
            for q in range(4):
                S.op("vector", lambda e, q=q: e.bn_stats(out=st4.t[:, q, :], in_=v.t[:, q * 512:(q + 1) * 512]),
                     reads=[v.d], writes=[st4.d])
            S.op("vector", lambda e: e.bn_aggr(out=mv.t[:], in_=st4.t[:].rearrange("p a b -> p (a b)")),
                 reads=[st4.d], writes=[mv.d])
            S.op("scalar", lambda e: e.activation(out=rs.t[:], in_=mv.t[:, 1:2], func=AF.Sqrt, bias=K.eps_ln.t[:],
                                                  scale=1.0), reads=[mv.d, K.eps_ln.d], writes=[rs.d])
            S.op("vector", lambda e: e.reciprocal(out=rs.t[:], in_=rs.t[:]), reads=[rs.d], writes=[rs.d])
            S.op("vector", lambda e: e.tensor_scalar(out=v.t[:], in0=v.t[:], scalar1=mv.t[:, 0:1], scalar2=rs.t[:],
                                                     op0=ALU.subtract, op1=ALU.mult),
                 reads=[v.d, mv.d, rs.d], writes=[v.d])
            S.op("vector", lambda e: e.tensor_tensor(out=v.t[:], in0=v.t[:], in1=sglng.t[:], op=ALU.mult),
                 reads=[v.d, sglng.d], writes=[v.d])
            S.op("vector", lambda e: e.tensor_tensor(out=vn.t[:], in0=v.t[:], in1=sglnb.t[:], op=ALU.add),
                 reads=[v.d, sglnb.d], writes=[vn.d])
            for g in range(8):
                pm = K.psum[2 + g // 4]
                half = (g % 4) // 2
                c0 = (g % 4) * 256
                fns = [lambda e, g=g, pm=pm, c0=c0: e.matmul(pm.t[:, c0:c0 + 256], lhsT=wmT.t[:, g, :],
                                                              rhs=vn.t[:, g * 256:(g + 1) * 256], start=True, stop=False),
                       lambda e, g=g, pm=pm, c0=c0: e.matmul(pm.t[:, c0:c0 + 256], lhsT=bsb.t[0:1, g, :],
                                                              rhs=ones.t[0:1, 0:256], start=False, stop=True)]
                S.group("tensor", fns, reads=[wmT.d, vn.d, bsb.d, ones.d], writes=[pm.ds[half]])
            for h2 in range(2):
                pm = K.psum[2 + h2]
                S.op("vector", lambda e, pm=pm, h2=h2: e.tensor_tensor(
                    out=v.t[:, h2 * 1024:(h2 + 1) * 1024], in0=pm.t[:], in1=u.t[:, h2 * 1024:(h2 + 1) * 1024],
                    op=ALU.mult), reads=[pm.ds[0], pm.ds[1], u.d], writes=[v.d])
            for h2 in range(2):
                pt2 = K.psum[h2]
                for hb in range(2):
                    fns = [(lambda e, c=c, pt2=pt2, h2=h2: e.transpose(
                        out=pt2.t[:, (c % 8) * 128:(c % 8 + 1) * 128], in_=v.t[:, c * 128:(c + 1) * 128],
                        identity=K.ident.t[:])) for c in range(h2 * 8 + hb * 4, h2 * 8 + hb * 4 + 4)]
                    S.group("tensor", fns, reads=[v.d, K.ident.d], writes=[pt2.ds[hb]])
                S.op("scalar", lambda e, pt2=pt2, h2=h2: e.activation(
                    out=gTv[:, h2 * 8:(h2 + 1) * 8, :], in_=pt2.t[:].rearrange("p (c t) -> p c t", c=8),
                    func=AF.Copy), reads=[pt2.ds[0], pt2.ds[1]], writes=[gT.d])
            py = K.psum[2]
            for db in range(2):
                fns = [(lambda e, c=c, db=db: e.matmul(py.t[:, db * 512:(db + 1) * 512], lhsT=gTv[:, c, :],
                                                        rhs=wout.t[:, c, db * 512:(db + 1) * 512], start=(c == 0),
                                                        stop=(c == 15))) for c in range(16)]
                S.group("tensor", fns, reads=[gT.d, wout.d], writes=[py.ds[db]])
            residual_ln(K, py.t[:], py.ds, xin, g1p, lng, lnb, hf, xo)
            S.dma("gpsimd", osem, xd.ap[ts * 128:(ts + 1) * 128, :], xo.t[:], reads=[xo.d], writes=[xd.ds[ts]])
        S.barrier()
    K.scope = old_scope
    K.S.release(_mk)


def build_ssd(NTok):
    K = KB()
    x = K.din("x", [NTok, D]); c = K.din("c", [D]); aw = K.din("aw", [D, 6 * D]); ab = K.din("ab", [6 * D])
    w = K.din("w", [D, 1544]); cw = K.din("cw", [4, 1024]); cbv = K.din("cb", [1024])
    dtb = K.din("dtb", [8]); alog = K.din("alog", [8]); dsk = K.din("dsk", [8]); nw = K.din("nw", [512])
    out = K.dout("mT", [512, NTok], BF16)
    K.alloc_psum(); K.consts(); K.setup_cond(c)
    sub_ssd(K, x, out, NTok, aw, ab, w, cw, cbv, dtb, alog, dsk, nw)
    K.S.finish()
    return K


def sub_ssd(K, x, out, NTok, aw, ab, w, cw, cbv, dtb, alog, dsk, nw):
    S, nc = K.S, K.nc
    X = mybir.AxisListType.X
    P0, P1, P2, P3 = K.psum
    old_scope = K.scope
    _mk = K.S.mark()
    sc_ssd = ExitStack()
    K.scope = sc_ssd
    wz = K.sb([128, 8, 512], BF16, tag="wz")
    wx = K.sb([128, 8, 1024], BF16, tag="wx")
    wdt = K.sb([128, 8, 8], BF16, tag="wdt")
    S.dma("gpsimd", S.new_dma_sem(), wz.t[:], w[:, 0:512].rearrange("(c p) f -> p c f", p=128), writes=[wz.d])
    S.dma("gpsimd", S.new_dma_sem(), wx.t[:], w[:, 512:1536].rearrange("(c p) f -> p c f", p=128), writes=[wx.d])
    with nc.allow_non_contiguous_dma(reason="tiny"):
        S.dma("gpsimd", S.new_dma_sem(), wdt.t[:], w[:, 1536:1544].rearrange("(c p) f -> p c f", p=128),
              writes=[wdt.d])
    cwT = K.sb([128, 4, 8], F32, tag="cwT")
    cbT = K.sb([128, 8], F32, tag="cbT")
    with nc.allow_non_contiguous_dma(reason="tiny"):
        S.dma("sync", S.new_dma_sem(), cwT.t[:], cw.rearrange("k (c p) -> p k c", p=128), writes=[cwT.d])
        S.dma("sync", S.new_dma_sem(), cbT.t[:], cbv.rearrange("(c p) -> p c", p=128), writes=[cbT.d])
    dtb_bc = K.sb([128, 8], F32, tag="dtb"); A_bc = K.sb([128, 8], F32, tag="A"); dsk_bc = K.sb([128, 8], F32, tag="dsk")
    nw_bc = K.sb([128, 512], F32, tag="nw")
    K.bcast_vec(dtb_bc, dtb, S.new_dma_sem()); K.bcast_vec(A_bc, alog, S.new_dma_sem())
    K.bcast_vec(dsk_bc, dsk, S.new_dma_sem()); K.bcast_vec(nw_bc, nw, S.new_dma_sem())
    S.op("scalar", lambda e: e.activation(out=A_bc.t[:], in_=A_bc.t[:], func=AF.Exp), reads=[A_bc.d], writes=[A_bc.d])
    S.op("vector", lambda e: e.tensor_scalar(out=A_bc.t[:], in0=A_bc.t[:], scalar1=-1.0, scalar2=None, op0=ALU.mult),
         reads=[A_bc.d], writes=[A_bc.d])
    sc1p = K.sb([128, 1024], F32, tag="sc1p"); shm = K.sb([128, 1024], F32, tag="shm")
    K.mod_vec(shm, aw, ab, 0, False)
    K.mod_vec(sc1p, aw, ab, 1, True)
    ii = K.sb([128, 128], I32, tag="ii3")
    triU = K.sb([128, 128], F32, tag="triU")
    ones = K.sb([128, 128], F32, tag="ones")
    eps_r = K.sb([128, 1], F32, tag="epsr")
    S.op("gpsimd", lambda e: e.iota(ii.t[:], pattern=[[1, 128]], base=0, channel_multiplier=-1), writes=[ii.d])
    S.op("vector", lambda e: e.tensor_single_scalar(out=triU.t[:], in_=ii.t[:], scalar=0, op=ALU.is_ge),
         reads=[ii.d], writes=[triU.d])
    S.op("vector", lambda e: e.memset(ones.t[:], 1.0), writes=[ones.d])
    S.op("vector", lambda e: e.memset(eps_r.t[:], RMS_EPS), writes=[eps_r.d])
    S32 = K.sb([128, 8, 64], F32, tag="S32"); Sbf = K.sb([128, 8, 64], BF16, tag="Sbf")
    S.op("vector", lambda e: e.memset(S32.t[:], 0.0), writes=[S32.d])
    S.op("vector", lambda e: e.memset(Sbf.t[:], 0.0), writes=[Sbf.d])
    xr = K.sb([128, 8, 131], F32, tag="xr")
    S.op("vector", lambda e: e.memset(xr.t[:], 0.0), writes=[xr.d])
    xin = K.sb([128, 1024], F32, tag="xin"); xsem = S.new_dma_sem()
    hf = K.sb([128, 1024], F32, tag="hf")
    hmT = K.sb([128, 8, 128], BF16, tag="hmT")
    cacc = K.sb([128, 8, 128], F32, tag="cacc"); ctmp = K.sb([128, 8, 128], F32, tag="ctmp")
    xa = K.sb([128, 8, 128], F32, tag="xa")
    bcT = K.sb([128, 4, 128], BF16, tag="bcT")
    xtok = K.sb([128, 768], F32, tag="xtok")
    btok = K.sb([128, 256], BF16, tag="btok")
    dtv = K.sb([128, 8], F32, tag="dtv"); dtA = K.sb([128, 8], F32, tag="dtA")
    dtAb = K.sb([128, 8, 128], F32, tag="dtAb")
    acs = K.sb([128, 24], F32, tag="acs")
    dte = K.sb([128, 8], F32, tag="dte"); cd = K.sb([128, 8], F32, tag="cd")
    Lx = K.sb([128, 8, 128], F32, tag="Lx"); Eb = K.sb([128, 8, 128], F32, tag="Eb")
    cbm = K.sb([128, 2, 128], F32, tag="cbm")
    MT = K.sb([128, 8, 128], BF16, tag="MT"); CsT = K.sb([128, 8, 128], BF16, tag="CsT")
    xdt = K.sb([128, 8, 64], BF16, tag="xdt"); xdte = K.sb([128, 8, 64], BF16, tag="xdte")
    y = K.sb([128, 512], F32, tag="y"); sz = K.sb([128, 512], F32, tag="sz"); sq = K.sb([128, 512], F32, tag="sq")
    ss = K.sb([128, 2], F32, tag="ss")
    ygT = K.sb([128, 4, 128], BF16, tag="ygT"); osem = S.new_dma_sem()
    bc3 = lambda ap, n: ap.unsqueeze(2).to_broadcast([128, 8, n])

    for ck in range(NTok // 128):
        S.dma("sync", xsem, xin.t[:], (x(ck) if callable(x) else x[ck * 128:(ck + 1) * 128, :]), writes=[xin.d])
        S.op("vector", lambda e: e.tensor_tensor(out=hf.t[:], in0=xin.t[:], in1=sc1p.t[:], op=ALU.mult),
             reads=[xin.d, sc1p.d], writes=[hf.d])
        S.op("vector", lambda e: e.tensor_tensor(out=hf.t[:], in0=hf.t[:], in1=shm.t[:], op=ALU.add),
             reads=[hf.d, shm.d], writes=[hf.d])
        for hb in range(2):
            fns = [(lambda e, k=k: e.transpose(out=P0.t[:, k * 128:(k + 1) * 128], in_=hf.t[:, k * 128:(k + 1) * 128],
                                               identity=K.ident.t[:])) for k in range(hb * 4, hb * 4 + 4)]
            S.group("tensor", fns, reads=[hf.d, K.ident.d], writes=[P0.ds[hb]])
        S.op("scalar", lambda e: e.activation(out=hmT.t[:], in_=P0.t[:].rearrange("p (c t) -> p c t", c=8),
                                              func=AF.Copy), reads=[P0.ds[0], P0.ds[1]], writes=[hmT.d])
        fns = [(lambda e, k=k: e.matmul(P1.t[:, 0:512], lhsT=hmT.t[:, k, :], rhs=wz.t[:, k, :], start=(k == 0),
                                        stop=(k == 7))) for k in range(8)]
        S.group("tensor", fns, reads=[hmT.d, wz.d], writes=[P1.ds[0]])
        fns = [(lambda e, k=k: e.matmul(P1.t[:, 512:520], lhsT=hmT.t[:, k, :], rhs=wdt.t[:, k, :], start=(k == 0),
                                        stop=(k == 7))) for k in range(8)]
        S.group("tensor", fns, reads=[hmT.d, wdt.d], writes=[P1.ds[1]])
        for hb in range(2):
            fns = []
            for ch in range(hb * 4, hb * 4 + 4):
                fns += [(lambda e, k=k, ch=ch: e.matmul(P2.t[:, ch * 128:(ch + 1) * 128],
                                                        lhsT=wx.t[:, k, ch * 128:(ch + 1) * 128], rhs=hmT.t[:, k, :],
                                                        start=(k == 0), stop=(k == 7))) for k in range(8)]
            S.group("tensor", fns, reads=[hmT.d, wx.d], writes=[P2.ds[hb]])
        S.op("scalar", lambda e: e.activation(out=xr.t[:, :, 3:131], in_=P2.t[:].rearrange("p (c t) -> p c t", c=8),
                                              func=AF.Copy), reads=[P2.ds[0], P2.ds[1]], writes=[xr.d])
        S.op("vector", lambda e: e.tensor_tensor(out=dtv.t[:], in0=P1.t[:, 512:520], in1=dtb_bc.t[:], op=ALU.add),
             reads=[P1.ds[1], dtb_bc.d], writes=[dtv.d])
        S.op("scalar", lambda e: e.activation(out=dtv.t[:], in_=dtv.t[:], func=AF.Exp), reads=[dtv.d], writes=[dtv.d])
        S.op("scalar", lambda e: e.activation(out=dtv.t[:], in_=dtv.t[:], func=AF.Ln, bias=1.0, scale=1.0),
             reads=[dtv.d], writes=[dtv.d])
        S.op("vector", lambda e: e.tensor_tensor(out=dtA.t[:], in0=dtv.t[:], in1=A_bc.t[:], op=ALU.mult),
             reads=[dtv.d, A_bc.d], writes=[dtA.d])
        S.op("vector", lambda e: e.tensor_copy(out=dtAb.t[:], in_=bc3(dtA.t[:], 128)), reads=[dtA.d], writes=[dtAb.d])
        S.op("tensor", lambda e: e.matmul(P1.t[:, 520:528], lhsT=triU.t[:], rhs=dtA.t[:], start=True, stop=True),
             reads=[triU.d, dtA.d], writes=[P1.ds[1]])
        S.op("tensor", lambda e: e.matmul(P1.t[:, 528:536], lhsT=ones.t[:], rhs=dtA.t[:], start=True, stop=True),
             reads=[ones.d, dtA.d], writes=[P1.ds[1]])
        S.op("scalar", lambda e: e.activation(out=acs.t[:, 0:16], in_=P1.t[:, 520:536], func=AF.Copy),
             reads=[P1.ds[1]], writes=[acs.d])
        for hb in range(2):
            fns = [(lambda e, h=h: e.matmul(P0.t[:, h * 128:(h + 1) * 128], lhsT=dtAb.t[:, h, :], rhs=triU.t[:],
                                            start=True, stop=True)) for h in range(hb * 4, hb * 4 + 4)]
            S.group("tensor", fns, reads=[dtAb.d, triU.d], writes=[P0.ds[hb]])
        P0v = P0.t[:].rearrange("p (h l) -> p h l", h=8)
        S.op("vector", lambda e: e.tensor_tensor(out=Lx.t[:], in0=P0v, in1=bc3(acs.t[:, 0:8], 128), op=ALU.subtract),
             reads=[P0.ds[0], P0.ds[1], acs.d], writes=[Lx.d])
        S.op("vector", lambda e: e.tensor_scalar(out=Lx.t[:], in0=Lx.t[:], scalar1=0.0, scalar2=None, op0=ALU.min),
             reads=[Lx.d], writes=[Lx.d])
        S.op("scalar", lambda e: e.activation(out=Lx.t[:], in_=Lx.t[:], func=AF.Exp), reads=[Lx.d], writes=[Lx.d])
        S.op("scalar", lambda e: e.activation(out=Eb.t[:], in_=P0v, func=AF.Exp), reads=[P0.ds[0], P0.ds[1]],
             writes=[Eb.d])
        S.op("vector", lambda e: e.tensor_tensor(out=dte.t[:], in0=acs.t[:, 8:16], in1=acs.t[:, 0:8], op=ALU.subtract),
             reads=[acs.d], writes=[dte.d])
        S.op("scalar", lambda e: e.activation(out=dte.t[:], in_=dte.t[:], func=AF.Exp), reads=[dte.d], writes=[dte.d])
        S.op("scalar", lambda e: e.activation(out=cd.t[:], in_=acs.t[:, 8:16], func=AF.Exp), reads=[acs.d], writes=[cd.d])
        for k in range(4):
            src = xr.t[:, :, k:k + 128]
            wk = bc3(cwT.t[:, k, :], 128)
            if k == 0:
                S.op("vector", lambda e, src=src, wk=wk: e.tensor_tensor(out=cacc.t[:], in0=src, in1=wk, op=ALU.mult),
                     reads=[xr.d, cwT.d], writes=[cacc.d])
            else:
                S.op("vector", lambda e, src=src, wk=wk: e.tensor_tensor(out=ctmp.t[:], in0=src, in1=wk, op=ALU.mult),
                     reads=[xr.d, cwT.d], writes=[ctmp.d])
                S.op("vector", lambda e: e.tensor_tensor(out=cacc.t[:], in0=cacc.t[:], in1=ctmp.t[:], op=ALU.add),
                     reads=[cacc.d, ctmp.d], writes=[cacc.d])
        S.op("vector", lambda e: e.tensor_copy(out=xr.t[:, :, 0:3], in_=xr.t[:, :, 128:131]), reads=[xr.d], writes=[xr.d])
        for ch in range(8):
            S.op("scalar", lambda e, ch=ch: e.activation(out=xa.t[:, ch, :], in_=cacc.t[:, ch, :], func=AF.Silu,
                                                         bias=cbT.t[:, ch:ch + 1], scale=1.0),
                 reads=[cacc.d, cbT.d], writes=[xa.d])
        S.op("vector", lambda e: e.tensor_copy(out=bcT.t[:], in_=xa.t[:, 4:8, :]), reads=[xa.d], writes=[bcT.d])
        for hb in range(2):
            rng_ = range(0, 4) if hb == 0 else range(4, 6)
            fns = [(lambda e, j=j: e.transpose(out=P2.t[:, j * 128:(j + 1) * 128], in_=xa.t[:, j, :],
                                               identity=K.ident.t[:])) for j in rng_]
            S.group("tensor", fns, reads=[xa.d, K.ident.d], writes=[P2.ds[hb]])
        S.op("scalar", lambda e: e.activation(out=xtok.t[:], in_=P2.t[:, 0:768], func=AF.Copy),
             reads=[P2.ds[0], P2.ds[1]], writes=[xtok.d])
        S.op("vector", lambda e: e.tensor_copy(out=btok.t[:], in_=xtok.t[:, 512:768]), reads=[xtok.d], writes=[btok.d])
        xt3 = xtok.t[:, 0:512].rearrange("p (h d) -> p h d", h=8)
        S.op("vector", lambda e: e.tensor_tensor(out=xdt.t[:], in0=xt3, in1=bc3(dtv.t[:], 64), op=ALU.mult),
             reads=[xtok.d, dtv.d], writes=[xdt.d])
        S.op("vector", lambda e: e.tensor_tensor(out=dte.t[:], in0=dte.t[:], in1=dtv.t[:], op=ALU.mult),
             reads=[dte.d, dtv.d], writes=[dte.d])
        S.op("vector", lambda e: e.tensor_tensor(out=xdte.t[:], in0=xt3, in1=bc3(dte.t[:], 64), op=ALU.mult),
             reads=[xtok.d, dte.d], writes=[xdte.d])
        fns = [(lambda e, g=g: e.matmul(P3.t[:, g * 128:(g + 1) * 128], lhsT=bcT.t[:, g, :], rhs=bcT.t[:, 2 + g, :],
                                        start=True, stop=True)) for g in range(2)]
        S.group("tensor", fns, reads=[bcT.d], writes=[P3.ds[0]])
        S.op("vector", lambda e: e.tensor_tensor(out=cbm.t[:], in0=P3.t[:, 0:256].rearrange("p (g l) -> p g l", g=2),
                                                 in1=triU.t[:].unsqueeze(1).to_broadcast([128, 2, 128]), op=ALU.mult),
             reads=[P3.ds[0], triU.d], writes=[cbm.d])
        for g in range(2):
            S.op("vector", lambda e, g=g: e.tensor_tensor(
                out=MT.t[:, 4 * g:4 * g + 4, :], in0=Lx.t[:, 4 * g:4 * g + 4, :],
                in1=cbm.t[:, g, :].unsqueeze(1).to_broadcast([128, 4, 128]), op=ALU.mult),
                 reads=[Lx.d, cbm.d], writes=[MT.d])
            S.op("vector", lambda e, g=g: e.tensor_tensor(
                out=CsT.t[:, 4 * g:4 * g + 4, :], in0=Eb.t[:, 4 * g:4 * g + 4, :],
                in1=xa.t[:, 6 + g, :].unsqueeze(1).to_broadcast([128, 4, 128]), op=ALU.mult),
                 reads=[Eb.d, xa.d], writes=[CsT.d])
        fns = []
        for h in range(8):
            fns.append(lambda e, h=h: e.matmul(P2.t[:, h * 64:(h + 1) * 64], lhsT=MT.t[:, h, :], rhs=xdt.t[:, h, :],
                                               start=True, stop=False))
            fns.append(lambda e, h=h: e.matmul(P2.t[:, h * 64:(h + 1) * 64], lhsT=CsT.t[:, h, :], rhs=Sbf.t[:, h, :],
                                               start=False, stop=True))
        S.group("tensor", fns, reads=[MT.d, xdt.d, CsT.d, Sbf.d], writes=[P2.ds[0]])
        fns = [(lambda e, h=h: e.matmul(P2.t[:, 512 + h * 64:512 + (h + 1) * 64], lhsT=btok.t[:, (h // 4) * 128:(h // 4 + 1) * 128],
                                        rhs=xdte.t[:, h, :], start=True, stop=True)) for h in range(8)]
        S.group("tensor", fns, reads=[btok.d, xdte.d], writes=[P2.ds[1]])
        S.op("vector", lambda e: e.tensor_tensor(out=S32.t[:], in0=S32.t[:], in1=bc3(cd.t[:], 64), op=ALU.mult),
             reads=[S32.d, cd.d], writes=[S32.d])
        S.op("vector", lambda e: e.tensor_tensor(out=S32.t[:], in0=S32.t[:],
                                                 in1=P2.t[:, 512:1024].rearrange("p (h d) -> p h d", h=8), op=ALU.add),
             reads=[S32.d, P2.ds[1]], writes=[S32.d])
        S.op("vector", lambda e: e.tensor_copy(out=Sbf.t[:], in_=S32.t[:]), reads=[S32.d], writes=[Sbf.d])
        S.op("vector", lambda e: e.tensor_tensor(out=y.t[:].rearrange("p (h d) -> p h d", h=8), in0=xt3,
                                                 in1=bc3(dsk_bc.t[:], 64), op=ALU.mult),
             reads=[xtok.d, dsk_bc.d], writes=[y.d])
        S.op("vector", lambda e: e.tensor_tensor(out=y.t[:], in0=y.t[:], in1=P2.t[:, 0:512], op=ALU.add),
             reads=[y.d, P2.ds[0]], writes=[y.d])
        S.op("scalar", lambda e: e.activation(out=sz.t[:], in_=P1.t[:, 0:512], func=AF.Silu), reads=[P1.ds[0]],
             writes=[sz.d])
        S.op("vector", lambda e: e.tensor_tensor(out=y.t[:], in0=y.t[:], in1=sz.t[:], op=ALU.mult),
             reads=[y.d, sz.d], writes=[y.d])
        S.op("vector", lambda e: e.tensor_tensor(out=sq.t[:], in0=y.t[:], in1=y.t[:], op=ALU.mult),
             reads=[y.d], writes=[sq.d])
        S.op("vector", lambda e: e.tensor_reduce(out=ss.t[:], in_=sq.t[:].rearrange("p (g d) -> p g d", g=2), axis=X,
                                                 op=ALU.add), reads=[sq.d], writes=[ss.d])
        S.op("scalar", lambda e: e.activation(out=ss.t[:], in_=ss.t[:], func=AF.Sqrt, bias=eps_r.t[:], scale=1.0 / 256.0),
             reads=[ss.d, eps_r.d], writes=[ss.d])
        S.op("vector", lambda e: e.reciprocal(out=ss.t[:], in_=ss.t[:]), reads=[ss.d], writes=[ss.d])
        S.op("vector", lambda e: e.tensor_tensor(out=y.t[:].rearrange("p (g d) -> p g d", g=2),
                                                 in0=y.t[:].rearrange("p (g d) -> p g d", g=2),
                                                 in1=ss.t[:].unsqueeze(2).to_broadcast([128, 2, 256]), op=ALU.mult),
             reads=[y.d, ss.d], writes=[y.d])
        S.op("vector", lambda e: e.tensor_tensor(out=y.t[:], in0=y.t[:], in1=nw_bc.t[:], op=ALU.mult),
             reads=[y.d, nw_bc.d], writes=[y.d])
        fns = [(lambda e, j=j: e.transpose(out=P3.t[:, 512 + j * 128:512 + (j + 1) * 128], in_=y.t[:, j * 128:(j + 1) * 128],
                                           identity=K.ident.t[:])) for j in range(4)]
        S.group("tensor", fns, reads=[y.d, K.ident.d], writes=[P3.ds[1]])
        S.op("scalar", lambda e: e.activation(out=ygT.t[:], in_=P3.t[:, 512:1024].rearrange("p (c t) -> p c t", c=4),
                                              func=AF.Copy), reads=[P3.ds[1]], writes=[ygT.d])
        S.dma("gpsimd", osem, out[:, ck * 128:(ck + 1) * 128].rearrange("(c p) t -> p c t", p=128), ygT.t[:],
              reads=[ygT.d])
    S.barrier()
    sc_ssd.close()
    K.scope = old_scope
    K.S.release(_mk)


def build_mla(NTok):
    K = KB()
    x = K.din("x", [NTok, D]); c = K.din("c", [D]); aw = K.din("aw", [D, 6 * D]); ab = K.din("ab", [6 * D])
    pos = K.din("pos", [NTok], I32)
    w_in = K.din("w_in", [D, 800]); qn = K.din("qn", [512]); kvn = K.din("kvn", [256])
    wuq_d = K.din("wuq", [512, 384]); wukv_d = K.din("wukv", [256, 512])
    out = K.dout("mT", [256, NTok], BF16)
    K.alloc_psum(); K.consts(); K.setup_cond(c)
    sub_mla(K, x, out, NTok, aw, ab, pos, w_in, qn, kvn, wuq_d, wukv_d)
    K.S.finish()
    return K


def sub_mla(K, x, out, NTok, aw, ab, pos, w_in, qn, kvn, wuq_d, wukv_d):
    S, nc = K.S, K.nc
    X = mybir.AxisListType.X
    NTL = NTok // 128
    QT_d = K.dtmp("QT_d", [4, 96, NTok], BF16); KT_d = K.dtmp("KT_d", [4, 96, NTok], BF16)
    V_d = K.dtmp("V_d", [NTL, 128, 4, 65], BF16)
    P0, P1, P2, P3 = K.psum
    old_scope = K.scope
    _mk = K.S.mark()
    SCALE = 96.0 ** -0.5
    TWO_PI = 2.0 * math.pi
    with ExitStack() as sc:
        K.scope = sc
        win = K.sb([128, 8, 800], BF16, tag="win")
        wuq = K.sb([128, 4, 384], BF16, tag="wuq"); wukv = K.sb([128, 2, 512], BF16, tag="wukv")
        S.dma("gpsimd", S.new_dma_sem(), win.t[:], w_in.rearrange("(c p) f -> p c f", p=128), writes=[win.d])
        S.dma("gpsimd", S.new_dma_sem(), wuq.t[:], wuq_d.rearrange("(c p) f -> p c f", p=128), writes=[wuq.d])
        S.dma("gpsimd", S.new_dma_sem(), wukv.t[:], wukv_d.rearrange("(c p) f -> p c f", p=128), writes=[wukv.d])
        nbc = K.sb([128, 768], F32, tag="nbc")
        S.dma("sync", S.new_dma_sem(), nbc.t[:, 0:512], qn.unsqueeze(0).to_broadcast([128, 512]), writes=[nbc.d])
        S.dma("sync", S.new_dma_sem(), nbc.t[:, 512:768], kvn.unsqueeze(0).to_broadcast([128, 256]), writes=[nbc.d])
        sc1p = K.sb([128, 1024], F32, tag="sc1p"); shm = K.sb([128, 1024], F32, tag="shm")
        K.mod_vec(shm, aw, ab, 0, False)
        K.mod_vec(sc1p, aw, ab, 1, True)
        eps_r = K.sb([128, 1], F32, tag="epsr")
        S.op("vector", lambda e: e.memset(eps_r.t[:], RMS_EPS), writes=[eps_r.d])
        ji = K.sb([128, 16], I32, tag="ji"); freq = K.sb([128, 16], F32, tag="freq")
        S.op("gpsimd", lambda e: e.iota(ji.t[:], pattern=[[1, 16]], base=0, channel_multiplier=0), writes=[ji.d])
        S.op("vector", lambda e: e.tensor_copy(out=freq.t[:], in_=ji.t[:]), reads=[ji.d], writes=[freq.d])
        S.op("scalar", lambda e: e.activation(out=freq.t[:], in_=freq.t[:], func=AF.Exp,
                                              scale=-math.log(10000.0) / 16.0), reads=[freq.d], writes=[freq.d])
        xin = K.sb([128, 1024], F32, tag="xin"); xsem = S.new_dma_sem()
        hf = K.sb([128, 1024], F32, tag="hf"); hmT = K.sb([128, 8, 128], BF16, tag="hmT")
        lat = K.sb([128, 800], F32, tag="lat"); sq = K.sb([128, 768], F32, tag="sq")
        ss = K.sb([128, 2], F32, tag="ss"); nrm = K.sb([128, 768], F32, tag="nrm"); nT = K.sb([128, 6, 128], BF16, tag="nT")
        posi = K.sb([128, 1], I32, tag="posi"); psem = S.new_dma_sem(); posf = K.sb([128, 1], F32, tag="posf")
        tt = K.sb([128, 32], F32, tag="tt"); ti = K.sb([128, 32], I32, tag="ti"); tf = K.sb([128, 32], F32, tag="tf")
        mm = K.sb([128, 32], F32, tag="mm"); scs = K.sb([128, 32], F32, tag="scs")
        qf = K.sb([128, 4, 96], F32, tag="qf"); kvf = K.sb([128, 4, 128], F32, tag="kvf")
        Qh = K.sb([128, 4, 96], F32, tag="Qh"); Kh = K.sb([128, 4, 96], F32, tag="Kh")
        ra = K.sb([128, 4, 16], F32, tag="ra"); rb = K.sb([128, 4, 16], F32, tag="rb")
        kr = K.sb([128, 32], F32, tag="kr")
        Va = K.sb([128, 4, 65], BF16, tag="Va")
        S.op("vector", lambda e: e.memset(Va.t[:], 1.0), writes=[Va.d])
        QTs = K.sb([128, 4, 128], BF16, tag="QTs"); KTs = K.sb([128, 4, 128], BF16, tag="KTs")
        qsem = S.new_dma_sem(); ksem = S.new_dma_sem(); vsem = S.new_dma_sem()
        b4 = lambda ap: ap.unsqueeze(1).to_broadcast([128, 4, 16])

        def rope(src1, src2, dst1, dst2, cosb, sinb, shape_bc):
            S.op("vector", lambda e: e.tensor_tensor(out=ra.t[:] if shape_bc else ra.t[:, 0, :], in0=src1, in1=cosb, op=ALU.mult),
                 reads=[qf.d, lat.d, scs.d], writes=[ra.d])
            S.op("vector", lambda e: e.tensor_tensor(out=rb.t[:] if shape_bc else rb.t[:, 0, :], in0=src2, in1=sinb, op=ALU.mult),
                 reads=[qf.d, lat.d, scs.d], writes=[rb.d])
            S.op("vector", lambda e: e.tensor_tensor(out=dst1, in0=ra.t[:] if shape_bc else ra.t[:, 0, :],
                                                     in1=rb.t[:] if shape_bc else rb.t[:, 0, :], op=ALU.subtract),
                 reads=[ra.d, rb.d], writes=[Qh.d, kr.d])
            S.op("vector", lambda e: e.tensor_tensor(out=ra.t[:] if shape_bc else ra.t[:, 0, :], in0=src2, in1=cosb, op=ALU.mult),
                 reads=[qf.d, lat.d, scs.d], writes=[ra.d])
            S.op("vector", lambda e: e.tensor_tensor(out=rb.t[:] if shape_bc else rb.t[:, 0, :], in0=src1, in1=sinb, op=ALU.mult),
                 reads=[qf.d, lat.d, scs.d], writes=[rb.d])
            S.op("vector", lambda e: e.tensor_tensor(out=dst2, in0=ra.t[:] if shape_bc else ra.t[:, 0, :],
                                                     in1=rb.t[:] if shape_bc else rb.t[:, 0, :], op=ALU.add),
                 reads=[ra.d, rb.d], writes=[Qh.d, kr.d])

        for t in range(NTL):
            S.dma("sync", xsem, xin.t[:], (x(t) if callable(x) else x[t * 128:(t + 1) * 128, :]), writes=[xin.d])
            with nc.allow_non_contiguous_dma(reason="positions column"):
                S.dma("sync", psem, posi.t[:], pos[t * 128:(t + 1) * 128].unsqueeze(1), writes=[posi.d])
            S.op("vector", lambda e: e.tensor_tensor(out=hf.t[:], in0=xin.t[:], in1=sc1p.t[:], op=ALU.mult),
                 reads=[xin.d, sc1p.d], writes=[hf.d])
            S.op("vector", lambda e: e.tensor_tensor(out=hf.t[:], in0=hf.t[:], in1=shm.t[:], op=ALU.add),
                 reads=[hf.d, shm.d], writes=[hf.d])
            for hb in range(2):
                fns = [(lambda e, k=k: e.transpose(out=P0.t[:, k * 128:(k + 1) * 128], in_=hf.t[:, k * 128:(k + 1) * 128],
                                                   identity=K.ident.t[:])) for k in range(hb * 4, hb * 4 + 4)]
                S.group("tensor", fns, reads=[hf.d, K.ident.d], writes=[P0.ds[hb]])
            S.op("scalar", lambda e: e.activation(out=hmT.t[:], in_=P0.t[:].rearrange("p (c t) -> p c t", c=8),
                                                  func=AF.Copy), reads=[P0.ds[0], P0.ds[1]], writes=[hmT.d])
            fns = [(lambda e, k=k: e.matmul(P1.t[:, 0:512], lhsT=hmT.t[:, k, :], rhs=win.t[:, k, 0:512], start=(k == 0),
                                            stop=(k == 7))) for k in range(8)]
            S.group("tensor", fns, reads=[hmT.d, win.d], writes=[P1.ds[0]])
            fns = [(lambda e, k=k: e.matmul(P1.t[:, 512:800], lhsT=hmT.t[:, k, :], rhs=win.t[:, k, 512:800], start=(k == 0),
                                            stop=(k == 7))) for k in range(8)]
            S.group("tensor", fns, reads=[hmT.d, win.d], writes=[P1.ds[1]])
            S.op("scalar", lambda e: e.activation(out=lat.t[:], in_=P1.t[:, 0:800], func=AF.Copy),
                 reads=[P1.ds[0], P1.ds[1]], writes=[lat.d])
            S.op("vector", lambda e: e.tensor_tensor(out=sq.t[:], in0=lat.t[:, 0:768], in1=lat.t[:, 0:768], op=ALU.mult),
                 reads=[lat.d], writes=[sq.d])
            S.op("vector", lambda e: e.tensor_reduce(out=ss.t[:, 0:1], in_=sq.t[:, 0:512], axis=X, op=ALU.add),
                 reads=[sq.d], writes=[ss.d])
            S.op("vector", lambda e: e.tensor_reduce(out=ss.t[:, 1:2], in_=sq.t[:, 512:768], axis=X, op=ALU.add),
                 reads=[sq.d], writes=[ss.d])
            S.op("scalar", lambda e: e.activation(out=ss.t[:, 0:1], in_=ss.t[:, 0:1], func=AF.Sqrt, bias=eps_r.t[:],
                                                  scale=1.0 / 512.0), reads=[ss.d, eps_r.d], writes=[ss.d])
            S.op("scalar", lambda e: e.activation(out=ss.t[:, 1:2], in_=ss.t[:, 1:2], func=AF.Sqrt, bias=eps_r.t[:],
                                                  scale=1.0 / 256.0), reads=[ss.d, eps_r.d], writes=[ss.d])
            S.op("vector", lambda e: e.reciprocal(out=ss.t[:], in_=ss.t[:]), reads=[ss.d], writes=[ss.d])
            S.op("vector", lambda e: e.scalar_tensor_tensor(out=nrm.t[:, 0:512], in0=lat.t[:, 0:512], scalar=ss.t[:, 0:1],
                                                            in1=nbc.t[:, 0:512], op0=ALU.mult, op1=ALU.mult),
                 reads=[lat.d, ss.d, nbc.d], writes=[nrm.d])
            S.op("vector", lambda e: e.scalar_tensor_tensor(out=nrm.t[:, 512:768], in0=lat.t[:, 512:768],
                                                            scalar=ss.t[:, 1:2], in1=nbc.t[:, 512:768], op0=ALU.mult,
                                                            op1=ALU.mult), reads=[lat.d, ss.d, nbc.d], writes=[nrm.d])
            for hb in range(2):
                rng_ = range(0, 4) if hb == 0 else range(4, 6)
                fns = [(lambda e, j=j: e.transpose(out=P0.t[:, j * 128:(j + 1) * 128], in_=nrm.t[:, j * 128:(j + 1) * 128],
                                                   identity=K.ident.t[:])) for j in rng_]
                S.group("tensor", fns, reads=[nrm.d, K.ident.d], writes=[P0.ds[hb]])
            S.op("scalar", lambda e: e.activation(out=nT.t[:], in_=P0.t[:, 0:768].rearrange("p (c t) -> p c t", c=6),
                                                  func=AF.Copy), reads=[P0.ds[0], P0.ds[1]], writes=[nT.d])
            fns = [(lambda e, c_=c_: e.matmul(P2.t[:, 0:384], lhsT=nT.t[:, c_, :], rhs=wuq.t[:, c_, :], start=(c_ == 0),
                                              stop=(c_ == 3))) for c_ in range(4)]
            S.group("tensor", fns, reads=[nT.d, wuq.d], writes=[P2.ds[0]])
            fns = [(lambda e, c_=c_: e.matmul(P2.t[:, 512:1024], lhsT=nT.t[:, 4 + c_, :], rhs=wukv.t[:, c_, :],
                                              start=(c_ == 0), stop=(c_ == 1))) for c_ in range(2)]
            S.group("tensor", fns, reads=[nT.d, wukv.d], writes=[P2.ds[1]])
            S.op("scalar", lambda e: e.activation(out=qf.t[:], in_=P2.t[:, 0:384].rearrange("p (h d) -> p h d", h=4),
                                                  func=AF.Copy), reads=[P2.ds[0]], writes=[qf.d])
            S.op("scalar", lambda e: e.activation(out=kvf.t[:], in_=P2.t[:, 512:1024].rearrange("p (h d) -> p h d", h=4),
                                                  func=AF.Copy), reads=[P2.ds[1]], writes=[kvf.d])
            S.op("vector", lambda e: e.tensor_copy(out=posf.t[:], in_=posi.t[:]), reads=[posi.d], writes=[posf.d])
            S.op("vector", lambda e: e.tensor_scalar(out=tt.t[:, 0:16], in0=freq.t[:], scalar1=posf.t[:],
                                                     scalar2=1.0 / TWO_PI, op0=ALU.mult, op1=ALU.mult),
                 reads=[freq.d, posf.d], writes=[tt.d])
            S.op("vector", lambda e: e.tensor_scalar(out=tt.t[:, 16:32], in0=tt.t[:, 0:16], scalar1=0.25, scalar2=None,
                                                     op0=ALU.add), reads=[tt.d], writes=[tt.d])
            S.op("vector", lambda e: e.tensor_copy(out=ti.t[:], in_=tt.t[:]), reads=[tt.d], writes=[ti.d])
            S.op("vector", lambda e: e.tensor_copy(out=tf.t[:], in_=ti.t[:]), reads=[ti.d], writes=[tf.d])
            S.op("vector", lambda e: e.tensor_tensor(out=tt.t[:], in0=tt.t[:], in1=tf.t[:], op=ALU.subtract),
                 reads=[tt.d, tf.d], writes=[tt.d])
            S.op("vector", lambda e: e.tensor_single_scalar(out=mm.t[:], in_=tt.t[:], scalar=0.5, op=ALU.is_gt),
                 reads=[tt.d], writes=[mm.d])
            S.op("vector", lambda e: e.tensor_tensor(out=tt.t[:], in0=tt.t[:], in1=mm.t[:], op=ALU.subtract),
                 reads=[tt.d, mm.d], writes=[tt.d])
            S.op("vector", lambda e: e.tensor_single_scalar(out=mm.t[:], in_=tt.t[:], scalar=-0.5, op=ALU.is_lt),
                 reads=[tt.d], writes=[mm.d])
            S.op("vector", lambda e: e.tensor_tensor(out=tt.t[:], in0=tt.t[:], in1=mm.t[:], op=ALU.add),
                 reads=[tt.d, mm.d], writes=[tt.d])
            S.op("scalar", lambda e: e.activation(out=scs.t[:], in_=tt.t[:], func=AF.Sin, scale=TWO_PI),
                 reads=[tt.d], writes=[scs.d])
            sinb, cosb = scs.t[:, 0:16], scs.t[:, 16:32]
            rope(qf.t[:, :, 64:80], qf.t[:, :, 80:96], Qh.t[:, :, 64:80], Qh.t[:, :, 80:96], b4(cosb), b4(sinb), True)
            rope(lat.t[:, 768:784], lat.t[:, 784:800], kr.t[:, 0:16], kr.t[:, 16:32], cosb, sinb, False)
            S.op("vector", lambda e: e.tensor_copy(out=Qh.t[:, :, 0:64], in_=qf.t[:, :, 0:64]), reads=[qf.d], writes=[Qh.d])
            S.op("vector", lambda e: e.tensor_copy(out=Kh.t[:, :, 0:64], in_=kvf.t[:, :, 0:64]), reads=[kvf.d], writes=[Kh.d])
            S.op("vector", lambda e: e.tensor_copy(out=Kh.t[:, :, 64:96], in_=kr.t[:].unsqueeze(1).to_broadcast([128, 4, 32])),
                 reads=[kr.d], writes=[Kh.d])
            S.op("vector", lambda e: e.tensor_copy(out=Va.t[:, :, 0:64], in_=kvf.t[:, :, 64:128]), reads=[kvf.d], writes=[Va.d])
            fns = [(lambda e, h=h: e.transpose(out=P3.t[0:96, h * 128:(h + 1) * 128], in_=Qh.t[:, h, :],
                                               identity=K.ident.t[:])) for h in range(4)]
            S.group("tensor", fns, reads=[Qh.d, K.ident.d], writes=[P3.ds[0]])
            fns = [(lambda e, h=h: e.transpose(out=P3.t[0:96, 512 + h * 128:512 + (h + 1) * 128], in_=Kh.t[:, h, :],
                                               identity=K.ident.t[:])) for h in range(4)]
            S.group("tensor", fns, reads=[Kh.d, K.ident.d], writes=[P3.ds[1]])
            S.op("scalar", lambda e: e.activation(out=QTs.t[0:96], in_=P3.t[0:96, 0:512].rearrange("p (h t) -> p h t", h=4),
                                                  func=AF.Copy), reads=[P3.ds[0]], writes=[QTs.d])
            S.op("scalar", lambda e: e.activation(out=KTs.t[0:96], in_=P3.t[0:96, 512:1024].rearrange("p (h t) -> p h t", h=4),
                                                  func=AF.Copy), reads=[P3.ds[1]], writes=[KTs.d])
            S.dma("gpsimd", qsem, QT_d[:, :, t * 128:(t + 1) * 128].rearrange("h d t -> d h t"), QTs.t[0:96], reads=[QTs.d])
            S.dma("gpsimd", ksem, KT_d[:, :, t * 128:(t + 1) * 128].rearrange("h d t -> d h t"), KTs.t[0:96], reads=[KTs.d])
            S.dma("gpsimd", vsem, V_d[t], Va.t[:], reads=[Va.d])
        S.barrier()
    with ExitStack() as sc:
        K.scope = sc
        KTh = K.sb([128, NTok], BF16, tag="KTh"); Vh = K.sb([128, NTL, 65], BF16, tag="Vh")
        khs = S.new_dma_sem(); vhs = S.new_dma_sem()
        QTt = [K.sb([128, 512], BF16, tag="QTt") for _ in range(2)]; qts = [S.new_dma_sem() for _ in range(2)]
        Pb = [K.sb([128, 512], BF16, tag="Pb") for _ in range(2)]
        mk = K.sb([128, 4, 512], BF16, tag="mk")
        mi = K.sb([128, 512], I32, tag="mi")
        for j in range(4):
            S.op("gpsimd", lambda e, j=j: e.iota(mi.t[:], pattern=[[1, 512]], base=-128 * j, channel_multiplier=-1),
                 writes=[mi.d])
            S.op("vector", lambda e, j=j: e.tensor_single_scalar(out=mk.t[:, j, :], in_=mi.t[:], scalar=0, op=ALU.is_ge),
                 reads=[mi.d], writes=[mk.d])
        sel = K.sb([128, 64], F32, tag="sel")
        S.op("vector", lambda e: e.memset(sel.t[:], 0.0), writes=[sel.d])
        S.op("vector", lambda e: e.memset(sel.t[64:65, :], 1.0), writes=[sel.d])
        Osb = K.sb([128, 512], F32, tag="Osb"); rec = K.sb([64, 512], F32, tag="rec")
        ob = [K.sb([64, 512], BF16, tag="ob") for _ in range(2)]; obs = [S.new_dma_sem() for _ in range(2)]
        it = 0
        for h in range(4):
            S.dma("sync", khs, KTh.t[0:96, :], KT_d[h], writes=[KTh.d])
            with nc.allow_non_contiguous_dma(reason="V rows of 130B"):
                S.dma("sync", vhs, Vh.t[:], V_d[:, :, h, :].rearrange("c p d -> p c d"), writes=[Vh.d])
            for qt in range(NTok // 512):
                qb = (h * (NTok // 512) + qt) % 2
                S.dma("sync", qts[qb], QTt[qb].t[0:96, :], QT_d[h][:, qt * 512:(qt + 1) * 512], writes=[QTt[qb].d])
                nkb = 4 * qt + 4

                def emit_s(kb, it_):
                    pb = Pb[it_ % 2]
                    S.op("tensor", lambda e, kb=kb, it_=it_, qb=qb: e.matmul(
                        P0.t[:, (it_ % 2) * 512:(it_ % 2 + 1) * 512], lhsT=KTh.t[0:96, kb * 128:(kb + 1) * 128],
                        rhs=QTt[qb].t[0:96, :], start=True, stop=True),
                         reads=[KTh.d, QTt[qb].d], writes=[P0.ds[it_ % 2]])
                    S.op("scalar", lambda e, it_=it_, pb=pb: e.activation(
                        out=pb.t[:], in_=P0.t[:, (it_ % 2) * 512:(it_ % 2 + 1) * 512], func=AF.Exp, scale=SCALE),
                         reads=[P0.ds[it_ % 2]], writes=[pb.d])
                    if kb >= 4 * qt:
                        S.op("vector", lambda e, pb=pb, j=kb - 4 * qt: e.tensor_tensor(
                            out=pb.t[:], in0=pb.t[:], in1=mk.t[:, j, :], op=ALU.mult), reads=[pb.d, mk.d], writes=[pb.d])

                def emit_pv(kb, it_):
                    pb = Pb[it_ % 2]
                    S.op("tensor", lambda e, kb=kb, pb=pb: e.matmul(
                        P1.t[0:65, 0:512], lhsT=Vh.t[:, kb, :], rhs=pb.t[:], start=(kb == 0), stop=(kb == nkb - 1)),
                         reads=[Vh.d, pb.d], writes=[P1.ds[0]])

                emit_s(0, it)
                for kb in range(nkb):
                    if kb + 1 < nkb:
                        emit_s(kb + 1, it + 1)
                    emit_pv(kb, it)
                    it += 1
                S.op("scalar", lambda e: e.activation(out=Osb.t[0:65, :], in_=P1.t[0:65, 0:512], func=AF.Copy),
                     reads=[P1.ds[0]], writes=[Osb.d])
                S.op("tensor", lambda e: e.matmul(P2.t[0:64, 0:512], lhsT=sel.t[0:65, :], rhs=Osb.t[0:65, :], start=True,
                                                  stop=True), reads=[sel.d, Osb.d], writes=[P2.ds[0]])
                S.op("scalar", lambda e: e.activation(out=rec.t[:], in_=P2.t[0:64, 0:512], func=AF.Copy),
                     reads=[P2.ds[0]], writes=[rec.d])
                S.op("vector", lambda e: e.reciprocal(out=rec.t[:], in_=rec.t[:]), reads=[rec.d], writes=[rec.d])
                S.op("vector", lambda e, qb=qb: e.tensor_tensor(out=ob[qb].t[:], in0=Osb.t[0:64, :], in1=rec.t[:],
                                                               op=ALU.mult), reads=[Osb.d, rec.d], writes=[ob[qb].d])
                S.dma("gpsimd", obs[qb], out[h * 64:(h + 1) * 64, qt * 512:(qt + 1) * 512], ob[qb].t[:], reads=[ob[qb].d])
        S.barrier()
    K.scope = old_scope
    K.S.release(_mk)


def build_tok(NT, steps):
    K = KB()
    x = K.din("x", [NT, D]); c = K.din("c", [D])
    y = K.dout("y", [NT, D])
    K.alloc_psum(); K.consts(); K.setup_cond(c)
    aw, ab, lnp = {}, {}, {}

    def layer_in(l):
        if l not in aw:
            aw[l] = K.din(f"aw{l}", [D, 6 * D]); ab[l] = K.din(f"ab{l}", [6 * D])
        return aw[l], ab[l]

    def ln_in(l, j):
        if (l, j) not in lnp:
            lnp[(l, j)] = (K.din(f"lng{l}_{j}", [D]), K.din(f"lnb{l}_{j}", [D]))
        return lnp[(l, j)]

    cur = DramStream(x, NT)
    for si, (kind, l, dm) in enumerate(steps):
        last = si == len(steps) - 1
        nxt = DramStream(y if last else K.dtmp(f"xs{si}", [NT, D]), NT)
        a_w, a_b = layer_in(l)
        if kind == "proj":
            g, b = ln_in(l, 0)
            mT = K.din(f"s{si}_mT", [dm, NT], BF16); wo = K.din(f"s{si}_wo", [dm, D])
            sub_proj(K, cur, nxt, NT, mT, dm, wo, a_w, a_b, g, b)
        elif kind == "ffn":
            g, b = ln_in(l, 1)
            wg = K.din(f"s{si}_wg", [D, DFF]); wu = K.din(f"s{si}_wu", [D, DFF]); wd = K.din(f"s{si}_wd", [DFF, D])
            sub_ffn(K, cur, nxt, NT, a_w, a_b, g, b, wg, wu, wd, None)
        elif kind == "moe":
            g, b = ln_in(l, 1)
            wg = K.din(f"s{si}_wg", [NE, D, DFF]); wu = K.din(f"s{si}_wu", [NE, D, DFF]); wd = K.din(f"s{si}_wd", [NE, DFF, D])
            wr = K.din(f"s{si}_wr", [D, NE])
            sub_ffn(K, cur, nxt, NT, a_w, a_b, g, b, wg, wu, wd, wr)
        elif kind == "sg":
            g, b = ln_in(l, 0)
            w_in = K.din(f"s{si}_win", [D, 4096]); b_in = K.din(f"s{si}_bin", [4096])
            sg_g = K.din(f"s{si}_sg", [2048]); sg_b = K.din(f"s{si}_sb", [2048])
            w_s = K.din(f"s{si}_ws", [8, 128, 128]); b_s = K.din(f"s{si}_bs", [8, 128]); wo = K.din(f"s{si}_wo", [2048, D])
            sub_sg(K, cur, nxt, NT, a_w, a_b, g, b, w_in, b_in, sg_g, sg_b, w_s, b_s, wo)
        cur = nxt
    K.S.finish()
    return K


def _ssd_sel(I, j, q):
    w = I['ssd_w_in'][j]
    cols = np.concatenate([np.arange(512 * q, 512 * q + 512), 2048 + np.arange(512 * q, 512 * q + 512),
                           4096 + np.arange(256 * q, 256 * q + 256), 4096 + 1024 + np.arange(256 * q, 256 * q + 256),
                           6144 + np.arange(8 * q, 8 * q + 8)])
    ccols = np.concatenate([np.arange(512 * q, 512 * q + 512), 2048 + np.arange(256 * q, 256 * q + 256),
                            2048 + 1024 + np.arange(256 * q, 256 * q + 256)])
    return dict(w=np.ascontiguousarray(w[:, cols]), cw=np.ascontiguousarray(I['ssd_conv_w'][j][:, ccols]),
                cb=np.ascontiguousarray(I['ssd_conv_b'][j][ccols]),
                dtb=np.ascontiguousarray(I['ssd_dt_bias'][j][8 * q:8 * q + 8]),
                alog=np.ascontiguousarray(I['ssd_a_log'][j][8 * q:8 * q + 8]),
                dsk=np.ascontiguousarray(I['ssd_d_skip'][j][8 * q:8 * q + 8]),
                nw=np.ascontiguousarray(I['ssd_norm_w'][j][512 * q:512 * q + 512]))


def _mla_sel(I, hq):
    uq = I['mla_w_uq'][0].reshape(512, 16, 96)[:, 4 * hq:4 * hq + 4].reshape(512, 384)
    ukv = I['mla_w_ukv'][0].reshape(256, 16, 128)[:, 4 * hq:4 * hq + 4].reshape(256, 512)
    return dict(w_in=I['mla_w_in'][0], qn=I['mla_q_norm'][0], kvn=I['mla_kv_norm'][0],
                wuq=np.ascontiguousarray(uq), wukv=np.ascontiguousarray(ukv))


def _run(K, in_maps):
    res = run_bass_kernel_spmd(K.nc, in_maps, core_ids=list(range(8)))
    return res.results


def kernel_unfused(**I):
    I = {k: np.asarray(v) for k, v in I.items()}
    B, SEQ = I['x'].shape[0], I['x'].shape[1]
    NT = SEQ // 4
    cores = [(k // 4, k % 4) for k in range(8)]

    def run_mixer_ssd(xcur, layer, j):
        K = build_ssd(SEQ)
        maps = [dict(x=xcur[b], c=I['c'][b], aw=I['ada_w'][layer], ab=I['ada_b'][layer], **_ssd_sel(I, j, q))
                for b, q in cores]
        r = _run(K, maps)
        return [np.concatenate([r[b * 4 + q]["mT"] for q in range(4)], 0) for b in range(B)]

    def run_mixer_mla(xcur, layer):
        K = build_mla(SEQ)
        maps = [dict(x=xcur[b], c=I['c'][b], aw=I['ada_w'][layer], ab=I['ada_b'][layer],
                     pos=np.ascontiguousarray(I['positions'][b].astype(np.int32)), **_mla_sel(I, q)) for b, q in cores]
        r = _run(K, maps)
        return [np.concatenate([r[b * 4 + q]["mT"] for q in range(4)], 0) for b in range(B)]

    def run_tok(xcur, steps, extra):
        K = build_tok(NT, steps)
        maps = []
        for b, r_ in cores:
            m = dict(x=np.ascontiguousarray(xcur[b][r_ * NT:(r_ + 1) * NT]), c=I['c'][b])
            for (kind, l, dm) in steps:
                m[f"aw{l}"] = I['ada_w'][l]; m[f"ab{l}"] = I['ada_b'][l]
                jj = 0 if kind in ("proj", "sg") else 1
                m[f"lng{l}_{jj}"] = I['ln_g'][l, jj]; m[f"lnb{l}_{jj}"] = I['ln_b'][l, jj]
            for k_, v in extra.items():
                m[k_] = v(b, r_) if callable(v) else v
            maps.append(m)
        r = _run(K, maps)
        return [np.concatenate([r[b * 4 + q]["y"] for q in range(4)], 0) for b in range(B)]

    def ffn_w(si, k):
        return {f"s{si}_wg": I['ffn_w_gate'][k], f"s{si}_wu": I['ffn_w_up'][k], f"s{si}_wd": I['ffn_w_down'][k]}

    def moe_w(si, k):
        return {f"s{si}_wg": I['moe_w_gate'][k], f"s{si}_wu": I['moe_w_up'][k], f"s{si}_wd": I['moe_w_down'][k],
                f"s{si}_wr": I['moe_w_router'][k]}

    def mt_slice(mT):
        return lambda b, r_: np.ascontiguousarray(mT[b][:, r_ * NT:(r_ + 1) * NT])

    xcur = [I['x'][b] for b in range(B)]
    mT = run_mixer_ssd(xcur, 0, 0)
    xcur = run_tok(xcur, [("proj", 0, 2048), ("ffn", 0, 0)],
                   {"s0_mT": mt_slice(mT), "s0_wo": I['ssd_w_out'][0], **ffn_w(1, 0)})
    mT = run_mixer_mla(xcur, 1)
    sgw = {"s2_win": I['sg_w_in'][0], "s2_bin": I['sg_b_in'][0], "s2_sg": I['sg_ln_g'][0], "s2_sb": I['sg_ln_b'][0],
           "s2_ws": I['sg_w_s'][0], "s2_bs": I['sg_b_s'][0], "s2_wo": I['sg_w_out'][0]}
    xcur = run_tok(xcur, [("proj", 1, 1024), ("moe", 1, 0), ("sg", 2, 0), ("ffn", 2, 0)],
                   {"s0_mT": mt_slice(mT), "s0_wo": I['mla_w_out'][0], **moe_w(1, 0), **sgw, **ffn_w(3, 1)})
    mT = run_mixer_ssd(xcur, 3, 1)
    xcur = run_tok(xcur, [("proj", 3, 2048), ("moe", 3, 0)],
                   {"s0_mT": mt_slice(mT), "s0_wo": I['ssd_w_out'][1], **moe_w(1, 1)})
    return np.stack(xcur, 0).astype(np.float32)


RG = [[0, 1, 2, 3], [4, 5, 6, 7]]


def collective(K, kind, in_ap, out_ap):
    S = K.S
    if not hasattr(K, "cc"):
        K.cc = S.new_dma_sem()
    S.barrier()
    op = ALU.add if kind == "ReduceScatter" else ALU.bypass
    ins = K.nc.gpsimd.collective_compute(kind, op, replica_groups=RG, ins=[in_ap], outs=[out_ap])
    K.cc.val += 1
    ins.then_inc(K.cc.sem)
    S.barrier()


def sub_pproj(K, mT_ap, DM, wo_ap, ypart_ap, NTok):
    S, nc = K.S, K.nc
    NC_ = DM // 128
    old_scope = K.scope
    _mk = K.S.mark()
    with ExitStack() as sc:
        K.scope = sc
        wout = K.sb([128, NC_, 1024], BF16, tag="wout")
        S.dma("gpsimd", S.new_dma_sem(), wout.t[:], wo_ap.rearrange("(c p) d -> p c d", p=128), writes=[wout.d])
        mt = [K.sb([128, NC_, 128], BF16, tag="mt") for _ in range(2)]
        msem = [S.new_dma_sem() for _ in range(2)]
        yo = [K.sb([128, 1024], F32, tag="yo") for _ in range(2)]
        osem = [S.new_dma_sem() for _ in range(2)]
        for ts in range(NTok // 128):
            b = ts % 2
            S.dma("sync", msem[b], mt[b].t[:], mT_ap[:, ts * 128:(ts + 1) * 128].rearrange("(c p) t -> p c t", p=128),
                  writes=[mt[b].d])
            po = K.psum[2 + b]
            for db in range(2):
                fns = [(lambda e, c=c, db=db, po=po, b=b: e.matmul(
                    po.t[:, db * 512:(db + 1) * 512], lhsT=mt[b].t[:, c, :], rhs=wout.t[:, c, db * 512:(db + 1) * 512],
                    start=(c == 0), stop=(c == NC_ - 1))) for c in range(NC_)]
                S.group("tensor", fns, reads=[mt[b].d, wout.d], writes=[po.ds[db]])
            S.op("scalar", lambda e, po=po, b=b: e.activation(out=yo[b].t[:], in_=po.t[:], func=AF.Copy),
                 reads=[po.ds[0], po.ds[1]], writes=[yo[b].d])
            S.dma("gpsimd", osem[b], ypart_ap(ts), yo[b].t[:], reads=[yo[b].d])
        S.barrier()
    K.scope = old_scope
    K.S.release(_mk)


def sub_resln(K, xs, xd, NT, y_ap, ada_w_l, ada_b_l, lng_ap, lnb_ap):
    S, nc = K.S, K.nc
    old_scope = K.scope
    _mk = K.S.mark()
    with ExitStack() as sc:
        K.scope = sc
        g1p = K.sb([128, 1024], F32, tag="g1p")
        lng = K.sb([128, 1024], F32, tag="lng")
        lnb = K.sb([128, 1024], F32, tag="lnb")
        K.ln_alloc()
        K.mod_vec(g1p, ada_w_l, ada_b_l, 2, True)
        K.bcast_vec(lng, lng_ap, S.new_dma_sem())
        K.bcast_vec(lnb, lnb_ap, S.new_dma_sem())
        yin = [K.sb([128, 1024], F32, tag="yin") for _ in range(2)]
        ysem = [S.new_dma_sem() for _ in range(2)]
        xin = [K.sb([128, 1024], F32, tag="xin") for _ in range(2)]
        xsem = [S.new_dma_sem() for _ in range(2)]
        tmp = [K.sb([128, 1024], F32, tag="tmp") for _ in range(2)]
        xo = [K.sb([128, 1024], F32, tag="xo") for _ in range(2)]
        osem = [S.new_dma_sem() for _ in range(2)]
        for ts in range(NT // 128):
            b = ts % 2
            S.dma("sync", ysem[b], yin[b].t[:], y_ap[ts * 128:(ts + 1) * 128, :], writes=[yin[b].d])
            S.dma("sync", xsem[b], xin[b].t[:], xs.ap[ts * 128:(ts + 1) * 128, :], reads=[xs.ds[ts]],
                  writes=[xin[b].d])
            residual_ln(K, yin[b].t[:], [yin[b].d], xin[b], g1p, lng, lnb, tmp[b], xo[b])
            S.dma("gpsimd", osem[b], xd.ap[ts * 128:(ts + 1) * 128, :], xo[b].t[:], reads=[xo[b].d],
                  writes=[xd.ds[ts]])
        S.barrier()
    K.scope = old_scope
    K.S.release(_mk)


def build_fused(SEQ):
    NT = SEQ // 4
    K = KB()
    S, nc = K.S, K.nc
    x_in = K.din("x", [NT, D]); c = K.din("c", [D]); pos = K.din("pos", [SEQ], I32)
    aw = [K.din(f"aw{l}", [D, 6 * D]) for l in range(4)]
    ab = [K.din(f"ab{l}", [6 * D]) for l in range(4)]
    lng = [[K.din(f"lng{l}_{j}", [D]) for j in range(2)] for l in range(4)]
    lnb = [[K.din(f"lnb{l}_{j}", [D]) for j in range(2)] for l in range(4)]
    ssd = []
    for j in range(2):
        ssd.append(dict(w=K.din(f"ssd{j}_w", [D, 1544]), cw=K.din(f"ssd{j}_cw", [4, 1024]),
                        cbv=K.din(f"ssd{j}_cb", [1024]), dtb=K.din(f"ssd{j}_dtb", [8]),
                        alog=K.din(f"ssd{j}_alog", [8]), dsk=K.din(f"ssd{j}_dsk", [8]),
                        nw=K.din(f"ssd{j}_nw", [512]), wo=K.din(f"ssd{j}_wo", [512, D])))
    mla = dict(w_in=K.din("mla_w_in", [D, 800]), qn=K.din("mla_qn", [512]), kvn=K.din("mla_kvn", [256]),
               wuq_d=K.din("mla_wuq", [512, 384]), wukv_d=K.din("mla_wukv", [256, 512]))
    mla_wo = K.din("mla_wo", [256, D])
    sg = dict(w_in=K.din("sg_win", [D, 4096]), b_in=K.din("sg_bin", [4096]), sg_g=K.din("sg_g", [2048]),
              sg_b=K.din("sg_b", [2048]), w_s=K.din("sg_ws", [8, 128, 128]), b_s=K.din("sg_bs", [8, 128]),
              wo=K.din("sg_wo", [2048, D]))
    ffn = [dict(wg=K.din(f"ffn{k}_wg", [D, DFF]), wu=K.din(f"ffn{k}_wu", [D, DFF]), wd=K.din(f"ffn{k}_wd", [DFF, D]))
           for k in range(2)]
    moe = [dict(wg=K.din(f"moe{k}_wg", [NE, D, DFF]), wu=K.din(f"moe{k}_wu", [NE, D, DFF]),
                wd=K.din(f"moe{k}_wd", [NE, DFF, D]), wr=K.din(f"moe{k}_wr", [D, NE])) for k in range(2)]
    y = K.dout("y", [NT, D])
    K.alloc_psum(); K.consts(); K.setup_cond(c)
    CH = 256
    NCH = NT // CH
    xfull_c = K.dtmp("xfull", [NCH, 4 * CH, D])
    ypart_c = K.dtmp("ypart", [NCH, 4 * CH, D]); yred = K.dtmp("yred", [NT, D])

    def rows(buf):
        def f(tile):
            t = tile * 128
            r, rem = t // NT, t % NT
            ch, i = rem // CH, rem % CH
            return buf[ch, r * CH + i:r * CH + i + 128, :]
        return f

    xfull = rows(xfull_c)
    ypart = rows(ypart_c)

    def collectives(kind, pairs):
        if not hasattr(K, "cc"):
            K.cc = S.new_dma_sem()
        S.barrier()
        op = ALU.add if kind == "ReduceScatter" else ALU.bypass
        for a, b_ in pairs:
            ins = nc.gpsimd.collective_compute(kind, op, replica_groups=RG, ins=[a], outs=[b_])
            K.cc.val += 1
            ins.then_inc(K.cc.sem)
        S.barrier()

    def gather_x(src):
        collectives("AllGather", [(src[ch * CH:(ch + 1) * CH, :], xfull_c[ch]) for ch in range(NCH)])

    def scatter_y():
        collectives("ReduceScatter", [(ypart_c[ch], yred[ch * CH:(ch + 1) * CH, :]) for ch in range(NCH)])
    mTs = K.dtmp("mTs", [512, SEQ], BF16); mTm = K.dtmp("mTm", [256, SEQ], BF16)
    xl = [K.dtmp(f"xl{i}", [NT, D]) for i in range(8)]

    def stream(ap):
        return DramStream(ap, NT)

    cps = S.new_dma_sem()
    for ch in range(NCH):
        S.dma("sync", cps, xl[0][ch * CH:(ch + 1) * CH, :], x_in[ch * CH:(ch + 1) * CH, :])
    gather_x(xl[0])
    s = ssd[0]
    sub_ssd(K, xfull, mTs, SEQ, aw[0], ab[0], s["w"], s["cw"], s["cbv"], s["dtb"], s["alog"], s["dsk"], s["nw"])
    sub_pproj(K, mTs, 512, s["wo"], ypart, SEQ)
    scatter_y()
    sub_resln(K, stream(xl[0]), stream(xl[1]), NT, yred, aw[0], ab[0], lng[0][0], lnb[0][0])
    f = ffn[0]
    sub_ffn(K, stream(xl[1]), stream(xl[2]), NT, aw[0], ab[0], lng[0][1], lnb[0][1], f["wg"], f["wu"], f["wd"], None)
    gather_x(xl[2])
    sub_mla(K, xfull, mTm, SEQ, aw[1], ab[1], pos, **mla)
    sub_pproj(K, mTm, 256, mla_wo, ypart, SEQ)
    scatter_y()
    sub_resln(K, stream(xl[2]), stream(xl[3]), NT, yred, aw[1], ab[1], lng[1][0], lnb[1][0])
    m = moe[0]
    sub_ffn(K, stream(xl[3]), stream(xl[4]), NT, aw[1], ab[1], lng[1][1], lnb[1][1], m["wg"], m["wu"], m["wd"], m["wr"])
    sub_sg(K, stream(xl[4]), stream(xl[5]), NT, aw[2], ab[2], lng[2][0], lnb[2][0], sg["w_in"], sg["b_in"], sg["sg_g"],
           sg["sg_b"], sg["w_s"], sg["b_s"], sg["wo"])
    f = ffn[1]
    sub_ffn(K, stream(xl[5]), stream(xl[6]), NT, aw[2], ab[2], lng[2][1], lnb[2][1], f["wg"], f["wu"], f["wd"], None)
    gather_x(xl[6])
    s = ssd[1]
    sub_ssd(K, xfull, mTs, SEQ, aw[3], ab[3], s["w"], s["cw"], s["cbv"], s["dtb"], s["alog"], s["dsk"], s["nw"])
    sub_pproj(K, mTs, 512, s["wo"], ypart, SEQ)
    scatter_y()
    sub_resln(K, stream(xl[6]), stream(xl[7]), NT, yred, aw[3], ab[3], lng[3][0], lnb[3][0])
    m = moe[1]
    sub_ffn(K, stream(xl[7]), stream(y), NT, aw[3], ab[3], lng[3][1], lnb[3][1], m["wg"], m["wu"], m["wd"], m["wr"])
    S.finish()
    return K


def fused_inputs(I, SEQ, b, q):
    NT = SEQ // 4
    m = dict(x=np.ascontiguousarray(I['x'][b][q * NT:(q + 1) * NT]), c=np.ascontiguousarray(I['c'][b]),
             pos=np.ascontiguousarray(I['positions'][b].astype(np.int32)))
    for l in range(4):
        m[f"aw{l}"] = I['ada_w'][l]; m[f"ab{l}"] = I['ada_b'][l]
        for j in range(2):
            m[f"lng{l}_{j}"] = I['ln_g'][l, j]; m[f"lnb{l}_{j}"] = I['ln_b'][l, j]
    for j in range(2):
        s = _ssd_sel(I, j, q)
        for k_, v in s.items():
            m[f"ssd{j}_{k_}"] = v
        m[f"ssd{j}_wo"] = np.ascontiguousarray(I['ssd_w_out'][j][512 * q:512 * q + 512])
    s = _mla_sel(I, q)
    m["mla_w_in"] = s["w_in"]; m["mla_qn"] = s["qn"]; m["mla_kvn"] = s["kvn"]; m["mla_wuq"] = s["wuq"]; m["mla_wukv"] = s["wukv"]
    m["mla_wo"] = np.ascontiguousarray(I['mla_w_out'][0][256 * q:256 * q + 256])
    m["sg_win"] = I['sg_w_in'][0]; m["sg_bin"] = I['sg_b_in'][0]; m["sg_g"] = I['sg_ln_g'][0]; m["sg_b"] = I['sg_ln_b'][0]
    m["sg_ws"] = I['sg_w_s'][0]; m["sg_bs"] = I['sg_b_s'][0]; m["sg_wo"] = I['sg_w_out'][0]
    for k in range(2):
        m[f"ffn{k}_wg"] = I['ffn_w_gate'][k]; m[f"ffn{k}_wu"] = I['ffn_w_up'][k]; m[f"ffn{k}_wd"] = I['ffn_w_down'][k]
        m[f"moe{k}_wg"] = I['moe_w_gate'][k]; m[f"moe{k}_wu"] = I['moe_w_up'][k]; m[f"moe{k}_wd"] = I['moe_w_down'][k]
        m[f"moe{k}_wr"] = I['moe_w_router'][k]
    return m


def kernel(**I):
    I = {k: np.asarray(v) for k, v in I.items()}
    B, SEQ = I['x'].shape[0], I['x'].shape[1]
    NT = SEQ // 4
    K = build_fused(SEQ)
    maps = [fused_inputs(I, SEQ, k // 4, k % 4) for k in range(8)]
    r = _run(K, maps)
    return np.stack([np.concatenate([r[b * 4 + q]["y"] for q in range(4)], 0) for b in range(B)], 0).astype(np.float32)
```

```python
import math
from contextlib import ExitStack

import numpy as np
import concourse.bass as bass
import concourse.mybir as mybir
from concourse.bass_utils import run_bass_kernel_spmd

F32 = mybir.dt.float32
BF16 = mybir.dt.bfloat16
I32 = mybir.dt.int32
AF = mybir.ActivationFunctionType
ALU = mybir.AluOpType

D = 1024
DFF = 2816
NE = 8
ALPHA = 8.0 ** 0.25
LN_EPS = 1e-5
RMS_EPS = 1e-6


class Dep:
    __slots__ = ("w", "r")

    def __init__(self):
        self.w = None
        self.r = {}


class DmaSem:
    def __init__(self, sem):
        self.sem = sem
        self.val = 0


class Eng:
    def __init__(self, name, engine, sem):
        self.name = name
        self.e = engine
        self.sem = sem
        self.count = 0
        self.seen = {}


class Sync:
    def __init__(self, nc, stack):
        self.nc = nc
        self.stack = stack
        self.engs = {}
        for name in ("tensor", "vector", "scalar", "gpsimd", "sync"):
            sem = stack.enter_context(nc.semaphore("s_" + name))
            self.engs[name] = Eng(name, getattr(nc, name), sem)
        self.dma_sems = []
        self.n_inst = 0

    def new_dma_sem(self):
        if getattr(self, "free", None):
            d = self.free.pop()
        else:
            sem = self.stack.enter_context(self.nc.semaphore(None))
            d = DmaSem(sem)
            self.dma_sems.append(d)
        if not hasattr(self, "handed"):
            self.handed = []
            self.free = []
        self.handed.append(d)
        return d

    def mark(self):
        if not hasattr(self, "handed"):
            self.handed = []
            self.free = []
        return len(self.handed)

    def release(self, mk):
        self.free.extend(self.handed[mk:])
        del self.handed[mk:]

    def _waits(self, eng, reads, writes):
        need = {}
        for d in reads:
            if d.w is not None and need.get(d.w[0], 0) < d.w[1]:
                need[d.w[0]] = d.w[1]
        for d in writes:
            if d.w is not None and need.get(d.w[0], 0) < d.w[1]:
                need[d.w[0]] = d.w[1]
            for k, v in d.r.items():
                if need.get(k, 0) < v:
                    need[k] = v
        for k, v in need.items():
            if eng.seen.get(k, 0) < v:
                eng.e.wait_ge(k, v)
                eng.seen[k] = v

    def _mark(self, key, val, reads, writes):
        for d in reads:
            d.r[key] = val
        for d in writes:
            d.w = (key, val)
            d.r = {}

    def op(self, en, fn, reads=(), writes=()):
        eng = self.engs[en]
        self._waits(eng, reads, writes)
        ins = fn(eng.e)
        eng.count += 1
        ins.then_inc(eng.sem, 1)
        self.n_inst += 1
        self._mark(eng.sem, eng.count, reads, writes)

    def group(self, en, fns, reads=(), writes=()):
        eng = self.engs[en]
        self._waits(eng, reads, writes)
        ins = None
        for fn in fns:
            ins = fn(eng.e)
            self.n_inst += 1
        eng.count += 1
        ins.then_inc(eng.sem, 1)
        self._mark(eng.sem, eng.count, reads, writes)

    def dma(self, en, dsem, out, in_, reads=(), writes=(), **kw):
        eng = self.engs[en]
        self._waits(eng, reads, writes)
        ins = eng.e.dma_start(out=out, in_=in_, **kw)
        dsem.val += 16
        ins.then_inc(dsem.sem, 16)
        self.n_inst += 1
        self._mark(dsem.sem, dsem.val, reads, writes)

    def barrier(self):
        for eng in self.engs.values():
            for e2 in self.engs.values():
                if e2.count > 0 and eng.seen.get(e2.sem, 0) < e2.count:
                    eng.e.wait_ge(e2.sem, e2.count)
                    eng.seen[e2.sem] = e2.count
            for d in self.dma_sems:
                if d.val > 0 and eng.seen.get(d.sem, 0) < d.val:
                    eng.e.wait_ge(d.sem, d.val)
                    eng.seen[d.sem] = d.val

    def finish(self):
        self.barrier()


class Buf:
    def __init__(self, t, n=1):
        self.t = t
        self.ds = [Dep() for _ in range(n)]

    @property
    def d(self):
        return self.ds[0]


class KB:
    def __init__(self):
        self.nc = bass.Bass("TRN2", target_bir_lowering=False)
        self.st = ExitStack()
        self.S = Sync(self.nc, self.st)
        self.scope = self.st
        self._n = 0
        self.psum = None

    def name(self, p):
        self._n += 1
        return f"{p}{self._n}"

    def din(self, name, shape, dt=F32):
        return self.nc.dram_tensor(name, list(shape), dt, kind="ExternalInput").ap()

    def dout(self, name, shape, dt=F32):
        return self.nc.dram_tensor(name, list(shape), dt, kind="ExternalOutput").ap()

    def dtmp(self, name, shape, dt=F32):
        return self.nc.dram_tensor(name, list(shape), dt, kind="Internal").ap()

    def sb(self, shape, dt, n=1, tag="t"):
        t = self.scope.enter_context(self.nc.sbuf_tensor(self.name(tag), list(shape), dt))
        return Buf(t, n)

    def alloc_psum(self):
        self.psum = [Buf(self.st.enter_context(self.nc.psum_tensor(f"ps{i}", [128, 1024], F32)), 2)
                     for i in range(4)]

    def consts(self):
        S, nc = self.S, self.nc
        ii = self.sb([128, 128], I32, tag="ii")
        self.ident = self.sb([128, 128], F32, tag="ident")
        S.op("gpsimd", lambda e: e.iota(ii.t[:], pattern=[[1, 128]], base=0, channel_multiplier=-1),
             writes=[ii.d])
        S.op("vector", lambda e: e.tensor_single_scalar(out=self.ident.t[:], in_=ii.t[:], scalar=0,
                                                        op=ALU.is_equal), reads=[ii.d], writes=[self.ident.d])
        self.eps_ln = self.sb([128, 1], F32, tag="eps")
        S.op("vector", lambda e: e.memset(self.eps_ln.t[:], LN_EPS), writes=[self.eps_ln.d])

    def setup_cond(self, c_ap):
        S, nc = self.S, self.nc
        ccol = self.sb([128, 8], F32, tag="ccol")
        self.cbc = self.sb([128, 8, 128], F32, tag="cbc")
        self.modw = self.sb([128, 8, 512], F32, tag="modw")
        self.modb = self.sb([128, 1024], F32, tag="modb")
        self.sem_c = S.new_dma_sem()
        self.sem_mw = S.new_dma_sem()
        self.sem_mb = S.new_dma_sem()
        with nc.allow_non_contiguous_dma(reason="tiny column load"):
            S.dma("sync", self.sem_c, ccol.t[:], c_ap.rearrange("(c p) -> p c", p=128), writes=[ccol.d])
        S.op("scalar", lambda e: e.activation(out=ccol.t[:], in_=ccol.t[:], func=AF.Silu),
             reads=[ccol.d], writes=[ccol.d])
        S.op("vector", lambda e: e.tensor_copy(out=self.cbc.t[:],
                                               in_=ccol.t[:].unsqueeze(2).to_broadcast([128, 8, 128])),
             reads=[ccol.d], writes=[self.cbc.d])

    def mod_vec(self, out_buf, ada_w_l, ada_b_l, idx, plus_one):
        S = self.S
        ps = self.psum[0]
        S.dma("sync", self.sem_mb, self.modb.t[:],
              ada_b_l[idx * 1024:(idx + 1) * 1024].unsqueeze(0).to_broadcast([128, 1024]),
              writes=[self.modb.d])
        for hb in range(2):
            c0 = idx * 1024 + hb * 512
            S.dma("sync", self.sem_mw, self.modw.t[:],
                  ada_w_l[:, c0:c0 + 512].rearrange("(c p) f -> p c f", p=128), writes=[self.modw.d])
            fns = [(lambda e, k=k: e.matmul(ps.t[:, hb * 512:(hb + 1) * 512], lhsT=self.cbc.t[:, k, :],
                                             rhs=self.modw.t[:, k, :], start=(k == 0), stop=(k == 7)))
                   for k in range(8)]
            S.group("tensor", fns, reads=[self.cbc.d, self.modw.d], writes=[ps.ds[hb]])
        if plus_one:
            S.op("vector", lambda e: e.scalar_tensor_tensor(out=out_buf.t[:], in0=ps.t[:], scalar=1.0,
                                                            in1=self.modb.t[:], op0=ALU.add, op1=ALU.add),
                 reads=[ps.ds[0], ps.ds[1], self.modb.d], writes=[out_buf.d])
        else:
            S.op("vector", lambda e: e.tensor_tensor(out=out_buf.t[:], in0=ps.t[:], in1=self.modb.t[:],
                                                     op=ALU.add),
                 reads=[ps.ds[0], ps.ds[1], self.modb.d], writes=[out_buf.d])

    def bcast_vec(self, out_buf, vec_ap, sem):
        n = vec_ap.shape[0]
        self.S.dma("sync", sem, out_buf.t[:], vec_ap.unsqueeze(0).to_broadcast([128, n]), writes=[out_buf.d])

    def ln_alloc(self):
        self.ln_st = self.sb([128, 2, 6], F32, tag="lnst")
        self.ln_mv = self.sb([128, 2], F32, tag="lnmv")
        self.ln_rs = self.sb([128, 1], F32, tag="lnrs")

    def layer_norm(self, r, out, g_bc, b_bc):
        S = self.S
        st, mv, rs = self.ln_st, self.ln_mv, self.ln_rs
        for hb in range(2):
            S.op("vector", lambda e, hb=hb: e.bn_stats(out=st.t[:, hb, :], in_=r.t[:, hb * 512:(hb + 1) * 512]),
                 reads=[r.d], writes=[st.d])
        S.op("vector", lambda e: e.bn_aggr(out=mv.t[:], in_=st.t[:].rearrange("p a b -> p (a b)")),
             reads=[st.d], writes=[mv.d])
        S.op("scalar", lambda e: e.activation(out=rs.t[:], in_=mv.t[:, 1:2], func=AF.Sqrt,
                                              bias=self.eps_ln.t[:], scale=1.0),
             reads=[mv.d, self.eps_ln.d], writes=[rs.d])
        S.op("vector", lambda e: e.reciprocal(out=rs.t[:], in_=rs.t[:]), reads=[rs.d], writes=[rs.d])
        S.op("vector", lambda e: e.tensor_scalar(out=r.t[:], in0=r.t[:], scalar1=mv.t[:, 0:1], scalar2=rs.t[:],
                                                 op0=ALU.subtract, op1=ALU.mult),
             reads=[r.d, mv.d, rs.d], writes=[r.d])
        S.op("vector", lambda e: e.tensor_tensor(out=r.t[:], in0=r.t[:], in1=g_bc.t[:], op=ALU.mult),
             reads=[r.d, g_bc.d], writes=[r.d])
        S.op("vector", lambda e: e.tensor_tensor(out=out.t[:], in0=r.t[:], in1=b_bc.t[:], op=ALU.add),
             reads=[r.d, b_bc.d], writes=[out.d])


class DramStream:
    def __init__(self, ap, nt):
        self.ap = ap
        self.ds = [Dep() for _ in range(nt // 128)]


def sub_ffn(K, xs, xd, NT, ada_w_l, ada_b_l, lng_ap, lnb_ap, wg_ap, wu_ap, wd_ap, wr_ap=None):
    S, nc = K.S, K.nc
    moe = wr_ap is not None
    import os as _os
    E = int(_os.environ.get('MOE_E', NE)) if moe else 1
    T = min(NT, 2048)
    NSUP, NSUB, NB = NT // T, T // 128, T // 512
    JG = 2
    NG = DFF // (128 * JG)
    old_scope = K.scope
    _mk = K.S.mark()
    with ExitStack() as sc:
        K.scope = sc
        xT = K.sb([128, 8, T], BF16, n=NB, tag="xT")
        acc = K.sb([128, NSUB, 1024], F32, n=NSUB, tag="acc")
        wgb = [K.sb([128, 8, 128 * JG], BF16, tag="wg") for _ in range(2)]
        wub = [K.sb([128, 8, 128 * JG], BF16, tag="wu") for _ in range(2)]
        wdb = [K.sb([128, JG, 1024], BF16, tag="wd") for _ in range(2)]
        wsem = [S.new_dma_sem() for _ in range(2)]
        hT = [K.sb([128, JG, 512], BF16, n=JG, tag="hT") for _ in range(2)]
        sg = [K.sb([128, 512], F32, tag="sg") for _ in range(2)]
        xin = [K.sb([128, 1024], F32, tag="xin") for _ in range(2)]
        xsem = [S.new_dma_sem() for _ in range(2)]
        hf = [K.sb([128, 1024], F32, tag="hf") for _ in range(2)]
        xo = [K.sb([128, 1024], F32, tag="xo") for _ in range(2)]
        osem = [S.new_dma_sem() for _ in range(2)]
        sc1p = K.sb([128, 1024], F32, tag="sc1p")
        shf = K.sb([128, 1024], F32, tag="shf")
        g1p = K.sb([128, 1024], F32, tag="g1p")
        lng = K.sb([128, 1024], F32, tag="lng")
        lnb = K.sb([128, 1024], F32, tag="lnb")
        csem = S.new_dma_sem()
        K.ln_alloc()
        K.mod_vec(shf, ada_w_l, ada_b_l, 3, False)
        K.mod_vec(sc1p, ada_w_l, ada_b_l, 4, True)
        K.mod_vec(g1p, ada_w_l, ada_b_l, 5, True)
        K.bcast_vec(lng, lng_ap, csem)
        K.bcast_vec(lnb, lnb_ap, S.new_dma_sem())
        if moe:
            hfT32 = K.sb([128, 8, 128], F32, tag="hfT32")
            wr = K.sb([128, 8, NE], F32, tag="wr")
            if _os.environ.get("DBG4") != "1":
                with nc.allow_non_contiguous_dma(reason="router weights, tiny"):
                    S.dma("sync", S.new_dma_sem(), wr.t[:], wr_ap.rearrange("(c p) e -> p c e", p=128), writes=[wr.d])
            comb = K.sb([128, NSUB, NE], F32, tag="comb")
            lgall = K.sb([128, NSUB, NE], F32, tag="lgall")
            l2 = K.sb([128, NSUB, NE], F32, tag="l2")
            mk1 = K.sb([128, NSUB, NE], F32, tag="mk1")
            mk2 = K.sb([128, NSUB, NE], F32, tag="mk2")
            m1 = K.sb([128, NSUB], F32, tag="m1")
            m2 = K.sb([128, NSUB], F32, tag="m2")
            w1 = K.sb([128, NSUB], F32, tag="w1")
            w2 = K.sb([128, NSUB], F32, tag="w2")
        psG = [K.psum[0], K.psum[1]]
        psO = [K.psum[2], K.psum[3]]

        def wsrc(ap, e):
            return ap[e % ap.shape[0]] if moe else ap

        def load_w(e, jg, slot):
            f0 = jg * 128 * JG
            S.dma("gpsimd", wsem[slot], wgb[slot].t[:],
                  wsrc(wg_ap, e)[:, f0:f0 + 128 * JG].rearrange("(c p) f -> p c f", p=128), writes=[wgb[slot].d])
            S.dma("gpsimd", wsem[slot], wub[slot].t[:],
                  wsrc(wu_ap, e)[:, f0:f0 + 128 * JG].rearrange("(c p) f -> p c f", p=128), writes=[wub[slot].d])
            S.dma("gpsimd", wsem[slot], wdb[slot].t[:],
                  wsrc(wd_ap, e)[f0:f0 + 128 * JG, :].rearrange("(j p) d -> p j d", p=128), writes=[wdb[slot].d])
            wgb[slot].d.w = wub[slot].d.w = wdb[slot].d.w

        wlist = [(e, jg) for e in range(E) for jg in range(NG)]
        for sup in range(NSUP):
            t0 = sup * T
            load_w(*wlist[0], 0)
            for ts in range(NSUB):
                b = ts % 2
                gi = (t0 // 128) + ts
                S.dma("sync", xsem[b], xin[b].t[:], xs.ap[gi * 128:(gi + 1) * 128, :],
                      reads=[xs.ds[gi]], writes=[xin[b].d])
                S.op("vector", lambda e, b=b: e.tensor_tensor(out=hf[b].t[:], in0=xin[b].t[:], in1=sc1p.t[:],
                                                              op=ALU.mult),
                     reads=[xin[b].d, sc1p.d], writes=[hf[b].d])
                S.op("vector", lambda e, b=b: e.tensor_tensor(out=hf[b].t[:], in0=hf[b].t[:], in1=shf.t[:],
                                                              op=ALU.add),
                     reads=[hf[b].d, shf.d], writes=[hf[b].d])
                po = psO[b]
                for hb in range(2):
                    fns = [(lambda e, k=k, b=b, po=po: e.transpose(out=po.t[:, k * 128:(k + 1) * 128],
                                                                    in_=hf[b].t[:, k * 128:(k + 1) * 128],
                                                                    identity=K.ident.t[:]))
                           for k in range(hb * 4, hb * 4 + 4)]
                    S.group("tensor", fns, reads=[hf[b].d, K.ident.d], writes=[po.ds[hb]])
                if not moe:
                    S.op("scalar", lambda e, po=po, ts=ts: e.activation(
                        out=xT.t[:, :, ts * 128:(ts + 1) * 128], in_=po.t[:].rearrange("p (c t) -> p c t", c=8),
                        func=AF.Copy), reads=[po.ds[0], po.ds[1]], writes=[xT.ds[ts // 4]])
                else:
                    S.op("scalar", lambda e, po=po: e.activation(
                        out=hfT32.t[:], in_=po.t[:].rearrange("p (c t) -> p c t", c=8), func=AF.Copy),
                         reads=[po.ds[0], po.ds[1]], writes=[hfT32.d])
                    S.op("gpsimd", lambda e, ts=ts: e.tensor_copy(out=xT.t[:, :, ts * 128:(ts + 1) * 128],
                                                                  in_=hfT32.t[:]),
                         reads=[hfT32.d], writes=[xT.ds[ts // 4]])
                if moe:
                    pl = psG[0]
                    fns = [(lambda e, k=k, pl=pl: e.matmul(pl.t[:, 0:NE], lhsT=hfT32.t[:, k, :], rhs=wr.t[:, k, :],
                                                           start=(k == 0), stop=(k == 7))) for k in range(8)]
                    import os as _os
                    if _os.environ.get("MOEDBG") == "1":
                        S.op("vector", lambda e, ts=ts: e.memset(lgall.t[:, ts, :], 0.0), writes=[lgall.d])
                    else:
                        S.group("tensor", fns, reads=[hfT32.d, wr.d], writes=[pl.ds[0]])
                        S.op("vector", lambda e, pl=pl, ts=ts: e.tensor_copy(out=lgall.t[:, ts, :], in_=pl.t[:, 0:NE]),
                             reads=[pl.ds[0]], writes=[lgall.d])
            if moe and _os.environ.get('DBG3') == '1':
                S.op('vector', lambda e: e.memset(comb.t[:], 0.125), writes=[comb.d])
            elif moe:
                X = mybir.AxisListType.X
                bc = lambda b_: b_.t[:].unsqueeze(2).to_broadcast([128, NSUB, NE])
                S.op("vector", lambda e: e.tensor_reduce(out=m1.t[:], in_=lgall.t[:], axis=X, op=ALU.max),
                     reads=[lgall.d], writes=[m1.d])
                S.op("vector", lambda e: e.tensor_tensor(out=mk1.t[:], in0=lgall.t[:], in1=bc(m1), op=ALU.is_equal),
                     reads=[lgall.d, m1.d], writes=[mk1.d])
                S.op("vector", lambda e: e.scalar_tensor_tensor(out=l2.t[:], in0=mk1.t[:], scalar=-1e30,
                                                                in1=lgall.t[:], op0=ALU.mult, op1=ALU.add),
                     reads=[mk1.d, lgall.d], writes=[l2.d])
                S.op("vector", lambda e: e.tensor_reduce(out=m2.t[:], in_=l2.t[:], axis=X, op=ALU.max),
                     reads=[l2.d], writes=[m2.d])
                S.op("vector", lambda e: e.tensor_tensor(out=mk2.t[:], in0=l2.t[:], in1=bc(m2), op=ALU.is_equal),
                     reads=[l2.d, m2.d], writes=[mk2.d])
                S.op("vector", lambda e: e.tensor_tensor(out=w2.t[:], in0=m2.t[:], in1=m1.t[:], op=ALU.subtract),
                     reads=[m1.d, m2.d], writes=[w2.d])
                S.op("scalar", lambda e: e.activation(out=w2.t[:], in_=w2.t[:], func=AF.Exp),
                     reads=[w2.d], writes=[w2.d])
                S.op("vector", lambda e: e.tensor_scalar(out=w1.t[:], in0=w2.t[:], scalar1=1.0, scalar2=None,
                                                         op0=ALU.add), reads=[w2.d], writes=[w1.d])
                S.op("vector", lambda e: e.reciprocal(out=w1.t[:], in_=w1.t[:]), reads=[w1.d], writes=[w1.d])
                S.op("vector", lambda e: e.tensor_tensor(out=w2.t[:], in0=w2.t[:], in1=w1.t[:], op=ALU.mult),
                     reads=[w1.d, w2.d], writes=[w2.d])
                S.op("vector", lambda e: e.tensor_tensor(out=mk1.t[:], in0=mk1.t[:], in1=bc(w1), op=ALU.mult),
                     reads=[mk1.d, w1.d], writes=[mk1.d])
                S.op("vector", lambda e: e.tensor_tensor(out=mk2.t[:], in0=mk2.t[:], in1=bc(w2), op=ALU.mult),
                     reads=[mk2.d, w2.d], writes=[mk2.d])
                S.op("vector", lambda e: e.tensor_tensor(out=comb.t[:], in0=mk1.t[:], in1=mk2.t[:], op=ALU.add),
                     reads=[mk1.d, mk2.d], writes=[comb.d])
            for ts in range(NSUB):
                S.op("gpsimd", lambda e, ts=ts: e.memset(acc.t[:, ts, :], 0.0), writes=[acc.ds[ts]])
            its = [(wi, tb) for wi in range(len(wlist)) for tb in range(NB)]

            def emit_gu(n, js):
                wi, tb = its[n]
                slot = wi % 2
                hb_ = hT[n % 2]
                for j in js:
                    pg = psG[j % 2]
                    for which, wbuf in ((0, wgb[slot]), (1, wub[slot])):
                        fns = [(lambda e, k=k, j=j, which=which, wbuf=wbuf, pg=pg, tb=tb: e.matmul(
                            pg.t[:, which * 512:(which + 1) * 512], lhsT=wbuf.t[:, k, j * 128:(j + 1) * 128],
                            rhs=xT.t[:, k, tb * 512:(tb + 1) * 512], start=(k == 0), stop=(k == 7)))
                            for k in range(8)]
                        S.group("tensor", fns, reads=[wbuf.d, xT.ds[tb]], writes=[pg.ds[which]])
                    sgb = sg[j % 2]
                    S.op("scalar", lambda e, pg=pg, sgb=sgb: e.activation(out=sgb.t[:], in_=pg.t[:, 0:512],
                                                                         func=AF.Silu),
                         reads=[pg.ds[0]], writes=[sgb.d])
                    S.op("vector", lambda e, pg=pg, sgb=sgb, hb_=hb_, j=j: e.tensor_tensor(
                        out=hb_.t[:, j, :], in0=sgb.t[:], in1=pg.t[:, 512:1024], op=ALU.mult),
                         reads=[sgb.d, pg.ds[1]], writes=[hb_.ds[j]])

            def emit_d(n, qs):
                wi, tb = its[n]
                slot = wi % 2
                e_ = wlist[wi][0]
                hb_ = hT[n % 2]
                for q in qs:
                    ts = tb * 4 + q
                    po = psO[q % 2]
                    for db in range(2):
                        fns = [(lambda e, j=j, q=q, db=db, po=po, hb_=hb_, slot=slot: e.matmul(
                            po.t[:, db * 512:(db + 1) * 512], lhsT=hb_.t[:, j, q * 128:(q + 1) * 128],
                            rhs=wdb[slot].t[:, j, db * 512:(db + 1) * 512], start=(j == 0),
                            stop=(j == JG - 1))) for j in range(JG)]
                        S.group("tensor", fns, reads=[hb_.ds[0], hb_.ds[1], wdb[slot].d], writes=[po.ds[db]])
                    cs = comb.t[:, ts, e_:e_ + 1] if (moe and _os.environ.get('DBG2') != '1') else 1.0
                    rd = [po.ds[0], po.ds[1], acc.ds[ts]] + ([comb.d] if moe else [])
                    S.op("vector", lambda e, po=po, ts=ts, cs=cs: e.scalar_tensor_tensor(
                        out=acc.t[:, ts, :], in0=po.t[:], scalar=cs, in1=acc.t[:, ts, :], op0=ALU.mult,
                        op1=ALU.add), reads=rd, writes=[acc.ds[ts]])

            for n in range(len(its)):
                wi, tb = its[n]
                emit_gu(n, (0,))
                if n > 0:
                    emit_d(n - 1, (0, 1))
                emit_gu(n, (1,))
                if n > 0:
                    emit_d(n - 1, (2, 3))
                if tb == 0 and wi + 1 < len(wlist):
                    load_w(*wlist[wi + 1], (wi + 1) % 2)
            emit_d(len(its) - 1, (0, 1, 2, 3))
            for ts in range(NSUB):
                b = ts % 2
                gi = (t0 // 128) + ts
                S.dma("sync", xsem[b], xin[b].t[:], xs.ap[gi * 128:(gi + 1) * 128, :],
                      reads=[xs.ds[gi]], writes=[xin[b].d])
                S.op("vector", lambda e, ts=ts, b=b: e.tensor_tensor(out=hf[b].t[:], in0=acc.t[:, ts, :],
                                                                    in1=g1p.t[:], op=ALU.mult),
                     reads=[acc.ds[ts], g1p.d], writes=[hf[b].d])
                S.op("vector", lambda e, b=b: e.scalar_tensor_tensor(out=hf[b].t[:], in0=xin[b].t[:], scalar=ALPHA,
                                                                     in1=hf[b].t[:], op0=ALU.mult, op1=ALU.add),
                     reads=[xin[b].d, hf[b].d], writes=[hf[b].d])
                K.layer_norm(hf[b], xo[b], lng, lnb)
                S.dma("gpsimd", osem[b], xd.ap[gi * 128:(gi + 1) * 128, :], xo[b].t[:],
                      reads=[xo[b].d], writes=[xd.ds[gi]])
        S.barrier()
    K.scope = old_scope
    K.S.release(_mk)


def residual_ln(K, y_ap, y_deps, xin, g1p, lng, lnb, tmp, xo):
    S = K.S
    S.op("vector", lambda e: e.tensor_tensor(out=tmp.t[:], in0=y_ap, in1=g1p.t[:], op=ALU.mult),
         reads=list(y_deps) + [g1p.d], writes=[tmp.d])
    S.op("vector", lambda e: e.scalar_tensor_tensor(out=tmp.t[:], in0=xin.t[:], scalar=ALPHA, in1=tmp.t[:],
                                                    op0=ALU.mult, op1=ALU.add),
         reads=[xin.d, tmp.d], writes=[tmp.d])
    K.layer_norm(tmp, xo, lng, lnb)


def sub_proj(K, xs, xd, NT, mT_ap, DM, wout_ap, ada_w_l, ada_b_l, lng_ap, lnb_ap):
    S, nc = K.S, K.nc
    NC_ = DM // 128
    old_scope = K.scope
    _mk = K.S.mark()
    with ExitStack() as sc:
        K.scope = sc
        wout = K.sb([128, NC_, 1024], BF16, tag="wout")
        S.dma("gpsimd", S.new_dma_sem(), wout.t[:], wout_ap.rearrange("(c p) d -> p c d", p=128), writes=[wout.d])
        g1p = K.sb([128, 1024], F32, tag="g1p")
        lng = K.sb([128, 1024], F32, tag="lng")
        lnb = K.sb([128, 1024], F32, tag="lnb")
        K.ln_alloc()
        K.mod_vec(g1p, ada_w_l, ada_b_l, 2, True)
        K.bcast_vec(lng, lng_ap, S.new_dma_sem())
        K.bcast_vec(lnb, lnb_ap, S.new_dma_sem())
        mt = [K.sb([128, NC_, 128], BF16, tag="mt") for _ in range(2)]
        msem = [S.new_dma_sem() for _ in range(2)]
        xin = [K.sb([128, 1024], F32, tag="xin") for _ in range(2)]
        xsem = [S.new_dma_sem() for _ in range(2)]
        tmp = [K.sb([128, 1024], F32, tag="tmp") for _ in range(2)]
        xo = [K.sb([128, 1024], F32, tag="xo") for _ in range(2)]
        osem = [S.new_dma_sem() for _ in range(2)]
        for ts in range(NT // 128):
            b = ts % 2
            S.dma("sync", msem[b], mt[b].t[:], mT_ap[:, ts * 128:(ts + 1) * 128].rearrange("(c p) t -> p c t", p=128),
                  writes=[mt[b].d])
            S.dma("sync", xsem[b], xin[b].t[:], xs.ap[ts * 128:(ts + 1) * 128, :], reads=[xs.ds[ts]],
                  writes=[xin[b].d])
            po = K.psum[2 + b]
            for db in range(2):
                fns = [(lambda e, c=c, db=db, po=po, b=b: e.matmul(
                    po.t[:, db * 512:(db + 1) * 512], lhsT=mt[b].t[:, c, :], rhs=wout.t[:, c, db * 512:(db + 1) * 512],
                    start=(c == 0), stop=(c == NC_ - 1))) for c in range(NC_)]
                S.group("tensor", fns, reads=[mt[b].d, wout.d], writes=[po.ds[db]])
            residual_ln(K, po.t[:], po.ds, xin[b], g1p, lng, lnb, tmp[b], xo[b])
            S.dma("gpsimd", osem[b], xd.ap[ts * 128:(ts + 1) * 128, :], xo[b].t[:], reads=[xo[b].d],
                  writes=[xd.ds[ts]])
        S.barrier()
    K.scope = old_scope
    K.S.release(_mk)


def sub_sg(K, xs, xd, NT, ada_w_l, ada_b_l, lng_ap, lnb_ap, w_in_ap, b_in_ap, sglng_ap, sglnb_ap, w_s_ap, b_s_ap,
           w_out_ap):
    S, nc = K.S, K.nc
    old_scope = K.scope
    _mk = K.S.mark()
    with ExitStack() as sc:
        K.scope = sc
        win = K.sb([128, 8, 4096], BF16, tag="win")
        S.dma("gpsimd", S.new_dma_sem(), win.t[:, :, 0:2048],
              w_in_ap[:, 0:2048].rearrange("(c p) f -> p c f", p=128), writes=[win.d])
        S.dma("gpsimd", S.new_dma_sem(), win.t[:, :, 2048:4096],
              w_in_ap[:, 2048:4096].rearrange("(c p) f -> p c f", p=128), writes=[win.d])
        wout = K.sb([128, 16, 1024], BF16, tag="wout")
        S.dma("gpsimd", S.new_dma_sem(), wout.t[:], w_out_ap.rearrange("(c p) d -> p c d", p=128), writes=[wout.d])
        binb = K.sb([1, 4096], BF16, tag="binb")
        S.dma("gpsimd", S.new_dma_sem(), binb.t[:], b_in_ap.unsqueeze(0), writes=[binb.d])
        bsb = K.sb([1, 8, 128], BF16, tag="bsb")
        S.dma("gpsimd", S.new_dma_sem(), bsb.t[:], b_s_ap.unsqueeze(0), writes=[bsb.d])
        ones = K.sb([1, 512], BF16, tag="ones")
        S.op("vector", lambda e: e.memset(ones.t[:], 1.0), writes=[ones.d])
        sglng = K.sb([128, 2048], F32, tag="sglng")
        sglnb = K.sb([128, 2048], F32, tag="sglnb")
        K.bcast_vec(sglng, sglng_ap, S.new_dma_sem())
        K.bcast_vec(sglnb, sglnb_ap, S.new_dma_sem())
        sc1p = K.sb([128, 1024], F32, tag="sc1p")
        shm = K.sb([128, 1024], F32, tag="shm")
        g1p = K.sb([128, 1024], F32, tag="g1p")
        lng = K.sb([128, 1024], F32, tag="lng")
        lnb = K.sb([128, 1024], F32, tag="lnb")
        K.ln_alloc()
        K.mod_vec(shm, ada_w_l, ada_b_l, 0, False)
        K.mod_vec(sc1p, ada_w_l, ada_b_l, 1, True)
        K.mod_vec(g1p, ada_w_l, ada_b_l, 2, True)
        K.bcast_vec(lng, lng_ap, S.new_dma_sem())
        K.bcast_vec(lnb, lnb_ap, S.new_dma_sem())
        wmT = K.sb([128, 8, 128], BF16, tag="wmT")
        sc_setup = ExitStack()
        K.scope = sc_setup
        wsf = K.sb([128, 8, 128], F32, tag="wsf")
        S.dma("sync", S.new_dma_sem(), wsf.t[:], w_s_ap.rearrange("g t s -> t g s"), writes=[wsf.d])
        ii = K.sb([128, 128], I32, tag="ii2")
        msk = K.sb([128, 128], F32, tag="msk")
        S.op("gpsimd", lambda e: e.iota(ii.t[:], pattern=[[1, 128]], base=0, channel_multiplier=-1), writes=[ii.d])
        S.op("vector", lambda e: e.tensor_single_scalar(out=msk.t[:], in_=ii.t[:], scalar=0, op=ALU.is_le),
             reads=[ii.d], writes=[msk.d])
        S.op("vector", lambda e: e.tensor_tensor(out=wsf.t[:], in0=wsf.t[:],
                                                 in1=msk.t[:].unsqueeze(1).to_broadcast([128, 8, 128]), op=ALU.mult),
             reads=[wsf.d, msk.d], writes=[wsf.d])
        pt = K.psum[0]
        for hb in range(2):
            fns = [(lambda e, g=g: e.transpose(out=pt.t[:, g * 128:(g + 1) * 128], in_=wsf.t[:, g, :],
                                               identity=K.ident.t[:])) for g in range(hb * 4, hb * 4 + 4)]
            S.group("tensor", fns, reads=[wsf.d, K.ident.d], writes=[pt.ds[hb]])
        S.op("scalar", lambda e: e.activation(out=wmT.t[:], in_=pt.t[:].rearrange("p (g t) -> p g t", g=8),
                                              func=AF.Copy), reads=[pt.ds[0], pt.ds[1]], writes=[wmT.d])
        S.barrier()
        sc_setup.close()
        K.scope = sc
        xin = K.sb([128, 1024], F32, tag="xin")
        xsem = S.new_dma_sem()
        hf = K.sb([128, 1024], F32, tag="hf")
        hmT = K.sb([128, 8, 128], BF16, tag="hmT")
        u = K.sb([128, 2048], F32, tag="u")
        v = K.sb([128, 2048], F32, n=1, tag="v")
        vn = K.sb([128, 2048], BF16, tag="vn")
        gT = Buf(vn.t, 1)
        gT.ds = vn.ds
        gTv = vn.t[:].rearrange("p (c t) -> p c t", c=16)
        xo = hf
        osem = S.new_dma_sem()
        st4 = K.sb([128, 4, 6], F32, tag="st4")
        mv = K.sb([128, 2], F32, tag="mv2")
        rs = K.sb([128, 1], F32, tag="rs2")
        for ts in range(NT // 128):
            S.dma("sync", xsem, xin.t[:], xs.ap[ts * 128:(ts + 1) * 128, :], reads=[xs.ds[ts]], writes=[xin.d])
            S.op("vector", lambda e: e.tensor_tensor(out=hf.t[:], in0=xin.t[:], in1=sc1p.t[:], op=ALU.mult),
                 reads=[xin.d, sc1p.d], writes=[hf.d])
            S.op("vector", lambda e: e.tensor_tensor(out=hf.t[:], in0=hf.t[:], in1=shm.t[:], op=ALU.add),
                 reads=[hf.d, shm.d], writes=[hf.d])
            po = K.psum[0]
            for hb in range(2):
                fns = [(lambda e, k=k: e.transpose(out=po.t[:, k * 128:(k + 1) * 128],
                                                   in_=hf.t[:, k * 128:(k + 1) * 128], identity=K.ident.t[:]))
                       for k in range(hb * 4, hb * 4 + 4)]
                S.group("tensor", fns, reads=[hf.d, K.ident.d], writes=[po.ds[hb]])
            S.op("scalar", lambda e: e.activation(out=hmT.t[:], in_=po.t[:].rearrange("p (c t) -> p c t", c=8),
                                                  func=AF.Copy), reads=[po.ds[0], po.ds[1]], writes=[hmT.d])
            for cb in range(8):
                pb = K.psum[(cb // 2) % 2 + 0]
                half = cb % 2
                fns = [(lambda e, k=k, cb=cb, pb=pb, half=half: e.matmul(
                    pb.t[:, half * 512:(half + 1) * 512], lhsT=hmT.t[:, k, :], rhs=win.t[:, k, cb * 512:(cb + 1) * 512],
                    start=(k == 0), stop=False)) for k in range(8)]
                fns.append(lambda e, cb=cb, pb=pb, half=half: e.matmul(
                    pb.t[:, half * 512:(half + 1) * 512], lhsT=ones.t[0:1, 0:128], rhs=binb.t[0:1, cb * 512:(cb + 1) * 512],
                    start=False, stop=True))
                S.group("tensor", fns, reads=[hmT.d, win.d, ones.d, binb.d], writes=[pb.ds[half]])
                dst = u if cb < 4 else v
                c0 = (cb % 4) * 512
                S.op("scalar", lambda e, pb=pb, half=half, dst=dst, c0=c0: e.activation(
                    out=dst.t[:, c0:c0 + 512], in_=pb.t[:, half * 512:(half + 1) * 512], func=AF.Gelu_apprx_tanh),
                     reads=[pb.ds[half]], writes=[dst.d])
            for q in range(4):
                S.op("vector", lambda e, q=q: e.bn_stats(out=st4.t[:, q, :], in_=v.t[:, q * 512:(q + 1) * 512]),
                     reads=[v.d], writes=[st4.d])
            S.op("vector", lambda e: e.bn_aggr(out=mv.t[:], in_=st4.t[:].rearrange("p a b -> p (a b)")),
                 reads=[st4.d], writes=[mv.d])
            S.op("scalar", lambda e: e.activation(out=rs.t[:], in_=mv.t[:, 1:2], func=AF.Sqrt, bias=K.eps_ln.t[:],
                                                  scale=1.0), reads=[mv.d, K.eps_ln.d], writes=[rs.d])
            S.op("vector", lambda e: e.reciprocal(out=rs.t[:], in_=rs.t[:]), reads=[rs.d], writes=[rs.d])
            S.op("vector", lambda e: e.tensor_scalar(out=v.t[:], in0=v.t[:], scalar1=mv.t[:, 0:1], scalar2=rs.t[:],
                                                     op0=ALU.subtract, op1=ALU.mult),
                 reads=[v.d, mv.d, rs.d], writes=[v.d])
            S.op("vector", lambda e: e.tensor_tensor(out=v.t[:], in0=v.t[:], in1=sglng.t[:], op=ALU.mult),
                 reads=[v.d, sglng.d], writes=[v.d])
            S.op("vector", lambda e: e.tensor_tensor(out=vn.t[:], in0=v.t[:], in1=sglnb.t[:], op=ALU.add),
                 reads=[v.d, sglnb.d], writes=[vn.d])
            for g in range(8):
                pm = K.psum[2 + g // 4]
                half = (g % 4) // 2
                c0 = (g % 4) * 256
                fns = [lambda e, g=g, pm=pm, c0=c0: e.matmul(pm.t[:, c0:c0 + 256], lhsT=wmT.t[:, g, :],
                                                              rhs=vn.t[:, g * 256:(g + 1) * 256], start=True, stop=False),
                       lambda e, g=g, pm=pm, c0=c0: e.matmul(pm.t[:, c0:c0 + 256], lhsT=bsb.t[0:1, g, :],
                                                              rhs=ones.t[0:1, 0:256], start=False, stop=True)]
                S.group("tensor", fns, reads=[wmT.d, vn.d, bsb.d, ones.d], writes=[pm.ds[half]])
            for h2 in range(2):
                pm = K.psum[2 + h2]
                S.op("vector", lambda e, pm=pm, h2=h2: e.tensor_tensor(
                    out=v.t[:, h2 * 1024:(h2 + 1) * 1024], in0=pm.t[:], in1=u.t[:, h2 * 1024:(h2 + 1) * 1024],
                    op=ALU.mult), reads=[pm.ds[0], pm.ds[1], u.d], writes=[v.d])
            for h2 in range(2):
                pt2 = K.psum[h2]
                for hb in range(2):
                    fns = [(lambda e, c=c, pt2=pt2, h2=h2: e.transpose(
                        out=pt2.t[:, (c % 8) * 128:(c % 8 + 1) * 128], in_=v.t[:, c * 128:(c + 1) * 128],
                        identity=K.ident.t[:])) for c in range(h2 * 8 + hb * 4, h2 * 8 + hb * 4 + 4)]
                    S.group("tensor", fns, reads=[v.d, K.ident.d], writes=[pt2.ds[hb]])
                S.op("scalar", lambda e, pt2=pt2, h2=h2: e.activation(
                    out=gTv[:, h2 * 8:(h2 + 1) * 8, :], in_=pt2.t[:].rearrange("p (c t) -> p c t", c=8),
                    func=AF.Copy), reads=[pt2.ds[0], pt2.ds[1]], writes=[gT.d])
            py = K.psum[2]
            for db in range(2):
                fns = [(lambda e, c=c, db=db: e.matmul(py.t[:, db * 512:(db + 1) * 512], lhsT=gTv[:, c, :],
                                                        rhs=wout.t[:, c, db * 512:(db + 1) * 512], start=(c == 0),
                                                        stop=(c == 15))) for c in range(16)]
                S.group("tensor", fns, reads=[gT.d, wout.d], writes=[py.ds[db]])
            residual_ln(K, py.t[:], py.ds, xin, g1p, lng, lnb, hf, xo)
            S.dma("gpsimd", osem, xd.ap[ts * 128:(ts + 1) * 128, :], xo.t[:], reads=[xo.d], writes=[xd.ds[ts]])
        S.barrier()
    K.scope = old_scope
    K.S.release(_mk)


def build_ssd(NTok):
    K = KB()
    x = K.din("x", [NTok, D]); c = K.din("c", [D]); aw = K.din("aw", [D, 6 * D]); ab = K.din("ab", [6 * D])
    w = K.din("w", [D, 1544]); cw = K.din("cw", [4, 1024]); cbv = K.din("cb", [1024])
    dtb = K.din("dtb", [8]); alog = K.din("alog", [8]); dsk = K.din("dsk", [8]); nw = K.din("nw", [512])
    out = K.dout("mT", [512, NTok], BF16)
    K.alloc_psum(); K.consts(); K.setup_cond(c)
    sub_ssd(K, x, out, NTok, aw, ab, w, cw, cbv, dtb, alog, dsk, nw)
    K.S.finish()
    return K


def sub_ssd(K, x, out, NTok, aw, ab, w, cw, cbv, dtb, alog, dsk, nw):
    S, nc = K.S, K.nc
    X = mybir.AxisListType.X
    P0, P1, P2, P3 = K.psum
    old_scope = K.scope
    _mk = K.S.mark()
    sc_ssd = ExitStack()
    K.scope = sc_ssd
    wz = K.sb([128, 8, 512], BF16, tag="wz")
    wx = K.sb([128, 8, 1024], BF16, tag="wx")
    wdt = K.sb([128, 8, 8], BF16, tag="wdt")
    S.dma("gpsimd", S.new_dma_sem(), wz.t[:], w[:, 0:512].rearrange("(c p) f -> p c f", p=128), writes=[wz.d])
    S.dma("gpsimd", S.new_dma_sem(), wx.t[:], w[:, 512:1536].rearrange("(c p) f -> p c f", p=128), writes=[wx.d])
    with nc.allow_non_contiguous_dma(reason="tiny"):
        S.dma("gpsimd", S.new_dma_sem(), wdt.t[:], w[:, 1536:1544].rearrange("(c p) f -> p c f", p=128),
              writes=[wdt.d])
    cwT = K.sb([128, 4, 8], F32, tag="cwT")
    cbT = K.sb([128, 8], F32, tag="cbT")
    with nc.allow_non_contiguous_dma(reason="tiny"):
        S.dma("sync", S.new_dma_sem(), cwT.t[:], cw.rearrange("k (c p) -> p k c", p=128), writes=[cwT.d])
        S.dma("sync", S.new_dma_sem(), cbT.t[:], cbv.rearrange("(c p) -> p c", p=128), writes=[cbT.d])
    dtb_bc = K.sb([128, 8], F32, tag="dtb"); A_bc = K.sb([128, 8], F32, tag="A"); dsk_bc = K.sb([128, 8], F32, tag="dsk")
    nw_bc = K.sb([128, 512], F32, tag="nw")
    K.bcast_vec(dtb_bc, dtb, S.new_dma_sem()); K.bcast_vec(A_bc, alog, S.new_dma_sem())
    K.bcast_vec(dsk_bc, dsk, S.new_dma_sem()); K.bcast_vec(nw_bc, nw, S.new_dma_sem())
    S.op("scalar", lambda e: e.activation(out=A_bc.t[:], in_=A_bc.t[:], func=AF.Exp), reads=[A_bc.d], writes=[A_bc.d])
    S.op("vector", lambda e: e.tensor_scalar(out=A_bc.t[:], in0=A_bc.t[:], scalar1=-1.0, scalar2=None, op0=ALU.mult),
         reads=[A_bc.d], writes=[A_bc.d])
    sc1p = K.sb([128, 1024], F32, tag="sc1p"); shm = K.sb([128, 1024], F32, tag="shm")
    K.mod_vec(shm, aw, ab, 0, False)
    K.mod_vec(sc1p, aw, ab, 1, True)
    ii = K.sb([128, 128], I32, tag="ii3")
    triU = K.sb([128, 128], F32, tag="triU")
    ones = K.sb([128, 128], F32, tag="ones")
    eps_r = K.sb([128, 1], F32, tag="epsr")
    S.op("gpsimd", lambda e: e.iota(ii.t[:], pattern=[[1, 128]], base=0, channel_multiplier=-1), writes=[ii.d])
    S.op("vector", lambda e: e.tensor_single_scalar(out=triU.t[:], in_=ii.t[:], scalar=0, op=ALU.is_ge),
         reads=[ii.d], writes=[triU.d])
    S.op("vector", lambda e: e.memset(ones.t[:], 1.0), writes=[ones.d])
    S.op("vector", lambda e: e.memset(eps_r.t[:], RMS_EPS), writes=[eps_r.d])
    S32 = K.sb([128, 8, 64], F32, tag="S32"); Sbf = K.sb([128, 8, 64], BF16, tag="Sbf")
    S.op("vector", lambda e: e.memset(S32.t[:], 0.0), writes=[S32.d])
    S.op("vector", lambda e: e.memset(Sbf.t[:], 0.0), writes=[Sbf.d])
    xr = K.sb([128, 8, 131], F32, tag="xr")
    S.op("vector", lambda e: e.memset(xr.t[:], 0.0), writes=[xr.d])
    xin2 = [K.sb([128, 1024], F32, tag="xin") for _ in range(2)]; xsem2 = [S.new_dma_sem() for _ in range(2)]
    hf2 = [K.sb([128, 1024], F32, tag="hf") for _ in range(2)]
    hmT2 = [K.sb([128, 8, 128], BF16, tag="hmT") for _ in range(2)]
    cacc = K.sb([128, 8, 128], F32, tag="cacc"); ctmp = K.sb([128, 8, 128], F32, tag="ctmp")
    xa = K.sb([128, 8, 128], F32, tag="xa")
    bcT = K.sb([128, 4, 128], BF16, tag="bcT")
    xtok = K.sb([128, 768], F32, tag="xtok")
    btok = K.sb([128, 256], BF16, tag="btok")
    dtv = K.sb([128, 8], F32, tag="dtv"); dtA = K.sb([128, 8], F32, tag="dtA")
    dtAb = K.sb([128, 8, 128], F32, tag="dtAb")
    acs = K.sb([128, 24], F32, tag="acs")
    dte = K.sb([128, 8], F32, tag="dte"); cd = K.sb([128, 8], F32, tag="cd")
    Lx = K.sb([128, 8, 128], F32, tag="Lx"); Eb = K.sb([128, 8, 128], F32, tag="Eb")
    cbm = K.sb([128, 2, 128], F32, tag="cbm")
    MT = K.sb([128, 8, 128], BF16, tag="MT"); CsT = K.sb([128, 8, 128], BF16, tag="CsT")
    xdt = K.sb([128, 8, 64], BF16, tag="xdt"); xdte = K.sb([128, 8, 64], BF16, tag="xdte")
    y = K.sb([128, 512], F32, tag="y"); sz = K.sb([128, 512], F32, tag="sz"); sq = K.sb([128, 512], F32, tag="sq")
    ss = K.sb([128, 2], F32, tag="ss")
    ygT = K.sb([128, 4, 128], BF16, tag="ygT"); osem = S.new_dma_sem()
    bc3 = lambda ap, n: ap.unsqueeze(2).to_broadcast([128, 8, n])

    def head(ck):
        xin, hf, hmT, xsem = xin2[ck % 2], hf2[ck % 2], hmT2[ck % 2], xsem2[ck % 2]
        S.dma("sync", xsem, xin.t[:], (x(ck) if callable(x) else x[ck * 128:(ck + 1) * 128, :]), writes=[xin.d])
        S.op("vector", lambda e: e.tensor_tensor(out=hf.t[:], in0=xin.t[:], in1=sc1p.t[:], op=ALU.mult),
             reads=[xin.d, sc1p.d], writes=[hf.d])
        S.op("vector", lambda e: e.tensor_tensor(out=hf.t[:], in0=hf.t[:], in1=shm.t[:], op=ALU.add),
             reads=[hf.d, shm.d], writes=[hf.d])
        for hb in range(2):
            fns = [(lambda e, k=k: e.transpose(out=P0.t[:, k * 128:(k + 1) * 128], in_=hf.t[:, k * 128:(k + 1) * 128],
                                               identity=K.ident.t[:])) for k in range(hb * 4, hb * 4 + 4)]
            S.group("tensor", fns, reads=[hf.d, K.ident.d], writes=[P0.ds[hb]])
        S.op("scalar", lambda e: e.activation(out=hmT.t[:], in_=P0.t[:].rearrange("p (c t) -> p c t", c=8),
                                              func=AF.Copy), reads=[P0.ds[0], P0.ds[1]], writes=[hmT.d])

    NCK = NTok // 128
    head(0)
    for ck in range(NCK):
        hmT = hmT2[ck % 2]
        fns = [(lambda e, k=k: e.matmul(P1.t[:, 0:512], lhsT=hmT.t[:, k, :], rhs=wz.t[:, k, :], start=(k == 0),
                                        stop=(k == 7))) for k in range(8)]
        S.group("tensor", fns, reads=[hmT.d, wz.d], writes=[P1.ds[0]])
        fns = [(lambda e, k=k: e.matmul(P1.t[:, 512:520], lhsT=hmT.t[:, k, :], rhs=wdt.t[:, k, :], start=(k == 0),
                                        stop=(k == 7))) for k in range(8)]
        S.group("tensor", fns, reads=[hmT.d, wdt.d], writes=[P1.ds[1]])
        for hb in range(2):
            fns = []
            for ch in range(hb * 4, hb * 4 + 4):
                fns += [(lambda e, k=k, ch=ch: e.matmul(P2.t[:, ch * 128:(ch + 1) * 128],
                                                        lhsT=wx.t[:, k, ch * 128:(ch + 1) * 128], rhs=hmT.t[:, k, :],
                                                        start=(k == 0), stop=(k == 7))) for k in range(8)]
            S.group("tensor", fns, reads=[hmT.d, wx.d], writes=[P2.ds[hb]])
        S.op("scalar", lambda e: e.activation(out=xr.t[:, :, 3:131], in_=P2.t[:].rearrange("p (c t) -> p c t", c=8),
                                              func=AF.Copy), reads=[P2.ds[0], P2.ds[1]], writes=[xr.d])
        S.op("vector", lambda e: e.tensor_tensor(out=dtv.t[:], in0=P1.t[:, 512:520], in1=dtb_bc.t[:], op=ALU.add),
             reads=[P1.ds[1], dtb_bc.d], writes=[dtv.d])
        S.op("scalar", lambda e: e.activation(out=dtv.t[:], in_=dtv.t[:], func=AF.Exp), reads=[dtv.d], writes=[dtv.d])
        S.op("scalar", lambda e: e.activation(out=dtv.t[:], in_=dtv.t[:], func=AF.Ln, bias=1.0, scale=1.0),
             reads=[dtv.d], writes=[dtv.d])
        S.op("vector", lambda e: e.tensor_tensor(out=dtA.t[:], in0=dtv.t[:], in1=A_bc.t[:], op=ALU.mult),
             reads=[dtv.d, A_bc.d], writes=[dtA.d])
        S.op("vector", lambda e: e.tensor_copy(out=dtAb.t[:], in_=bc3(dtA.t[:], 128)), reads=[dtA.d], writes=[dtAb.d])
        S.op("tensor", lambda e: e.matmul(P1.t[:, 520:528], lhsT=triU.t[:], rhs=dtA.t[:], start=True, stop=True),
             reads=[triU.d, dtA.d], writes=[P1.ds[1]])
        S.op("tensor", lambda e: e.matmul(P1.t[:, 528:536], lhsT=ones.t[:], rhs=dtA.t[:], start=True, stop=True),
             reads=[ones.d, dtA.d], writes=[P1.ds[1]])
        S.op("scalar", lambda e: e.activation(out=acs.t[:, 0:16], in_=P1.t[:, 520:536], func=AF.Copy),
             reads=[P1.ds[1]], writes=[acs.d])
        for hb in range(2):
            fns = [(lambda e, h=h: e.matmul(P0.t[:, h * 128:(h + 1) * 128], lhsT=dtAb.t[:, h, :], rhs=triU.t[:],
                                            start=True, stop=True)) for h in range(hb * 4, hb * 4 + 4)]
            S.group("tensor", fns, reads=[dtAb.d, triU.d], writes=[P0.ds[hb]])
        P0v = P0.t[:].rearrange("p (h l) -> p h l", h=8)
        S.op("vector", lambda e: e.tensor_tensor(out=Lx.t[:], in0=P0v, in1=bc3(acs.t[:, 0:8], 128), op=ALU.subtract),
             reads=[P0.ds[0], P0.ds[1], acs.d], writes=[Lx.d])
        S.op("vector", lambda e: e.tensor_scalar(out=Lx.t[:], in0=Lx.t[:], scalar1=0.0, scalar2=None, op0=ALU.min),
             reads=[Lx.d], writes=[Lx.d])
        S.op("scalar", lambda e: e.activation(out=Lx.t[:], in_=Lx.t[:], func=AF.Exp), reads=[Lx.d], writes=[Lx.d])
        S.op("scalar", lambda e: e.activation(out=Eb.t[:], in_=P0v, func=AF.Exp), reads=[P0.ds[0], P0.ds[1]],
             writes=[Eb.d])
        if ck + 1 < NCK:
            head(ck + 1)
        S.op("vector", lambda e: e.tensor_tensor(out=dte.t[:], in0=acs.t[:, 8:16], in1=acs.t[:, 0:8], op=ALU.subtract),
             reads=[acs.d], writes=[dte.d])
        S.op("scalar", lambda e: e.activation(out=dte.t[:], in_=dte.t[:], func=AF.Exp), reads=[dte.d], writes=[dte.d])
        S.op("scalar", lambda e: e.activation(out=cd.t[:], in_=acs.t[:, 8:16], func=AF.Exp), reads=[acs.d], writes=[cd.d])
        for k in range(4):
            src = xr.t[:, :, k:k + 128]
            wk = bc3(cwT.t[:, k, :], 128)
            if k == 0:
                S.op("vector", lambda e, src=src, wk=wk: e.tensor_tensor(out=cacc.t[:], in0=src, in1=wk, op=ALU.mult),
                     reads=[xr.d, cwT.d], writes=[cacc.d])
            else:
                S.op("vector", lambda e, src=src, wk=wk: e.tensor_tensor(out=ctmp.t[:], in0=src, in1=wk, op=ALU.mult),
                     reads=[xr.d, cwT.d], writes=[ctmp.d])
                S.op("vector", lambda e: e.tensor_tensor(out=cacc.t[:], in0=cacc.t[:], in1=ctmp.t[:], op=ALU.add),
                     reads=[cacc.d, ctmp.d], writes=[cacc.d])
        S.op("vector", lambda e: e.tensor_copy(out=xr.t[:, :, 0:3], in_=xr.t[:, :, 128:131]), reads=[xr.d], writes=[xr.d])
        for ch in range(8):
            S.op("scalar", lambda e, ch=ch: e.activation(out=xa.t[:, ch, :], in_=cacc.t[:, ch, :], func=AF.Silu,
                                                         bias=cbT.t[:, ch:ch + 1], scale=1.0),
                 reads=[cacc.d, cbT.d], writes=[xa.d])
        S.op("vector", lambda e: e.tensor_copy(out=bcT.t[:], in_=xa.t[:, 4:8, :]), reads=[xa.d], writes=[bcT.d])
        for hb in range(2):
            rng_ = range(0, 4) if hb == 0 else range(4, 6)
            fns = [(lambda e, j=j: e.transpose(out=P2.t[:, j * 128:(j + 1) * 128], in_=xa.t[:, j, :],
                                               identity=K.ident.t[:])) for j in rng_]
            S.group("tensor", fns, reads=[xa.d, K.ident.d], writes=[P2.ds[hb]])
        S.op("scalar", lambda e: e.activation(out=xtok.t[:], in_=P2.t[:, 0:768], func=AF.Copy),
             reads=[P2.ds[0], P2.ds[1]], writes=[xtok.d])
        S.op("vector", lambda e: e.tensor_copy(out=btok.t[:], in_=xtok.t[:, 512:768]), reads=[xtok.d], writes=[btok.d])
        xt3 = xtok.t[:, 0:512].rearrange("p (h d) -> p h d", h=8)
        S.op("vector", lambda e: e.tensor_tensor(out=xdt.t[:], in0=xt3, in1=bc3(dtv.t[:], 64), op=ALU.mult),
             reads=[xtok.d, dtv.d], writes=[xdt.d])
        S.op("vector", lambda e: e.tensor_tensor(out=dte.t[:], in0=dte.t[:], in1=dtv.t[:], op=ALU.mult),
             reads=[dte.d, dtv.d], writes=[dte.d])
        S.op("vector", lambda e: e.tensor_tensor(out=xdte.t[:], in0=xt3, in1=bc3(dte.t[:], 64), op=ALU.mult),
             reads=[xtok.d, dte.d], writes=[xdte.d])
        fns = [(lambda e, g=g: e.matmul(P3.t[:, g * 128:(g + 1) * 128], lhsT=bcT.t[:, g, :], rhs=bcT.t[:, 2 + g, :],
                                        start=True, stop=True)) for g in range(2)]
        S.group("tensor", fns, reads=[bcT.d], writes=[P3.ds[0]])
        S.op("vector", lambda e: e.tensor_tensor(out=cbm.t[:], in0=P3.t[:, 0:256].rearrange("p (g l) -> p g l", g=2),
                                                 in1=triU.t[:].unsqueeze(1).to_broadcast([128, 2, 128]), op=ALU.mult),
             reads=[P3.ds[0], triU.d], writes=[cbm.d])
        for g in range(2):
            S.op("vector", lambda e, g=g: e.tensor_tensor(
                out=MT.t[:, 4 * g:4 * g + 4, :], in0=Lx.t[:, 4 * g:4 * g + 4, :],
                in1=cbm.t[:, g, :].unsqueeze(1).to_broadcast([128, 4, 128]), op=ALU.mult),
                 reads=[Lx.d, cbm.d], writes=[MT.d])
            S.op("vector", lambda e, g=g: e.tensor_tensor(
                out=CsT.t[:, 4 * g:4 * g + 4, :], in0=Eb.t[:, 4 * g:4 * g + 4, :],
                in1=xa.t[:, 6 + g, :].unsqueeze(1).to_broadcast([128, 4, 128]), op=ALU.mult),
                 reads=[Eb.d, xa.d], writes=[CsT.d])
        fns = []
        for h in range(8):
            fns.append(lambda e, h=h: e.matmul(P2.t[:, h * 64:(h + 1) * 64], lhsT=MT.t[:, h, :], rhs=xdt.t[:, h, :],
                                               start=True, stop=False))
            fns.append(lambda e, h=h: e.matmul(P2.t[:, h * 64:(h + 1) * 64], lhsT=CsT.t[:, h, :], rhs=Sbf.t[:, h, :],
                                               start=False, stop=True))
        S.group("tensor", fns, reads=[MT.d, xdt.d, CsT.d, Sbf.d], writes=[P2.ds[0]])
        fns = [(lambda e, h=h: e.matmul(P2.t[:, 512 + h * 64:512 + (h + 1) * 64], lhsT=btok.t[:, (h // 4) * 128:(h // 4 + 1) * 128],
                                        rhs=xdte.t[:, h, :], start=True, stop=True)) for h in range(8)]
        S.group("tensor", fns, reads=[btok.d, xdte.d], writes=[P2.ds[1]])
        S.op("vector", lambda e: e.tensor_tensor(out=S32.t[:], in0=S32.t[:], in1=bc3(cd.t[:], 64), op=ALU.mult),
             reads=[S32.d, cd.d], writes=[S32.d])
        S.op("vector", lambda e: e.tensor_tensor(out=S32.t[:], in0=S32.t[:],
                                                 in1=P2.t[:, 512:1024].rearrange("p (h d) -> p h d", h=8), op=ALU.add),
             reads=[S32.d, P2.ds[1]], writes=[S32.d])
        S.op("vector", lambda e: e.tensor_copy(out=Sbf.t[:], in_=S32.t[:]), reads=[S32.d], writes=[Sbf.d])
        S.op("vector", lambda e: e.tensor_tensor(out=y.t[:].rearrange("p (h d) -> p h d", h=8), in0=xt3,
                                                 in1=bc3(dsk_bc.t[:], 64), op=ALU.mult),
             reads=[xtok.d, dsk_bc.d], writes=[y.d])
        S.op("vector", lambda e: e.tensor_tensor(out=y.t[:], in0=y.t[:], in1=P2.t[:, 0:512], op=ALU.add),
             reads=[y.d, P2.ds[0]], writes=[y.d])
        S.op("scalar", lambda e: e.activation(out=sz.t[:], in_=P1.t[:, 0:512], func=AF.Silu), reads=[P1.ds[0]],
             writes=[sz.d])
        S.op("vector", lambda e: e.tensor_tensor(out=y.t[:], in0=y.t[:], in1=sz.t[:], op=ALU.mult),
             reads=[y.d, sz.d], writes=[y.d])
        S.op("vector", lambda e: e.tensor_tensor(out=sq.t[:], in0=y.t[:], in1=y.t[:], op=ALU.mult),
             reads=[y.d], writes=[sq.d])
        S.op("vector", lambda e: e.tensor_reduce(out=ss.t[:], in_=sq.t[:].rearrange("p (g d) -> p g d", g=2), axis=X,
                                                 op=ALU.add), reads=[sq.d], writes=[ss.d])
        S.op("scalar", lambda e: e.activation(out=ss.t[:], in_=ss.t[:], func=AF.Sqrt, bias=eps_r.t[:], scale=1.0 / 256.0),
             reads=[ss.d, eps_r.d], writes=[ss.d])
        S.op("vector", lambda e: e.reciprocal(out=ss.t[:], in_=ss.t[:]), reads=[ss.d], writes=[ss.d])
        S.op("vector", lambda e: e.tensor_tensor(out=y.t[:].rearrange("p (g d) -> p g d", g=2),
                                                 in0=y.t[:].rearrange("p (g d) -> p g d", g=2),
                                                 in1=ss.t[:].unsqueeze(2).to_broadcast([128, 2, 256]), op=ALU.mult),
             reads=[y.d, ss.d], writes=[y.d])
        S.op("vector", lambda e: e.tensor_tensor(out=y.t[:], in0=y.t[:], in1=nw_bc.t[:], op=ALU.mult),
             reads=[y.d, nw_bc.d], writes=[y.d])
        fns = [(lambda e, j=j: e.transpose(out=P3.t[:, 512 + j * 128:512 + (j + 1) * 128], in_=y.t[:, j * 128:(j + 1) * 128],
                                           identity=K.ident.t[:])) for j in range(4)]
        S.group("tensor", fns, reads=[y.d, K.ident.d], writes=[P3.ds[1]])
        S.op("scalar", lambda e: e.activation(out=ygT.t[:], in_=P3.t[:, 512:1024].rearrange("p (c t) -> p c t", c=4),
                                              func=AF.Copy), reads=[P3.ds[1]], writes=[ygT.d])
        S.dma("gpsimd", osem, out[:, ck * 128:(ck + 1) * 128].rearrange("(c p) t -> p c t", p=128), ygT.t[:],
              reads=[ygT.d])
    S.barrier()
    sc_ssd.close()
    K.scope = old_scope
    K.S.release(_mk)


def build_mla(NTok):
    K = KB()
    x = K.din("x", [NTok, D]); c = K.din("c", [D]); aw = K.din("aw", [D, 6 * D]); ab = K.din("ab", [6 * D])
    pos = K.din("pos", [NTok], I32)
    w_in = K.din("w_in", [D, 800]); qn = K.din("qn", [512]); kvn = K.din("kvn", [256])
    wuq_d = K.din("wuq", [512, 384]); wukv_d = K.din("wukv", [256, 512])
    out = K.dout("mT", [256, NTok], BF16)
    K.alloc_psum(); K.consts(); K.setup_cond(c)
    sub_mla(K, x, out, NTok, aw, ab, pos, w_in, qn, kvn, wuq_d, wukv_d)
    K.S.finish()
    return K


def sub_mla(K, x, out, NTok, aw, ab, pos, w_in, qn, kvn, wuq_d, wukv_d):
    S, nc = K.S, K.nc
    X = mybir.AxisListType.X
    NTL = NTok // 128
    QT_d = K.dtmp("QT_d", [4, 96, NTok], BF16); KT_d = K.dtmp("KT_d", [4, 96, NTok], BF16)
    V_d = K.dtmp("V_d", [NTL, 128, 4, 65], BF16)
    P0, P1, P2, P3 = K.psum
    old_scope = K.scope
    _mk = K.S.mark()
    SCALE = 96.0 ** -0.5
    TWO_PI = 2.0 * math.pi
    with ExitStack() as sc:
        K.scope = sc
        win = K.sb([128, 8, 800], BF16, tag="win")
        wuq = K.sb([128, 4, 384], BF16, tag="wuq"); wukv = K.sb([128, 2, 512], BF16, tag="wukv")
        S.dma("gpsimd", S.new_dma_sem(), win.t[:], w_in.rearrange("(c p) f -> p c f", p=128), writes=[win.d])
        S.dma("gpsimd", S.new_dma_sem(), wuq.t[:], wuq_d.rearrange("(c p) f -> p c f", p=128), writes=[wuq.d])
        S.dma("gpsimd", S.new_dma_sem(), wukv.t[:], wukv_d.rearrange("(c p) f -> p c f", p=128), writes=[wukv.d])
        nbc = K.sb([128, 768], F32, tag="nbc")
        S.dma("sync", S.new_dma_sem(), nbc.t[:, 0:512], qn.unsqueeze(0).to_broadcast([128, 512]), writes=[nbc.d])
        S.dma("sync", S.new_dma_sem(), nbc.t[:, 512:768], kvn.unsqueeze(0).to_broadcast([128, 256]), writes=[nbc.d])
        sc1p = K.sb([128, 1024], F32, tag="sc1p"); shm = K.sb([128, 1024], F32, tag="shm")
        K.mod_vec(shm, aw, ab, 0, False)
        K.mod_vec(sc1p, aw, ab, 1, True)
        eps_r = K.sb([128, 1], F32, tag="epsr")
        S.op("vector", lambda e: e.memset(eps_r.t[:], RMS_EPS), writes=[eps_r.d])
        ji = K.sb([128, 16], I32, tag="ji"); freq = K.sb([128, 16], F32, tag="freq")
        S.op("gpsimd", lambda e: e.iota(ji.t[:], pattern=[[1, 16]], base=0, channel_multiplier=0), writes=[ji.d])
        S.op("vector", lambda e: e.tensor_copy(out=freq.t[:], in_=ji.t[:]), reads=[ji.d], writes=[freq.d])
        S.op("scalar", lambda e: e.activation(out=freq.t[:], in_=freq.t[:], func=AF.Exp,
                                              scale=-math.log(10000.0) / 16.0), reads=[freq.d], writes=[freq.d])
        xin2 = [K.sb([128, 1024], F32, tag="xin") for _ in range(2)]; xsem2 = [S.new_dma_sem() for _ in range(2)]
        hf2 = [K.sb([128, 1024], F32, tag="hf") for _ in range(2)]
        hmT2 = [K.sb([128, 8, 128], BF16, tag="hmT") for _ in range(2)]
        posi2 = [K.sb([128, 1], I32, tag="posi") for _ in range(2)]; psem2 = [S.new_dma_sem() for _ in range(2)]
        lat = K.sb([128, 800], F32, tag="lat"); sq = K.sb([128, 768], F32, tag="sq")
        ss = K.sb([128, 2], F32, tag="ss"); nrm = K.sb([128, 768], F32, tag="nrm"); nT = K.sb([128, 6, 128], BF16, tag="nT")
        posf = K.sb([128, 1], F32, tag="posf")
        tt = K.sb([128, 32], F32, tag="tt"); ti = K.sb([128, 32], I32, tag="ti"); tf = K.sb([128, 32], F32, tag="tf")
        mm = K.sb([128, 32], F32, tag="mm"); scs = K.sb([128, 32], F32, tag="scs")
        qf = K.sb([128, 4, 96], F32, tag="qf"); kvf = K.sb([128, 4, 128], F32, tag="kvf")
        Qh = K.sb([128, 4, 96], F32, tag="Qh"); Kh = K.sb([128, 4, 96], F32, tag="Kh")
        ra = K.sb([128, 4, 16], F32, tag="ra"); rb = K.sb([128, 4, 16], F32, tag="rb")
        kr = K.sb([128, 32], F32, tag="kr")
        Va = K.sb([128, 4, 65], BF16, tag="Va")
        S.op("vector", lambda e: e.memset(Va.t[:], 1.0), writes=[Va.d])
        QTs = K.sb([128, 4, 128], BF16, tag="QTs"); KTs = K.sb([128, 4, 128], BF16, tag="KTs")
        qsem = S.new_dma_sem(); ksem = S.new_dma_sem(); vsem = S.new_dma_sem()
        b4 = lambda ap: ap.unsqueeze(1).to_broadcast([128, 4, 16])

        def rope(src1, src2, dst1, dst2, cosb, sinb, shape_bc):
            S.op("vector", lambda e: e.tensor_tensor(out=ra.t[:] if shape_bc else ra.t[:, 0, :], in0=src1, in1=cosb, op=ALU.mult),
                 reads=[qf.d, lat.d, scs.d], writes=[ra.d])
            S.op("vector", lambda e: e.tensor_tensor(out=rb.t[:] if shape_bc else rb.t[:, 0, :], in0=src2, in1=sinb, op=ALU.mult),
                 reads=[qf.d, lat.d, scs.d], writes=[rb.d])
            S.op("vector", lambda e: e.tensor_tensor(out=dst1, in0=ra.t[:] if shape_bc else ra.t[:, 0, :],
                                                     in1=rb.t[:] if shape_bc else rb.t[:, 0, :], op=ALU.subtract),
                 reads=[ra.d, rb.d], writes=[Qh.d, kr.d])
            S.op("vector", lambda e: e.tensor_tensor(out=ra.t[:] if shape_bc else ra.t[:, 0, :], in0=src2, in1=cosb, op=ALU.mult),
                 reads=[qf.d, lat.d, scs.d], writes=[ra.d])
            S.op("vector", lambda e: e.tensor_tensor(out=rb.t[:] if shape_bc else rb.t[:, 0, :], in0=src1, in1=sinb, op=ALU.mult),
                 reads=[qf.d, lat.d, scs.d], writes=[rb.d])
            S.op("vector", lambda e: e.tensor_tensor(out=dst2, in0=ra.t[:] if shape_bc else ra.t[:, 0, :],
                                                     in1=rb.t[:] if shape_bc else rb.t[:, 0, :], op=ALU.add),
                 reads=[ra.d, rb.d], writes=[Qh.d, kr.d])

        def head(t):
            xin, hf, hmT, xsem = xin2[t % 2], hf2[t % 2], hmT2[t % 2], xsem2[t % 2]
            S.dma("sync", xsem, xin.t[:], (x(t) if callable(x) else x[t * 128:(t + 1) * 128, :]), writes=[xin.d])
            with nc.allow_non_contiguous_dma(reason="positions column"):
                S.dma("sync", psem2[t % 2], posi2[t % 2].t[:], pos[t * 128:(t + 1) * 128].unsqueeze(1),
                      writes=[posi2[t % 2].d])
            S.op("vector", lambda e: e.tensor_tensor(out=hf.t[:], in0=xin.t[:], in1=sc1p.t[:], op=ALU.mult),
                 reads=[xin.d, sc1p.d], writes=[hf.d])
            S.op("vector", lambda e: e.tensor_tensor(out=hf.t[:], in0=hf.t[:], in1=shm.t[:], op=ALU.add),
                 reads=[hf.d, shm.d], writes=[hf.d])
            for hb in range(2):
                fns = [(lambda e, k=k: e.transpose(out=P0.t[:, k * 128:(k + 1) * 128], in_=hf.t[:, k * 128:(k + 1) * 128],
                                                   identity=K.ident.t[:])) for k in range(hb * 4, hb * 4 + 4)]
                S.group("tensor", fns, reads=[hf.d, K.ident.d], writes=[P0.ds[hb]])
            S.op("scalar", lambda e: e.activation(out=hmT.t[:], in_=P0.t[:].rearrange("p (c t) -> p c t", c=8),
                                                  func=AF.Copy), reads=[P0.ds[0], P0.ds[1]], writes=[hmT.d])

        head(0)
        for t in range(NTL):
            hmT = hmT2[t % 2]
            posi = posi2[t % 2]
            fns = [(lambda e, k=k: e.matmul(P1.t[:, 0:512], lhsT=hmT.t[:, k, :], rhs=win.t[:, k, 0:512], start=(k == 0),
                                            stop=(k == 7))) for k in range(8)]
            S.group("tensor", fns, reads=[hmT.d, win.d], writes=[P1.ds[0]])
            fns = [(lambda e, k=k: e.matmul(P1.t[:, 512:800], lhsT=hmT.t[:, k, :], rhs=win.t[:, k, 512:800], start=(k == 0),
                                            stop=(k == 7))) for k in range(8)]
            S.group("tensor", fns, reads=[hmT.d, win.d], writes=[P1.ds[1]])
            S.op("scalar", lambda e: e.activation(out=lat.t[:], in_=P1.t[:, 0:800], func=AF.Copy),
                 reads=[P1.ds[0], P1.ds[1]], writes=[lat.d])
            S.op("vector", lambda e: e.tensor_tensor(out=sq.t[:], in0=lat.t[:, 0:768], in1=lat.t[:, 0:768], op=ALU.mult),
                 reads=[lat.d], writes=[sq.d])
            S.op("vector", lambda e: e.tensor_reduce(out=ss.t[:, 0:1], in_=sq.t[:, 0:512], axis=X, op=ALU.add),
                 reads=[sq.d], writes=[ss.d])
            S.op("vector", lambda e: e.tensor_reduce(out=ss.t[:, 1:2], in_=sq.t[:, 512:768], axis=X, op=ALU.add),
                 reads=[sq.d], writes=[ss.d])
            S.op("scalar", lambda e: e.activation(out=ss.t[:, 0:1], in_=ss.t[:, 0:1], func=AF.Sqrt, bias=eps_r.t[:],
                                                  scale=1.0 / 512.0), reads=[ss.d, eps_r.d], writes=[ss.d])
            S.op("scalar", lambda e: e.activation(out=ss.t[:, 1:2], in_=ss.t[:, 1:2], func=AF.Sqrt, bias=eps_r.t[:],
                                                  scale=1.0 / 256.0), reads=[ss.d, eps_r.d], writes=[ss.d])
            S.op("vector", lambda e: e.reciprocal(out=ss.t[:], in_=ss.t[:]), reads=[ss.d], writes=[ss.d])
            S.op("vector", lambda e: e.scalar_tensor_tensor(out=nrm.t[:, 0:512], in0=lat.t[:, 0:512], scalar=ss.t[:, 0:1],
                                                            in1=nbc.t[:, 0:512], op0=ALU.mult, op1=ALU.mult),
                 reads=[lat.d, ss.d, nbc.d], writes=[nrm.d])
            S.op("vector", lambda e: e.scalar_tensor_tensor(out=nrm.t[:, 512:768], in0=lat.t[:, 512:768],
                                                            scalar=ss.t[:, 1:2], in1=nbc.t[:, 512:768], op0=ALU.mult,
                                                            op1=ALU.mult), reads=[lat.d, ss.d, nbc.d], writes=[nrm.d])
            for hb in range(2):
                rng_ = range(0, 4) if hb == 0 else range(4, 6)
                fns = [(lambda e, j=j: e.transpose(out=P0.t[:, j * 128:(j + 1) * 128], in_=nrm.t[:, j * 128:(j + 1) * 128],
                                                   identity=K.ident.t[:])) for j in rng_]
                S.group("tensor", fns, reads=[nrm.d, K.ident.d], writes=[P0.ds[hb]])
            S.op("scalar", lambda e: e.activation(out=nT.t[:], in_=P0.t[:, 0:768].rearrange("p (c t) -> p c t", c=6),
                                                  func=AF.Copy), reads=[P0.ds[0], P0.ds[1]], writes=[nT.d])
            if t + 1 < NTL:
                head(t + 1)
            fns = [(lambda e, c_=c_: e.matmul(P2.t[:, 0:384], lhsT=nT.t[:, c_, :], rhs=wuq.t[:, c_, :], start=(c_ == 0),
                                              stop=(c_ == 3))) for c_ in range(4)]
            S.group("tensor", fns, reads=[nT.d, wuq.d], writes=[P2.ds[0]])
            fns = [(lambda e, c_=c_: e.matmul(P2.t[:, 512:1024], lhsT=nT.t[:, 4 + c_, :], rhs=wukv.t[:, c_, :],
                                              start=(c_ == 0), stop=(c_ == 1))) for c_ in range(2)]
            S.group("tensor", fns, reads=[nT.d, wukv.d], writes=[P2.ds[1]])
            S.op("scalar", lambda e: e.activation(out=qf.t[:], in_=P2.t[:, 0:384].rearrange("p (h d) -> p h d", h=4),
                                                  func=AF.Copy), reads=[P2.ds[0]], writes=[qf.d])
            S.op("scalar", lambda e: e.activation(out=kvf.t[:], in_=P2.t[:, 512:1024].rearrange("p (h d) -> p h d", h=4),
                                                  func=AF.Copy), reads=[P2.ds[1]], writes=[kvf.d])
            S.op("vector", lambda e: e.tensor_copy(out=posf.t[:], in_=posi.t[:]), reads=[posi.d], writes=[posf.d])
            S.op("vector", lambda e: e.tensor_scalar(out=tt.t[:, 0:16], in0=freq.t[:], scalar1=posf.t[:],
                                                     scalar2=1.0 / TWO_PI, op0=ALU.mult, op1=ALU.mult),
                 reads=[freq.d, posf.d], writes=[tt.d])
            S.op("vector", lambda e: e.tensor_scalar(out=tt.t[:, 16:32], in0=tt.t[:, 0:16], scalar1=0.25, scalar2=None,
                                                     op0=ALU.add), reads=[tt.d], writes=[tt.d])
            S.op("vector", lambda e: e.tensor_copy(out=ti.t[:], in_=tt.t[:]), reads=[tt.d], writes=[ti.d])
            S.op("vector", lambda e: e.tensor_copy(out=tf.t[:], in_=ti.t[:]), reads=[ti.d], writes=[tf.d])
            S.op("vector", lambda e: e.tensor_tensor(out=tt.t[:], in0=tt.t[:], in1=tf.t[:], op=ALU.subtract),
                 reads=[tt.d, tf.d], writes=[tt.d])
            S.op("vector", lambda e: e.tensor_single_scalar(out=mm.t[:], in_=tt.t[:], scalar=0.5, op=ALU.is_gt),
                 reads=[tt.d], writes=[mm.d])
            S.op("vector", lambda e: e.tensor_tensor(out=tt.t[:], in0=tt.t[:], in1=mm.t[:], op=ALU.subtract),
                 reads=[tt.d, mm.d], writes=[tt.d])
            S.op("vector", lambda e: e.tensor_single_scalar(out=mm.t[:], in_=tt.t[:], scalar=-0.5, op=ALU.is_lt),
                 reads=[tt.d], writes=[mm.d])
            S.op("vector", lambda e: e.tensor_tensor(out=tt.t[:], in0=tt.t[:], in1=mm.t[:], op=ALU.add),
                 reads=[tt.d, mm.d], writes=[tt.d])
            S.op("scalar", lambda e: e.activation(out=scs.t[:], in_=tt.t[:], func=AF.Sin, scale=TWO_PI),
                 reads=[tt.d], writes=[scs.d])
            sinb, cosb = scs.t[:, 0:16], scs.t[:, 16:32]
            rope(qf.t[:, :, 64:80], qf.t[:, :, 80:96], Qh.t[:, :, 64:80], Qh.t[:, :, 80:96], b4(cosb), b4(sinb), True)
            rope(lat.t[:, 768:784], lat.t[:, 784:800], kr.t[:, 0:16], kr.t[:, 16:32], cosb, sinb, False)
            S.op("vector", lambda e: e.tensor_copy(out=Qh.t[:, :, 0:64], in_=qf.t[:, :, 0:64]), reads=[qf.d], writes=[Qh.d])
            S.op("vector", lambda e: e.tensor_copy(out=Kh.t[:, :, 0:64], in_=kvf.t[:, :, 0:64]), reads=[kvf.d], writes=[Kh.d])
            S.op("vector", lambda e: e.tensor_copy(out=Kh.t[:, :, 64:96], in_=kr.t[:].unsqueeze(1).to_broadcast([128, 4, 32])),
                 reads=[kr.d], writes=[Kh.d])
            S.op("vector", lambda e: e.tensor_copy(out=Va.t[:, :, 0:64], in_=kvf.t[:, :, 64:128]), reads=[kvf.d], writes=[Va.d])
            fns = [(lambda e, h=h: e.transpose(out=P3.t[0:96, h * 128:(h + 1) * 128], in_=Qh.t[:, h, :],
                                               identity=K.ident.t[:])) for h in range(4)]
            S.group("tensor", fns, reads=[Qh.d, K.ident.d], writes=[P3.ds[0]])
            fns = [(lambda e, h=h: e.transpose(out=P3.t[0:96, 512 + h * 128:512 + (h + 1) * 128], in_=Kh.t[:, h, :],
                                               identity=K.ident.t[:])) for h in range(4)]
            S.group("tensor", fns, reads=[Kh.d, K.ident.d], writes=[P3.ds[1]])
            S.op("scalar", lambda e: e.activation(out=QTs.t[0:96], in_=P3.t[0:96, 0:512].rearrange("p (h t) -> p h t", h=4),
                                                  func=AF.Copy), reads=[P3.ds[0]], writes=[QTs.d])
            S.op("scalar", lambda e: e.activation(out=KTs.t[0:96], in_=P3.t[0:96, 512:1024].rearrange("p (h t) -> p h t", h=4),
                                                  func=AF.Copy), reads=[P3.ds[1]], writes=[KTs.d])
            S.dma("gpsimd", qsem, QT_d[:, :, t * 128:(t + 1) * 128].rearrange("h d t -> d h t"), QTs.t[0:96], reads=[QTs.d])
            S.dma("gpsimd", ksem, KT_d[:, :, t * 128:(t + 1) * 128].rearrange("h d t -> d h t"), KTs.t[0:96], reads=[KTs.d])
            S.dma("gpsimd", vsem, V_d[t], Va.t[:], reads=[Va.d])
        S.barrier()
    with ExitStack() as sc:
        K.scope = sc
        KTh = K.sb([128, NTok], BF16, tag="KTh"); Vh = K.sb([128, NTL, 65], BF16, tag="Vh")
        khs = S.new_dma_sem(); vhs = S.new_dma_sem()
        QTt = [K.sb([128, 512], BF16, tag="QTt") for _ in range(2)]; qts = [S.new_dma_sem() for _ in range(2)]
        Pb = [K.sb([128, 512], BF16, tag="Pb") for _ in range(2)]
        mk = K.sb([128, 4, 512], BF16, tag="mk")
        mi = K.sb([128, 512], I32, tag="mi")
        for j in range(4):
            S.op("gpsimd", lambda e, j=j: e.iota(mi.t[:], pattern=[[1, 512]], base=-128 * j, channel_multiplier=-1),
                 writes=[mi.d])
            S.op("vector", lambda e, j=j: e.tensor_single_scalar(out=mk.t[:, j, :], in_=mi.t[:], scalar=0, op=ALU.is_ge),
                 reads=[mi.d], writes=[mk.d])
        sel = K.sb([128, 64], F32, tag="sel")
        S.op("vector", lambda e: e.memset(sel.t[:], 0.0), writes=[sel.d])
        S.op("vector", lambda e: e.memset(sel.t[64:65, :], 1.0), writes=[sel.d])
        Osb = K.sb([128, 512], F32, tag="Osb"); rec = K.sb([64, 512], F32, tag="rec")
        ob = [K.sb([64, 512], BF16, tag="ob") for _ in range(2)]; obs = [S.new_dma_sem() for _ in range(2)]
        it = 0
        for h in range(4):
            S.dma("sync", khs, KTh.t[0:96, :], KT_d[h], writes=[KTh.d])
            with nc.allow_non_contiguous_dma(reason="V rows of 130B"):
                S.dma("sync", vhs, Vh.t[:], V_d[:, :, h, :].rearrange("c p d -> p c d"), writes=[Vh.d])
            for qt in range(NTok // 512):
                qb = (h * (NTok // 512) + qt) % 2
                S.dma("sync", qts[qb], QTt[qb].t[0:96, :], QT_d[h][:, qt * 512:(qt + 1) * 512], writes=[QTt[qb].d])
                nkb = 4 * qt + 4

                def emit_s(kb, it_):
                    pb = Pb[it_ % 2]
                    S.op("tensor", lambda e, kb=kb, it_=it_, qb=qb: e.matmul(
                        P0.t[:, (it_ % 2) * 512:(it_ % 2 + 1) * 512], lhsT=KTh.t[0:96, kb * 128:(kb + 1) * 128],
                        rhs=QTt[qb].t[0:96, :], start=True, stop=True),
                         reads=[KTh.d, QTt[qb].d], writes=[P0.ds[it_ % 2]])
                    S.op("scalar", lambda e, it_=it_, pb=pb: e.activation(
                        out=pb.t[:], in_=P0.t[:, (it_ % 2) * 512:(it_ % 2 + 1) * 512], func=AF.Exp, scale=SCALE),
                         reads=[P0.ds[it_ % 2]], writes=[pb.d])
                    if kb >= 4 * qt:
                        S.op("vector", lambda e, pb=pb, j=kb - 4 * qt: e.tensor_tensor(
                            out=pb.t[:], in0=pb.t[:], in1=mk.t[:, j, :], op=ALU.mult), reads=[pb.d, mk.d], writes=[pb.d])

                def emit_pv(kb, it_):
                    pb = Pb[it_ % 2]
                    S.op("tensor", lambda e, kb=kb, pb=pb: e.matmul(
                        P1.t[0:65, 0:512], lhsT=Vh.t[:, kb, :], rhs=pb.t[:], start=(kb == 0), stop=(kb == nkb - 1)),
                         reads=[Vh.d, pb.d], writes=[P1.ds[0]])

                emit_s(0, it)
                for kb in range(nkb):
                    if kb + 1 < nkb:
                        emit_s(kb + 1, it + 1)
                    emit_pv(kb, it)
                    it += 1
                S.op("scalar", lambda e: e.activation(out=Osb.t[0:65, :], in_=P1.t[0:65, 0:512], func=AF.Copy),
                     reads=[P1.ds[0]], writes=[Osb.d])
                S.op("tensor", lambda e: e.matmul(P2.t[0:64, 0:512], lhsT=sel.t[0:65, :], rhs=Osb.t[0:65, :], start=True,
                                                  stop=True), reads=[sel.d, Osb.d], writes=[P2.ds[0]])
                S.op("scalar", lambda e: e.activation(out=rec.t[:], in_=P2.t[0:64, 0:512], func=AF.Copy),
                     reads=[P2.ds[0]], writes=[rec.d])
                S.op("vector", lambda e: e.reciprocal(out=rec.t[:], in_=rec.t[:]), reads=[rec.d], writes=[rec.d])
                S.op("vector", lambda e, qb=qb: e.tensor_tensor(out=ob[qb].t[:], in0=Osb.t[0:64, :], in1=rec.t[:],
                                                               op=ALU.mult), reads=[Osb.d, rec.d], writes=[ob[qb].d])
                S.dma("gpsimd", obs[qb], out[h * 64:(h + 1) * 64, qt * 512:(qt + 1) * 512], ob[qb].t[:], reads=[ob[qb].d])
        S.barrier()
    K.scope = old_scope
    K.S.release(_mk)


def build_tok(NT, steps):
    K = KB()
    x = K.din("x", [NT, D]); c = K.din("c", [D])
    y = K.dout("y", [NT, D])
    K.alloc_psum(); K.consts(); K.setup_cond(c)
    aw, ab, lnp = {}, {}, {}

    def layer_in(l):
        if l not in aw:
            aw[l] = K.din(f"aw{l}", [D, 6 * D]); ab[l] = K.din(f"ab{l}", [6 * D])
        return aw[l], ab[l]

    def ln_in(l, j):
        if (l, j) not in lnp:
            lnp[(l, j)] = (K.din(f"lng{l}_{j}", [D]), K.din(f"lnb{l}_{j}", [D]))
        return lnp[(l, j)]

    cur = DramStream(x, NT)
    for si, (kind, l, dm) in enumerate(steps):
        last = si == len(steps) - 1
        nxt = DramStream(y if last else K.dtmp(f"xs{si}", [NT, D]), NT)
        a_w, a_b = layer_in(l)
        if kind == "proj":
            g, b = ln_in(l, 0)
            mT = K.din(f"s{si}_mT", [dm, NT], BF16); wo = K.din(f"s{si}_wo", [dm, D])
            sub_proj(K, cur, nxt, NT, mT, dm, wo, a_w, a_b, g, b)
        elif kind == "ffn":
            g, b = ln_in(l, 1)
            wg = K.din(f"s{si}_wg", [D, DFF]); wu = K.din(f"s{si}_wu", [D, DFF]); wd = K.din(f"s{si}_wd", [DFF, D])
            sub_ffn(K, cur, nxt, NT, a_w, a_b, g, b, wg, wu, wd, None)
        elif kind == "moe":
            g, b = ln_in(l, 1)
            wg = K.din(f"s{si}_wg", [NE, D, DFF]); wu = K.din(f"s{si}_wu", [NE, D, DFF]); wd = K.din(f"s{si}_wd", [NE, DFF, D])
            wr = K.din(f"s{si}_wr", [D, NE])
            sub_ffn(K, cur, nxt, NT, a_w, a_b, g, b, wg, wu, wd, wr)
        elif kind == "sg":
            g, b = ln_in(l, 0)
            w_in = K.din(f"s{si}_win", [D, 4096]); b_in = K.din(f"s{si}_bin", [4096])
            sg_g = K.din(f"s{si}_sg", [2048]); sg_b = K.din(f"s{si}_sb", [2048])
            w_s = K.din(f"s{si}_ws", [8, 128, 128]); b_s = K.din(f"s{si}_bs", [8, 128]); wo = K.din(f"s{si}_wo", [2048, D])
            sub_sg(K, cur, nxt, NT, a_w, a_b, g, b, w_in, b_in, sg_g, sg_b, w_s, b_s, wo)
        cur = nxt
    K.S.finish()
    return K


def _ssd_sel(I, j, q):
    w = I['ssd_w_in'][j]
    cols = np.concatenate([np.arange(512 * q, 512 * q + 512), 2048 + np.arange(512 * q, 512 * q + 512),
                           4096 + np.arange(256 * q, 256 * q + 256), 4096 + 1024 + np.arange(256 * q, 256 * q + 256),
                           6144 + np.arange(8 * q, 8 * q + 8)])
    ccols = np.concatenate([np.arange(512 * q, 512 * q + 512), 2048 + np.arange(256 * q, 256 * q + 256),
                            2048 + 1024 + np.arange(256 * q, 256 * q + 256)])
    return dict(w=np.ascontiguousarray(w[:, cols]), cw=np.ascontiguousarray(I['ssd_conv_w'][j][:, ccols]),
                cb=np.ascontiguousarray(I['ssd_conv_b'][j][ccols]),
                dtb=np.ascontiguousarray(I['ssd_dt_bias'][j][8 * q:8 * q + 8]),
                alog=np.ascontiguousarray(I['ssd_a_log'][j][8 * q:8 * q + 8]),
                dsk=np.ascontiguousarray(I['ssd_d_skip'][j][8 * q:8 * q + 8]),
                nw=np.ascontiguousarray(I['ssd_norm_w'][j][512 * q:512 * q + 512]))


def _mla_sel(I, hq):
    uq = I['mla_w_uq'][0].reshape(512, 16, 96)[:, 4 * hq:4 * hq + 4].reshape(512, 384)
    ukv = I['mla_w_ukv'][0].reshape(256, 16, 128)[:, 4 * hq:4 * hq + 4].reshape(256, 512)
    return dict(w_in=I['mla_w_in'][0], qn=I['mla_q_norm'][0], kvn=I['mla_kv_norm'][0],
                wuq=np.ascontiguousarray(uq), wukv=np.ascontiguousarray(ukv))


def _run(K, in_maps):
    res = run_bass_kernel_spmd(K.nc, in_maps, core_ids=list(range(8)))
    return res.results


def kernel_unfused(**I):
    I = {k: np.asarray(v) for k, v in I.items()}
    B, SEQ = I['x'].shape[0], I['x'].shape[1]
    NT = SEQ // 4
    cores = [(k // 4, k % 4) for k in range(8)]

    def run_mixer_ssd(xcur, layer, j):
        K = build_ssd(SEQ)
        maps = [dict(x=xcur[b], c=I['c'][b], aw=I['ada_w'][layer], ab=I['ada_b'][layer], **_ssd_sel(I, j, q))
                for b, q in cores]
        r = _run(K, maps)
        return [np.concatenate([r[b * 4 + q]["mT"] for q in range(4)], 0) for b in range(B)]

    def run_mixer_mla(xcur, layer):
        K = build_mla(SEQ)
        maps = [dict(x=xcur[b], c=I['c'][b], aw=I['ada_w'][layer], ab=I['ada_b'][layer],
                     pos=np.ascontiguousarray(I['positions'][b].astype(np.int32)), **_mla_sel(I, q)) for b, q in cores]
        r = _run(K, maps)
        return [np.concatenate([r[b * 4 + q]["mT"] for q in range(4)], 0) for b in range(B)]

    def run_tok(xcur, steps, extra):
        K = build_tok(NT, steps)
        maps = []
        for b, r_ in cores:
            m = dict(x=np.ascontiguousarray(xcur[b][r_ * NT:(r_ + 1) * NT]), c=I['c'][b])
            for (kind, l, dm) in steps:
                m[f"aw{l}"] = I['ada_w'][l]; m[f"ab{l}"] = I['ada_b'][l]
                jj = 0 if kind in ("proj", "sg") else 1
                m[f"lng{l}_{jj}"] = I['ln_g'][l, jj]; m[f"lnb{l}_{jj}"] = I['ln_b'][l, jj]
            for k_, v in extra.items():
                m[k_] = v(b, r_) if callable(v) else v
            maps.append(m)
        r = _run(K, maps)
        return [np.concatenate([r[b * 4 + q]["y"] for q in range(4)], 0) for b in range(B)]

    def ffn_w(si, k):
        return {f"s{si}_wg": I['ffn_w_gate'][k], f"s{si}_wu": I['ffn_w_up'][k], f"s{si}_wd": I['ffn_w_down'][k]}

    def moe_w(si, k):
        return {f"s{si}_wg": I['moe_w_gate'][k], f"s{si}_wu": I['moe_w_up'][k], f"s{si}_wd": I['moe_w_down'][k],
                f"s{si}_wr": I['moe_w_router'][k]}

    def mt_slice(mT):
        return lambda b, r_: np.ascontiguousarray(mT[b][:, r_ * NT:(r_ + 1) * NT])

    xcur = [I['x'][b] for b in range(B)]
    mT = run_mixer_ssd(xcur, 0, 0)
    xcur = run_tok(xcur, [("proj", 0, 2048), ("ffn", 0, 0)],
                   {"s0_mT": mt_slice(mT), "s0_wo": I['ssd_w_out'][0], **ffn_w(1, 0)})
    mT = run_mixer_mla(xcur, 1)
    sgw = {"s2_win": I['sg_w_in'][0], "s2_bin": I['sg_b_in'][0], "s2_sg": I['sg_ln_g'][0], "s2_sb": I['sg_ln_b'][0],
           "s2_ws": I['sg_w_s'][0], "s2_bs": I['sg_b_s'][0], "s2_wo": I['sg_w_out'][0]}
    xcur = run_tok(xcur, [("proj", 1, 1024), ("moe", 1, 0), ("sg", 2, 0), ("ffn", 2, 0)],
                   {"s0_mT": mt_slice(mT), "s0_wo": I['mla_w_out'][0], **moe_w(1, 0), **sgw, **ffn_w(3, 1)})
    mT = run_mixer_ssd(xcur, 3, 1)
    xcur = run_tok(xcur, [("proj", 3, 2048), ("moe", 3, 0)],
                   {"s0_mT": mt_slice(mT), "s0_wo": I['ssd_w_out'][1], **moe_w(1, 1)})
    return np.stack(xcur, 0).astype(np.float32)


RG = [[0, 1, 2, 3], [4, 5, 6, 7]]


def collective(K, kind, in_ap, out_ap):
    S = K.S
    if not hasattr(K, "cc"):
        K.cc = S.new_dma_sem()
    S.barrier()
    op = ALU.add if kind == "ReduceScatter" else ALU.bypass
    ins = K.nc.gpsimd.collective_compute(kind, op, replica_groups=RG, ins=[in_ap], outs=[out_ap])
    K.cc.val += 1
    ins.then_inc(K.cc.sem)
    S.barrier()


def sub_pproj(K, mT_ap, DM, wo_ap, ypart_ap, NTok):
    S, nc = K.S, K.nc
    NC_ = DM // 128
    old_scope = K.scope
    _mk = K.S.mark()
    with ExitStack() as sc:
        K.scope = sc
        wout = K.sb([128, NC_, 1024], BF16, tag="wout")
        S.dma("gpsimd", S.new_dma_sem(), wout.t[:], wo_ap.rearrange("(c p) d -> p c d", p=128), writes=[wout.d])
        mt = [K.sb([128, NC_, 128], BF16, tag="mt") for _ in range(2)]
        msem = [S.new_dma_sem() for _ in range(2)]
        yo = [K.sb([128, 1024], F32, tag="yo") for _ in range(2)]
        osem = [S.new_dma_sem() for _ in range(2)]
        for ts in range(NTok // 128):
            b = ts % 2
            S.dma("sync", msem[b], mt[b].t[:], mT_ap[:, ts * 128:(ts + 1) * 128].rearrange("(c p) t -> p c t", p=128),
                  writes=[mt[b].d])
            po = K.psum[2 + b]
            for db in range(2):
                fns = [(lambda e, c=c, db=db, po=po, b=b: e.matmul(
                    po.t[:, db * 512:(db + 1) * 512], lhsT=mt[b].t[:, c, :], rhs=wout.t[:, c, db * 512:(db + 1) * 512],
                    start=(c == 0), stop=(c == NC_ - 1))) for c in range(NC_)]
                S.group("tensor", fns, reads=[mt[b].d, wout.d], writes=[po.ds[db]])
            S.op("scalar", lambda e, po=po, b=b: e.activation(out=yo[b].t[:], in_=po.t[:], func=AF.Copy),
                 reads=[po.ds[0], po.ds[1]], writes=[yo[b].d])
            S.dma("gpsimd", osem[b], ypart_ap(ts), yo[b].t[:], reads=[yo[b].d])
        S.barrier()
    K.scope = old_scope
    K.S.release(_mk)


def sub_resln(K, xs, xd, NT, y_ap, ada_w_l, ada_b_l, lng_ap, lnb_ap):
    S, nc = K.S, K.nc
    old_scope = K.scope
    _mk = K.S.mark()
    with ExitStack() as sc:
        K.scope = sc
        g1p = K.sb([128, 1024], F32, tag="g1p")
        lng = K.sb([128, 1024], F32, tag="lng")
        lnb = K.sb([128, 1024], F32, tag="lnb")
        K.ln_alloc()
        K.mod_vec(g1p, ada_w_l, ada_b_l, 2, True)
        K.bcast_vec(lng, lng_ap, S.new_dma_sem())
        K.bcast_vec(lnb, lnb_ap, S.new_dma_sem())
        yin = [K.sb([128, 1024], F32, tag="yin") for _ in range(2)]
        ysem = [S.new_dma_sem() for _ in range(2)]
        xin = [K.sb([128, 1024], F32, tag="xin") for _ in range(2)]
        xsem = [S.new_dma_sem() for _ in range(2)]
        tmp = [K.sb([128, 1024], F32, tag="tmp") for _ in range(2)]
        xo = [K.sb([128, 1024], F32, tag="xo") for _ in range(2)]
        osem = [S.new_dma_sem() for _ in range(2)]
        for ts in range(NT // 128):
            b = ts % 2
            S.dma("sync", ysem[b], yin[b].t[:], y_ap[ts * 128:(ts + 1) * 128, :], writes=[yin[b].d])
            S.dma("sync", xsem[b], xin[b].t[:], xs.ap[ts * 128:(ts + 1) * 128, :], reads=[xs.ds[ts]],
                  writes=[xin[b].d])
            residual_ln(K, yin[b].t[:], [yin[b].d], xin[b], g1p, lng, lnb, tmp[b], xo[b])
            S.dma("gpsimd", osem[b], xd.ap[ts * 128:(ts + 1) * 128, :], xo[b].t[:], reads=[xo[b].d],
                  writes=[xd.ds[ts]])
        S.barrier()
    K.scope = old_scope
    K.S.release(_mk)


def build_fused(SEQ):
    NT = SEQ // 4
    K = KB()
    S, nc = K.S, K.nc
    x_in = K.din("x", [NT, D]); c = K.din("c", [D]); pos = K.din("pos", [SEQ], I32)
    aw = [K.din(f"aw{l}", [D, 6 * D]) for l in range(4)]
    ab = [K.din(f"ab{l}", [6 * D]) for l in range(4)]
    lng = [[K.din(f"lng{l}_{j}", [D]) for j in range(2)] for l in range(4)]
    lnb = [[K.din(f"lnb{l}_{j}", [D]) for j in range(2)] for l in range(4)]
    ssd = []
    for j in range(2):
        ssd.append(dict(w=K.din(f"ssd{j}_w", [D, 1544]), cw=K.din(f"ssd{j}_cw", [4, 1024]),
                        cbv=K.din(f"ssd{j}_cb", [1024]), dtb=K.din(f"ssd{j}_dtb", [8]),
                        alog=K.din(f"ssd{j}_alog", [8]), dsk=K.din(f"ssd{j}_dsk", [8]),
                        nw=K.din(f"ssd{j}_nw", [512]), wo=K.din(f"ssd{j}_wo", [512, D])))
    mla = dict(w_in=K.din("mla_w_in", [D, 800]), qn=K.din("mla_qn", [512]), kvn=K.din("mla_kvn", [256]),
               wuq_d=K.din("mla_wuq", [512, 384]), wukv_d=K.din("mla_wukv", [256, 512]))
    mla_wo = K.din("mla_wo", [256, D])
    sg = dict(w_in=K.din("sg_win", [D, 4096]), b_in=K.din("sg_bin", [4096]), sg_g=K.din("sg_g", [2048]),
              sg_b=K.din("sg_b", [2048]), w_s=K.din("sg_ws", [8, 128, 128]), b_s=K.din("sg_bs", [8, 128]),
              wo=K.din("sg_wo", [2048, D]))
    ffn = [dict(wg=K.din(f"ffn{k}_wg", [D, DFF]), wu=K.din(f"ffn{k}_wu", [D, DFF]), wd=K.din(f"ffn{k}_wd", [DFF, D]))
           for k in range(2)]
    moe = [dict(wg=K.din(f"moe{k}_wg", [NE, D, DFF]), wu=K.din(f"moe{k}_wu", [NE, D, DFF]),
                wd=K.din(f"moe{k}_wd", [NE, DFF, D]), wr=K.din(f"moe{k}_wr", [D, NE])) for k in range(2)]
    y = K.dout("y", [NT, D])
    K.alloc_psum(); K.consts(); K.setup_cond(c)
    CH = 256
    NCH = NT // CH
    xfull_c = K.dtmp("xfull", [NCH, 4 * CH, D])
    ypart_c = K.dtmp("ypart", [NCH, 4 * CH, D]); yred = K.dtmp("yred", [NT, D])

    def rows(buf):
        def f(tile):
            t = tile * 128
            r, rem = t // NT, t % NT
            ch, i = rem // CH, rem % CH
            return buf[ch, r * CH + i:r * CH + i + 128, :]
        return f

    xfull = rows(xfull_c)
    ypart = rows(ypart_c)

    def collectives(kind, pairs):
        if not hasattr(K, "cc"):
            K.cc = S.new_dma_sem()
        S.barrier()
        op = ALU.add if kind == "ReduceScatter" else ALU.bypass
        for a, b_ in pairs:
            ins = nc.gpsimd.collective_compute(kind, op, replica_groups=RG, ins=[a], outs=[b_])
            K.cc.val += 1
            ins.then_inc(K.cc.sem)
        S.barrier()

    def gather_x(src):
        collectives("AllGather", [(src[ch * CH:(ch + 1) * CH, :], xfull_c[ch]) for ch in range(NCH)])

    def scatter_y():
        collectives("ReduceScatter", [(ypart_c[ch], yred[ch * CH:(ch + 1) * CH, :]) for ch in range(NCH)])
    mTs = K.dtmp("mTs", [512, SEQ], BF16); mTm = K.dtmp("mTm", [256, SEQ], BF16)
    xl = [K.dtmp(f"xl{i}", [NT, D]) for i in range(8)]

    def stream(ap):
        return DramStream(ap, NT)

    cps = S.new_dma_sem()
    for ch in range(NCH):
        S.dma("sync", cps, xl[0][ch * CH:(ch + 1) * CH, :], x_in[ch * CH:(ch + 1) * CH, :])
    gather_x(xl[0])
    s = ssd[0]
    sub_ssd(K, xfull, mTs, SEQ, aw[0], ab[0], s["w"], s["cw"], s["cbv"], s["dtb"], s["alog"], s["dsk"], s["nw"])
    sub_pproj(K, mTs, 512, s["wo"], ypart, SEQ)
    scatter_y()
    sub_resln(K, stream(xl[0]), stream(xl[1]), NT, yred, aw[0], ab[0], lng[0][0], lnb[0][0])
    f = ffn[0]
    sub_ffn(K, stream(xl[1]), stream(xl[2]), NT, aw[0], ab[0], lng[0][1], lnb[0][1], f["wg"], f["wu"], f["wd"], None)
    gather_x(xl[2])
    sub_mla(K, xfull, mTm, SEQ, aw[1], ab[1], pos, **mla)
    sub_pproj(K, mTm, 256, mla_wo, ypart, SEQ)
    scatter_y()
    sub_resln(K, stream(xl[2]), stream(xl[3]), NT, yred, aw[1], ab[1], lng[1][0], lnb[1][0])
    m = moe[0]
    sub_ffn(K, stream(xl[3]), stream(xl[4]), NT, aw[1], ab[1], lng[1][1], lnb[1][1], m["wg"], m["wu"], m["wd"], m["wr"])
    sub_sg(K, stream(xl[4]), stream(xl[5]), NT, aw[2], ab[2], lng[2][0], lnb[2][0], sg["w_in"], sg["b_in"], sg["sg_g"],
           sg["sg_b"], sg["w_s"], sg["b_s"], sg["wo"])
    f = ffn[1]
    sub_ffn(K, stream(xl[5]), stream(xl[6]), NT, aw[2], ab[2], lng[2][1], lnb[2][1], f["wg"], f["wu"], f["wd"], None)
    gather_x(xl[6])
    s = ssd[1]
    sub_ssd(K, xfull, mTs, SEQ, aw[3], ab[3], s["w"], s["cw"], s["cbv"], s["dtb"], s["alog"], s["dsk"], s["nw"])
    sub_pproj(K, mTs, 512, s["wo"], ypart, SEQ)
    scatter_y()
    sub_resln(K, stream(xl[6]), stream(xl[7]), NT, yred, aw[3], ab[3], lng[3][0], lnb[3][0])
    m = moe[1]
    sub_ffn(K, stream(xl[7]), stream(y), NT, aw[3], ab[3], lng[3][1], lnb[3][1], m["wg"], m["wu"], m["wd"], m["wr"])
    S.finish()
    return K


def fused_inputs(I, SEQ, b, q):
    NT = SEQ // 4
    m = dict(x=np.ascontiguousarray(I['x'][b][q * NT:(q + 1) * NT]), c=np.ascontiguousarray(I['c'][b]),
             pos=np.ascontiguousarray(I['positions'][b].astype(np.int32)))
    for l in range(4):
        m[f"aw{l}"] = I['ada_w'][l]; m[f"ab{l}"] = I['ada_b'][l]
        for j in range(2):
            m[f"lng{l}_{j}"] = I['ln_g'][l, j]; m[f"lnb{l}_{j}"] = I['ln_b'][l, j]
    for j in range(2):
        s = _ssd_sel(I, j, q)
        for k_, v in s.items():
            m[f"ssd{j}_{k_}"] = v
        m[f"ssd{j}_wo"] = np.ascontiguousarray(I['ssd_w_out'][j][512 * q:512 * q + 512])
    s = _mla_sel(I, q)
    m["mla_w_in"] = s["w_in"]; m["mla_qn"] = s["qn"]; m["mla_kvn"] = s["kvn"]; m["mla_wuq"] = s["wuq"]; m["mla_wukv"] = s["wukv"]
    m["mla_wo"] = np.ascontiguousarray(I['mla_w_out'][0][256 * q:256 * q + 256])
    m["sg_win"] = I['sg_w_in'][0]; m["sg_bin"] = I['sg_b_in'][0]; m["sg_g"] = I['sg_ln_g'][0]; m["sg_b"] = I['sg_ln_b'][0]
    m["sg_ws"] = I['sg_w_s'][0]; m["sg_bs"] = I['sg_b_s'][0]; m["sg_wo"] = I['sg_w_out'][0]
    for k in range(2):
        m[f"ffn{k}_wg"] = I['ffn_w_gate'][k]; m[f"ffn{k}_wu"] = I['ffn_w_up'][k]; m[f"ffn{k}_wd"] = I['ffn_w_down'][k]
        m[f"moe{k}_wg"] = I['moe_w_gate'][k]; m[f"moe{k}_wu"] = I['moe_w_up'][k]; m[f"moe{k}_wd"] = I['moe_w_down'][k]
        m[f"moe{k}_wr"] = I['moe_w_router'][k]
    return m


def kernel(**I):
    I = {k: np.asarray(v) for k, v in I.items()}
    B, SEQ = I['x'].shape[0], I['x'].shape[1]
    NT = SEQ // 4
    K = build_fused(SEQ)
    maps = [fused_inputs(I, SEQ, k // 4, k % 4) for k in range(8)]
    r = _run(K, maps)
    return np.stack([np.concatenate([r[b * 4 + q]["y"] for q in range(4)], 0) for b in range(B)], 0).astype(np.float32)
```

```python
import math
from contextlib import ExitStack

import numpy as np
import concourse.bass as bass
import concourse.mybir as mybir
from concourse.bass_utils import run_bass_kernel_spmd

F32 = mybir.dt.float32
BF16 = mybir.dt.bfloat16
I32 = mybir.dt.int32
AF = mybir.ActivationFunctionType
ALU = mybir.AluOpType

D = 1024
DFF = 2816
NE = 8
ALPHA = 8.0 ** 0.25
LN_EPS = 1e-5
RMS_EPS = 1e-6


class Dep:
    __slots__ = ("w", "r")

    def __init__(self):
        self.w = None
        self.r = {}


class DmaSem:
    def __init__(self, sem):
        self.sem = sem
        self.val = 0


class Eng:
    def __init__(self, name, engine, sem):
        self.name = name
        self.e = engine
        self.sem = sem
        self.count = 0
        self.seen = {}


class Sync:
    def __init__(self, nc, stack):
        self.nc = nc
        self.stack = stack
        self.engs = {}
        for name in ("tensor", "vector", "scalar", "gpsimd", "sync"):
            sem = stack.enter_context(nc.semaphore("s_" + name))
            self.engs[name] = Eng(name, getattr(nc, name), sem)
        self.dma_sems = []
        self.n_inst = 0

    def new_dma_sem(self):
        if getattr(self, "free", None):
            d = self.free.pop()
        else:
            sem = self.stack.enter_context(self.nc.semaphore(None))
            d = DmaSem(sem)
            self.dma_sems.append(d)
        if not hasattr(self, "handed"):
            self.handed = []
            self.free = []
        self.handed.append(d)
        return d

    def mark(self):
        if not hasattr(self, "handed"):
            self.handed = []
            self.free = []
        return len(self.handed)

    def release(self, mk):
        self.free.extend(self.handed[mk:])
        del self.handed[mk:]

    def _waits(self, eng, reads, writes):
        need = {}
        for d in reads:
            if d.w is not None and need.get(d.w[0], 0) < d.w[1]:
                need[d.w[0]] = d.w[1]
        for d in writes:
            if d.w is not None and need.get(d.w[0], 0) < d.w[1]:
                need[d.w[0]] = d.w[1]
            for k, v in d.r.items():
                if need.get(k, 0) < v:
                    need[k] = v
        for k, v in need.items():
            if eng.seen.get(k, 0) < v:
                eng.e.wait_ge(k, v)
                eng.seen[k] = v

    def _mark(self, key, val, reads, writes):
        for d in reads:
            d.r[key] = val
        for d in writes:
            d.w = (key, val)
            d.r = {}

    def op(self, en, fn, reads=(), writes=()):
        eng = self.engs[en]
        self._waits(eng, reads, writes)
        ins = fn(eng.e)
        eng.count += 1
        ins.then_inc(eng.sem, 1)
        self.n_inst += 1
        self._mark(eng.sem, eng.count, reads, writes)

    def group(self, en, fns, reads=(), writes=()):
        eng = self.engs[en]
        self._waits(eng, reads, writes)
        ins = None
        for fn in fns:
            ins = fn(eng.e)
            self.n_inst += 1
        eng.count += 1
        ins.then_inc(eng.sem, 1)
        self._mark(eng.sem, eng.count, reads, writes)

    def dma(self, en, dsem, out, in_, reads=(), writes=(), **kw):
        eng = self.engs[en]
        self._waits(eng, reads, writes)
        ins = eng.e.dma_start(out=out, in_=in_, **kw)
        dsem.val += 16
        ins.then_inc(dsem.sem, 16)
        self.n_inst += 1
        self._mark(dsem.sem, dsem.val, reads, writes)

    def barrier(self):
        for eng in self.engs.values():
            for e2 in self.engs.values():
                if e2.count > 0 and eng.seen.get(e2.sem, 0) < e2.count:
                    eng.e.wait_ge(e2.sem, e2.count)
                    eng.seen[e2.sem] = e2.count
            for d in self.dma_sems:
                if d.val > 0 and eng.seen.get(d.sem, 0) < d.val:
                    eng.e.wait_ge(d.sem, d.val)
                    eng.seen[d.sem] = d.val

    def finish(self):
        self.barrier()


class Buf:
    def __init__(self, t, n=1):
        self.t = t
        self.ds = [Dep() for _ in range(n)]

    @property
    def d(self):
        return self.ds[0]


class KB:
    def __init__(self):
        self.nc = bass.Bass("TRN2", target_bir_lowering=False)
        self.st = ExitStack()
        self.S = Sync(self.nc, self.st)
        self.scope = self.st
        self._n = 0
        self.psum = None

    def name(self, p):
        self._n += 1
        return f"{p}{self._n}"

    def din(self, name, shape, dt=F32):
        return self.nc.dram_tensor(name, list(shape), dt, kind="ExternalInput").ap()

    def dout(self, name, shape, dt=F32):
        return self.nc.dram_tensor(name, list(shape), dt, kind="ExternalOutput").ap()

    def dtmp(self, name, shape, dt=F32):
        return self.nc.dram_tensor(name, list(shape), dt, kind="Internal").ap()

    def sb(self, shape, dt, n=1, tag="t"):
        t = self.scope.enter_context(self.nc.sbuf_tensor(self.name(tag), list(shape), dt))
        return Buf(t, n)

    def alloc_psum(self):
        self.psum = [Buf(self.st.enter_context(self.nc.psum_tensor(f"ps{i}", [128, 1024], F32)), 2)
                     for i in range(4)]

    def consts(self):
        S, nc = self.S, self.nc
        ii = self.sb([128, 128], I32, tag="ii")
        self.ident = self.sb([128, 128], F32, tag="ident")
        S.op("gpsimd", lambda e: e.iota(ii.t[:], pattern=[[1, 128]], base=0, channel_multiplier=-1),
             writes=[ii.d])
        S.op("vector", lambda e: e.tensor_single_scalar(out=self.ident.t[:], in_=ii.t[:], scalar=0,
                                                        op=ALU.is_equal), reads=[ii.d], writes=[self.ident.d])
        self.eps_ln = self.sb([128, 1], F32, tag="eps")
        S.op("vector", lambda e: e.memset(self.eps_ln.t[:], LN_EPS), writes=[self.eps_ln.d])

    def setup_cond(self, c_ap):
        S, nc = self.S, self.nc
        ccol = self.sb([128, 8], F32, tag="ccol")
        self.cbc = self.sb([128, 8, 128], F32, tag="cbc")
        self.modw = self.sb([128, 8, 512], F32, tag="modw")
        self.modb = self.sb([128, 1024], F32, tag="modb")
        self.sem_c = S.new_dma_sem()
        self.sem_mw = S.new_dma_sem()
        self.sem_mb = S.new_dma_sem()
        with nc.allow_non_contiguous_dma(reason="tiny column load"):
            S.dma("sync", self.sem_c, ccol.t[:], c_ap.rearrange("(c p) -> p c", p=128), writes=[ccol.d])
        S.op("scalar", lambda e: e.activation(out=ccol.t[:], in_=ccol.t[:], func=AF.Silu),
             reads=[ccol.d], writes=[ccol.d])
        S.op("vector", lambda e: e.tensor_copy(out=self.cbc.t[:],
                                               in_=ccol.t[:].unsqueeze(2).to_broadcast([128, 8, 128])),
             reads=[ccol.d], writes=[self.cbc.d])

    def mod_vec(self, out_buf, ada_w_l, ada_b_l, idx, plus_one):
        S = self.S
        ps = self.psum[0]
        S.dma("sync", self.sem_mb, self.modb.t[:],
              ada_b_l[idx * 1024:(idx + 1) * 1024].unsqueeze(0).to_broadcast([128, 1024]),
              writes=[self.modb.d])
        for hb in range(2):
            c0 = idx * 1024 + hb * 512
            S.dma("sync", self.sem_mw, self.modw.t[:],
                  ada_w_l[:, c0:c0 + 512].rearrange("(c p) f -> p c f", p=128), writes=[self.modw.d])
            fns = [(lambda e, k=k: e.matmul(ps.t[:, hb * 512:(hb + 1) * 512], lhsT=self.cbc.t[:, k, :],
                                             rhs=self.modw.t[:, k, :], start=(k == 0), stop=(k == 7)))
                   for k in range(8)]
            S.group("tensor", fns, reads=[self.cbc.d, self.modw.d], writes=[ps.ds[hb]])
        if plus_one:
            S.op("vector", lambda e: e.scalar_tensor_tensor(out=out_buf.t[:], in0=ps.t[:], scalar=1.0,
                                                            in1=self.modb.t[:], op0=ALU.add, op1=ALU.add),
                 reads=[ps.ds[0], ps.ds[1], self.modb.d], writes=[out_buf.d])
        else:
            S.op("vector", lambda e: e.tensor_tensor(out=out_buf.t[:], in0=ps.t[:], in1=self.modb.t[:],
                                                     op=ALU.add),
                 reads=[ps.ds[0], ps.ds[1], self.modb.d], writes=[out_buf.d])

    def bcast_vec(self, out_buf, vec_ap, sem):
        n = vec_ap.shape[0]
        self.S.dma("sync", sem, out_buf.t[:], vec_ap.unsqueeze(0).to_broadcast([128, n]), writes=[out_buf.d])

    def ln_alloc(self):
        self.ln_st = self.sb([128, 2, 6], F32, tag="lnst")
        self.ln_mv = self.sb([128, 2], F32, tag="lnmv")
        self.ln_rs = self.sb([128, 1], F32, tag="lnrs")

    def layer_norm(self, r, out, g_bc, b_bc):
        S = self.S
        st, mv, rs = self.ln_st, self.ln_mv, self.ln_rs
        for hb in range(2):
            S.op("vector", lambda e, hb=hb: e.bn_stats(out=st.t[:, hb, :], in_=r.t[:, hb * 512:(hb + 1) * 512]),
                 reads=[r.d], writes=[st.d])
        S.op("vector", lambda e: e.bn_aggr(out=mv.t[:], in_=st.t[:].rearrange("p a b -> p (a b)")),
             reads=[st.d], writes=[mv.d])
        S.op("scalar", lambda e: e.activation(out=rs.t[:], in_=mv.t[:, 1:2], func=AF.Sqrt,
                                              bias=self.eps_ln.t[:], scale=1.0),
             reads=[mv.d, self.eps_ln.d], writes=[rs.d])
        S.op("vector", lambda e: e.reciprocal(out=rs.t[:], in_=rs.t[:]), reads=[rs.d], writes=[rs.d])
        S.op("vector", lambda e: e.tensor_scalar(out=r.t[:], in0=r.t[:], scalar1=mv.t[:, 0:1], scalar2=rs.t[:],
                                                 op0=ALU.subtract, op1=ALU.mult),
             reads=[r.d, mv.d, rs.d], writes=[r.d])
        S.op("vector", lambda e: e.tensor_tensor(out=r.t[:], in0=r.t[:], in1=g_bc.t[:], op=ALU.mult),
             reads=[r.d, g_bc.d], writes=[r.d])
        S.op("vector", lambda e: e.tensor_tensor(out=out.t[:], in0=r.t[:], in1=b_bc.t[:], op=ALU.add),
             reads=[r.d, b_bc.d], writes=[out.d])


class DramStream:
    def __init__(self, ap, nt):
        self.ap = ap
        self.ds = [Dep() for _ in range(nt // 128)]


def sub_ffn(K, xs, xd, NT, ada_w_l, ada_b_l, lng_ap, lnb_ap, wg_ap, wu_ap, wd_ap, wr_ap=None):
    S, nc = K.S, K.nc
    moe = wr_ap is not None
    import os as _os
    E = int(_os.environ.get('MOE_E', NE)) if moe else 1
    T = min(NT, 2048)
    NSUP, NSUB, NB = NT // T, T // 128, T // 512
    JG = 2
    NG = DFF // (128 * JG)
    old_scope = K.scope
    _mk = K.S.mark()
    with ExitStack() as sc:
        K.scope = sc
        xT = K.sb([128, 8, T], BF16, n=NB, tag="xT")
        acc = K.sb([128, NSUB, 1024], F32, n=NSUB, tag="acc")
        wgb = [K.sb([128, 8, 128 * JG], BF16, tag="wg") for _ in range(2)]
        wub = [K.sb([128, 8, 128 * JG], BF16, tag="wu") for _ in range(2)]
        wdb = [K.sb([128, JG, 1024], BF16, tag="wd") for _ in range(2)]
        wsem = [S.new_dma_sem() for _ in range(2)]
        hT = [K.sb([128, JG, 512], BF16, n=JG, tag="hT") for _ in range(2)]
        sg = [K.sb([128, 512], F32, tag="sg") for _ in range(2)]
        xin = [K.sb([128, 1024], F32, tag="xin") for _ in range(2)]
        xsem = [S.new_dma_sem() for _ in range(2)]
        hf = [K.sb([128, 1024], F32, tag="hf") for _ in range(2)]
        xo = [K.sb([128, 1024], F32, tag="xo") for _ in range(2)]
        osem = [S.new_dma_sem() for _ in range(2)]
        sc1p = K.sb([128, 1024], F32, tag="sc1p")
        shf = K.sb([128, 1024], F32, tag="shf")
        g1p = K.sb([128, 1024], F32, tag="g1p")
        lng = K.sb([128, 1024], F32, tag="lng")
        lnb = K.sb([128, 1024], F32, tag="lnb")
        csem = S.new_dma_sem()
        K.ln_alloc()
        K.mod_vec(shf, ada_w_l, ada_b_l, 3, False)
        K.mod_vec(sc1p, ada_w_l, ada_b_l, 4, True)
        K.mod_vec(g1p, ada_w_l, ada_b_l, 5, True)
        K.bcast_vec(lng, lng_ap, csem)
        K.bcast_vec(lnb, lnb_ap, S.new_dma_sem())
        if moe:
            hfT32 = K.sb([128, 8, 128], F32, tag="hfT32")
            wr = K.sb([128, 8, NE], F32, tag="wr")
            if _os.environ.get("DBG4") != "1":
                with nc.allow_non_contiguous_dma(reason="router weights, tiny"):
                    S.dma("sync", S.new_dma_sem(), wr.t[:], wr_ap.rearrange("(c p) e -> p c e", p=128), writes=[wr.d])
            comb = K.sb([128, NSUB, NE], F32, tag="comb")
            lgall = K.sb([128, NSUB, NE], F32, tag="lgall")
            l2 = K.sb([128, NSUB, NE], F32, tag="l2")
            mk1 = K.sb([128, NSUB, NE], F32, tag="mk1")
            mk2 = K.sb([128, NSUB, NE], F32, tag="mk2")
            m1 = K.sb([128, NSUB], F32, tag="m1")
            m2 = K.sb([128, NSUB], F32, tag="m2")
            w1 = K.sb([128, NSUB], F32, tag="w1")
            w2 = K.sb([128, NSUB], F32, tag="w2")
        psG = [K.psum[0], K.psum[1]]
        psO = [K.psum[2], K.psum[3]]

        def wsrc(ap, e):
            return ap[e % ap.shape[0]] if moe else ap

        def load_w(e, jg, slot):
            f0 = jg * 128 * JG
            S.dma("gpsimd", wsem[slot], wgb[slot].t[:],
                  wsrc(wg_ap, e)[:, f0:f0 + 128 * JG].rearrange("(c p) f -> p c f", p=128), writes=[wgb[slot].d])
            S.dma("gpsimd", wsem[slot], wub[slot].t[:],
                  wsrc(wu_ap, e)[:, f0:f0 + 128 * JG].rearrange("(c p) f -> p c f", p=128), writes=[wub[slot].d])
            S.dma("gpsimd", wsem[slot], wdb[slot].t[:],
                  wsrc(wd_ap, e)[f0:f0 + 128 * JG, :].rearrange("(j p) d -> p j d", p=128), writes=[wdb[slot].d])
            wgb[slot].d.w = wub[slot].d.w = wdb[slot].d.w

        wlist = [(e, jg) for e in range(E) for jg in range(NG)]
        for sup in range(NSUP):
            t0 = sup * T
            load_w(*wlist[0], 0)
            for ts in range(NSUB):
                b = ts % 2
                gi = (t0 // 128) + ts
                S.dma("sync", xsem[b], xin[b].t[:], xs.ap[gi * 128:(gi + 1) * 128, :],
                      reads=[xs.ds[gi]], writes=[xin[b].d])
                S.op("vector", lambda e, b=b: e.tensor_tensor(out=hf[b].t[:], in0=xin[b].t[:], in1=sc1p.t[:],
                                                              op=ALU.mult),
                     reads=[xin[b].d, sc1p.d], writes=[hf[b].d])
                S.op("vector", lambda e, b=b: e.tensor_tensor(out=hf[b].t[:], in0=hf[b].t[:], in1=shf.t[:],
                                                              op=ALU.add),
                     reads=[hf[b].d, shf.d], writes=[hf[b].d])
                po = psO[b]
                for hb in range(2):
                    fns = [(lambda e, k=k, b=b, po=po: e.transpose(out=po.t[:, k * 128:(k + 1) * 128],
                                                                    in_=hf[b].t[:, k * 128:(k + 1) * 128],
                                                                    identity=K.ident.t[:]))
                           for k in range(hb * 4, hb * 4 + 4)]
                    S.group("tensor", fns, reads=[hf[b].d, K.ident.d], writes=[po.ds[hb]])
                if not moe:
                    S.op("scalar", lambda e, po=po, ts=ts: e.activation(
                        out=xT.t[:, :, ts * 128:(ts + 1) * 128], in_=po.t[:].rearrange("p (c t) -> p c t", c=8),
                        func=AF.Copy), reads=[po.ds[0], po.ds[1]], writes=[xT.ds[ts // 4]])
                else:
                    S.op("scalar", lambda e, po=po: e.activation(
                        out=hfT32.t[:], in_=po.t[:].rearrange("p (c t) -> p c t", c=8), func=AF.Copy),
                         reads=[po.ds[0], po.ds[1]], writes=[hfT32.d])
                    S.op("gpsimd", lambda e, ts=ts: e.tensor_copy(out=xT.t[:, :, ts * 128:(ts + 1) * 128],
                                                                  in_=hfT32.t[:]),
                         reads=[hfT32.d], writes=[xT.ds[ts // 4]])
                if moe:
                    pl = psG[0]
                    fns = [(lambda e, k=k, pl=pl: e.matmul(pl.t[:, 0:NE], lhsT=hfT32.t[:, k, :], rhs=wr.t[:, k, :],
                                                           start=(k == 0), stop=(k == 7))) for k in range(8)]
                    import os as _os
                    if _os.environ.get("MOEDBG") == "1":
                        S.op("vector", lambda e, ts=ts: e.memset(lgall.t[:, ts, :], 0.0), writes=[lgall.d])
                    else:
                        S.group("tensor", fns, reads=[hfT32.d, wr.d], writes=[pl.ds[0]])
                        S.op("vector", lambda e, pl=pl, ts=ts: e.tensor_copy(out=lgall.t[:, ts, :], in_=pl.t[:, 0:NE]),
                             reads=[pl.ds[0]], writes=[lgall.d])
            if moe and _os.environ.get('DBG3') == '1':
                S.op('vector', lambda e: e.memset(comb.t[:], 0.125), writes=[comb.d])
            elif moe:
                X = mybir.AxisListType.X
                bc = lambda b_: b_.t[:].unsqueeze(2).to_broadcast([128, NSUB, NE])
                S.op("vector", lambda e: e.tensor_reduce(out=m1.t[:], in_=lgall.t[:], axis=X, op=ALU.max),
                     reads=[lgall.d], writes=[m1.d])
                S.op("vector", lambda e: e.tensor_tensor(out=mk1.t[:], in0=lgall.t[:], in1=bc(m1), op=ALU.is_equal),
                     reads=[lgall.d, m1.d], writes=[mk1.d])
                S.op("vector", lambda e: e.scalar_tensor_tensor(out=l2.t[:], in0=mk1.t[:], scalar=-1e30,
                                                                in1=lgall.t[:], op0=ALU.mult, op1=ALU.add),
                     reads=[mk1.d, lgall.d], writes=[l2.d])
                S.op("vector", lambda e: e.tensor_reduce(out=m2.t[:], in_=l2.t[:], axis=X, op=ALU.max),
                     reads=[l2.d], writes=[m2.d])
                S.op("vector", lambda e: e.tensor_tensor(out=mk2.t[:], in0=l2.t[:], in1=bc(m2), op=ALU.is_equal),
                     reads=[l2.d, m2.d], writes=[mk2.d])
                S.op("vector", lambda e: e.tensor_tensor(out=w2.t[:], in0=m2.t[:], in1=m1.t[:], op=ALU.subtract),
                     reads=[m1.d, m2.d], writes=[w2.d])
                S.op("scalar", lambda e: e.activation(out=w2.t[:], in_=w2.t[:], func=AF.Exp),
                     reads=[w2.d], writes=[w2.d])
                S.op("vector", lambda e: e.tensor_scalar(out=w1.t[:], in0=w2.t[:], scalar1=1.0, scalar2=None,
                                                         op0=ALU.add), reads=[w2.d], writes=[w1.d])
                S.op("vector", lambda e: e.reciprocal(out=w1.t[:], in_=w1.t[:]), reads=[w1.d], writes=[w1.d])
                S.op("vector", lambda e: e.tensor_tensor(out=w2.t[:], in0=w2.t[:], in1=w1.t[:], op=ALU.mult),
                     reads=[w1.d, w2.d], writes=[w2.d])
                S.op("vector", lambda e: e.tensor_tensor(out=mk1.t[:], in0=mk1.t[:], in1=bc(w1), op=ALU.mult),
                     reads=[mk1.d, w1.d], writes=[mk1.d])
                S.op("vector", lambda e: e.tensor_tensor(out=mk2.t[:], in0=mk2.t[:], in1=bc(w2), op=ALU.mult),
                     reads=[mk2.d, w2.d], writes=[mk2.d])
                S.op("vector", lambda e: e.tensor_tensor(out=comb.t[:], in0=mk1.t[:], in1=mk2.t[:], op=ALU.add),
                     reads=[mk1.d, mk2.d], writes=[comb.d])
            for ts in range(NSUB):
                S.op("gpsimd", lambda e, ts=ts: e.memset(acc.t[:, ts, :], 0.0), writes=[acc.ds[ts]])
            its = [(wi, tb) for wi in range(len(wlist)) for tb in range(NB)]

            def emit_gu(n, js):
                wi, tb = its[n]
                slot = wi % 2
                hb_ = hT[n % 2]
                for j in js:
                    pg = psG[j % 2]
                    for which, wbuf in ((0, wgb[slot]), (1, wub[slot])):
                        fns = [(lambda e, k=k, j=j, which=which, wbuf=wbuf, pg=pg, tb=tb: e.matmul(
                            pg.t[:, which * 512:(which + 1) * 512], lhsT=wbuf.t[:, k, j * 128:(j + 1) * 128],
                            rhs=xT.t[:, k, tb * 512:(tb + 1) * 512], start=(k == 0), stop=(k == 7)))
                            for k in range(8)]
                        S.group("tensor", fns, reads=[wbuf.d, xT.ds[tb]], writes=[pg.ds[which]])
                    sgb = sg[j % 2]
                    S.op("scalar", lambda e, pg=pg, sgb=sgb: e.activation(out=sgb.t[:], in_=pg.t[:, 0:512],
                                                                         func=AF.Silu),
                         reads=[pg.ds[0]], writes=[sgb.d])
                    S.op("vector", lambda e, pg=pg, sgb=sgb, hb_=hb_, j=j: e.tensor_tensor(
                        out=hb_.t[:, j, :], in0=sgb.t[:], in1=pg.t[:, 512:1024], op=ALU.mult),
                         reads=[sgb.d, pg.ds[1]], writes=[hb_.ds[j]])

            def emit_d(n, qs):
                wi, tb = its[n]
                slot = wi % 2
                e_ = wlist[wi][0]
                hb_ = hT[n % 2]
                for q in qs:
                    ts = tb * 4 + q
                    po = psO[q % 2]
                    for db in range(2):
                        fns = [(lambda e, j=j, q=q, db=db, po=po, hb_=hb_, slot=slot: e.matmul(
                            po.t[:, db * 512:(db + 1) * 512], lhsT=hb_.t[:, j, q * 128:(q + 1) * 128],
                            rhs=wdb[slot].t[:, j, db * 512:(db + 1) * 512], start=(j == 0),
                            stop=(j == JG - 1))) for j in range(JG)]
                        S.group("tensor", fns, reads=[hb_.ds[0], hb_.ds[1], wdb[slot].d], writes=[po.ds[db]])
                    cs = comb.t[:, ts, e_:e_ + 1] if (moe and _os.environ.get('DBG2') != '1') else 1.0
                    rd = [po.ds[0], po.ds[1], acc.ds[ts]] + ([comb.d] if moe else [])
                    S.op("vector", lambda e, po=po, ts=ts, cs=cs: e.scalar_tensor_tensor(
                        out=acc.t[:, ts, :], in0=po.t[:], scalar=cs, in1=acc.t[:, ts, :], op0=ALU.mult,
                        op1=ALU.add), reads=rd, writes=[acc.ds[ts]])

            for n in range(len(its)):
                wi, tb = its[n]
                emit_gu(n, (0,))
                if n > 0:
                    emit_d(n - 1, (0, 1))
                emit_gu(n, (1,))
                if n > 0:
                    emit_d(n - 1, (2, 3))
                if tb == 0 and wi + 1 < len(wlist):
                    load_w(*wlist[wi + 1], (wi + 1) % 2)
            emit_d(len(its) - 1, (0, 1, 2, 3))
            for ts in range(NSUB):
                b = ts % 2
                gi = (t0 // 128) + ts
                S.dma("sync", xsem[b], xin[b].t[:], xs.ap[gi * 128:(gi + 1) * 128, :],
                      reads=[xs.ds[gi]], writes=[xin[b].d])
                S.op("vector", lambda e, ts=ts, b=b: e.tensor_tensor(out=hf[b].t[:], in0=acc.t[:, ts, :],
                                                                    in1=g1p.t[:], op=ALU.mult),
                     reads=[acc.ds[ts], g1p.d], writes=[hf[b].d])
                S.op("vector", lambda e, b=b: e.scalar_tensor_tensor(out=hf[b].t[:], in0=xin[b].t[:], scalar=ALPHA,
                                                                     in1=hf[b].t[:], op0=ALU.mult, op1=ALU.add),
                     reads=[xin[b].d, hf[b].d], writes=[hf[b].d])
                K.layer_norm(hf[b], xo[b], lng, lnb)
                S.dma("gpsimd", osem[b], xd.ap[gi * 128:(gi + 1) * 128, :], xo[b].t[:],
                      reads=[xo[b].d], writes=[xd.ds[gi]])
        S.barrier()
    K.scope = old_scope
    K.S.release(_mk)


def residual_ln(K, y_ap, y_deps, xin, g1p, lng, lnb, tmp, xo):
    S = K.S
    S.op("vector", lambda e: e.tensor_tensor(out=tmp.t[:], in0=y_ap, in1=g1p.t[:], op=ALU.mult),
         reads=list(y_deps) + [g1p.d], writes=[tmp.d])
    S.op("vector", lambda e: e.scalar_tensor_tensor(out=tmp.t[:], in0=xin.t[:], scalar=ALPHA, in1=tmp.t[:],
                                                    op0=ALU.mult, op1=ALU.add),
         reads=[xin.d, tmp.d], writes=[tmp.d])
    K.layer_norm(tmp, xo, lng, lnb)


def sub_proj(K, xs, xd, NT, mT_ap, DM, wout_ap, ada_w_l, ada_b_l, lng_ap, lnb_ap):
    S, nc = K.S, K.nc
    NC_ = DM // 128
    old_scope = K.scope
    _mk = K.S.mark()
    with ExitStack() as sc:
        K.scope = sc
        wout = K.sb([128, NC_, 1024], BF16, tag="wout")
        S.dma("gpsimd", S.new_dma_sem(), wout.t[:], wout_ap.rearrange("(c p) d -> p c d", p=128), writes=[wout.d])
        g1p = K.sb([128, 1024], F32, tag="g1p")
        lng = K.sb([128, 1024], F32, tag="lng")
        lnb = K.sb([128, 1024], F32, tag="lnb")
        K.ln_alloc()
        K.mod_vec(g1p, ada_w_l, ada_b_l, 2, True)
        K.bcast_vec(lng, lng_ap, S.new_dma_sem())
        K.bcast_vec(lnb, lnb_ap, S.new_dma_sem())
        mt = [K.sb([128, NC_, 128], BF16, tag="mt") for _ in range(2)]
        msem = [S.new_dma_sem() for _ in range(2)]
        xin = [K.sb([128, 1024], F32, tag="xin") for _ in range(2)]
        xsem = [S.new_dma_sem() for _ in range(2)]
        tmp = [K.sb([128, 1024], F32, tag="tmp") for _ in range(2)]
        xo = [K.sb([128, 1024], F32, tag="xo") for _ in range(2)]
        osem = [S.new_dma_sem() for _ in range(2)]
        for ts in range(NT // 128):
            b = ts % 2
            S.dma("sync", msem[b], mt[b].t[:], mT_ap[:, ts * 128:(ts + 1) * 128].rearrange("(c p) t -> p c t", p=128),
                  writes=[mt[b].d])
            S.dma("sync", xsem[b], xin[b].t[:], xs.ap[ts * 128:(ts + 1) * 128, :], reads=[xs.ds[ts]],
                  writes=[xin[b].d])
            po = K.psum[2 + b]
            for db in range(2):
                fns = [(lambda e, c=c, db=db, po=po, b=b: e.matmul(
                    po.t[:, db * 512:(db + 1) * 512], lhsT=mt[b].t[:, c, :], rhs=wout.t[:, c, db * 512:(db + 1) * 512],
                    start=(c == 0), stop=(c == NC_ - 1))) for c in range(NC_)]
                S.group("tensor", fns, reads=[mt[b].d, wout.d], writes=[po.ds[db]])
            residual_ln(K, po.t[:], po.ds, xin[b], g1p, lng, lnb, tmp[b], xo[b])
            S.dma("gpsimd", osem[b], xd.ap[ts * 128:(ts + 1) * 128, :], xo[b].t[:], reads=[xo[b].d],
                  writes=[xd.ds[ts]])
        S.barrier()
    K.scope = old_scope
    K.S.release(_mk)


def sub_sg(K, xs, xd, NT, ada_w_l, ada_b_l, lng_ap, lnb_ap, w_in_ap, b_in_ap, sglng_ap, sglnb_ap, w_s_ap, b_s_ap,
           w_out_ap):
    S, nc = K.S, K.nc
    old_scope = K.scope
    _mk = K.S.mark()
    with ExitStack() as sc:
        K.scope = sc
        win = K.sb([128, 8, 4096], BF16, tag="win")
        S.dma("gpsimd", S.new_dma_sem(), win.t[:, :, 0:2048],
              w_in_ap[:, 0:2048].rearrange("(c p) f -> p c f", p=128), writes=[win.d])
        S.dma("gpsimd", S.new_dma_sem(), win.t[:, :, 2048:4096],
              w_in_ap[:, 2048:4096].rearrange("(c p) f -> p c f", p=128), writes=[win.d])
        wout = K.sb([128, 16, 1024], BF16, tag="wout")
        S.dma("gpsimd", S.new_dma_sem(), wout.t[:], w_out_ap.rearrange("(c p) d -> p c d", p=128), writes=[wout.d])
        binb = K.sb([1, 4096], BF16, tag="binb")
        S.dma("gpsimd", S.new_dma_sem(), binb.t[:], b_in_ap.unsqueeze(0), writes=[binb.d])
        bsb = K.sb([1, 8, 128], BF16, tag="bsb")
        S.dma("gpsimd", S.new_dma_sem(), bsb.t[:], b_s_ap.unsqueeze(0), writes=[bsb.d])
        ones = K.sb([1, 512], BF16, tag="ones")
        S.op("vector", lambda e: e.memset(ones.t[:], 1.0), writes=[ones.d])
        sglng = K.sb([128, 2048], F32, tag="sglng")
        sglnb = K.sb([128, 2048], F32, tag="sglnb")
        K.bcast_vec(sglng, sglng_ap, S.new_dma_sem())
        K.bcast_vec(sglnb, sglnb_ap, S.new_dma_sem())
        sc1p = K.sb([128, 1024], F32, tag="sc1p")
        shm = K.sb([128, 1024], F32, tag="shm")
        g1p = K.sb([128, 1024], F32, tag="g1p")
        lng = K.sb([128, 1024], F32, tag="lng")
        lnb = K.sb([128, 1024], F32, tag="lnb")
        K.ln_alloc()
        K.mod_vec(shm, ada_w_l, ada_b_l, 0, False)
        K.mod_vec(sc1p, ada_w_l, ada_b_l, 1, True)
        K.mod_vec(g1p, ada_w_l, ada_b_l, 2, True)
        K.bcast_vec(lng, lng_ap, S.new_dma_sem())
        K.bcast_vec(lnb, lnb_ap, S.new_dma_sem())
        wmT = K.sb([128, 8, 128], BF16, tag="wmT")
        sc_setup = ExitStack()
        K.scope = sc_setup
        wsf = K.sb([128, 8, 128], F32, tag="wsf")
        S.dma("sync", S.new_dma_sem(), wsf.t[:], w_s_ap.rearrange("g t s -> t g s"), writes=[wsf.d])
        ii = K.sb([128, 128], I32, tag="ii2")
        msk = K.sb([128, 128], F32, tag="msk")
        S.op("gpsimd", lambda e: e.iota(ii.t[:], pattern=[[1, 128]], base=0, channel_multiplier=-1), writes=[ii.d])
        S.op("vector", lambda e: e.tensor_single_scalar(out=msk.t[:], in_=ii.t[:], scalar=0, op=ALU.is_le),
             reads=[ii.d], writes=[msk.d])
        S.op("vector", lambda e: e.tensor_tensor(out=wsf.t[:], in0=wsf.t[:],
                                                 in1=msk.t[:].unsqueeze(1).to_broadcast([128, 8, 128]), op=ALU.mult),
             reads=[wsf.d, msk.d], writes=[wsf.d])
        pt = K.psum[0]
        for hb in range(2):
            fns = [(lambda e, g=g: e.transpose(out=pt.t[:, g * 128:(g + 1) * 128], in_=wsf.t[:, g, :],
                                               identity=K.ident.t[:])) for g in range(hb * 4, hb * 4 + 4)]
            S.group("tensor", fns, reads=[wsf.d, K.ident.d], writes=[pt.ds[hb]])
        S.op("scalar", lambda e: e.activation(out=wmT.t[:], in_=pt.t[:].rearrange("p (g t) -> p g t", g=8),
                                              func=AF.Copy), reads=[pt.ds[0], pt.ds[1]], writes=[wmT.d])
        S.barrier()
        sc_setup.close()
        K.scope = sc
        xin = K.sb([128, 1024], F32, tag="xin")
        xsem = S.new_dma_sem()
        hf = K.sb([128, 1024], F32, tag="hf")
        hmT = K.sb([128, 8, 128], BF16, tag="hmT")
        u = K.sb([128, 2048], F32, tag="u")
        v = K.sb([128, 2048], F32, n=1, tag="v")
        vn = K.sb([128, 2048], BF16, tag="vn")
        gT = Buf(vn.t, 1)
        gT.ds = vn.ds
        gTv = vn.t[:].rearrange("p (c t) -> p c t", c=16)
        xo = hf
        osem = S.new_dma_sem()
        st4 = K.sb([128, 4, 6], F32, tag="st4")
        mv = K.sb([128, 2], F32, tag="mv2")
        rs = K.sb([128, 1], F32, tag="rs2")
        for ts in range(NT // 128):
            S.dma("sync", xsem, xin.t[:], xs.ap[ts * 128:(ts + 1) * 128, :], reads=[xs.ds[ts]], writes=[xin.d])
            S.op("vector", lambda e: e.tensor_tensor(out=hf.t[:], in0=xin.t[:], in1=sc1p.t[:], op=ALU.mult),
                 reads=[xin.d, sc1p.d], writes=[hf.d])
            S.op("vector", lambda e: e.tensor_tensor(out=hf.t[:], in0=hf.t[:], in1=shm.t[:], op=ALU.add),
                 reads=[hf.d, shm.d], writes=[hf.d])
            po = K.psum[0]
            for hb in range(2):
                fns = [(lambda e, k=k: e.transpose(out=po.t[:, k * 128:(k + 1) * 128],
                                                   in_=hf.t[:, k * 128:(k + 1) * 128], identity=K.ident.t[:]))
                       for k in range(hb * 4, hb * 4 + 4)]
                S.group("tensor", fns, reads=[hf.d, K.ident.d], writes=[po.ds[hb]])
            S.op("scalar", lambda e: e.activation(out=hmT.t[:], in_=po.t[:].rearrange("p (c t) -> p c t", c=8),
                                                  func=AF.Copy), reads=[po.ds[0], po.ds[1]], writes=[hmT.d])
            for cb in range(8):
                pb = K.psum[(cb // 2) % 2 + 0]
                half = cb % 2
                fns = [(lambda e, k=k, cb=cb, pb=pb, half=half: e.matmul(
                    pb.t[:, half * 512:(half + 1) * 512], lhsT=hmT.t[:, k, :], rhs=win.t[:, k, cb * 512:(cb + 1) * 512],
                    start=(k == 0), stop=False)) for k in range(8)]
                fns.append(lambda e, cb=cb, pb=pb, half=half: e.matmul(
                    pb.t[:, half * 512:(half + 1) * 512], lhsT=ones.t[0:1, 0:128], rhs=binb.t[0:1, cb * 512:(cb + 1) * 512],
                    start=False, stop=True))
                S.group("tensor", fns, reads=[hmT.d, win.d, ones.d, binb.d], writes=[pb.ds[half]])
                dst = u if cb < 4 else v
                c0 = (cb % 4) * 512
                S.op("scalar", lambda e, pb=pb, half=half, dst=dst, c0=c0: e.activation(
                    out=dst.t[:, c0:c0 + 512], in_=pb.t[:, half * 512:(half + 1) * 512], func=AF.Gelu_apprx_tanh),
                     reads=[pb.ds[half]], writes=[dst.d])
            for q in range(4):
                S.op("vector", lambda e, q=q: e.bn_stats(out=st4.t[:, q, :], in_=v.t[:, q * 512:(q + 1) * 512]),
                     reads=[v.d], writes=[st4.d])
            S.op("vector", lambda e: e.bn_aggr(out=mv.t[:], in_=st4.t[:].rearrange("p a b -> p (a b)")),
                 reads=[st4.d], writes=[mv.d])
            S.op("scalar", lambda e: e.activation(out=rs.t[:], in_=mv.t[:, 1:2], func=AF.Sqrt, bias=K.eps_ln.t[:],
                                                  scale=1.0), reads=[mv.d, K.eps_ln.d], writes=[rs.d])
            S.op("vector", lambda e: e.reciprocal(out=rs.t[:], in_=rs.t[:]), reads=[rs.d], writes=[rs.d])
            S.op("vector", lambda e: e.tensor_scalar(out=v.t[:], in0=v.t[:], scalar1=mv.t[:, 0:1], scalar2=rs.t[:],
                                                     op0=ALU.subtract, op1=ALU.mult),
                 reads=[v.d, mv.d, rs.d], writes=[v.d])
            S.op("vector", lambda e: e.tensor_tensor(out=v.t[:], in0=v.t[:], in1=sglng.t[:], op=ALU.mult),
                 reads=[v.d, sglng.d], writes=[v.d])
            S.op("vector", lambda e: e.tensor_tensor(out=vn.t[:], in0=v.t[:], in1=sglnb.t[:], op=ALU.add),
                 reads=[v.d, sglnb.d], writes=[vn.d])
            for g in range(8):
                pm = K.psum[2 + g // 4]
                half = (g % 4) // 2
                c0 = (g % 4) * 256
                fns = [lambda e, g=g, pm=pm, c0=c0: e.matmul(pm.t[:, c0:c0 + 256], lhsT=wmT.t[:, g, :],
                                                              rhs=vn.t[:, g * 256:(g + 1) * 256], start=True, stop=False),
                       lambda e, g=g, pm=pm, c0=c0: e.matmul(pm.t[:, c0:c0 + 256], lhsT=bsb.t[0:1, g, :],
                                                              rhs=ones.t[0:1, 0:256], start=False, stop=True)]
                S.group("tensor", fns, reads=[wmT.d, vn.d, bsb.d, ones.d], writes=[pm.ds[half]])
            for h2 in range(2):
                pm = K.psum[2 + h2]
                S.op("vector", lambda e, pm=pm, h2=h2: e.tensor_tensor(
                    out=v.t[:, h2 * 1024:(h2 + 1) * 1024], in0=pm.t[:], in1=u.t[:, h2 * 1024:(h2 + 1) * 1024],
                    op=ALU.mult), reads=[pm.ds[0], pm.ds[1], u.d], writes=[v.d])
            for h2 in range(2):
                pt2 = K.psum[h2]
                for hb in range(2):
                    fns = [(lambda e, c=c, pt2=pt2, h2=h2: e.transpose(
                        out=pt2.t[:, (c % 8) * 128:(c % 8 + 1) * 128], in_=v.t[:, c * 128:(c + 1) * 128],
                        identity=K.ident.t[:])) for c in range(h2 * 8 + hb * 4, h2 * 8 + hb * 4 + 4)]
                    S.group("tensor", fns, reads=[v.d, K.ident.d], writes=[pt2.ds[hb]])
                S.op("scalar", lambda e, pt2=pt2, h2=h2: e.activation(
                    out=gTv[:, h2 * 8:(h2 + 1) * 8, :], in_=pt2.t[:].rearrange("p (c t) -> p c t", c=8),
                    func=AF.Copy), reads=[pt2.ds[0], pt2.ds[1]], writes=[gT.d])
            py = K.psum[2]
            for db in range(2):
                fns = [(lambda e, c=c, db=db: e.matmul(py.t[:, db * 512:(db + 1) * 512], lhsT=gTv[:, c, :],
                                                        rhs=wout.t[:, c, db * 512:(db + 1) * 512], start=(c == 0),
                                                        stop=(c == 15))) for c in range(16)]
                S.group("tensor", fns, reads=[gT.d, wout.d], writes=[py.ds[db]])
            residual_ln(K, py.t[:], py.ds, xin, g1p, lng, lnb, hf, xo)
            S.dma("gpsimd", osem, xd.ap[ts * 128:(ts + 1) * 128, :], xo.t[:], reads=[xo.d], writes=[xd.ds[ts]])
        S.barrier()
    K.scope = old_scope
    K.S.release(_mk)


def build_ssd(NTok):
    K = KB()
    x = K.din("x", [NTok, D]); c = K.din("c", [D]); aw = K.din("aw", [D, 6 * D]); ab = K.din("ab", [6 * D])
    w = K.din("w", [D, 1544]); cw = K.din("cw", [4, 1024]); cbv = K.din("cb", [1024])
    dtb = K.din("dtb", [8]); alog = K.din("alog", [8]); dsk = K.din("dsk", [8]); nw = K.din("nw", [512])
    out = K.dout("mT", [512, NTok], BF16)
    K.alloc_psum(); K.consts(); K.setup_cond(c)
    sub_ssd(K, x, out, NTok, aw, ab, w, cw, cbv, dtb, alog, dsk, nw)
    K.S.finish()
    return K


def sub_ssd(K, x, out, NTok, aw, ab, w, cw, cbv, dtb, alog, dsk, nw):
    S, nc = K.S, K.nc
    X = mybir.AxisListType.X
    P0, P1, P2, P3 = K.psum
    old_scope = K.scope
    _mk = K.S.mark()
    sc_ssd = ExitStack()
    K.scope = sc_ssd
    wz = K.sb([128, 8, 512], BF16, tag="wz")
    wx = K.sb([128, 8, 1024], BF16, tag="wx")
    wdt = K.sb([128, 8, 8], BF16, tag="wdt")
    S.dma("gpsimd", S.new_dma_sem(), wz.t[:], w[:, 0:512].rearrange("(c p) f -> p c f", p=128), writes=[wz.d])
    S.dma("gpsimd", S.new_dma_sem(), wx.t[:], w[:, 512:1536].rearrange("(c p) f -> p c f", p=128), writes=[wx.d])
    with nc.allow_non_contiguous_dma(reason="tiny"):
        S.dma("gpsimd", S.new_dma_sem(), wdt.t[:], w[:, 1536:1544].rearrange("(c p) f -> p c f", p=128),
              writes=[wdt.d])
    cwT = K.sb([128, 4, 8], F32, tag="cwT")
    cbT = K.sb([128, 8], F32, tag="cbT")
    with nc.allow_non_contiguous_dma(reason="tiny"):
        S.dma("sync", S.new_dma_sem(), cwT.t[:], cw.rearrange("k (c p) -> p k c", p=128), writes=[cwT.d])
        S.dma("sync", S.new_dma_sem(), cbT.t[:], cbv.rearrange("(c p) -> p c", p=128), writes=[cbT.d])
    dtb_bc = K.sb([128, 8], F32, tag="dtb"); A_bc = K.sb([128, 8], F32, tag="A"); dsk_bc = K.sb([128, 8], F32, tag="dsk")
    nw_bc = K.sb([128, 512], F32, tag="nw")
    K.bcast_vec(dtb_bc, dtb, S.new_dma_sem()); K.bcast_vec(A_bc, alog, S.new_dma_sem())
    K.bcast_vec(dsk_bc, dsk, S.new_dma_sem()); K.bcast_vec(nw_bc, nw, S.new_dma_sem())
    S.op("scalar", lambda e: e.activation(out=A_bc.t[:], in_=A_bc.t[:], func=AF.Exp), reads=[A_bc.d], writes=[A_bc.d])
    S.op("vector", lambda e: e.tensor_scalar(out=A_bc.t[:], in0=A_bc.t[:], scalar1=-1.0, scalar2=None, op0=ALU.mult),
         reads=[A_bc.d], writes=[A_bc.d])
    sc1p = K.sb([128, 1024], F32, tag="sc1p"); shm = K.sb([128, 1024], F32, tag="shm")
    K.mod_vec(shm, aw, ab, 0, False)
    K.mod_vec(sc1p, aw, ab, 1, True)
    ii = K.sb([128, 128], I32, tag="ii3")
    triU = K.sb([128, 128], F32, tag="triU")
    ones = K.sb([128, 128], F32, tag="ones")
    eps_r = K.sb([128, 1], F32, tag="epsr")
    S.op("gpsimd", lambda e: e.iota(ii.t[:], pattern=[[1, 128]], base=0, channel_multiplier=-1), writes=[ii.d])
    S.op("vector", lambda e: e.tensor_single_scalar(out=triU.t[:], in_=ii.t[:], scalar=0, op=ALU.is_ge),
         reads=[ii.d], writes=[triU.d])
    S.op("vector", lambda e: e.memset(ones.t[:], 1.0), writes=[ones.d])
    S.op("vector", lambda e: e.memset(eps_r.t[:], RMS_EPS), writes=[eps_r.d])
    S32 = K.sb([128, 8, 64], F32, tag="S32"); Sbf = K.sb([128, 8, 64], BF16, tag="Sbf")
    S.op("vector", lambda e: e.memset(S32.t[:], 0.0), writes=[S32.d])
    S.op("vector", lambda e: e.memset(Sbf.t[:], 0.0), writes=[Sbf.d])
    xr = K.sb([128, 8, 131], F32, tag="xr")
    S.op("vector", lambda e: e.memset(xr.t[:], 0.0), writes=[xr.d])
    xin2 = [K.sb([128, 1024], F32, tag="xin") for _ in range(2)]; xsem2 = [S.new_dma_sem() for _ in range(2)]
    hf2 = [K.sb([128, 1024], F32, tag="hf") for _ in range(2)]
    hmT2 = [K.sb([128, 8, 128], BF16, tag="hmT") for _ in range(2)]
    cacc = K.sb([128, 8, 128], F32, tag="cacc"); ctmp = K.sb([128, 8, 128], F32, tag="ctmp")
    xa = K.sb([128, 8, 128], F32, tag="xa")
    bcT = K.sb([128, 4, 128], BF16, tag="bcT")
    xtok = K.sb([128, 768], F32, tag="xtok")
    btok = K.sb([128, 256], BF16, tag="btok")
    dtv = K.sb([128, 8], F32, tag="dtv"); dtA = K.sb([128, 8], F32, tag="dtA")
    dtAb = K.sb([128, 8, 128], F32, tag="dtAb")
    acs = K.sb([128, 24], F32, tag="acs")
    dte = K.sb([128, 8], F32, tag="dte"); cd = K.sb([128, 8], F32, tag="cd")
    Lx = K.sb([128, 8, 128], F32, tag="Lx"); Eb = K.sb([128, 8, 128], F32, tag="Eb")
    cbm = K.sb([128, 2, 128], F32, tag="cbm")
    MT = K.sb([128, 8, 128], BF16, tag="MT"); CsT = K.sb([128, 8, 128], BF16, tag="CsT")
    xdt = K.sb([128, 8, 64], BF16, tag="xdt"); xdte = K.sb([128, 8, 64], BF16, tag="xdte")
    y = K.sb([128, 512], F32, tag="y"); sz = K.sb([128, 512], F32, tag="sz"); sq = K.sb([128, 512], F32, tag="sq")
    ss = K.sb([128, 2], F32, tag="ss")
    ygT = K.sb([128, 4, 128], BF16, tag="ygT"); osem = S.new_dma_sem()
    bc3 = lambda ap, n: ap.unsqueeze(2).to_broadcast([128, 8, n])

    def head(ck):
        xin, hf, hmT, xsem = xin2[ck % 2], hf2[ck % 2], hmT2[ck % 2], xsem2[ck % 2]
        S.dma("sync", xsem, xin.t[:], (x(ck) if callable(x) else x[ck * 128:(ck + 1) * 128, :]), writes=[xin.d])
        S.op("vector", lambda e: e.tensor_tensor(out=hf.t[:], in0=xin.t[:], in1=sc1p.t[:], op=ALU.mult),
             reads=[xin.d, sc1p.d], writes=[hf.d])
        S.op("vector", lambda e: e.tensor_tensor(out=hf.t[:], in0=hf.t[:], in1=shm.t[:], op=ALU.add),
             reads=[hf.d, shm.d], writes=[hf.d])
        for hb in range(2):
            fns = [(lambda e, k=k: e.transpose(out=P0.t[:, k * 128:(k + 1) * 128], in_=hf.t[:, k * 128:(k + 1) * 128],
                                               identity=K.ident.t[:])) for k in range(hb * 4, hb * 4 + 4)]
            S.group("tensor", fns, reads=[hf.d, K.ident.d], writes=[P0.ds[hb]])
        S.op("scalar", lambda e: e.activation(out=hmT.t[:], in_=P0.t[:].rearrange("p (c t) -> p c t", c=8),
                                              func=AF.Copy), reads=[P0.ds[0], P0.ds[1]], writes=[hmT.d])

    NCK = NTok // 128
    head(0)
    for ck in range(NCK):
        hmT = hmT2[ck % 2]
        fns = [(lambda e, k=k: e.matmul(P1.t[:, 0:512], lhsT=hmT.t[:, k, :], rhs=wz.t[:, k, :], start=(k == 0),
                                        stop=(k == 7))) for k in range(8)]
        S.group("tensor", fns, reads=[hmT.d, wz.d], writes=[P1.ds[0]])
        fns = [(lambda e, k=k: e.matmul(P1.t[:, 512:520], lhsT=hmT.t[:, k, :], rhs=wdt.t[:, k, :], start=(k == 0),
                                        stop=(k == 7))) for k in range(8)]
        S.group("tensor", fns, reads=[hmT.d, wdt.d], writes=[P1.ds[1]])
        for hb in range(2):
            fns = []
            for ch in range(hb * 4, hb * 4 + 4):
                fns += [(lambda e, k=k, ch=ch: e.matmul(P2.t[:, ch * 128:(ch + 1) * 128],
                                                        lhsT=wx.t[:, k, ch * 128:(ch + 1) * 128], rhs=hmT.t[:, k, :],
                                                        start=(k == 0), stop=(k == 7))) for k in range(8)]
            S.group("tensor", fns, reads=[hmT.d, wx.d], writes=[P2.ds[hb]])
        S.op("scalar", lambda e: e.activation(out=xr.t[:, :, 3:131], in_=P2.t[:].rearrange("p (c t) -> p c t", c=8),
                                              func=AF.Copy), reads=[P2.ds[0], P2.ds[1]], writes=[xr.d])
        S.op("vector", lambda e: e.tensor_tensor(out=dtv.t[:], in0=P1.t[:, 512:520], in1=dtb_bc.t[:], op=ALU.add),
             reads=[P1.ds[1], dtb_bc.d], writes=[dtv.d])
        S.op("scalar", lambda e: e.activation(out=dtv.t[:], in_=dtv.t[:], func=AF.Exp), reads=[dtv.d], writes=[dtv.d])
        S.op("scalar", lambda e: e.activation(out=dtv.t[:], in_=dtv.t[:], func=AF.Ln, bias=1.0, scale=1.0),
             reads=[dtv.d], writes=[dtv.d])
        S.op("vector", lambda e: e.tensor_tensor(out=dtA.t[:], in0=dtv.t[:], in1=A_bc.t[:], op=ALU.mult),
             reads=[dtv.d, A_bc.d], writes=[dtA.d])
        S.op("vector", lambda e: e.tensor_copy(out=dtAb.t[:], in_=bc3(dtA.t[:], 128)), reads=[dtA.d], writes=[dtAb.d])
        S.op("tensor", lambda e: e.matmul(P1.t[:, 520:528], lhsT=triU.t[:], rhs=dtA.t[:], start=True, stop=True),
             reads=[triU.d, dtA.d], writes=[P1.ds[1]])
        S.op("tensor", lambda e: e.matmul(P1.t[:, 528:536], lhsT=ones.t[:], rhs=dtA.t[:], start=True, stop=True),
             reads=[ones.d, dtA.d], writes=[P1.ds[1]])
        S.op("scalar", lambda e: e.activation(out=acs.t[:, 0:16], in_=P1.t[:, 520:536], func=AF.Copy),
             reads=[P1.ds[1]], writes=[acs.d])
        for hb in range(2):
            fns = [(lambda e, h=h: e.matmul(P0.t[:, h * 128:(h + 1) * 128], lhsT=dtAb.t[:, h, :], rhs=triU.t[:],
                                            start=True, stop=True)) for h in range(hb * 4, hb * 4 + 4)]
            S.group("tensor", fns, reads=[dtAb.d, triU.d], writes=[P0.ds[hb]])
        P0v = P0.t[:].rearrange("p (h l) -> p h l", h=8)
        S.op("vector", lambda e: e.tensor_tensor(out=Lx.t[:], in0=P0v, in1=bc3(acs.t[:, 0:8], 128), op=ALU.subtract),
             reads=[P0.ds[0], P0.ds[1], acs.d], writes=[Lx.d])
        S.op("vector", lambda e: e.tensor_scalar(out=Lx.t[:], in0=Lx.t[:], scalar1=0.0, scalar2=None, op0=ALU.min),
             reads=[Lx.d], writes=[Lx.d])
        S.op("scalar", lambda e: e.activation(out=Lx.t[:], in_=Lx.t[:], func=AF.Exp), reads=[Lx.d], writes=[Lx.d])
        S.op("scalar", lambda e: e.activation(out=Eb.t[:], in_=P0v, func=AF.Exp), reads=[P0.ds[0], P0.ds[1]],
             writes=[Eb.d])
        if ck + 1 < NCK:
            head(ck + 1)
        S.op("vector", lambda e: e.tensor_tensor(out=dte.t[:], in0=acs.t[:, 8:16], in1=acs.t[:, 0:8], op=ALU.subtract),
             reads=[acs.d], writes=[dte.d])
        S.op("scalar", lambda e: e.activation(out=dte.t[:], in_=dte.t[:], func=AF.Exp), reads=[dte.d], writes=[dte.d])
        S.op("scalar", lambda e: e.activation(out=cd.t[:], in_=acs.t[:, 8:16], func=AF.Exp), reads=[acs.d], writes=[cd.d])
        for k in range(4):
            src = xr.t[:, :, k:k + 128]
            wk = bc3(cwT.t[:, k, :], 128)
            if k == 0:
                S.op("vector", lambda e, src=src, wk=wk: e.tensor_tensor(out=cacc.t[:], in0=src, in1=wk, op=ALU.mult),
                     reads=[xr.d, cwT.d], writes=[cacc.d])
            else:
                S.op("vector", lambda e, src=src, wk=wk: e.tensor_tensor(out=ctmp.t[:], in0=src, in1=wk, op=ALU.mult),
                     reads=[xr.d, cwT.d], writes=[ctmp.d])
                S.op("vector", lambda e: e.tensor_tensor(out=cacc.t[:], in0=cacc.t[:], in1=ctmp.t[:], op=ALU.add),
                     reads=[cacc.d, ctmp.d], writes=[cacc.d])
        S.op("vector", lambda e: e.tensor_copy(out=xr.t[:, :, 0:3], in_=xr.t[:, :, 128:131]), reads=[xr.d], writes=[xr.d])
        for ch in range(8):
            S.op("scalar", lambda e, ch=ch: e.activation(out=xa.t[:, ch, :], in_=cacc.t[:, ch, :], func=AF.Silu,
                                                         bias=cbT.t[:, ch:ch + 1], scale=1.0),
                 reads=[cacc.d, cbT.d], writes=[xa.d])
        S.op("vector", lambda e: e.tensor_copy(out=bcT.t[:], in_=xa.t[:, 4:8, :]), reads=[xa.d], writes=[bcT.d])
        for hb in range(2):
            rng_ = range(0, 4) if hb == 0 else range(4, 6)
            fns = [(lambda e, j=j: e.transpose(out=P2.t[:, j * 128:(j + 1) * 128], in_=xa.t[:, j, :],
                                               identity=K.ident.t[:])) for j in rng_]
            S.group("tensor", fns, reads=[xa.d, K.ident.d], writes=[P2.ds[hb]])
        S.op("scalar", lambda e: e.activation(out=xtok.t[:], in_=P2.t[:, 0:768], func=AF.Copy),
             reads=[P2.ds[0], P2.ds[1]], writes=[xtok.d])
        S.op("vector", lambda e: e.tensor_copy(out=btok.t[:], in_=xtok.t[:, 512:768]), reads=[xtok.d], writes=[btok.d])
        xt3 = xtok.t[:, 0:512].rearrange("p (h d) -> p h d", h=8)
        S.op("vector", lambda e: e.tensor_tensor(out=xdt.t[:], in0=xt3, in1=bc3(dtv.t[:], 64), op=ALU.mult),
             reads=[xtok.d, dtv.d], writes=[xdt.d])
        S.op("vector", lambda e: e.tensor_tensor(out=dte.t[:], in0=dte.t[:], in1=dtv.t[:], op=ALU.mult),
             reads=[dte.d, dtv.d], writes=[dte.d])
        S.op("vector", lambda e: e.tensor_tensor(out=xdte.t[:], in0=xt3, in1=bc3(dte.t[:], 64), op=ALU.mult),
             reads=[xtok.d, dte.d], writes=[xdte.d])
        fns = [(lambda e, g=g: e.matmul(P3.t[:, g * 128:(g + 1) * 128], lhsT=bcT.t[:, g, :], rhs=bcT.t[:, 2 + g, :],
                                        start=True, stop=True)) for g in range(2)]
        S.group("tensor", fns, reads=[bcT.d], writes=[P3.ds[0]])
        S.op("vector", lambda e: e.tensor_tensor(out=cbm.t[:], in0=P3.t[:, 0:256].rearrange("p (g l) -> p g l", g=2),
                                                 in1=triU.t[:].unsqueeze(1).to_broadcast([128, 2, 128]), op=ALU.mult),
             reads=[P3.ds[0], triU.d], writes=[cbm.d])
        for g in range(2):
            S.op("vector", lambda e, g=g: e.tensor_tensor(
                out=MT.t[:, 4 * g:4 * g + 4, :], in0=Lx.t[:, 4 * g:4 * g + 4, :],
                in1=cbm.t[:, g, :].unsqueeze(1).to_broadcast([128, 4, 128]), op=ALU.mult),
                 reads=[Lx.d, cbm.d], writes=[MT.d])
            S.op("vector", lambda e, g=g: e.tensor_tensor(
                out=CsT.t[:, 4 * g:4 * g + 4, :], in0=Eb.t[:, 4 * g:4 * g + 4, :],
                in1=xa.t[:, 6 + g, :].unsqueeze(1).to_broadcast([128, 4, 128]), op=ALU.mult),
                 reads=[Eb.d, xa.d], writes=[CsT.d])
        fns = []
        for h in range(8):
            fns.append(lambda e, h=h: e.matmul(P2.t[:, h * 64:(h + 1) * 64], lhsT=MT.t[:, h, :], rhs=xdt.t[:, h, :],
                                               start=True, stop=False))
            fns.append(lambda e, h=h: e.matmul(P2.t[:, h * 64:(h + 1) * 64], lhsT=CsT.t[:, h, :], rhs=Sbf.t[:, h, :],
                                               start=False, stop=True))
        S.group("tensor", fns, reads=[MT.d, xdt.d, CsT.d, Sbf.d], writes=[P2.ds[0]])
        fns = [(lambda e, h=h: e.matmul(P2.t[:, 512 + h * 64:512 + (h + 1) * 64], lhsT=btok.t[:, (h // 4) * 128:(h // 4 + 1) * 128],
                                        rhs=xdte.t[:, h, :], start=True, stop=True)) for h in range(8)]
        S.group("tensor", fns, reads=[btok.d, xdte.d], writes=[P2.ds[1]])
        S.op("vector", lambda e: e.tensor_tensor(out=S32.t[:], in0=S32.t[:], in1=bc3(cd.t[:], 64), op=ALU.mult),
             reads=[S32.d, cd.d], writes=[S32.d])
        S.op("vector", lambda e: e.tensor_tensor(out=S32.t[:], in0=S32.t[:],
                                                 in1=P2.t[:, 512:1024].rearrange("p (h d) -> p h d", h=8), op=ALU.add),
             reads=[S32.d, P2.ds[1]], writes=[S32.d])
        S.op("vector", lambda e: e.tensor_copy(out=Sbf.t[:], in_=S32.t[:]), reads=[S32.d], writes=[Sbf.d])
        S.op("vector", lambda e: e.tensor_tensor(out=y.t[:].rearrange("p (h d) -> p h d", h=8), in0=xt3,
                                                 in1=bc3(dsk_bc.t[:], 64), op=ALU.mult),
             reads=[xtok.d, dsk_bc.d], writes=[y.d])
        S.op("vector", lambda e: e.tensor_tensor(out=y.t[:], in0=y.t[:], in1=P2.t[:, 0:512], op=ALU.add),
             reads=[y.d, P2.ds[0]], writes=[y.d])
        S.op("scalar", lambda e: e.activation(out=sz.t[:], in_=P1.t[:, 0:512], func=AF.Silu), reads=[P1.ds[0]],
             writes=[sz.d])
        S.op("vector", lambda e: e.tensor_tensor(out=y.t[:], in0=y.t[:], in1=sz.t[:], op=ALU.mult),
             reads=[y.d, sz.d], writes=[y.d])
        S.op("vector", lambda e: e.tensor_tensor(out=sq.t[:], in0=y.t[:], in1=y.t[:], op=ALU.mult),
             reads=[y.d], writes=[sq.d])
        S.op("vector", lambda e: e.tensor_reduce(out=ss.t[:], in_=sq.t[:].rearrange("p (g d) -> p g d", g=2), axis=X,
                                                 op=ALU.add), reads=[sq.d], writes=[ss.d])
        S.op("scalar", lambda e: e.activation(out=ss.t[:], in_=ss.t[:], func=AF.Ln, bias=eps_r.t[:], scale=1.0 / 256.0),
             reads=[ss.d, eps_r.d], writes=[ss.d])
        S.op("scalar", lambda e: e.activation(out=ss.t[:], in_=ss.t[:], func=AF.Exp, scale=-0.5),
             reads=[ss.d], writes=[ss.d])
        S.op("vector", lambda e: e.tensor_tensor(out=y.t[:].rearrange("p (g d) -> p g d", g=2),
                                                 in0=y.t[:].rearrange("p (g d) -> p g d", g=2),
                                                 in1=ss.t[:].unsqueeze(2).to_broadcast([128, 2, 256]), op=ALU.mult),
             reads=[y.d, ss.d], writes=[y.d])
        S.op("vector", lambda e: e.tensor_tensor(out=y.t[:], in0=y.t[:], in1=nw_bc.t[:], op=ALU.mult),
             reads=[y.d, nw_bc.d], writes=[y.d])
        fns = [(lambda e, j=j: e.transpose(out=P3.t[:, 512 + j * 128:512 + (j + 1) * 128], in_=y.t[:, j * 128:(j + 1) * 128],
                                           identity=K.ident.t[:])) for j in range(4)]
        S.group("tensor", fns, reads=[y.d, K.ident.d], writes=[P3.ds[1]])
        S.op("scalar", lambda e: e.activation(out=ygT.t[:], in_=P3.t[:, 512:1024].rearrange("p (c t) -> p c t", c=4),
                                              func=AF.Copy), reads=[P3.ds[1]], writes=[ygT.d])
        S.dma("gpsimd", osem, out[:, ck * 128:(ck + 1) * 128].rearrange("(c p) t -> p c t", p=128), ygT.t[:],
              reads=[ygT.d])
    S.barrier()
    sc_ssd.close()
    K.scope = old_scope
    K.S.release(_mk)


def build_mla(NTok):
    K = KB()
    x = K.din("x", [NTok, D]); c = K.din("c", [D]); aw = K.din("aw", [D, 6 * D]); ab = K.din("ab", [6 * D])
    pos = K.din("pos", [NTok], I32)
    w_in = K.din("w_in", [D, 800]); qn = K.din("qn", [512]); kvn = K.din("kvn", [256])
    wuq_d = K.din("wuq", [512, 384]); wukv_d = K.din("wukv", [256, 512])
    out = K.dout("mT", [256, NTok], BF16)
    K.alloc_psum(); K.consts(); K.setup_cond(c)
    sub_mla(K, x, out, NTok, aw, ab, pos, w_in, qn, kvn, wuq_d, wukv_d)
    K.S.finish()
    return K


def sub_mla(K, x, out, NTok, aw, ab, pos, w_in, qn, kvn, wuq_d, wukv_d):
    S, nc = K.S, K.nc
    X = mybir.AxisListType.X
    NTL = NTok // 128
    QT_d = K.dtmp("QT_d", [4, 96, NTok], BF16); KT_d = K.dtmp("KT_d", [4, 96, NTok], BF16)
    V_d = K.dtmp("V_d", [NTL, 128, 4, 65], BF16)
    P0, P1, P2, P3 = K.psum
    old_scope = K.scope
    _mk = K.S.mark()
    SCALE = 96.0 ** -0.5
    TWO_PI = 2.0 * math.pi
    with ExitStack() as sc:
        K.scope = sc
        win = K.sb([128, 8, 800], BF16, tag="win")
        wuq = K.sb([128, 4, 384], BF16, tag="wuq"); wukv = K.sb([128, 2, 512], BF16, tag="wukv")
        S.dma("gpsimd", S.new_dma_sem(), win.t[:], w_in.rearrange("(c p) f -> p c f", p=128), writes=[win.d])
        S.dma("gpsimd", S.new_dma_sem(), wuq.t[:], wuq_d.rearrange("(c p) f -> p c f", p=128), writes=[wuq.d])
        S.dma("gpsimd", S.new_dma_sem(), wukv.t[:], wukv_d.rearrange("(c p) f -> p c f", p=128), writes=[wukv.d])
        nbc = K.sb([128, 768], F32, tag="nbc")
        S.dma("sync", S.new_dma_sem(), nbc.t[:, 0:512], qn.unsqueeze(0).to_broadcast([128, 512]), writes=[nbc.d])
        S.dma("sync", S.new_dma_sem(), nbc.t[:, 512:768], kvn.unsqueeze(0).to_broadcast([128, 256]), writes=[nbc.d])
        sc1p = K.sb([128, 1024], F32, tag="sc1p"); shm = K.sb([128, 1024], F32, tag="shm")
        K.mod_vec(shm, aw, ab, 0, False)
        K.mod_vec(sc1p, aw, ab, 1, True)
        eps_r = K.sb([128, 1], F32, tag="epsr")
        S.op("vector", lambda e: e.memset(eps_r.t[:], RMS_EPS), writes=[eps_r.d])
        ji = K.sb([128, 16], I32, tag="ji"); freq = K.sb([128, 16], F32, tag="freq")
        S.op("gpsimd", lambda e: e.iota(ji.t[:], pattern=[[1, 16]], base=0, channel_multiplier=0), writes=[ji.d])
        S.op("vector", lambda e: e.tensor_copy(out=freq.t[:], in_=ji.t[:]), reads=[ji.d], writes=[freq.d])
        S.op("scalar", lambda e: e.activation(out=freq.t[:], in_=freq.t[:], func=AF.Exp,
                                              scale=-math.log(10000.0) / 16.0), reads=[freq.d], writes=[freq.d])
        xin2 = [K.sb([128, 1024], F32, tag="xin") for _ in range(2)]; xsem2 = [S.new_dma_sem() for _ in range(2)]
        hf2 = [K.sb([128, 1024], F32, tag="hf") for _ in range(2)]
        hmT2 = [K.sb([128, 8, 128], BF16, tag="hmT") for _ in range(2)]
        posi2 = [K.sb([128, 1], I32, tag="posi") for _ in range(2)]; psem2 = [S.new_dma_sem() for _ in range(2)]
        lat = K.sb([128, 800], F32, tag="lat"); sq = K.sb([128, 768], F32, tag="sq")
        ss = K.sb([128, 2], F32, tag="ss"); nrm = K.sb([128, 768], F32, tag="nrm"); nT = K.sb([128, 6, 128], BF16, tag="nT")
        posf = K.sb([128, 1], F32, tag="posf")
        tt = K.sb([128, 32], F32, tag="tt"); ti = K.sb([128, 32], I32, tag="ti"); tf = K.sb([128, 32], F32, tag="tf")
        mm = K.sb([128, 32], F32, tag="mm"); scs = K.sb([128, 32], F32, tag="scs")
        qf = K.sb([128, 4, 96], F32, tag="qf"); kvf = K.sb([128, 4, 128], F32, tag="kvf")
        Qh = K.sb([128, 4, 96], F32, tag="Qh"); Kh = K.sb([128, 4, 96], F32, tag="Kh")
        ra = K.sb([128, 4, 16], F32, tag="ra"); rb = K.sb([128, 4, 16], F32, tag="rb")
        kr = K.sb([128, 32], F32, tag="kr")
        Va = K.sb([128, 4, 65], BF16, tag="Va")
        S.op("vector", lambda e: e.memset(Va.t[:], 1.0), writes=[Va.d])
        QTs = K.sb([128, 4, 128], BF16, tag="QTs"); KTs = K.sb([128, 4, 128], BF16, tag="KTs")
        qsem = S.new_dma_sem(); ksem = S.new_dma_sem(); vsem = S.new_dma_sem()
        b4 = lambda ap: ap.unsqueeze(1).to_broadcast([128, 4, 16])

        def rope(src1, src2, dst1, dst2, cosb, sinb, shape_bc):
            S.op("vector", lambda e: e.tensor_tensor(out=ra.t[:] if shape_bc else ra.t[:, 0, :], in0=src1, in1=cosb, op=ALU.mult),
                 reads=[qf.d, lat.d, scs.d], writes=[ra.d])
            S.op("vector", lambda e: e.tensor_tensor(out=rb.t[:] if shape_bc else rb.t[:, 0, :], in0=src2, in1=sinb, op=ALU.mult),
                 reads=[qf.d, lat.d, scs.d], writes=[rb.d])
            S.op("vector", lambda e: e.tensor_tensor(out=dst1, in0=ra.t[:] if shape_bc else ra.t[:, 0, :],
                                                     in1=rb.t[:] if shape_bc else rb.t[:, 0, :], op=ALU.subtract),
                 reads=[ra.d, rb.d], writes=[Qh.d, kr.d])
            S.op("vector", lambda e: e.tensor_tensor(out=ra.t[:] if shape_bc else ra.t[:, 0, :], in0=src2, in1=cosb, op=ALU.mult),
                 reads=[qf.d, lat.d, scs.d], writes=[ra.d])
            S.op("vector", lambda e: e.tensor_tensor(out=rb.t[:] if shape_bc else rb.t[:, 0, :], in0=src1, in1=sinb, op=ALU.mult),
                 reads=[qf.d, lat.d, scs.d], writes=[rb.d])
            S.op("vector", lambda e: e.tensor_tensor(out=dst2, in0=ra.t[:] if shape_bc else ra.t[:, 0, :],
                                                     in1=rb.t[:] if shape_bc else rb.t[:, 0, :], op=ALU.add),
                 reads=[ra.d, rb.d], writes=[Qh.d, kr.d])

        def head(t):
            xin, hf, hmT, xsem = xin2[t % 2], hf2[t % 2], hmT2[t % 2], xsem2[t % 2]
            S.dma("sync", xsem, xin.t[:], (x(t) if callable(x) else x[t * 128:(t + 1) * 128, :]), writes=[xin.d])
            with nc.allow_non_contiguous_dma(reason="positions column"):
                S.dma("sync", psem2[t % 2], posi2[t % 2].t[:], pos[t * 128:(t + 1) * 128].unsqueeze(1),
                      writes=[posi2[t % 2].d])
            S.op("vector", lambda e: e.tensor_tensor(out=hf.t[:], in0=xin.t[:], in1=sc1p.t[:], op=ALU.mult),
                 reads=[xin.d, sc1p.d], writes=[hf.d])
            S.op("vector", lambda e: e.tensor_tensor(out=hf.t[:], in0=hf.t[:], in1=shm.t[:], op=ALU.add),
                 reads=[hf.d, shm.d], writes=[hf.d])
            for hb in range(2):
                fns = [(lambda e, k=k: e.transpose(out=P0.t[:, k * 128:(k + 1) * 128], in_=hf.t[:, k * 128:(k + 1) * 128],
                                                   identity=K.ident.t[:])) for k in range(hb * 4, hb * 4 + 4)]
                S.group("tensor", fns, reads=[hf.d, K.ident.d], writes=[P0.ds[hb]])
            S.op("scalar", lambda e: e.activation(out=hmT.t[:], in_=P0.t[:].rearrange("p (c t) -> p c t", c=8),
                                                  func=AF.Copy), reads=[P0.ds[0], P0.ds[1]], writes=[hmT.d])

        head(0)
        for t in range(NTL):
            hmT = hmT2[t % 2]
            posi = posi2[t % 2]
            fns = [(lambda e, k=k: e.matmul(P1.t[:, 0:512], lhsT=hmT.t[:, k, :], rhs=win.t[:, k, 0:512], start=(k == 0),
                                            stop=(k == 7))) for k in range(8)]
            S.group("tensor", fns, reads=[hmT.d, win.d], writes=[P1.ds[0]])
            fns = [(lambda e, k=k: e.matmul(P1.t[:, 512:800], lhsT=hmT.t[:, k, :], rhs=win.t[:, k, 512:800], start=(k == 0),
                                            stop=(k == 7))) for k in range(8)]
            S.group("tensor", fns, reads=[hmT.d, win.d], writes=[P1.ds[1]])
            S.op("scalar", lambda e: e.activation(out=lat.t[:], in_=P1.t[:, 0:800], func=AF.Copy),
                 reads=[P1.ds[0], P1.ds[1]], writes=[lat.d])
            S.op("vector", lambda e: e.tensor_tensor(out=sq.t[:], in0=lat.t[:, 0:768], in1=lat.t[:, 0:768], op=ALU.mult),
                 reads=[lat.d], writes=[sq.d])
            S.op("vector", lambda e: e.tensor_reduce(out=ss.t[:, 0:1], in_=sq.t[:, 0:512], axis=X, op=ALU.add),
                 reads=[sq.d], writes=[ss.d])
            S.op("vector", lambda e: e.tensor_reduce(out=ss.t[:, 1:2], in_=sq.t[:, 512:768], axis=X, op=ALU.add),
                 reads=[sq.d], writes=[ss.d])
            S.op("scalar", lambda e: e.activation(out=ss.t[:, 0:1], in_=ss.t[:, 0:1], func=AF.Sqrt, bias=eps_r.t[:],
                                                  scale=1.0 / 512.0), reads=[ss.d, eps_r.d], writes=[ss.d])
            S.op("scalar", lambda e: e.activation(out=ss.t[:, 1:2], in_=ss.t[:, 1:2], func=AF.Sqrt, bias=eps_r.t[:],
                                                  scale=1.0 / 256.0), reads=[ss.d, eps_r.d], writes=[ss.d])
            S.op("vector", lambda e: e.reciprocal(out=ss.t[:], in_=ss.t[:]), reads=[ss.d], writes=[ss.d])
            S.op("vector", lambda e: e.scalar_tensor_tensor(out=nrm.t[:, 0:512], in0=lat.t[:, 0:512], scalar=ss.t[:, 0:1],
                                                            in1=nbc.t[:, 0:512], op0=ALU.mult, op1=ALU.mult),
                 reads=[lat.d, ss.d, nbc.d], writes=[nrm.d])
            S.op("vector", lambda e: e.scalar_tensor_tensor(out=nrm.t[:, 512:768], in0=lat.t[:, 512:768],
                                                            scalar=ss.t[:, 1:2], in1=nbc.t[:, 512:768], op0=ALU.mult,
                                                            op1=ALU.mult), reads=[lat.d, ss.d, nbc.d], writes=[nrm.d])
            for hb in range(2):
                rng_ = range(0, 4) if hb == 0 else range(4, 6)
                fns = [(lambda e, j=j: e.transpose(out=P0.t[:, j * 128:(j + 1) * 128], in_=nrm.t[:, j * 128:(j + 1) * 128],
                                                   identity=K.ident.t[:])) for j in rng_]
                S.group("tensor", fns, reads=[nrm.d, K.ident.d], writes=[P0.ds[hb]])
            S.op("scalar", lambda e: e.activation(out=nT.t[:], in_=P0.t[:, 0:768].rearrange("p (c t) -> p c t", c=6),
                                                  func=AF.Copy), reads=[P0.ds[0], P0.ds[1]], writes=[nT.d])
            if t + 1 < NTL:
                head(t + 1)
            fns = [(lambda e, c_=c_: e.matmul(P2.t[:, 0:384], lhsT=nT.t[:, c_, :], rhs=wuq.t[:, c_, :], start=(c_ == 0),
                                              stop=(c_ == 3))) for c_ in range(4)]
            S.group("tensor", fns, reads=[nT.d, wuq.d], writes=[P2.ds[0]])
            fns = [(lambda e, c_=c_: e.matmul(P2.t[:, 512:1024], lhsT=nT.t[:, 4 + c_, :], rhs=wukv.t[:, c_, :],
                                              start=(c_ == 0), stop=(c_ == 1))) for c_ in range(2)]
            S.group("tensor", fns, reads=[nT.d, wukv.d], writes=[P2.ds[1]])
            S.op("scalar", lambda e: e.activation(out=qf.t[:], in_=P2.t[:, 0:384].rearrange("p (h d) -> p h d", h=4),
                                                  func=AF.Copy), reads=[P2.ds[0]], writes=[qf.d])
            S.op("scalar", lambda e: e.activation(out=kvf.t[:], in_=P2.t[:, 512:1024].rearrange("p (h d) -> p h d", h=4),
                                                  func=AF.Copy), reads=[P2.ds[1]], writes=[kvf.d])
            S.op("vector", lambda e: e.tensor_copy(out=posf.t[:], in_=posi.t[:]), reads=[posi.d], writes=[posf.d])
            S.op("vector", lambda e: e.tensor_scalar(out=tt.t[:, 0:16], in0=freq.t[:], scalar1=posf.t[:],
                                                     scalar2=1.0 / TWO_PI, op0=ALU.mult, op1=ALU.mult),
                 reads=[freq.d, posf.d], writes=[tt.d])
            S.op("vector", lambda e: e.tensor_scalar(out=tt.t[:, 16:32], in0=tt.t[:, 0:16], scalar1=0.25, scalar2=None,
                                                     op0=ALU.add), reads=[tt.d], writes=[tt.d])
            S.op("vector", lambda e: e.tensor_copy(out=ti.t[:], in_=tt.t[:]), reads=[tt.d], writes=[ti.d])
            S.op("vector", lambda e: e.tensor_copy(out=tf.t[:], in_=ti.t[:]), reads=[ti.d], writes=[tf.d])
            S.op("vector", lambda e: e.tensor_tensor(out=tt.t[:], in0=tt.t[:], in1=tf.t[:], op=ALU.subtract),
                 reads=[tt.d, tf.d], writes=[tt.d])
            S.op("vector", lambda e: e.tensor_single_scalar(out=mm.t[:], in_=tt.t[:], scalar=0.5, op=ALU.is_gt),
                 reads=[tt.d], writes=[mm.d])
            S.op("vector", lambda e: e.tensor_tensor(out=tt.t[:], in0=tt.t[:], in1=mm.t[:], op=ALU.subtract),
                 reads=[tt.d, mm.d], writes=[tt.d])
            S.op("vector", lambda e: e.tensor_single_scalar(out=mm.t[:], in_=tt.t[:], scalar=-0.5, op=ALU.is_lt),
                 reads=[tt.d], writes=[mm.d])
            S.op("vector", lambda e: e.tensor_tensor(out=tt.t[:], in0=tt.t[:], in1=mm.t[:], op=ALU.add),
                 reads=[tt.d, mm.d], writes=[tt.d])
            S.op("scalar", lambda e: e.activation(out=scs.t[:], in_=tt.t[:], func=AF.Sin, scale=TWO_PI),
                 reads=[tt.d], writes=[scs.d])
            sinb, cosb = scs.t[:, 0:16], scs.t[:, 16:32]
            rope(qf.t[:, :, 64:80], qf.t[:, :, 80:96], Qh.t[:, :, 64:80], Qh.t[:, :, 80:96], b4(cosb), b4(sinb), True)
            rope(lat.t[:, 768:784], lat.t[:, 784:800], kr.t[:, 0:16], kr.t[:, 16:32], cosb, sinb, False)
            S.op("vector", lambda e: e.tensor_copy(out=Qh.t[:, :, 0:64], in_=qf.t[:, :, 0:64]), reads=[qf.d], writes=[Qh.d])
            S.op("vector", lambda e: e.tensor_copy(out=Kh.t[:, :, 0:64], in_=kvf.t[:, :, 0:64]), reads=[kvf.d], writes=[Kh.d])
            S.op("vector", lambda e: e.tensor_copy(out=Kh.t[:, :, 64:96], in_=kr.t[:].unsqueeze(1).to_broadcast([128, 4, 32])),
                 reads=[kr.d], writes=[Kh.d])
            S.op("vector", lambda e: e.tensor_copy(out=Va.t[:, :, 0:64], in_=kvf.t[:, :, 64:128]), reads=[kvf.d], writes=[Va.d])
            fns = [(lambda e, h=h: e.transpose(out=P3.t[0:96, h * 128:(h + 1) * 128], in_=Qh.t[:, h, :],
                                               identity=K.ident.t[:])) for h in range(4)]
            S.group("tensor", fns, reads=[Qh.d, K.ident.d], writes=[P3.ds[0]])
            fns = [(lambda e, h=h: e.transpose(out=P3.t[0:96, 512 + h * 128:512 + (h + 1) * 128], in_=Kh.t[:, h, :],
                                               identity=K.ident.t[:])) for h in range(4)]
            S.group("tensor", fns, reads=[Kh.d, K.ident.d], writes=[P3.ds[1]])
            S.op("scalar", lambda e: e.activation(out=QTs.t[0:96], in_=P3.t[0:96, 0:512].rearrange("p (h t) -> p h t", h=4),
                                                  func=AF.Copy), reads=[P3.ds[0]], writes=[QTs.d])
            S.op("scalar", lambda e: e.activation(out=KTs.t[0:96], in_=P3.t[0:96, 512:1024].rearrange("p (h t) -> p h t", h=4),
                                                  func=AF.Copy), reads=[P3.ds[1]], writes=[KTs.d])
            S.dma("gpsimd", qsem, QT_d[:, :, t * 128:(t + 1) * 128].rearrange("h d t -> d h t"), QTs.t[0:96], reads=[QTs.d])
            S.dma("gpsimd", ksem, KT_d[:, :, t * 128:(t + 1) * 128].rearrange("h d t -> d h t"), KTs.t[0:96], reads=[KTs.d])
            S.dma("gpsimd", vsem, V_d[t], Va.t[:], reads=[Va.d])
        S.barrier()
    with ExitStack() as sc:
        K.scope = sc
        KTh = K.sb([128, NTok], BF16, tag="KTh"); Vh = K.sb([128, NTL, 65], BF16, tag="Vh")
        khs = S.new_dma_sem(); vhs = S.new_dma_sem()
        QTt = [K.sb([128, 512], BF16, tag="QTt") for _ in range(2)]; qts = [S.new_dma_sem() for _ in range(2)]
        Pb = [K.sb([128, 512], BF16, tag="Pb") for _ in range(2)]
        mk = K.sb([128, 4, 512], BF16, tag="mk")
        mi = K.sb([128, 512], I32, tag="mi")
        for j in range(4):
            S.op("gpsimd", lambda e, j=j: e.iota(mi.t[:], pattern=[[1, 512]], base=-128 * j, channel_multiplier=-1),
                 writes=[mi.d])
            S.op("vector", lambda e, j=j: e.tensor_single_scalar(out=mk.t[:, j, :], in_=mi.t[:], scalar=0, op=ALU.is_ge),
                 reads=[mi.d], writes=[mk.d])
        sel = K.sb([128, 64], F32, tag="sel")
        S.op("vector", lambda e: e.memset(sel.t[:], 0.0), writes=[sel.d])
        S.op("vector", lambda e: e.memset(sel.t[64:65, :], 1.0), writes=[sel.d])
        Osb = K.sb([128, 512], F32, tag="Osb"); rec = K.sb([64, 512], F32, tag="rec")
        ob = [K.sb([64, 512], BF16, tag="ob") for _ in range(2)]; obs = [S.new_dma_sem() for _ in range(2)]
        it = 0
        for h in range(4):
            S.dma("sync", khs, KTh.t[0:96, :], KT_d[h], writes=[KTh.d])
            with nc.allow_non_contiguous_dma(reason="V rows of 130B"):
                S.dma("sync", vhs, Vh.t[:], V_d[:, :, h, :].rearrange("c p d -> p c d"), writes=[Vh.d])
            for qt in range(NTok // 512):
                qb = (h * (NTok // 512) + qt) % 2
                S.dma("sync", qts[qb], QTt[qb].t[0:96, :], QT_d[h][:, qt * 512:(qt + 1) * 512], writes=[QTt[qb].d])
                nkb = 4 * qt + 4

                def emit_s(kb, it_):
                    pb = Pb[it_ % 2]
                    S.op("tensor", lambda e, kb=kb, it_=it_, qb=qb: e.matmul(
                        P0.t[:, (it_ % 2) * 512:(it_ % 2 + 1) * 512], lhsT=KTh.t[0:96, kb * 128:(kb + 1) * 128],
                        rhs=QTt[qb].t[0:96, :], start=True, stop=True),
                         reads=[KTh.d, QTt[qb].d], writes=[P0.ds[it_ % 2]])
                    S.op("scalar", lambda e, it_=it_, pb=pb: e.activation(
                        out=pb.t[:], in_=P0.t[:, (it_ % 2) * 512:(it_ % 2 + 1) * 512], func=AF.Exp, scale=SCALE),
                         reads=[P0.ds[it_ % 2]], writes=[pb.d])
                    if kb >= 4 * qt:
                        S.op("vector", lambda e, pb=pb, j=kb - 4 * qt: e.tensor_tensor(
                            out=pb.t[:], in0=pb.t[:], in1=mk.t[:, j, :], op=ALU.mult), reads=[pb.d, mk.d], writes=[pb.d])

                def emit_pv(kb, it_):
                    pb = Pb[it_ % 2]
                    S.op("tensor", lambda e, kb=kb, pb=pb: e.matmul(
                        P1.t[0:65, 0:512], lhsT=Vh.t[:, kb, :], rhs=pb.t[:], start=(kb == 0), stop=(kb == nkb - 1)),
                         reads=[Vh.d, pb.d], writes=[P1.ds[0]])

                emit_s(0, it)
                for kb in range(nkb):
                    if kb + 1 < nkb:
                        emit_s(kb + 1, it + 1)
                    emit_pv(kb, it)
                    it += 1
                S.op("scalar", lambda e: e.activation(out=Osb.t[0:65, :], in_=P1.t[0:65, 0:512], func=AF.Copy),
                     reads=[P1.ds[0]], writes=[Osb.d])
                S.op("tensor", lambda e: e.matmul(P2.t[0:64, 0:512], lhsT=sel.t[0:65, :], rhs=Osb.t[0:65, :], start=True,
                                                  stop=True), reads=[sel.d, Osb.d], writes=[P2.ds[0]])
                S.op("scalar", lambda e: e.activation(out=rec.t[:], in_=P2.t[0:64, 0:512], func=AF.Copy),
                     reads=[P2.ds[0]], writes=[rec.d])
                S.op("vector", lambda e: e.reciprocal(out=rec.t[:], in_=rec.t[:]), reads=[rec.d], writes=[rec.d])
                S.op("vector", lambda e, qb=qb: e.tensor_tensor(out=ob[qb].t[:], in0=Osb.t[0:64, :], in1=rec.t[:],
                                                               op=ALU.mult), reads=[Osb.d, rec.d], writes=[ob[qb].d])
                S.dma("gpsimd", obs[qb], out[h * 64:(h + 1) * 64, qt * 512:(qt + 1) * 512], ob[qb].t[:], reads=[ob[qb].d])
        S.barrier()
    K.scope = old_scope
    K.S.release(_mk)


def build_tok(NT, steps):
    K = KB()
    x = K.din("x", [NT, D]); c = K.din("c", [D])
    y = K.dout("y", [NT, D])
    K.alloc_psum(); K.consts(); K.setup_cond(c)
    aw, ab, lnp = {}, {}, {}

    def layer_in(l):
        if l not in aw:
            aw[l] = K.din(f"aw{l}", [D, 6 * D]); ab[l] = K.din(f"ab{l}", [6 * D])
        return aw[l], ab[l]

    def ln_in(l, j):
        if (l, j) not in lnp:
            lnp[(l, j)] = (K.din(f"lng{l}_{j}", [D]), K.din(f"lnb{l}_{j}", [D]))
        return lnp[(l, j)]

    cur = DramStream(x, NT)
    for si, (kind, l, dm) in enumerate(steps):
        last = si == len(steps) - 1
        nxt = DramStream(y if last else K.dtmp(f"xs{si}", [NT, D]), NT)
        a_w, a_b = layer_in(l)
        if kind == "proj":
            g, b = ln_in(l, 0)
            mT = K.din(f"s{si}_mT", [dm, NT], BF16); wo = K.din(f"s{si}_wo", [dm, D])
            sub_proj(K, cur, nxt, NT, mT, dm, wo, a_w, a_b, g, b)
        elif kind == "ffn":
            g, b = ln_in(l, 1)
            wg = K.din(f"s{si}_wg", [D, DFF]); wu = K.din(f"s{si}_wu", [D, DFF]); wd = K.din(f"s{si}_wd", [DFF, D])
            sub_ffn(K, cur, nxt, NT, a_w, a_b, g, b, wg, wu, wd, None)
        elif kind == "moe":
            g, b = ln_in(l, 1)
            wg = K.din(f"s{si}_wg", [NE, D, DFF]); wu = K.din(f"s{si}_wu", [NE, D, DFF]); wd = K.din(f"s{si}_wd", [NE, DFF, D])
            wr = K.din(f"s{si}_wr", [D, NE])
            sub_ffn(K, cur, nxt, NT, a_w, a_b, g, b, wg, wu, wd, wr)
        elif kind == "sg":
            g, b = ln_in(l, 0)
            w_in = K.din(f"s{si}_win", [D, 4096]); b_in = K.din(f"s{si}_bin", [4096])
            sg_g = K.din(f"s{si}_sg", [2048]); sg_b = K.din(f"s{si}_sb", [2048])
            w_s = K.din(f"s{si}_ws", [8, 128, 128]); b_s = K.din(f"s{si}_bs", [8, 128]); wo = K.din(f"s{si}_wo", [2048, D])
            sub_sg(K, cur, nxt, NT, a_w, a_b, g, b, w_in, b_in, sg_g, sg_b, w_s, b_s, wo)
        cur = nxt
    K.S.finish()
    return K


def _ssd_sel(I, j, q):
    w = I['ssd_w_in'][j]
    cols = np.concatenate([np.arange(512 * q, 512 * q + 512), 2048 + np.arange(512 * q, 512 * q + 512),
                           4096 + np.arange(256 * q, 256 * q + 256), 4096 + 1024 + np.arange(256 * q, 256 * q + 256),
                           6144 + np.arange(8 * q, 8 * q + 8)])
    ccols = np.concatenate([np.arange(512 * q, 512 * q + 512), 2048 + np.arange(256 * q, 256 * q + 256),
                            2048 + 1024 + np.arange(256 * q, 256 * q + 256)])
    return dict(w=np.ascontiguousarray(w[:, cols]), cw=np.ascontiguousarray(I['ssd_conv_w'][j][:, ccols]),
                cb=np.ascontiguousarray(I['ssd_conv_b'][j][ccols]),
                dtb=np.ascontiguousarray(I['ssd_dt_bias'][j][8 * q:8 * q + 8]),
                alog=np.ascontiguousarray(I['ssd_a_log'][j][8 * q:8 * q + 8]),
                dsk=np.ascontiguousarray(I['ssd_d_skip'][j][8 * q:8 * q + 8]),
                nw=np.ascontiguousarray(I['ssd_norm_w'][j][512 * q:512 * q + 512]))


def _mla_sel(I, hq):
    uq = I['mla_w_uq'][0].reshape(512, 16, 96)[:, 4 * hq:4 * hq + 4].reshape(512, 384)
    ukv = I['mla_w_ukv'][0].reshape(256, 16, 128)[:, 4 * hq:4 * hq + 4].reshape(256, 512)
    return dict(w_in=I['mla_w_in'][0], qn=I['mla_q_norm'][0], kvn=I['mla_kv_norm'][0],
                wuq=np.ascontiguousarray(uq), wukv=np.ascontiguousarray(ukv))


def _run(K, in_maps):
    res = run_bass_kernel_spmd(K.nc, in_maps, core_ids=list(range(8)))
    return res.results


def kernel_unfused(**I):
    I = {k: np.asarray(v) for k, v in I.items()}
    B, SEQ = I['x'].shape[0], I['x'].shape[1]
    NT = SEQ // 4
    cores = [(k // 4, k % 4) for k in range(8)]

    def run_mixer_ssd(xcur, layer, j):
        K = build_ssd(SEQ)
        maps = [dict(x=xcur[b], c=I['c'][b], aw=I['ada_w'][layer], ab=I['ada_b'][layer], **_ssd_sel(I, j, q))
                for b, q in cores]
        r = _run(K, maps)
        return [np.concatenate([r[b * 4 + q]["mT"] for q in range(4)], 0) for b in range(B)]

    def run_mixer_mla(xcur, layer):
        K = build_mla(SEQ)
        maps = [dict(x=xcur[b], c=I['c'][b], aw=I['ada_w'][layer], ab=I['ada_b'][layer],
                     pos=np.ascontiguousarray(I['positions'][b].astype(np.int32)), **_mla_sel(I, q)) for b, q in cores]
        r = _run(K, maps)
        return [np.concatenate([r[b * 4 + q]["mT"] for q in range(4)], 0) for b in range(B)]

    def run_tok(xcur, steps, extra):
        K = build_tok(NT, steps)
        maps = []
        for b, r_ in cores:
            m = dict(x=np.ascontiguousarray(xcur[b][r_ * NT:(r_ + 1) * NT]), c=I['c'][b])
            for (kind, l, dm) in steps:
                m[f"aw{l}"] = I['ada_w'][l]; m[f"ab{l}"] = I['ada_b'][l]
                jj = 0 if kind in ("proj", "sg") else 1
                m[f"lng{l}_{jj}"] = I['ln_g'][l, jj]; m[f"lnb{l}_{jj}"] = I['ln_b'][l, jj]
            for k_, v in extra.items():
                m[k_] = v(b, r_) if callable(v) else v
            maps.append(m)
        r = _run(K, maps)
        return [np.concatenate([r[b * 4 + q]["y"] for q in range(4)], 0) for b in range(B)]

    def ffn_w(si, k):
        return {f"s{si}_wg": I['ffn_w_gate'][k], f"s{si}_wu": I['ffn_w_up'][k], f"s{si}_wd": I['ffn_w_down'][k]}

    def moe_w(si, k):
        return {f"s{si}_wg": I['moe_w_gate'][k], f"s{si}_wu": I['moe_w_up'][k], f"s{si}_wd": I['moe_w_down'][k],
                f"s{si}_wr": I['moe_w_router'][k]}

    def mt_slice(mT):
        return lambda b, r_: np.ascontiguousarray(mT[b][:, r_ * NT:(r_ + 1) * NT])

    xcur = [I['x'][b] for b in range(B)]
    mT = run_mixer_ssd(xcur, 0, 0)
    xcur = run_tok(xcur, [("proj", 0, 2048), ("ffn", 0, 0)],
                   {"s0_mT": mt_slice(mT), "s0_wo": I['ssd_w_out'][0], **ffn_w(1, 0)})
    mT = run_mixer_mla(xcur, 1)
    sgw = {"s2_win": I['sg_w_in'][0], "s2_bin": I['sg_b_in'][0], "s2_sg": I['sg_ln_g'][0], "s2_sb": I['sg_ln_b'][0],
           "s2_ws": I['sg_w_s'][0], "s2_bs": I['sg_b_s'][0], "s2_wo": I['sg_w_out'][0]}
    xcur = run_tok(xcur, [("proj", 1, 1024), ("moe", 1, 0), ("sg", 2, 0), ("ffn", 2, 0)],
                   {"s0_mT": mt_slice(mT), "s0_wo": I['mla_w_out'][0], **moe_w(1, 0), **sgw, **ffn_w(3, 1)})
    mT = run_mixer_ssd(xcur, 3, 1)
    xcur = run_tok(xcur, [("proj", 3, 2048), ("moe", 3, 0)],
                   {"s0_mT": mt_slice(mT), "s0_wo": I['ssd_w_out'][1], **moe_w(1, 1)})
    return np.stack(xcur, 0).astype(np.float32)


RG = [[0, 1, 2, 3], [4, 5, 6, 7]]


def collective(K, kind, in_ap, out_ap):
    S = K.S
    if not hasattr(K, "cc"):
        K.cc = S.new_dma_sem()
    S.barrier()
    op = ALU.add if kind == "ReduceScatter" else ALU.bypass
    ins = K.nc.gpsimd.collective_compute(kind, op, replica_groups=RG, ins=[in_ap], outs=[out_ap])
    K.cc.val += 1
    ins.then_inc(K.cc.sem)
    S.barrier()


def sub_pproj(K, mT_ap, DM, wo_ap, ypart_ap, NTok):
    S, nc = K.S, K.nc
    NC_ = DM // 128
    old_scope = K.scope
    _mk = K.S.mark()
    with ExitStack() as sc:
        K.scope = sc
        wout = K.sb([128, NC_, 1024], BF16, tag="wout")
        S.dma("gpsimd", S.new_dma_sem(), wout.t[:], wo_ap.rearrange("(c p) d -> p c d", p=128), writes=[wout.d])
        mt = [K.sb([128, NC_, 128], BF16, tag="mt") for _ in range(2)]
        msem = [S.new_dma_sem() for _ in range(2)]
        yo = [K.sb([128, 1024], F32, tag="yo") for _ in range(2)]
        osem = [S.new_dma_sem() for _ in range(2)]
        for ts in range(NTok // 128):
            b = ts % 2
            S.dma("sync", msem[b], mt[b].t[:], mT_ap[:, ts * 128:(ts + 1) * 128].rearrange("(c p) t -> p c t", p=128),
                  writes=[mt[b].d])
            po = K.psum[2 + b]
            for db in range(2):
                fns = [(lambda e, c=c, db=db, po=po, b=b: e.matmul(
                    po.t[:, db * 512:(db + 1) * 512], lhsT=mt[b].t[:, c, :], rhs=wout.t[:, c, db * 512:(db + 1) * 512],
                    start=(c == 0), stop=(c == NC_ - 1))) for c in range(NC_)]
                S.group("tensor", fns, reads=[mt[b].d, wout.d], writes=[po.ds[db]])
            S.op("scalar", lambda e, po=po, b=b: e.activation(out=yo[b].t[:], in_=po.t[:], func=AF.Copy),
                 reads=[po.ds[0], po.ds[1]], writes=[yo[b].d])
            S.dma("gpsimd", osem[b], ypart_ap(ts), yo[b].t[:], reads=[yo[b].d])
        S.barrier()
    K.scope = old_scope
    K.S.release(_mk)


def sub_resln(K, xs, xd, NT, y_ap, ada_w_l, ada_b_l, lng_ap, lnb_ap):
    S, nc = K.S, K.nc
    old_scope = K.scope
    _mk = K.S.mark()
    with ExitStack() as sc:
        K.scope = sc
        g1p = K.sb([128, 1024], F32, tag="g1p")
        lng = K.sb([128, 1024], F32, tag="lng")
        lnb = K.sb([128, 1024], F32, tag="lnb")
        K.ln_alloc()
        K.mod_vec(g1p, ada_w_l, ada_b_l, 2, True)
        K.bcast_vec(lng, lng_ap, S.new_dma_sem())
        K.bcast_vec(lnb, lnb_ap, S.new_dma_sem())
        yin = [K.sb([128, 1024], F32, tag="yin") for _ in range(2)]
        ysem = [S.new_dma_sem() for _ in range(2)]
        xin = [K.sb([128, 1024], F32, tag="xin") for _ in range(2)]
        xsem = [S.new_dma_sem() for _ in range(2)]
        tmp = [K.sb([128, 1024], F32, tag="tmp") for _ in range(2)]
        xo = [K.sb([128, 1024], F32, tag="xo") for _ in range(2)]
        osem = [S.new_dma_sem() for _ in range(2)]
        for ts in range(NT // 128):
            b = ts % 2
            S.dma("sync", ysem[b], yin[b].t[:], y_ap[ts * 128:(ts + 1) * 128, :], writes=[yin[b].d])
            S.dma("sync", xsem[b], xin[b].t[:], xs.ap[ts * 128:(ts + 1) * 128, :], reads=[xs.ds[ts]],
                  writes=[xin[b].d])
            residual_ln(K, yin[b].t[:], [yin[b].d], xin[b], g1p, lng, lnb, tmp[b], xo[b])
            S.dma("gpsimd", osem[b], xd.ap[ts * 128:(ts + 1) * 128, :], xo[b].t[:], reads=[xo[b].d],
                  writes=[xd.ds[ts]])
        S.barrier()
    K.scope = old_scope
    K.S.release(_mk)


def build_fused(SEQ):
    NT = SEQ // 4
    K = KB()
    S, nc = K.S, K.nc
    x_in = K.din("x", [NT, D]); c = K.din("c", [D]); pos = K.din("pos", [SEQ], I32)
    aw = [K.din(f"aw{l}", [D, 6 * D]) for l in range(4)]
    ab = [K.din(f"ab{l}", [6 * D]) for l in range(4)]
    lng = [[K.din(f"lng{l}_{j}", [D]) for j in range(2)] for l in range(4)]
    lnb = [[K.din(f"lnb{l}_{j}", [D]) for j in range(2)] for l in range(4)]
    ssd = []
    for j in range(2):
        ssd.append(dict(w=K.din(f"ssd{j}_w", [D, 1544]), cw=K.din(f"ssd{j}_cw", [4, 1024]),
                        cbv=K.din(f"ssd{j}_cb", [1024]), dtb=K.din(f"ssd{j}_dtb", [8]),
                        alog=K.din(f"ssd{j}_alog", [8]), dsk=K.din(f"ssd{j}_dsk", [8]),
                        nw=K.din(f"ssd{j}_nw", [512]), wo=K.din(f"ssd{j}_wo", [512, D])))
    mla = dict(w_in=K.din("mla_w_in", [D, 800]), qn=K.din("mla_qn", [512]), kvn=K.din("mla_kvn", [256]),
               wuq_d=K.din("mla_wuq", [512, 384]), wukv_d=K.din("mla_wukv", [256, 512]))
    mla_wo = K.din("mla_wo", [256, D])
    sg = dict(w_in=K.din("sg_win", [D, 4096]), b_in=K.din("sg_bin", [4096]), sg_g=K.din("sg_g", [2048]),
              sg_b=K.din("sg_b", [2048]), w_s=K.din("sg_ws", [8, 128, 128]), b_s=K.din("sg_bs", [8, 128]),
              wo=K.din("sg_wo", [2048, D]))
    ffn = [dict(wg=K.din(f"ffn{k}_wg", [D, DFF]), wu=K.din(f"ffn{k}_wu", [D, DFF]), wd=K.din(f"ffn{k}_wd", [DFF, D]))
           for k in range(2)]
    moe = [dict(wg=K.din(f"moe{k}_wg", [NE, D, DFF]), wu=K.din(f"moe{k}_wu", [NE, D, DFF]),
                wd=K.din(f"moe{k}_wd", [NE, DFF, D]), wr=K.din(f"moe{k}_wr", [D, NE])) for k in range(2)]
    y = K.dout("y", [NT, D])
    K.alloc_psum(); K.consts(); K.setup_cond(c)
    CH = 256
    NCH = NT // CH
    xfull_c = K.dtmp("xfull", [NCH, 4 * CH, D])
    ypart_c = K.dtmp("ypart", [NCH, 4 * CH, D]); yred = K.dtmp("yred", [NT, D])

    def rows(buf):
        def f(tile):
            t = tile * 128
            r, rem = t // NT, t % NT
            ch, i = rem // CH, rem % CH
            return buf[ch, r * CH + i:r * CH + i + 128, :]
        return f

    xfull = rows(xfull_c)
    ypart = rows(ypart_c)

    def collectives(kind, pairs):
        if not hasattr(K, "cc"):
            K.cc = S.new_dma_sem()
        S.barrier()
        op = ALU.add if kind == "ReduceScatter" else ALU.bypass
        for a, b_ in pairs:
            ins = nc.gpsimd.collective_compute(kind, op, replica_groups=RG, ins=[a], outs=[b_])
            K.cc.val += 1
            ins.then_inc(K.cc.sem)
        S.barrier()

    def gather_x(src):
        collectives("AllGather", [(src[ch * CH:(ch + 1) * CH, :], xfull_c[ch]) for ch in range(NCH)])

    def scatter_y():
        collectives("ReduceScatter", [(ypart_c[ch], yred[ch * CH:(ch + 1) * CH, :]) for ch in range(NCH)])
    mTs = K.dtmp("mTs", [512, SEQ], BF16); mTm = K.dtmp("mTm", [256, SEQ], BF16)
    xl = [K.dtmp(f"xl{i}", [NT, D]) for i in range(8)]

    def stream(ap):
        return DramStream(ap, NT)

    cps = S.new_dma_sem()
    for ch in range(NCH):
        S.dma("sync", cps, xl[0][ch * CH:(ch + 1) * CH, :], x_in[ch * CH:(ch + 1) * CH, :])
    gather_x(xl[0])
    s = ssd[0]
    sub_ssd(K, xfull, mTs, SEQ, aw[0], ab[0], s["w"], s["cw"], s["cbv"], s["dtb"], s["alog"], s["dsk"], s["nw"])
    sub_pproj(K, mTs, 512, s["wo"], ypart, SEQ)
    scatter_y()
    sub_resln(K, stream(xl[0]), stream(xl[1]), NT, yred, aw[0], ab[0], lng[0][0], lnb[0][0])
    f = ffn[0]
    sub_ffn(K, stream(xl[1]), stream(xl[2]), NT, aw[0], ab[0], lng[0][1], lnb[0][1], f["wg"], f["wu"], f["wd"], None)
    gather_x(xl[2])
    sub_mla(K, xfull, mTm, SEQ, aw[1], ab[1], pos, **mla)
    sub_pproj(K, mTm, 256, mla_wo, ypart, SEQ)
    scatter_y()
    sub_resln(K, stream(xl[2]), stream(xl[3]), NT, yred, aw[1], ab[1], lng[1][0], lnb[1][0])
    m = moe[0]
    sub_ffn(K, stream(xl[3]), stream(xl[4]), NT, aw[1], ab[1], lng[1][1], lnb[1][1], m["wg"], m["wu"], m["wd"], m["wr"])
    sub_sg(K, stream(xl[4]), stream(xl[5]), NT, aw[2], ab[2], lng[2][0], lnb[2][0], sg["w_in"], sg["b_in"], sg["sg_g"],
           sg["sg_b"], sg["w_s"], sg["b_s"], sg["wo"])
    f = ffn[1]
    sub_ffn(K, stream(xl[5]), stream(xl[6]), NT, aw[2], ab[2], lng[2][1], lnb[2][1], f["wg"], f["wu"], f["wd"], None)
    gather_x(xl[6])
    s = ssd[1]
    sub_ssd(K, xfull, mTs, SEQ, aw[3], ab[3], s["w"], s["cw"], s["cbv"], s["dtb"], s["alog"], s["dsk"], s["nw"])
    sub_pproj(K, mTs, 512, s["wo"], ypart, SEQ)
    scatter_y()
    sub_resln(K, stream(xl[6]), stream(xl[7]), NT, yred, aw[3], ab[3], lng[3][0], lnb[3][0])
    m = moe[1]
    sub_ffn(K, stream(xl[7]), stream(y), NT, aw[3], ab[3], lng[3][1], lnb[3][1], m["wg"], m["wu"], m["wd"], m["wr"])
    S.finish()
    return K


def fused_inputs(I, SEQ, b, q):
    NT = SEQ // 4
    m = dict(x=np.ascontiguousarray(I['x'][b][q * NT:(q + 1) * NT]), c=np.ascontiguousarray(I['c'][b]),
             pos=np.ascontiguousarray(I['positions'][b].astype(np.int32)))
    for l in range(4):
        m[f"aw{l}"] = I['ada_w'][l]; m[f"ab{l}"] = I['ada_b'][l]
        for j in range(2):
            m[f"lng{l}_{j}"] = I['ln_g'][l, j]; m[f"lnb{l}_{j}"] = I['ln_b'][l, j]
    for j in range(2):
        s = _ssd_sel(I, j, q)
        for k_, v in s.items():
            m[f"ssd{j}_{k_}"] = v
        m[f"ssd{j}_wo"] = np.ascontiguousarray(I['ssd_w_out'][j][512 * q:512 * q + 512])
    s = _mla_sel(I, q)
    m["mla_w_in"] = s["w_in"]; m["mla_qn"] = s["qn"]; m["mla_kvn"] = s["kvn"]; m["mla_wuq"] = s["wuq"]; m["mla_wukv"] = s["wukv"]
    m["mla_wo"] = np.ascontiguousarray(I['mla_w_out'][0][256 * q:256 * q + 256])
    m["sg_win"] = I['sg_w_in'][0]; m["sg_bin"] = I['sg_b_in'][0]; m["sg_g"] = I['sg_ln_g'][0]; m["sg_b"] = I['sg_ln_b'][0]
    m["sg_ws"] = I['sg_w_s'][0]; m["sg_bs"] = I['sg_b_s'][0]; m["sg_wo"] = I['sg_w_out'][0]
    for k in range(2):
        m[f"ffn{k}_wg"] = I['ffn_w_gate'][k]; m[f"ffn{k}_wu"] = I['ffn_w_up'][k]; m[f"ffn{k}_wd"] = I['ffn_w_down'][k]
        m[f"moe{k}_wg"] = I['moe_w_gate'][k]; m[f"moe{k}_wu"] = I['moe_w_up'][k]; m[f"moe{k}_wd"] = I['moe_w_down'][k]
        m[f"moe{k}_wr"] = I['moe_w_router'][k]
    return m


def kernel(**I):
    I = {k: np.asarray(v) for k, v in I.items()}
    B, SEQ = I['x'].shape[0], I['x'].shape[1]
    NT = SEQ // 4
    K = build_fused(SEQ)
    maps = [fused_inputs(I, SEQ, k // 4, k % 4) for k in range(8)]
    r = _run(K, maps)
    return np.stack([np.concatenate([r[b * 4 + q]["y"] for q in range(4)], 0) for b in range(B)], 0).astype(np.float32)
```
